# Optimizing a Trainium2 kernel written in Bass

```python
import math
import jax
import jax.numpy as jnp
from jax import lax
import numpy as np

D_MODEL = 1024
BATCH = 8
SEQ = 2048
DEPTH = 2

GRID_W = 64
CTX_LEN = 256
Q_BLOCK = 128
ROPE_BASE = 10000.0
EPS = 1e-6
DA_HEADS = 4
DA_DIM = 32
DA_VDIM = 64
S5_GROUPS = 16
S5_CH = 16
S5_STATE = 64
S5_WIDTH = S5_GROUPS * S5_CH
MLA_HEADS = 4
MLA_NOPE = 32
MLA_ROPE = 16
MLA_VDIM = 64
MLA_Q_RANK = 192
MLA_KV_RANK = 128
RW_HEADS = 4
RW_DIM = 64
RW_WIDTH = RW_HEADS * RW_DIM
RW_DECAY_RANK = 32
RW_ICL_RANK = 32
RW_GATE_RANK = 64
RW_LN_EPS = 64e-5
N_BRANCH = 4
BRANCH_WIDTH = 256
N_EXPERTS = 16
N_GROUPS = 4
EXPERTS_PER_GROUP = N_EXPERTS // N_GROUPS
TOP_K = 2
D_FF = 512

IN_SIZES = (2 * DA_HEADS * DA_DIM, 2 * DA_HEADS * DA_DIM, DA_HEADS * DA_VDIM, S5_WIDTH,
            MLA_Q_RANK, MLA_KV_RANK, MLA_ROPE, RW_WIDTH, RW_WIDTH, RW_WIDTH, RW_WIDTH,
            N_BRANCH * D_MODEL)
IN_OFFSETS = tuple(int(o) for o in np.cumsum(IN_SIZES)[:-1])
N_IN = int(sum(IN_SIZES))

kernel_name = 'hybrid_prefix_dit_block'


def rms_norm(x, g, eps=EPS):
    xf = x.astype(jnp.float32)
    xf = xf * lax.rsqrt(jnp.mean(jnp.square(xf), axis=-1, keepdims=True) + eps)
    return (xf * g.astype(jnp.float32)).astype(x.dtype)


def modulate(x, g, shift, scale):
    return rms_norm(x, g) * (1 + scale) + shift


def axial_rope_tables(n_tokens, rot_dim):
    rows = n_tokens // GRID_W
    row = jnp.repeat(jnp.arange(rows, dtype=jnp.float32), GRID_W)
    col = jnp.tile(jnp.arange(GRID_W, dtype=jnp.float32), rows)
    n_freq = rot_dim // 4
    inv_freq = ROPE_BASE ** (-jnp.arange(n_freq, dtype=jnp.float32) / n_freq)
    ang = jnp.concatenate([row[:, None] * inv_freq, col[:, None] * inv_freq], axis=-1)
    return jnp.cos(ang), jnp.sin(ang)


def apply_rope(x, cos, sin):
    half = x.shape[-1] // 2
    x1, x2 = x[..., :half], x[..., half:]
    c = cos[:, None, :].astype(x.dtype)
    s = sin[:, None, :].astype(x.dtype)
    return jnp.concatenate([x1 * c - x2 * s, x2 * c + x1 * s], axis=-1)


def attend(q, k, v, scale):
    b, m, h, lq, d = q.shape
    nb = lq // Q_BLOCK
    qb = jnp.moveaxis(q.reshape(b, m, h, nb, Q_BLOCK, d), 3, 0)

    def block(qi):
        s = jnp.einsum('bmhqd,bmhkd->bmhqk', qi, k).astype(jnp.float32) * scale
        p = jax.nn.softmax(s, axis=-1).astype(v.dtype)
        return jnp.einsum('bmhqk,bhkd->bmhqd', p, v)

    o = lax.map(block, qb)
    return jnp.moveaxis(o, 0, 3).reshape(b, m, h, lq, v.shape[-1])


def diff_attention(q_l, k_l, v_l, q_c, k_c, v_c, cos, sin, qk_g, lam, subln_g, lambda_init, need_ctx):
    def prep(q, k, v, rotate):
        b, n, _ = q.shape
        q = rms_norm(q.reshape(b, n, 2 * DA_HEADS, DA_DIM), qk_g[0])
        k = rms_norm(k.reshape(b, n, 2 * DA_HEADS, DA_DIM), qk_g[1])
        if rotate:
            q, k = apply_rope(q, cos, sin), apply_rope(k, cos, sin)
        q = q.reshape(b, n, DA_HEADS, 2, DA_DIM).transpose(0, 3, 2, 1, 4)
        k = k.reshape(b, n, DA_HEADS, 2, DA_DIM).transpose(0, 3, 2, 1, 4)
        v = v.reshape(b, n, DA_HEADS, DA_VDIM).transpose(0, 2, 1, 3)
        return q, k, v

    ql, kl, vl = prep(q_l, k_l, v_l, True)
    qc, kc, vc = prep(q_c, k_c, v_c, False)
    lam32 = lam.astype(jnp.float32)
    lmbda = jnp.exp(jnp.sum(lam32[0] * lam32[1])) - jnp.exp(jnp.sum(lam32[2] * lam32[3])) + lambda_init
    scale = DA_DIM ** -0.5

    def combine(o):
        o = o[:, 0] - lmbda.astype(o.dtype) * o[:, 1]
        o = rms_norm(o, subln_g) * (1 - lambda_init)
        b, h, n, dv = o.shape
        return o.transpose(0, 2, 1, 3).reshape(b, n, h * dv)

    k_all = jnp.concatenate([kl, kc], axis=3)
    v_all = jnp.concatenate([vl, vc], axis=2)
    y_lat = combine(attend(ql, k_all, v_all, scale))
    y_ctx = combine(attend(qc, kc, vc, scale)) if need_ctx else None
    return y_lat, y_ctx


def _linear_combine(e1, e2):
    a1, b1 = e1
    a2, b2 = e2
    return a2 * a1, a2 * b1 + b2


def s5_scan(u, lam_re, lam_im, log_dt, b_re, b_im, h0, reverse):
    lam = lax.complex(lam_re.astype(jnp.float32), lam_im.astype(jnp.float32))
    dt = jnp.exp(log_dt.astype(jnp.float32))[:, None]
    a_bar = jnp.exp(lam * dt)
    b_mat = lax.complex(b_re.astype(jnp.float32), b_im.astype(jnp.float32))
    b_bar = ((a_bar - 1.0) / lam)[..., None] * b_mat
    bu = jnp.einsum('bngc,gpc->bngp', u.astype(jnp.complex64), b_bar)
    if h0 is not None:
        edge = -1 if reverse else 0
        bu = bu.at[:, edge].add(a_bar * h0)
    a = jnp.broadcast_to(a_bar, bu.shape)
    _, h = lax.associative_scan(_linear_combine, (a, bu), reverse=reverse, axis=1)
    return h


def s5_readout(h, c_re, c_im):
    return (jnp.einsum('bngp,gcp->bngc', h.real, c_re.astype(jnp.float32))
            - jnp.einsum('bngp,gcp->bngc', h.imag, c_im.astype(jnp.float32)))


def s5_mixer(u_l, u_c, lam_re, lam_im, log_dt, b_re, b_im, c_re, c_im, d, w_glu, b_glu, need_ctx):
    out_dtype = u_l.dtype

    def groups(u):
        return u.astype(jnp.float32).reshape(u.shape[0], u.shape[1], S5_GROUPS, S5_CH)

    ul, uc = groups(u_l), groups(u_c)
    ys_l = [d * ul]
    ys_c = [d * uc]
    for dr in range(2):
        rev = dr == 1
        prm = (lam_re[dr], lam_im[dr], log_dt[dr], b_re[dr], b_im[dr])
        h_c = s5_scan(uc, *prm, None, rev)
        h_final = h_c[:, 0] if rev else h_c[:, -1]
        h_l = s5_scan(ul, *prm, h_final, rev)
        ys_l.append(s5_readout(h_l, c_re[dr], c_im[dr]))
        if need_ctx:
            ys_c.append(s5_readout(h_c, c_re[dr], c_im[dr]))

    def glu(ys):
        y = sum(ys)
        b, n = y.shape[:2]
        z = jax.nn.gelu(y.reshape(b, n, S5_WIDTH))
        return (z * jax.nn.sigmoid(z @ w_glu + b_glu)).astype(out_dtype)

    return glu(ys_l), (glu(ys_c) if need_ctx else None)


def mla(cq_l, ckv_l, kr_l, cq_c, ckv_c, kr_c, cos, sin, cq_g, ckv_g, w_uq, w_ukv, qk_g, need_ctx):
    def prep(cq, ckv, kr, rotate):
        b, n, _ = cq.shape
        q = (rms_norm(cq, cq_g) @ w_uq).reshape(b, n, MLA_HEADS, MLA_NOPE + MLA_ROPE)
        kv = (rms_norm(ckv, ckv_g) @ w_ukv).reshape(b, n, MLA_HEADS, MLA_NOPE + MLA_VDIM)
        k_nope, v = kv[..., :MLA_NOPE], kv[..., MLA_NOPE:]
        k_rope = jnp.broadcast_to(kr[:, :, None, :], (b, n, MLA_HEADS, MLA_ROPE))
        k = jnp.concatenate([k_nope, k_rope], axis=-1)
        q = rms_norm(q, qk_g[0])
        k = rms_norm(k, qk_g[1])
        if rotate:
            q = jnp.concatenate([q[..., :MLA_NOPE], apply_rope(q[..., MLA_NOPE:], cos, sin)], axis=-1)
            k = jnp.concatenate([k[..., :MLA_NOPE], apply_rope(k[..., MLA_NOPE:], cos, sin)], axis=-1)
        return (q.transpose(0, 2, 1, 3)[:, None], k.transpose(0, 2, 1, 3)[:, None],
                v.transpose(0, 2, 1, 3))

    ql, kl, vl = prep(cq_l, ckv_l, kr_l, True)
    qc, kc, vc = prep(cq_c, ckv_c, kr_c, False)
    scale = (MLA_NOPE + MLA_ROPE) ** -0.5

    def heads_out(o):
        o = o[:, 0]
        b, h, n, dv = o.shape
        return o.transpose(0, 2, 1, 3).reshape(b, n, h * dv)

    k_all = jnp.concatenate([kl, kc], axis=3)
    v_all = jnp.concatenate([vl, vc], axis=2)
    y_lat = heads_out(attend(ql, k_all, v_all, scale))
    y_ctx = heads_out(attend(qc, kc, vc, scale)) if need_ctx else None
    return y_lat, y_ctx


def centred_shift(x):
    pad = jnp.pad(x, ((0, 0), (1, 1), (0, 0)))
    return 0.5 * (pad[:, :-2] + pad[:, 2:])


def rwkv_direction(xd, k, w0, w1, w2, a0, a1, a2, k_a):
    w_raw = w0 + jnp.tanh(xd @ w1) @ w2
    decay = jnp.exp(-jnp.exp(-jax.nn.softplus(-w_raw) - 0.5))
    a = jax.nn.sigmoid(a0 + (xd @ a1) @ a2)
    k_mod = k * (1 + (a - 1) * k_a)
    return decay, a, k_mod


def rwkv_scan(s0, r, w, k, v, kk, a, reverse):
    xs = tuple(jnp.moveaxis(t, 1, 0) for t in (r, w, k, v, kk, a))

    def step(s, inp):
        r_t, w_t, k_t, v_t, kk_t, a_t = inp
        sa = jnp.einsum('bhvk,bhk->bhv', s, -kk_t)
        s = (s * w_t[:, :, None, :] + sa[..., None] * (kk_t * a_t)[:, :, None, :]
             + v_t[..., None] * k_t[:, :, None, :])
        return s, jnp.einsum('bhvk,bhk->bhv', s, r_t)

    s_final, ys = lax.scan(step, s0, xs, reverse=reverse)
    return s_final, jnp.moveaxis(ys, 0, 1)


def rwkv_bonus(r, k, v, r_k):
    return jnp.sum(r * k * r_k, axis=-1, keepdims=True) * v


def rwkv_mixer(r_l, k_l, v_l, d_l, r_c, k_c, v_c, d_c, mu, w0, w1, w2, a0, a1, a2, g1, g2,
               k_k, k_a, r_k, ln_g, ln_b, need_ctx):
    out_dtype = r_l.dtype

    def streams(*ss):
        return [(s + (centred_shift(s) - s) * mu[i]).astype(jnp.float32) for i, s in enumerate(ss)]

    def heads(t):
        return t.reshape(t.shape[0], t.shape[1], RW_HEADS, RW_DIM)

    def removal_key(k):
        kk = heads(k * k_k)
        return kk / jnp.maximum(jnp.sqrt(jnp.sum(kk * kk, axis=-1, keepdims=True)), 1e-12)

    rl, kl, vl, xl = streams(r_l, k_l, v_l, d_l)
    rc, kc, vc, xc = streams(r_c, k_c, v_c, d_c)
    kk_l, kk_c = removal_key(kl), removal_key(kc)
    s0 = jnp.zeros((rc.shape[0], RW_HEADS, RW_DIM, RW_DIM), jnp.float32)
    ys_l, bon_l, ys_c, bon_c = [], [], [], []
    for dr in range(2):
        rev = dr == 1
        prm = (w0[dr], w1[dr], w2[dr], a0[dr], a1[dr], a2[dr], k_a)
        dec_c, a_c, km_c = rwkv_direction(xc, kc, *prm)
        s_c, y_c = rwkv_scan(s0, heads(rc), heads(dec_c), heads(km_c), heads(vc), kk_c, heads(a_c), rev)
        dec_l, a_l, km_l = rwkv_direction(xl, kl, *prm)
        _, y_l = rwkv_scan(s_c, heads(rl), heads(dec_l), heads(km_l), heads(vl), kk_l, heads(a_l), rev)
        ys_l.append(y_l)
        bon_l.append(rwkv_bonus(heads(rl), heads(km_l), heads(vl), r_k))
        if need_ctx:
            ys_c.append(y_c)
            bon_c.append(rwkv_bonus(heads(rc), heads(km_c), heads(vc), r_k))

    def finish(ys, bons, xd):
        y = sum(ys)
        mean = jnp.mean(y, axis=-1, keepdims=True)
        var = jnp.mean(jnp.square(y - mean), axis=-1, keepdims=True)
        y = (y - mean) * lax.rsqrt(var + RW_LN_EPS)
        b, n = y.shape[:2]
        y = y.reshape(b, n, RW_WIDTH) * ln_g + ln_b + sum(bons).reshape(b, n, RW_WIDTH)
        g = jax.nn.sigmoid(xd @ g1) @ g2
        return (y * g).astype(out_dtype)

    y_lat = finish(ys_l, bon_l, xl)
    y_ctx = finish(ys_c, bon_c, xc) if need_ctx else None
    return y_lat, y_ctx


def merge_branches(branches, gate_cols, w_branch_l, w_out_l):
    b, n, _ = gate_cols.shape
    gates = jax.nn.sigmoid(gate_cols.reshape(b, n, N_BRANCH, D_MODEL))
    merged = sum(gates[:, :, i] * (y @ w_branch_l[i]) for i, y in enumerate(branches))
    return merged @ w_out_l


def moe(h, router_w, router_bias, w_gate, w_up, w_down):
    t = h.shape[0]
    scores = jax.nn.sigmoid((h @ router_w).astype(jnp.float32))
    biased = (scores + router_bias.astype(jnp.float32)).reshape(t, N_GROUPS, EXPERTS_PER_GROUP)
    group_score = jnp.sum(lax.top_k(biased, TOP_K)[0], axis=-1)
    in_group = jax.nn.one_hot(jnp.argmax(group_score, axis=-1), N_GROUPS, dtype=jnp.bool_)
    masked = jnp.where(in_group[..., None], biased, -jnp.inf).reshape(t, N_EXPERTS)
    _, idx = lax.top_k(masked, TOP_K)
    w = jnp.take_along_axis(scores, idx, axis=-1)
    w = w / jnp.sum(w, axis=-1, keepdims=True)
    combine = jnp.sum(jax.nn.one_hot(idx, N_EXPERTS, dtype=jnp.float32) * w[..., None], axis=1).astype(h.dtype)
    out = 0
    for e in range(N_EXPERTS):
        act = jax.nn.silu(h @ w_gate[e]) * (h @ w_up[e])
        out = out + combine[:, e:e + 1] * (act @ w_down[e])
    return out


def setup_inputs(seed: int = 0) -> dict:
    key = jax.random.key(seed)
    keys = iter(jax.random.split(key, 64))
    f32 = jnp.float32

    def nrm(shape, scale):
        return scale * jax.random.normal(next(keys), shape, f32)

    def unif(shape, lo, hi):
        return jax.random.uniform(next(keys), shape, f32, lo, hi)

    def gain(shape):
        return 1.0 + nrm(shape, 0.05)

    dm = D_MODEL
    g, p, ch = S5_GROUPS, S5_STATE, S5_CH
    e = N_EXPERTS
    n_idx = jnp.arange(p, dtype=f32)
    return {
        'x': nrm((BATCH, SEQ, dm), 1.0),
        'c': nrm((BATCH, dm), 1.0),
        'ctx': nrm((BATCH, CTX_LEN, dm), 1.0),
        'c_ctx': nrm((dm,), 1.0),
        'w_ada': nrm((DEPTH, dm, 6 * dm), 0.5 * dm ** -0.5),
        'b_ada': nrm((DEPTH, 6 * dm), 0.02),
        'norm_mix_g': gain((DEPTH, dm)),
        'norm_ffn_g': gain((DEPTH, dm)),
        'w_in': nrm((DEPTH, dm, N_IN), dm ** -0.5),
        'da_qk_norm_g': gain((DEPTH, 2, DA_DIM)),
        'da_lambda': nrm((DEPTH, 4, DA_DIM), 0.1),
        'da_subln_g': gain((DEPTH, DA_VDIM)),
        's5_lam_re': -0.5 + nrm((DEPTH, 2, g, p), 0.01),
        's5_lam_im': jnp.broadcast_to(math.pi * n_idx, (DEPTH, 2, g, p)),
        's5_log_dt': unif((DEPTH, 2, g), math.log(1e-3), math.log(1e-1)),
        's5_b_re': nrm((DEPTH, 2, g, p, ch), (2 * ch) ** -0.5),
        's5_b_im': nrm((DEPTH, 2, g, p, ch), (2 * ch) ** -0.5),
        's5_c_re': nrm((DEPTH, 2, g, ch, p), p ** -0.5),
        's5_c_im': nrm((DEPTH, 2, g, ch, p), p ** -0.5),
        's5_d': nrm((DEPTH, g, ch), 1.0),
        's5_w_glu': nrm((DEPTH, S5_WIDTH, S5_WIDTH), S5_WIDTH ** -0.5),
        's5_b_glu': nrm((DEPTH, S5_WIDTH), 0.02),
        'mla_cq_norm_g': gain((DEPTH, MLA_Q_RANK)),
        'mla_ckv_norm_g': gain((DEPTH, MLA_KV_RANK)),
        'mla_w_uq': nrm((DEPTH, MLA_Q_RANK, MLA_HEADS * (MLA_NOPE + MLA_ROPE)), MLA_Q_RANK ** -0.5),
        'mla_w_ukv': nrm((DEPTH, MLA_KV_RANK, MLA_HEADS * (MLA_NOPE + MLA_VDIM)), MLA_KV_RANK ** -0.5),
        'mla_qk_norm_g': gain((DEPTH, 2, MLA_NOPE + MLA_ROPE)),
        'rw_mu': unif((DEPTH, 4, RW_WIDTH), 0.0, 1.0),
        'rw_w0': nrm((DEPTH, 2, RW_WIDTH), 1.0),
        'rw_w1': nrm((DEPTH, 2, RW_WIDTH, RW_DECAY_RANK), RW_WIDTH ** -0.5),
        'rw_w2': nrm((DEPTH, 2, RW_DECAY_RANK, RW_WIDTH), 0.1 * RW_DECAY_RANK ** -0.5),
        'rw_a0': nrm((DEPTH, 2, RW_WIDTH), 0.5),
        'rw_a1': nrm((DEPTH, 2, RW_WIDTH, RW_ICL_RANK), RW_WIDTH ** -0.5),
        'rw_a2': nrm((DEPTH, 2, RW_ICL_RANK, RW_WIDTH), 0.1 * RW_ICL_RANK ** -0.5),
        'rw_g1': nrm((DEPTH, RW_WIDTH, RW_GATE_RANK), RW_WIDTH ** -0.5),
        'rw_g2': nrm((DEPTH, RW_GATE_RANK, RW_WIDTH), RW_GATE_RANK ** -0.5),
        'rw_k_k': 0.85 + nrm((DEPTH, RW_WIDTH), 0.05),
        'rw_k_a': gain((DEPTH, RW_WIDTH)),
        'rw_r_k': nrm((DEPTH, RW_HEADS, RW_DIM), 0.1),
        'rw_ln_g': gain((DEPTH, RW_WIDTH)),
        'rw_ln_b': nrm((DEPTH, RW_WIDTH), 0.02),
        'w_branch': nrm((DEPTH, N_BRANCH, BRANCH_WIDTH, dm), BRANCH_WIDTH ** -0.5),
        'w_out': nrm((DEPTH, dm, dm), dm ** -0.5),
        'router_w': nrm((dm, e), dm ** -0.5),
        'router_bias': nrm((e,), 0.01),
        'exp_w_gate': nrm((DEPTH, e, dm, D_FF), dm ** -0.5),
        'exp_w_up': nrm((DEPTH, e, dm, D_FF), dm ** -0.5),
        'exp_w_down': nrm((DEPTH, e, D_FF, dm), D_FF ** -0.5),
    }


def reference(x, c, ctx, c_ctx, w_ada, b_ada, norm_mix_g, norm_ffn_g, w_in,
              da_qk_norm_g, da_lambda, da_subln_g,
              s5_lam_re, s5_lam_im, s5_log_dt, s5_b_re, s5_b_im, s5_c_re, s5_c_im, s5_d, s5_w_glu, s5_b_glu,
              mla_cq_norm_g, mla_ckv_norm_g, mla_w_uq, mla_w_ukv, mla_qk_norm_g,
              rw_mu, rw_w0, rw_w1, rw_w2, rw_a0, rw_a1, rw_a2, rw_g1, rw_g2, rw_k_k, rw_k_a, rw_r_k,
              rw_ln_g, rw_ln_b,
              w_branch, w_out, router_w, router_bias, exp_w_gate, exp_w_up, exp_w_down):
    b, n_lat, _ = x.shape
    cos_da, sin_da = axial_rope_tables(n_lat, DA_DIM)
    cos_mla, sin_mla = axial_rope_tables(n_lat, MLA_ROPE)
    cx = ctx
    for l in range(DEPTH):
        need_ctx = l < DEPTH - 1
        lambda_init = 0.8 - 0.6 * math.exp(-0.3 * l)
        mod = (jax.nn.silu(c) @ w_ada[l] + b_ada[l]).reshape(b, 6, 1, D_MODEL)
        mod_c = (jax.nn.silu(c_ctx) @ w_ada[l] + b_ada[l]).reshape(6, D_MODEL)

        h_l = modulate(x, norm_mix_g[l], mod[:, 0], mod[:, 1])
        h_c = modulate(cx, norm_mix_g[l], mod_c[0], mod_c[1])
        p_l = jnp.split(h_l @ w_in[l], IN_OFFSETS, axis=-1)
        p_c = jnp.split(h_c @ w_in[l], IN_OFFSETS, axis=-1)
        ya_l, ya_c = diff_attention(p_l[0], p_l[1], p_l[2], p_c[0], p_c[1], p_c[2], cos_da, sin_da,
                                    da_qk_norm_g[l], da_lambda[l], da_subln_g[l], lambda_init, need_ctx)
        yb_l, yb_c = s5_mixer(p_l[3], p_c[3], s5_lam_re[l], s5_lam_im[l], s5_log_dt[l], s5_b_re[l],
                              s5_b_im[l], s5_c_re[l], s5_c_im[l], s5_d[l], s5_w_glu[l], s5_b_glu[l], need_ctx)
        yc_l, yc_c = mla(p_l[4], p_l[5], p_l[6], p_c[4], p_c[5], p_c[6], cos_mla, sin_mla,
                         mla_cq_norm_g[l], mla_ckv_norm_g[l], mla_w_uq[l], mla_w_ukv[l], mla_qk_norm_g[l],
                         need_ctx)
        yd_l, yd_c = rwkv_mixer(p_l[7], p_l[8], p_l[9], p_l[10], p_c[7], p_c[8], p_c[9], p_c[10],
                                rw_mu[l], rw_w0[l], rw_w1[l], rw_w2[l], rw_a0[l], rw_a1[l], rw_a2[l],
                                rw_g1[l], rw_g2[l], rw_k_k[l], rw_k_a[l], rw_r_k[l], rw_ln_g[l], rw_ln_b[l],
                                need_ctx)
        x = x + mod[:, 2] * merge_branches([ya_l, yb_l, yc_l, yd_l], p_l[11], w_branch[l], w_out[l])
        if need_ctx:
            cx = cx + mod_c[2] * merge_branches([ya_c, yb_c, yc_c, yd_c], p_c[11], w_branch[l], w_out[l])

        f_l = modulate(x, norm_ffn_g[l], mod[:, 3], mod[:, 4])
        if need_ctx:
            f_c = modulate(cx, norm_ffn_g[l], mod_c[3], mod_c[4])
            toks = jnp.concatenate([f_l.reshape(-1, D_MODEL), f_c.reshape(-1, D_MODEL)], axis=0)
            y = moe(toks, router_w, router_bias, exp_w_gate[l], exp_w_up[l], exp_w_down[l])
            x = x + mod[:, 5] * y[: b * n_lat].reshape(x.shape)
            cx = cx + mod_c[5] * y[b * n_lat:].reshape(cx.shape)
        else:
            y = moe(f_l.reshape(-1, D_MODEL), router_w, router_bias, exp_w_gate[l], exp_w_up[l], exp_w_down[l])
            x = x + mod[:, 5] * y.reshape(x.shape)
    return x
```

```python
import math
import os
import numpy as np
import concourse.bass as bass
import concourse.mybir as mybir
from concourse.bass_utils import run_bass_kernel_spmd

F32 = mybir.dt.float32
BF16 = mybir.dt.bfloat16
I32 = mybir.dt.int32
AF = mybir.ActivationFunctionType
ALU = mybir.AluOpType
AX = mybir.AxisListType

D = 1024
NT = 2304
NCTX = 256
NLAT = 2048
DEPTH = 2
EPS = 1e-6
GROUPS = [(0, 256)] + [(256 + 512 * i, 512) for i in range(4)]
NTT = 18


class Buf:
    __slots__ = ("name", "w", "r", "excl")

    def __init__(self, name="", excl=False):
        self.name = name
        self.w = None
        self.r = {}
        self.excl = excl


class Prog:
    NDMA = 8

    def __init__(self, nc):
        self.nc = nc
        self.eng = {"pe": nc.tensor, "act": nc.scalar, "dve": nc.vector, "pool": nc.gpsimd, "sp": nc.sync}
        self.sems = {}
        self.cnt = {}
        self.seen = {e: {} for e in self.eng}
        self.pend = {e: ([], []) for e in self.eng}
        for e in self.eng:
            self.sems[e] = nc.alloc_semaphore("s_" + e)
            self.cnt[e] = 0
        self.dq = {}
        for q in ("sp", "pool"):
            lst = []
            for i in range(self.NDMA):
                key = ("d", q, i)
                self.sems[key] = nc.alloc_semaphore(f"d_{q}{i}")
                self.cnt[key] = 0
                lst.append(key)
            self.dq[q] = [lst, 0, [None] * self.NDMA]
        self.ninst = 0

    def _wait(self, e, tok):
        key, val = tok
        if key == e:
            if e == "pe":
                return
            if val < self.cnt[e] - 1:
                return
        if self.seen[e].get(key, 0) >= val:
            return
        self.eng[e].wait_ge(self.sems[key], val)
        self.seen[e][key] = val
        self.ninst += 1

    def _deps(self, e, reads, writes):
        for b in reads:
            if b.w is not None:
                self._wait(e, b.w)
            if b.excl:
                for tok in b.r.values():
                    if tok[0] != e:
                        self._wait(e, tok)
        for b in writes:
            if b.w is not None:
                self._wait(e, b.w)
            for tok in b.r.values():
                if tok[0] != e:
                    self._wait(e, tok)

    def op(self, e, fn, reads=(), writes=(), inc=True):
        self._deps(e, reads, writes)
        inst = fn(self.eng[e])
        self.ninst += 1
        pr, pw = self.pend[e]
        pr.extend(reads)
        pw.extend(writes)
        if inc:
            self.cnt[e] += 1
            tok = (e, self.cnt[e])
            inst.then_inc(self.sems[e], 1)
            for b in pr:
                b.r[e] = tok
            for b in pw:
                b.w = tok
                b.r = {}
            self.pend[e] = ([], [])
        return inst

    def dma(self, q, out, in_, reads=(), writes=(), **kw):
        lst, rr, last = self.dq[q]
        k = rr
        self.dq[q][1] = (rr + 1) % self.NDMA
        if last[k] is not None:
            self._wait(q, last[k])
        self._deps(q, reads, writes)
        inst = self.eng[q].dma_start(out=out, in_=in_, **kw)
        self.ninst += 1
        key = lst[k]
        self.cnt[key] += 16
        tok = (key, self.cnt[key])
        inst.then_inc(self.sems[key], 16)
        last[k] = tok
        for b in reads:
            b.r[key] = tok
        for b in writes:
            b.w = tok
            b.r = {}
        return tok

    def finish(self, bufs, e="sp"):
        for b in bufs:
            if b.w is not None:
                self._wait(e, b.w)
            for tok in b.r.values():
                self._wait(e, tok)

    def barrier(self):
        toks = [(k, v) for k, v in self.cnt.items() if v > 0]
        for e in self.eng:
            for tok in toks:
                if tok[0] != e:
                    self._wait(e, tok)


from contextlib import ExitStack


class K:
    def __init__(self, nc, dbg=None):
        self.nc = nc
        self.P = Prog(nc)
        self.dbg = dbg or {}
        self.ins = {}
        self.uid = 0
        self.ps = []
        self.pb = []
        for i in range(8):
            self.ps.append(nc.alloc_psum_tensor(f"ps{i}", [128, 512], F32))
            self.pb.append(Buf(f"ps{i}", excl=True))
        self.rr = 0
        self.rrset = list(range(8))

    def din(self, name, shape, dtype=F32):
        t = self.nc.dram_tensor(name, list(shape), dtype, kind="ExternalInput").ap()
        self.ins[name] = t
        return t

    def sb(self, stack, shape, dtype, name=None):
        self.uid += 1
        nm = f"{name or 't'}_{self.uid}"
        t = stack.enter_context(self.nc.sbuf_tensor(nm, list(shape), dtype))
        nb = int(np.prod(shape[1:])) * (2 if dtype == BF16 else 4)
        self.live = getattr(self, "live", 0) + nb
        if self.live > getattr(self, "peak", 0):
            self.peak = self.live; self.peak_at = nm
        def _dec(nb=nb):
            self.live -= nb
        stack.callback(_dec)
        return t, Buf(nm)

    def bank(self):
        i = self.rrset[self.rr % len(self.rrset)]
        self.rr += 1
        return i

    def mm(self, bank, out, lhsT, rhs, start, stop, reads, inc=None):
        self.P.op("pe", lambda e: e.matmul(out, lhsT=lhsT, rhs=rhs, start=start, stop=stop, skip_group_check=True),
                  reads=reads, writes=[self.pb[bank]], inc=(stop if inc is None else inc))

    def act(self, out, in_, func, reads, writes, scale=None, bias=None, eng="act"):
        kw = {}
        if scale is not None:
            kw["scale"] = scale
        if bias is not None:
            kw["bias"] = bias
        self.P.op("act", lambda e: e.activation(out=out, in_=in_, func=func, **kw), reads=reads, writes=writes)

    def tt(self, eng, out, in0, in1, op, reads, writes):
        self.P.op(eng, lambda e: e.tensor_tensor(out=out, in0=in0, in1=in1, op=op), reads=reads, writes=writes)

    def ts(self, eng, out, in0, s1, s2, op0, op1, reads, writes):
        if op1 is None:
            self.P.op(eng, lambda e: e.tensor_scalar(out=out, in0=in0, scalar1=s1, scalar2=None, op0=op0),
                      reads=reads, writes=writes)
        else:
            self.P.op(eng, lambda e: e.tensor_scalar(out=out, in0=in0, scalar1=s1, scalar2=s2, op0=op0, op1=op1),
                      reads=reads, writes=writes)

    def stt(self, out, in0, scalar, in1, op0, op1, reads, writes):
        self.P.op("dve", lambda e: e.scalar_tensor_tensor(out=out, in0=in0, scalar=scalar, in1=in1, op0=op0, op1=op1),
                  reads=reads, writes=writes)

    def copy(self, eng, out, in_, reads, writes):
        if eng == "act":
            self.P.op("act", lambda e: e.copy(out=out, in_=in_), reads=reads, writes=writes)
        else:
            self.P.op(eng, lambda e: e.tensor_copy(out=out, in_=in_), reads=reads, writes=writes)

    def memset(self, eng, ap, val, writes):
        self.P.op(eng, lambda e: e.memset(ap, val), writes=writes)

    def load(self, q, out, in_, writes, reads=()):
        self.P.dma(q, out, in_, reads=reads, writes=writes)

    def rstd_from_ss(self, out, ss_ps, bank, inv_n, tmp, tmpb, writes):
        self.P.op("act", lambda e: e.activation(out=tmp, in_=ss_ps, func=AF.Ln, scale=inv_n, bias=self.eps_col[:]),
                  reads=[self.pb[bank], self.b_const], writes=[tmpb])
        self.P.op("act", lambda e: e.activation(out=out, in_=tmp, func=AF.Exp, scale=-0.5), reads=[tmpb], writes=writes)


def host_consts():
    ident = np.eye(128, dtype=np.float32)
    bo32 = np.kron(np.eye(4, dtype=np.float32), np.ones((32, 32), np.float32))
    bo64 = np.kron(np.eye(2, dtype=np.float32), np.ones((64, 64), np.float32))
    Rda = np.zeros((128, 128), np.float32)
    for h in range(4):
        for d in range(16):
            Rda[32 * h + d, 32 * h + d + 16] = -1.0
            Rda[32 * h + d + 16, 32 * h + d] = 1.0
    Rmla = np.zeros((128, 128), np.float32)
    for h in range(2):
        for d in range(8):
            Rmla[64 * h + 32 + d, 64 * h + 40 + d] = -1.0
            Rmla[64 * h + 40 + d, 64 * h + 32 + d] = 1.0
    cst = np.concatenate([ident, bo32, bo64, Rda.T.copy(), Rmla.T.copy()], axis=1)
    def tables(rot_dim):
        rows = NLAT // 64
        row = np.repeat(np.arange(rows, dtype=np.float32), 64)
        col = np.tile(np.arange(64, dtype=np.float32), rows)
        n_freq = rot_dim // 4
        inv = (10000.0 ** (-np.arange(n_freq, dtype=np.float32) / n_freq)).astype(np.float32)
        ang = np.concatenate([row[:, None] * inv, col[:, None] * inv], axis=-1)
        return np.cos(ang).astype(np.float32), np.sin(ang).astype(np.float32)
    c, s = tables(32)
    da_cos = np.ones((128, NT), np.float32)
    da_sin = np.zeros((128, NT), np.float32)
    for h in range(4):
        for d in range(32):
            da_cos[32 * h + d, NCTX:] = c[:, d % 16]
            da_sin[32 * h + d, NCTX:] = s[:, d % 16]
    c, s = tables(16)
    ml_cos = np.ones((128, NT), np.float32)
    ml_sin = np.zeros((128, NT), np.float32)
    for h in range(2):
        for d in range(16):
            ml_cos[64 * h + 32 + d, NCTX:] = c[:, d % 8]
            ml_sin[64 * h + 32 + d, NCTX:] = s[:, d % 8]
    return dict(cst=cst, da_cos=da_cos, da_sin=da_sin, ml_cos=ml_cos, ml_sin=ml_sin)


def fm_cols(v, ntile):
    return np.ascontiguousarray(np.asarray(v, np.float32).reshape(ntile, 128).T)


def build(stages, dbg_names=()):
    nc = bass.Bass("TRN2", target_bir_lowering=False)
    k = K(nc)
    P = k.P
    top = ExitStack()
    xin = k.din("xin", [NT, D])
    c2T = k.din("c2T", [128, 8, 2])
    cst_d = k.din("cst", [128, 640])
    da_cos_d = k.din("da_cos", [128, NT]); da_sin_d = k.din("da_sin", [128, NT])
    ml_cos_d = k.din("ml_cos", [128, NT]); ml_sin_d = k.din("ml_sin", [128, NT])
    w_ada = k.din("w_ada", [DEPTH, D, 6 * D])
    bada_d = k.din("bada", [DEPTH, 128, 48])
    gmix_d = k.din("gmix", [DEPTH, 128, 8]); gffn_d = k.din("gffn", [DEPTH, 128, 8])
    w_da_qk = k.din("w_da_qk", [DEPTH, D, 768]); w_da_v = k.din("w_da_v", [DEPTH, D, 256])
    da_g_d = k.din("da_g", [DEPTH, 128, 2])
    da_lam_d = k.din("da_lam", [DEPTH, 128, 128])
    da_sub_d = k.din("da_sub", [DEPTH, 128, 64])
    w_mla_c = k.din("w_mla_c", [DEPTH, D, 384]); w_mla_kr = k.din("w_mla_kr", [DEPTH, D, 128])
    w_uq_d = k.din("w_uq", [DEPTH, 128, 2, 256]); w_ukvk_d = k.din("w_ukvk", [DEPTH, 128, 256]); w_ukvv_d = k.din("w_ukvv", [DEPTH, 128, 256])
    mla_gc_d = k.din("mla_gc", [DEPTH, 128, 3])
    mla_g_d = k.din("mla_g", [DEPTH, 128, 2])
    w_gates_d = k.din("w_gates", [DEPTH, 8, 128, 4096]); w_branch_d = k.din("w_branch", [DEPTH, 128, 8192]); w_out_d = k.din("w_out", [DEPTH, D, D])
    router_w_d = k.din("router_w", [D, 16]); router_b_d = k.din("router_b", [128, 16]); sel_d = k.din("sel", [16, 16, 128])
    exp_g_d = k.din("exp_w_gate", [DEPTH, 16, D, 512]); exp_u_d = k.din("exp_w_up", [DEPTH, 16, D, 512]); exp_d_d = k.din("exp_w_down", [DEPTH, 16, 512, D])
    w_s5_d = k.din("w_s5", [DEPTH, D, 256])
    s5B_d = k.din("s5B", [DEPTH, 2, 2, 128, 1024])
    s5C_d = k.din("s5C", [DEPTH, 2, 2, 128, 8, 128])
    s5tm_d = k.din("s5tm", [DEPTH, 2, 3, 128, 1024])
    s5fm_d = k.din("s5fm", [DEPTH, 2, 128, 3, 8])
    s5d_d = k.din("s5d", [DEPTH, 128, 2]); s5bg_d = k.din("s5bg", [DEPTH, 128, 2])
    w_glu_d = k.din("w_glu", [DEPTH, 256, 256])
    posc_d = k.din("posc", [2, 128, 2]); posr_d = k.din("posr", [2, 128, 128]); tri_d = k.din("tri", [2, 128, 128])
    w_rw_d = k.din("w_rw", [DEPTH, D, 1024]); rw_mu_d = k.din("rw_mu_row", [DEPTH, 128, 1024])
    rw_cols_d = k.din("rw_cols", [DEPTH, 128, 2, 9])
    rw_w0row_d = k.din("rw_w0row", [DEPTH, 2, 1, 256])
    rw_w1_d = k.din("rw_w1", [DEPTH, 2, 256, 32]); rw_w2_d = k.din("rw_w2", [DEPTH, 2, 32, 256])
    rw_a1_d = k.din("rw_a1", [DEPTH, 2, 256, 32]); rw_a2_d = k.din("rw_a2", [DEPTH, 2, 32, 256])
    rw_g1_d = k.din("rw_g1", [DEPTH, 256, 64]); rw_g2_d = k.din("rw_g2", [DEPTH, 64, 256])
    rw_tri_d = k.din("rw_tri", [2, 128, 2, 128]); rw_ind_d = k.din("rw_ind", [128, 2])
    rw_m1_d = k.din("rw_m1", [2, 128, 256]); rw_mnt_d = k.din("rw_mnt", [2, 128, 128])
    out_d = nc.dram_tensor("out", [NLAT, D], F32, kind="ExternalOutput").ap()
    dbg_out = {}

    xT, b_xT = k.sb(top, [128, 8, NT], F32, "xT")
    cstf, b_cstf = k.sb(top, [128, 640], F32, "cstf")
    cstb, b_cstb = k.sb(top, [128, 640], BF16, "cstb")
    onesb, b_ones = k.sb(top, [128, 128], BF16, "ones")
    k.eps_col, k.b_const = k.sb(top, [128, 1], F32, "eps")
    MOD, b_MOD = k.sb(top, [128, DEPTH, 6, 8, 2], F32, "MOD")
    AB, b_AB = k.sb(top, [128, DEPTH, 2, 2, 8, 2], F32, "AB")
    rstd_b, b_rstd = k.sb(top, [128, NT], F32, "rstd")
    yall, b_yall = k.sb(top, [128, 4, 2, NT], BF16, "yall")
    yT = []
    for i in range(4):
        yT.append((yall[:, i], Buf(f"y{i}")))
    k.memset("dve", k.eps_col[:], EPS, [k.b_const])
    k.memset("dve", onesb[:], 1.0, [b_ones])
    k.load("sp", cstf[:], cst_d, [b_cstf])
    k.copy("dve", cstb[:], cstf[:], [b_cstf], [b_cstb])
    identf = cstf[:, 0:128]
    identb = cstb[:, 0:128]
    bo32 = cstb[:, 128:256]; bo64 = cstb[:, 256:384]; Rda = cstb[:, 384:512]; Rmla = cstb[:, 512:640]

    def dump(name, ap_sb, buf, shape, dtype=F32):
        if name in dbg_names:
            d = nc.dram_tensor("dbg_" + name, list(shape), dtype, kind="ExternalOutput").ap()
            dbg_out[name] = d
            P.dma("sp", d, ap_sb, reads=[buf])
            P.barrier()

    with ExitStack() as st:
        xs = [k.sb(st, [128, D], F32, "xstage") for _ in range(2)]
        for tt in range(NTT):
            xt, xb = xs[tt % 2]
            k.load("sp", xt[:], xin[tt * 128:(tt + 1) * 128, :], [xb])
            for half in range(2):
                bk = k.bank()
                for j in range(4):
                    kt = half * 4 + j
                    P.op("pe", lambda e: e.transpose(k.ps[bk][:, j * 128:(j + 1) * 128], xt[:, kt * 128:(kt + 1) * 128], identf),
                         reads=[xb, b_cstf], writes=[k.pb[bk]], inc=(j == 3))
                k.copy("dve" if half == 0 else "act", xT[:, half * 4:half * 4 + 4, tt * 128:(tt + 1) * 128],
                       k.ps[bk][:].rearrange("p (j t) -> p j t", j=4), [k.pb[bk]], [b_xT])
        scT, b_scT = k.sb(st, [128, 8, 2], F32, "scT")
        scb, b_scb = k.sb(st, [128, 8, 2], BF16, "scb")
        bada, b_bada = k.sb(st, [128, DEPTH, 48], F32, "bada")
        gm, b_gm = k.sb(st, [128, DEPTH, 2, 8], F32, "gm")
        k.load("sp", scT[:], c2T, [b_scT])
        k.act(scb[:], scT[:], AF.Silu, [b_scT], [b_scb])
        for l in range(DEPTH):
            k.load("sp", bada[:, l, :], bada_d[l], [b_bada])
            k.load("sp", gm[:, l, 0, :], gmix_d[l], [b_gm])
            k.load("sp", gm[:, l, 1, :], gffn_d[l], [b_gm])
        wa = [k.sb(st, [128, 8, 1024], BF16, "wada") for _ in range(2)]
        ci = 0
        for l in range(DEPTH):
            for ch in range(6):
                wt, wb = wa[ci % 2]; ci += 1
                k.load("pool", wt[:], w_ada[l, :, ch * 1024:(ch + 1) * 1024].rearrange("(kt p) n -> p kt n", p=128), [wb])
                bk = k.bank()
                for ft in range(8):
                    for kt in range(8):
                        k.mm(bk, k.ps[bk][:, ft * 2:ft * 2 + 2], wt[:, kt, ft * 128:(ft + 1) * 128], scb[:, kt, :],
                             kt == 0, kt == 7, [wb, b_scb])
                k.tt("dve", MOD[:, l, ch, :, :], k.ps[bk][:, 0:16].rearrange("p (f j) -> p f j", j=2),
                     bada[:, l, ch * 8:(ch + 1) * 8].unsqueeze(2).to_broadcast([128, 8, 2]), ALU.add,
                     [k.pb[bk], b_bada], [b_MOD])
        for l in range(DEPTH):
            for m in range(2):
                sh, sc = (0, 1) if m == 0 else (3, 4)
                P.op("dve", lambda e: e.scalar_tensor_tensor(out=AB[:, l, m, 0, :, :], in0=MOD[:, l, sc, :, :], scalar=1.0,
                                                             in1=gm[:, l, m, :].unsqueeze(2).to_broadcast([128, 8, 2]),
                                                             op0=ALU.add, op1=ALU.mult),
                     reads=[b_MOD, b_gm], writes=[b_AB])
                k.copy("dve", AB[:, l, m, 1, :, :], MOD[:, l, sh, :, :], [b_MOD], [b_AB])
        P.barrier()
    dump("xT", xT[:], b_xT, [128, 8, NT])
    dump("MOD", MOD[:], b_MOD, [128, DEPTH, 6, 8, 2])

    def jof(g):
        return 1 if g == 0 else 0

    def rms_stats(st):
        sq = [k.sb(st, [128, 512], BF16, "sq") for _ in range(2)]
        lnt, b_lnt = k.sb(st, [128, 512], F32, "lnt")
        i = 0
        for (c0, n) in GROUPS:
            bk = k.bank()
            for kt in range(8):
                s, sbf = sq[i % 2]; i += 1
                k.act(s[:, :n], xT[:, kt, c0:c0 + n], AF.Square, [b_xT], [sbf])
                k.mm(bk, k.ps[bk][:, :n], onesb[:], s[:, :n], kt == 0, kt == 7, [sbf, b_ones])
            k.rstd_from_ss(rstd_b[:, c0:c0 + n], k.ps[bk][:, :n], bk, 1.0 / D, lnt[:, :n], b_lnt, [b_rstd])

    def h_group(l, m, g, hg, hb, tmp, tmpb):
        c0, n = GROUPS[g]
        j = jof(g)
        for kt in range(8):
            k.tt("dve", tmp[:, :n], xT[:, kt, c0:c0 + n], rstd_b[:, c0:c0 + n], ALU.mult, [b_xT, b_rstd], [tmpb])
            P.op("act", lambda e: e.activation(out=hg[:, kt, :n], in_=tmp[:, :n], func=AF.Identity,
                                               scale=AB[:, l, m, 0, kt, j:j + 1], bias=AB[:, l, m, 1, kt, j:j + 1]),
                 reads=[tmpb, b_AB], writes=[hb])

    def load_w(q, st, src, ncols, name):
        wt, wb = k.sb(st, [128, 8, ncols], BF16, name)
        k.load(q, wt[:], src.rearrange("(kt p) n -> p kt n", p=128), [wb])
        return wt, wb

    def proj(bk, wt, wb, col0, hg, hb, n, start=True, stop=True):
        for kt in range(8):
            k.mm(bk, k.ps[bk][:, :n], wt[:, kt, col0:col0 + 128], hg[:, kt, :n], start and kt == 0, stop and kt == 7, [wb, hb])

    def headnorm_rope(st, src_bk, n, c0, blockones, inv_dim, gcol, gbuf, Rm, cos_d, sin_d, dst, dstb, scr, load=True):
        sq, b_sq, rs, b_rs, lnt, b_lnt, qn, b_qn, ct, b_ct, sn, b_sn, t1, b_t1, t2, b_t2 = scr
        src = k.ps[src_bk][:, :n]
        k.act(sq[:, :n], src, AF.Square, [k.pb[src_bk]], [b_sq])
        b2 = k.bank()
        k.mm(b2, k.ps[b2][:, :n], blockones, sq[:, :n], True, True, [b_sq, b_cstb])
        k.rstd_from_ss(rs[:, :n], k.ps[b2][:, :n], b2, inv_dim, lnt[:, :n], b_lnt, [b_rs])
        k.stt(qn[:, :n], src, gcol, rs[:, :n], ALU.mult, ALU.mult, [k.pb[src_bk], gbuf, b_rs], [b_qn])
        if load:
            k.load("sp", ct[:, :n], cos_d[:, c0:c0 + n], [b_ct])
            k.load("sp", sn[:, :n], sin_d[:, c0:c0 + n], [b_sn])
        b3 = k.bank()
        k.mm(b3, k.ps[b3][:, :n], Rm, qn[:, :n], True, True, [b_qn, b_cstb])
        k.tt("pool", t1[:, :n], qn[:, :n], ct[:, :n], ALU.mult, [b_qn, b_ct], [b_t1])
        k.tt("dve", t2[:, :n], k.ps[b3][:, :n], sn[:, :n], ALU.mult, [k.pb[b3], b_sn], [b_t2])
        k.tt("pool", dst, t1[:, :n], t2[:, :n], ALU.add, [b_t1, b_t2], [dstb])

    def norm_scratch(st):
        out = []
        for nm, dt in (("sq", BF16), ("rs", F32), ("lnt", F32), ("qn", BF16), ("ct", F32), ("sn", F32), ("t1", F32), ("t2", F32)):
            t, b = k.sb(st, [128, 512], dt, nm)
            out += [t, b]
        return out

    def attention(st, qT, b_q, kT, b_k, V, b_V, heads, scale, epilogue, skip_ctx=False):
        Et = [k.sb(st, [128, 512], BF16, "E") for _ in range(4)]
        sbanks = [0, 1, 2, 3]
        obanks = [4, 5, 6, 7]
        steps = []
        for g, (c0, n) in enumerate(GROUPS):
            if skip_ctx and g == 0:
                continue
            ktiles = [0, 1] if g == 0 else list(range(NTT))
            for hidx, (tile, pb, Kd, vh) in enumerate(heads):
                for ki, kt in enumerate(ktiles):
                    steps.append((g, c0, n, hidx, tile, pb, Kd, vh, ki, kt, len(ktiles)))

        def qk_exp(si):
            g, c0, n, hidx, tile, pb, Kd, vh, ki, kt, nk = steps[si]
            sbk = sbanks[si % 4]
            k.mm(sbk, k.ps[sbk][:, :n], kT[pb:pb + Kd, tile, kt * 128:(kt + 1) * 128], qT[pb:pb + Kd, tile, c0:c0 + n],
                 True, True, [b_k, b_q])
            E, Eb = Et[si % 4]
            k.act(E[:, :n], k.ps[sbk][:, :n], AF.Exp, [k.pb[sbk]], [Eb], scale=scale)
            return E, Eb

        oi = 0
        ob = None
        nxt = qk_exp(0)
        for si, (g, c0, n, hidx, tile, pb, Kd, vh, ki, kt, nk) in enumerate(steps):
            E, Eb = nxt
            if si + 1 < len(steps):
                nxt = qk_exp(si + 1)
            nq = n // 128
            if ki == 0:
                ob = obanks[oi % 4]; oi += 1
            for qi in range(nq):
                P.op("pe", lambda e: e.matmul(k.ps[ob][:, qi * 65:(qi + 1) * 65], lhsT=E[:, qi * 128:(qi + 1) * 128],
                                              rhs=V[:, kt, vh, :], start=(ki == 0 and qi == 0), stop=(ki == nk - 1),
                                              skip_group_check=True),
                     reads=[Eb, b_V], writes=[k.pb[ob]], inc=(qi == nq - 1))
            if ki == nk - 1:
                epilogue(g, c0, nq, hidx, ob)

    def transpose_out(ytok, b_ytok, nq, c0, ydst, b_ydst):
        for qi in range(nq):
            bk = k.bank()
            pv = k.ps[bk][:].bitcast(BF16)
            for tile in range(2):
                P.op("pe", lambda e: e.transpose(pv[:, tile * 128:(tile + 1) * 128], ytok[:, qi, tile * 128:(tile + 1) * 128], identb),
                     reads=[b_ytok, b_cstb], writes=[k.pb[bk]], inc=(tile == 1))
            k.copy("dve", ydst[:, :, c0 + qi * 128:c0 + (qi + 1) * 128], pv[:, 0:256].rearrange("p (j t) -> p j t", j=2),
                   [k.pb[bk]], [b_ydst])

    def da_mixer(l):
        lam_init = 0.8 - 0.6 * math.exp(-0.3 * l)
        ydst, b_ydst = yT[0]
        with ExitStack() as st:
            qT, b_q = k.sb(st, [128, 3, NT], BF16, "daq")
            kT, b_k = k.sb(st, [128, 3, NT], BF16, "dak")
            V, b_V = k.sb(st, [128, NTT, 4, 65], BF16, "dav")
            gcol, b_g = k.sb(st, [128, 2], F32, "dag")
            lam, b_lam = k.sb(st, [128, 128], F32, "dalam")
            lt, b_lt = k.sb(st, [128, 8], F32, "dalt")
            gsub, b_gsub = k.sb(st, [128, 64], F32, "dagsub")
            k.load("sp", gcol[:], da_g_d[l], [b_g])
            k.load("sp", lam[:], da_lam_d[l], [b_lam])
            k.load("sp", gsub[:], da_sub_d[l], [b_gsub])
            k.ts("dve", gsub[:], gsub[:], 1.0 - lam_init, None, ALU.mult, None, [b_gsub], [b_gsub])
            k.tt("dve", lam[:, 0:32], lam[:, 0:32], lam[:, 32:64], ALU.mult, [b_lam], [b_lam])
            k.tt("dve", lam[:, 64:96], lam[:, 64:96], lam[:, 96:128], ALU.mult, [b_lam], [b_lam])
            P.op("dve", lambda e: e.tensor_reduce(out=lt[:, 0:1], in_=lam[:, 0:32], axis=AX.X, op=ALU.add), reads=[b_lam], writes=[b_lt])
            P.op("dve", lambda e: e.tensor_reduce(out=lt[:, 1:2], in_=lam[:, 64:96], axis=AX.X, op=ALU.add), reads=[b_lam], writes=[b_lt])
            k.act(lt[:, 2:4], lt[:, 0:2], AF.Exp, [b_lt], [b_lt])
            k.tt("dve", lt[:, 4:5], lt[:, 3:4], lt[:, 2:3], ALU.subtract, [b_lt], [b_lt])
            k.ts("dve", lt[:, 4:5], lt[:, 4:5], -lam_init, None, ALU.add, None, [b_lt], [b_lt])
            k.memset("pool", V[:, :, :, 64:65], 1.0, [b_V])
            with ExitStack() as s2:
                wqk, b_wqk = load_w("pool", s2, w_da_qk[l], 768, "wqk")
                wv, b_wv = load_w("pool", s2, w_da_v[l], 256, "wv")
                hgs = [k.sb(s2, [128, 8, 512], BF16, "hg") for _ in range(2)]
                tmp, tmpb = k.sb(s2, [128, 512], F32, "htmp")
                scr = norm_scratch(s2)
                for g, (c0, n) in enumerate(GROUPS):
                    hg, hb = hgs[g % 2]
                    h_group(l, 0, g, hg, hb, tmp, tmpb)
                    for ti in range(6):
                        bk = k.bank()
                        proj(bk, wqk, b_wqk, ti * 128, hg, hb, n)
                        dst = (qT if ti < 3 else kT)
                        dstb = (b_q if ti < 3 else b_k)
                        headnorm_rope(s2, bk, n, c0, bo32, 1.0 / 32, gcol[:, (ti // 3):(ti // 3) + 1], b_g, Rda, da_cos_d, da_sin_d,
                                      dst[:, ti % 3, c0:c0 + n], dstb, scr, load=(ti == 0))
                    for qi in range(n // 128):
                        tt_ = (c0 + qi * 128) // 128
                        bk = k.bank()
                        for kt in range(8):
                            k.mm(bk, k.ps[bk][:, 0:256], hg[:, kt, qi * 128:(qi + 1) * 128], wv[:, kt, :], kt == 0, kt == 7, [hb, b_wv])
                        k.copy("act", V[:, tt_, :, 0:64], k.ps[bk][:, 0:256].rearrange("p (h d) -> p h d", h=4), [k.pb[bk]], [b_V])
                P.barrier()
            dump("da_q", qT[:], b_q, [128, 3, NT], BF16)
            dump("da_k", kT[:], b_k, [128, 3, NT], BF16)
            dump("da_v", V[:], b_V, [128, NTT, 4, 65], BF16)
            with ExitStack() as s3:
                ytok, b_ytok = k.sb(s3, [128, 4, 256], BF16, "ytok")
                o0, b_o0 = k.sb(s3, [128, 4, 64], F32, "o0")
                dd, b_dd = k.sb(s3, [128, 4, 64], F32, "dd")
                junk, b_junk = k.sb(s3, [128, 64], F32, "junk")
                rc, b_rc = k.sb(s3, [128, 4, 4], F32, "rc")
                lnt, b_lnt = k.sb(s3, [128, 4], F32, "lnt2")
                state = {}

                def epi(g, c0, nq, hidx, ob):
                    h, m = hidx // 2, hidx % 2
                    if m == 0:
                        state["ob0"] = ob
                        return
                    ob0 = state["ob0"]
                    O0 = k.ps[ob0][:, 0:nq * 65].rearrange("p (q c) -> p q c", c=65)
                    O1 = k.ps[ob][:, 0:nq * 65].rearrange("p (q c) -> p q c", c=65)
                    P.op("dve", lambda e: e.reciprocal(out=rc[:, 0, 0:nq], in_=O0[:, :, 64]), reads=[k.pb[ob0]], writes=[b_rc])
                    P.op("dve", lambda e: e.reciprocal(out=rc[:, 1, 0:nq], in_=O1[:, :, 64]), reads=[k.pb[ob]], writes=[b_rc])
                    k.ts("dve", rc[:, 1, 0:nq], rc[:, 1, 0:nq], lt[:, 4:5], None, ALU.mult, None, [b_rc, b_lt], [b_rc])
                    for qi in range(nq):
                        k.ts("dve", o0[:, qi, :], O0[:, qi, 0:64], rc[:, 0, qi:qi + 1], None, ALU.mult, None, [k.pb[ob0], b_rc], [b_o0])
                        k.stt(dd[:, qi, :], O1[:, qi, 0:64], rc[:, 1, qi:qi + 1], o0[:, qi, :], ALU.mult, ALU.add,
                              [k.pb[ob], b_rc, b_o0], [b_dd])
                        P.op("act", lambda e: e.activation(out=junk[:], in_=dd[:, qi, :], func=AF.Square, accum_out=rc[:, 2, qi:qi + 1]),
                             reads=[b_dd], writes=[b_junk, b_rc])
                    P.op("act", lambda e: e.activation(out=lnt[:, 0:nq], in_=rc[:, 2, 0:nq], func=AF.Ln, scale=1.0 / 64, bias=k.eps_col[:]),
                         reads=[b_rc, k.b_const], writes=[b_lnt])
                    k.act(rc[:, 3, 0:nq], lnt[:, 0:nq], AF.Exp, [b_lnt], [b_rc], scale=-0.5)
                    for qi in range(nq):
                        k.stt(ytok[:, qi, h * 64:(h + 1) * 64], dd[:, qi, :], rc[:, 3, qi:qi + 1], gsub[:], ALU.mult, ALU.mult,
                              [b_dd, b_rc, b_gsub], [b_ytok])
                    if h == 3:
                        k.rrset = [0, 1, 2, 3]
                        transpose_out(ytok, b_ytok, nq, c0, ydst, b_ydst)
                        k.rrset = list(range(8))

                heads = [(j // 3, 32 * (j % 3), 32, j // 2) for j in range(8)]
                attention(s3, qT, b_q, kT, b_k, V, b_V, heads, 32 ** -0.5, epi, skip_ctx=(l == DEPTH - 1))
                P.barrier()
        dump("ya", ydst[:], b_ydst, [128, 2, NT], BF16)

    def mla_mixer(l):
        ydst, b_ydst = yT[2]
        with ExitStack() as st:
            qT, b_q = k.sb(st, [128, 2, NT], BF16, "mlq")
            kT, b_k = k.sb(st, [128, 2, NT], BF16, "mlk")
            V, b_V = k.sb(st, [128, NTT, 4, 65], BF16, "mlv")
            gcol, b_g = k.sb(st, [128, 2], F32, "mlg")
            gc, b_gc = k.sb(st, [128, 3], F32, "mlgc")
            k.load("sp", gcol[:], mla_g_d[l], [b_g])
            k.load("sp", gc[:], mla_gc_d[l], [b_gc])
            k.memset("pool", V[:, :, :, 64:65], 1.0, [b_V])
            with ExitStack() as s2:
                wc, b_wc = load_w("pool", s2, w_mla_c[l], 384, "wc")
                wkr, b_wkr = load_w("pool", s2, w_mla_kr[l], 128, "wkr")
                wf, b_wf = k.sb(s2, [128, 4, 256], F32, "wf")
                wuq, b_wuq = k.sb(s2, [128, 2, 256], BF16, "wuq")
                wkk, b_wkk = k.sb(s2, [128, 256], BF16, "wkk")
                wvv, b_wvv = k.sb(s2, [128, 256], BF16, "wvv")
                k.load("sp", wf[:, 0:2, :], w_uq_d[l], [b_wf])
                k.load("sp", wf[:, 2, :], w_ukvk_d[l], [b_wf])
                k.load("sp", wf[:, 3, :], w_ukvv_d[l], [b_wf])
                for kt in range(2):
                    k.ts("dve", wuq[:, kt, :], wf[:, kt, :], gc[:, kt:kt + 1], None, ALU.mult, None, [b_wf, b_gc], [b_wuq])
                k.ts("dve", wkk[:], wf[:, 2, :], gc[:, 2:3], None, ALU.mult, None, [b_wf, b_gc], [b_wkk])
                k.ts("dve", wvv[:], wf[:, 3, :], gc[:, 2:3], None, ALU.mult, None, [b_wf, b_gc], [b_wvv])
                hgs = [k.sb(s2, [128, 8, 512], BF16, "hg") for _ in range(2)]
                tmp, tmpb = k.sb(s2, [128, 512], F32, "htmp")
                scr = norm_scratch(s2)
                sq2, b_sq2 = k.sb(s2, [128, 2, 512], BF16, "sq2")
                rsq, b_rsq = k.sb(s2, [128, 512], F32, "rsq")
                lnq, b_lnq = k.sb(s2, [128, 512], F32, "lnq")
                cqn, b_cqn = k.sb(s2, [128, 2, 512], BF16, "cqn")
                ckvn, b_ckvn = k.sb(s2, [128, 512], BF16, "ckvn")
                for g, (c0, n) in enumerate(GROUPS):
                    hg, hb = hgs[g % 2]
                    h_group(l, 0, g, hg, hb, tmp, tmpb)
                    bA = k.bank(); proj(bA, wc, b_wc, 0, hg, hb, n)
                    bB = k.bank(); proj(bB, wc, b_wc, 128, hg, hb, n)
                    k.act(sq2[:, 0, :n], k.ps[bA][:, :n], AF.Square, [k.pb[bA]], [b_sq2])
                    k.act(sq2[:, 1, :n], k.ps[bB][:, :n], AF.Square, [k.pb[bB]], [b_sq2])
                    bS = k.bank()
                    k.mm(bS, k.ps[bS][:, :n], onesb[:], sq2[:, 0, :n], True, False, [b_sq2, b_ones])
                    k.mm(bS, k.ps[bS][:, :n], onesb[:], sq2[:, 1, :n], False, True, [b_sq2, b_ones])
                    k.rstd_from_ss(rsq[:, :n], k.ps[bS][:, :n], bS, 1.0 / 192, lnq[:, :n], b_lnq, [b_rsq])
                    k.tt("dve", cqn[:, 0, :n], k.ps[bA][:, :n], rsq[:, :n], ALU.mult, [k.pb[bA], b_rsq], [b_cqn])
                    k.tt("dve", cqn[:, 1, :n], k.ps[bB][:, :n], rsq[:, :n], ALU.mult, [k.pb[bB], b_rsq], [b_cqn])
                    bC = k.bank(); proj(bC, wc, b_wc, 256, hg, hb, n)
                    k.act(sq2[:, 0, :n], k.ps[bC][:, :n], AF.Square, [k.pb[bC]], [b_sq2])
                    bS = k.bank()
                    k.mm(bS, k.ps[bS][:, :n], onesb[:], sq2[:, 0, :n], True, True, [b_sq2, b_ones])
                    k.rstd_from_ss(rsq[:, :n], k.ps[bS][:, :n], bS, 1.0 / 128, lnq[:, :n], b_lnq, [b_rsq])
                    k.tt("dve", ckvn[:, :n], k.ps[bC][:, :n], rsq[:, :n], ALU.mult, [k.pb[bC], b_rsq], [b_ckvn])
                    for tile in range(2):
                        bk = k.bank()
                        k.mm(bk, k.ps[bk][:, :n], wuq[:, 0, tile * 128:(tile + 1) * 128], cqn[:, 0, :n], True, False, [b_wuq, b_cqn])
                        k.mm(bk, k.ps[bk][:, :n], wuq[0:64, 1, tile * 128:(tile + 1) * 128], cqn[0:64, 1, :n], False, True, [b_wuq, b_cqn])
                        headnorm_rope(s2, bk, n, c0, bo64, 1.0 / 48, gcol[:, 0:1], b_g, Rmla, ml_cos_d, ml_sin_d,
                                      qT[:, tile, c0:c0 + n], b_q, scr, load=(tile == 0))
                    for tile in range(2):
                        bk = k.bank()
                        k.mm(bk, k.ps[bk][:, :n], wkk[:, tile * 128:(tile + 1) * 128], ckvn[:, :n], True, False, [b_wkk, b_ckvn])
                        for kt in range(8):
                            k.mm(bk, k.ps[bk][:, :n], wkr[:, kt, :], hg[:, kt, :n], False, kt == 7, [b_wkr, hb])
                        headnorm_rope(s2, bk, n, c0, bo64, 1.0 / 48, gcol[:, 1:2], b_g, Rmla, ml_cos_d, ml_sin_d,
                                      kT[:, tile, c0:c0 + n], b_k, scr, load=False)
                    for qi in range(n // 128):
                        tt_ = (c0 + qi * 128) // 128
                        bk = k.bank()
                        k.mm(bk, k.ps[bk][:, 0:256], ckvn[:, qi * 128:(qi + 1) * 128], wvv[:], True, True, [b_ckvn, b_wvv])
                        k.copy("act", V[:, tt_, :, 0:64], k.ps[bk][:, 0:256].rearrange("p (h d) -> p h d", h=4), [k.pb[bk]], [b_V])
                P.barrier()
            dump("ml_q", qT[:], b_q, [128, 2, NT], BF16)
            dump("ml_k", kT[:], b_k, [128, 2, NT], BF16)
            with ExitStack() as s3:
                ytok, b_ytok = k.sb(s3, [128, 4, 256], BF16, "ytok")
                rc, b_rc = k.sb(s3, [128, 4], F32, "rc")

                def epi(g, c0, nq, h, ob):
                    O = k.ps[ob][:, 0:nq * 65].rearrange("p (q c) -> p q c", c=65)
                    P.op("dve", lambda e: e.reciprocal(out=rc[:, 0:nq], in_=O[:, :, 64]), reads=[k.pb[ob]], writes=[b_rc])
                    for qi in range(nq):
                        k.ts("dve", ytok[:, qi, h * 64:(h + 1) * 64], O[:, qi, 0:64], rc[:, qi:qi + 1], None, ALU.mult, None,
                             [k.pb[ob], b_rc], [b_ytok])
                    if h == 3:
                        k.rrset = [0, 1, 2, 3]
                        transpose_out(ytok, b_ytok, nq, c0, ydst, b_ydst)
                        k.rrset = list(range(8))

                heads = [(h // 2, 64 * (h % 2), 48, h) for h in range(4)]
                attention(s3, qT, b_q, kT, b_k, V, b_V, heads, 48 ** -0.5, epi, skip_ctx=(l == DEPTH - 1))
                P.barrier()
        dump("yc", ydst[:], b_ydst, [128, 2, NT], BF16)

    def merge(l):
        with ExitStack() as st:
            wbr, b_wbr = k.sb(st, [128, 2, 4, D], BF16, "wbr")
            wout, b_wout = k.sb(st, [128, 8, D], BF16, "wout")
            wgs = [k.sb(st, [128, 8, 4, 128], BF16, "wg") for _ in range(2)]
            hgs = [k.sb(st, [128, 8, 512], BF16, "hg") for _ in range(2)]
            tmp, tmpb = k.sb(st, [128, 512], F32, "htmp")
            sig = [k.sb(st, [128, 512], F32, "sig") for _ in range(2)]
            prod = [k.sb(st, [128, 512], F32, "prod") for _ in range(2)]
            macc, b_macc = k.sb(st, [128, 512], F32, "macc")
            mbf, b_mbf = k.sb(st, [128, 8, 512], BF16, "mbf")
            wi = 0; si = 0
            for g, (c0, n) in enumerate(GROUPS):
                if l == DEPTH - 1 and g == 0:
                    continue
                j = jof(g)
                hg, hb = hgs[g % 2]
                h_group(l, 0, g, hg, hb, tmp, tmpb)
                for ct in range(8):
                    wg, b_wg = wgs[wi % 2]; wi += 1
                    k.load("pool", wg[:].rearrange("p a b c -> p (a b c)"), w_gates_d[l, ct], [b_wg])
                    if wi == 1:
                        k.load("pool", wbr[:].rearrange("p a b c -> p (a b c)"), w_branch_d[l], [b_wbr])
                    if wi == 3:
                        k.load("pool", wout[:], w_out_d[l].rearrange("(kt p) n -> p kt n", p=128), [b_wout])
                    for i in range(4):
                        bg = k.bank()
                        for kt in range(8):
                            k.mm(bg, k.ps[bg][:, :n], wg[:, kt, i, :], hg[:, kt, :n], kt == 0, kt == 7, [b_wg, hb])
                        sg, b_sg = sig[si % 2]; pr, b_pr = prod[si % 2]; si += 1
                        k.act(sg[:, :n], k.ps[bg][:, :n], AF.Sigmoid, [k.pb[bg]], [b_sg])
                        bp = k.bank()
                        for kt2 in range(2):
                            k.mm(bp, k.ps[bp][:, :n], wbr[:, kt2, i, ct * 128:(ct + 1) * 128], yT[i][0][:, kt2, c0:c0 + n],
                                 kt2 == 0, kt2 == 1, [b_wbr, yT[i][1]])
                        if i == 0:
                            k.tt("dve", macc[:, :n], k.ps[bp][:, :n], sg[:, :n], ALU.mult, [k.pb[bp], b_sg], [b_macc])
                        else:
                            k.tt("dve", pr[:, :n], k.ps[bp][:, :n], sg[:, :n], ALU.mult, [k.pb[bp], b_sg], [b_pr])
                            if i < 3:
                                k.tt("dve", macc[:, :n], macc[:, :n], pr[:, :n], ALU.add, [b_macc, b_pr], [b_macc])
                            else:
                                k.tt("dve", mbf[:, ct, :n], macc[:, :n], pr[:, :n], ALU.add, [b_macc, b_pr], [b_mbf])
                for co in range(8):
                    bo = k.bank()
                    for ct in range(8):
                        k.mm(bo, k.ps[bo][:, :n], wout[:, ct, co * 128:(co + 1) * 128], mbf[:, ct, :n], ct == 0, ct == 7, [b_wout, b_mbf])
                    k.stt(xT[:, co, c0:c0 + n], k.ps[bo][:, :n], MOD[:, l, 2, co, j:j + 1], xT[:, co, c0:c0 + n], ALU.mult, ALU.add,
                          [k.pb[bo], b_MOD, b_xT], [b_xT])
            P.barrier()
        dump("xmix", xT[:], b_xT, [128, 8, NT])

    def moe(l):
        fT = yall[:].rearrange("p i k t -> p (i k) t")
        b_fT = Buf("fT")
        with ExitStack() as st:
            rms_stats(st)
            P.barrier()
        with ExitStack() as st:
            combT, b_comb = k.sb(st, [16, NT], BF16, "combT")
            selb, b_sel = k.sb(st, [16, 16, 128], BF16, "selb")
            k.load("pool", selb[:], sel_d, [b_sel])
            wgs = [k.sb(st, [128, 8, 512], BF16, "ewg") for _ in range(2)]
            wus = [k.sb(st, [128, 8, 512], BF16, "ewu") for _ in range(2)]
            wds = [k.sb(st, [128, 4, D], BF16, "ewd") for _ in range(2)]

            def load_expert(e_):
                wg, b_wg = wgs[e_ % 2]; wu, b_wu = wus[e_ % 2]; wd, b_wd = wds[e_ % 2]
                k.load("pool", wg[:], exp_g_d[l, e_].rearrange("(kt p) n -> p kt n", p=128), [b_wg])
                k.load("pool", wu[:], exp_u_d[l, e_].rearrange("(kt p) n -> p kt n", p=128), [b_wu])
                k.load("pool", wd[:], exp_d_d[l, e_].rearrange("(kt p) n -> p kt n", p=128), [b_wd])
            load_expert(0)
            with ExitStack() as s2:
                tmp, tmpb = k.sb(s2, [128, 512], F32, "htmp")
                f32s = [k.sb(s2, [128, 512], F32, "f32") for _ in range(2)]
                rw, b_rw = k.sb(s2, [128, 8, 16], F32, "rw")
                k.load("sp", rw[:], router_w_d.rearrange("(kt p) n -> p kt n", p=128), [b_rw])
                rb, b_rb = k.sb(s2, [128, 16], F32, "rb")
                k.load("sp", rb[:], router_b_d, [b_rb])
                lg, b_lg = k.sb(s2, [128, NTT, 16], F32, "lg")
                fi_ = 0
                skip0 = (l == DEPTH - 1)
                if skip0:
                    k.memset("dve", lg[:, 0:2, :], 0.0, [b_lg])
                for g in range(5):
                    if skip0 and g == 0:
                        continue
                    c0, n = GROUPS[g]
                    j = jof(g)
                    nq = n // 128
                    bl = k.bank()
                    for kt in range(8):
                        k.tt("dve", tmp[:, :n], xT[:, kt, c0:c0 + n], rstd_b[:, c0:c0 + n], ALU.mult, [b_xT, b_rstd], [tmpb])
                        P.op("act", lambda e: e.activation(out=fT[:, kt, c0:c0 + n], in_=tmp[:, :n], func=AF.Identity,
                                                           scale=AB[:, l, 1, 0, kt, j:j + 1], bias=AB[:, l, 1, 1, kt, j:j + 1]),
                             reads=[tmpb, b_AB], writes=[b_fT])
                        f32, b_f32 = f32s[fi_ % 2]; fi_ += 1
                        k.ts("pool", f32[:, :n], tmp[:, :n], AB[:, l, 1, 0, kt, j:j + 1], AB[:, l, 1, 1, kt, j:j + 1], ALU.mult, ALU.add,
                             [tmpb, b_AB], [b_f32])
                        for qi in range(nq):
                            P.op("pe", lambda e: e.matmul(k.ps[bl][:, qi * 16:(qi + 1) * 16], lhsT=f32[:, qi * 128:(qi + 1) * 128], rhs=rw[:, kt, :],
                                                          start=(kt == 0 and qi == 0), stop=(kt == 7), skip_group_check=True),
                                 reads=[b_f32, b_rw], writes=[k.pb[bl]], inc=(qi == nq - 1))
                    tt0 = c0 // 128
                    k.copy("dve", lg[:, tt0:tt0 + nq, :], k.ps[bl][:, 0:nq * 16].rearrange("p (q e) -> p q e", e=16), [k.pb[bl]], [b_lg])
                r = {}
                NR = NTT * 16
                for nm, wd_ in (("sc", NR), ("bi", NR), ("m1", NR // 4), ("eq", NR), ("bi2", NR), ("m2", NR // 4),
                                ("gs", NR // 4), ("gm", NTT), ("gsel", NR // 4), ("sel", NR), ("w", NR), ("ws", NTT), ("cmb", NR)):
                    r[nm] = k.sb(s2, [128, wd_], F32, "r_" + nm)
                v4 = lambda ap: ap.rearrange("p (g e) -> p g e", e=4)
                b4 = lambda ap: ap.unsqueeze(2).to_broadcast([128, NR // 4, 4])
                sc, b_sc = r["sc"]; bi, b_bi = r["bi"]; m1, b_m1 = r["m1"]; eq, b_eq = r["eq"]; bi2, b_bi2 = r["bi2"]
                m2, b_m2 = r["m2"]; gs, b_gs = r["gs"]; gm_, b_gm_ = r["gm"]; gsel, b_gsel = r["gsel"]; sel, b_sl = r["sel"]
                w_, b_w = r["w"]; ws, b_ws = r["ws"]; cmb, b_cmb = r["cmb"]
                k.act(sc[:], lg[:].rearrange("p t e -> p (t e)"), AF.Sigmoid, [b_lg], [b_sc])
                k.tt("dve", sc[:].rearrange("p (t e) -> p t e", e=16) if False else bi[:].rearrange("p (t e) -> p t e", e=16),
                     sc[:].rearrange("p (t e) -> p t e", e=16), rb[:].unsqueeze(1).to_broadcast([128, NTT, 16]), ALU.add, [b_sc, b_rb], [b_bi])
                P.op("dve", lambda e: e.tensor_reduce(out=m1[:], in_=v4(bi[:]), axis=AX.X, op=ALU.max), reads=[b_bi], writes=[b_m1])
                k.tt("dve", v4(eq[:]), v4(bi[:]), b4(m1[:]), ALU.is_equal, [b_bi, b_m1], [b_eq])
                k.stt(bi2[:], eq[:], -1e9, bi[:], ALU.mult, ALU.add, [b_eq, b_bi], [b_bi2])
                P.op("dve", lambda e: e.tensor_reduce(out=m2[:], in_=v4(bi2[:]), axis=AX.X, op=ALU.max), reads=[b_bi2], writes=[b_m2])
                k.tt("dve", gs[:], m1[:], m2[:], ALU.add, [b_m1, b_m2], [b_gs])
                P.op("dve", lambda e: e.tensor_reduce(out=gm_[:], in_=v4(gs[:]), axis=AX.X, op=ALU.max), reads=[b_gs], writes=[b_gm_])
                k.tt("dve", v4(gsel[:]), v4(gs[:]), gm_[:].unsqueeze(2).to_broadcast([128, NTT, 4]), ALU.is_equal, [b_gs, b_gm_], [b_gsel])
                k.tt("dve", v4(sel[:]), v4(bi[:]), b4(m2[:]), ALU.is_ge, [b_bi, b_m2], [b_sl])
                k.tt("dve", v4(sel[:]), v4(sel[:]), b4(gsel[:]), ALU.mult, [b_sl, b_gsel], [b_sl])
                k.tt("dve", w_[:], sc[:], sel[:], ALU.mult, [b_sc, b_sl], [b_w])
                P.op("dve", lambda e: e.tensor_reduce(out=ws[:], in_=w_[:].rearrange("p (t e) -> p t e", e=16), axis=AX.X, op=ALU.add),
                     reads=[b_w], writes=[b_ws])
                P.op("dve", lambda e: e.reciprocal(out=ws[:], in_=ws[:]), reads=[b_ws], writes=[b_ws])
                k.tt("dve", cmb[:].rearrange("p (t e) -> p t e", e=16), w_[:].rearrange("p (t e) -> p t e", e=16),
                     ws[:].unsqueeze(2).to_broadcast([128, NTT, 16]), ALU.mult, [b_w, b_ws], [b_cmb])
                for tq in range(0, NTT, 4):
                    nn = min(4, NTT - tq)
                    bt = k.bank()
                    for i_ in range(nn):
                        P.op("pe", lambda e: e.transpose(k.ps[bt][0:16, i_ * 128:(i_ + 1) * 128], cmb[:, (tq + i_) * 16:(tq + i_ + 1) * 16], identf),
                             reads=[b_cmb, b_cstf], writes=[k.pb[bt]], inc=(i_ == nn - 1))
                    k.copy("act", combT[:, tq * 128:(tq + nn) * 128], k.ps[bt][0:16, 0:nn * 128], [k.pb[bt]], [b_comb])
                P.barrier()
            dump("combT", combT[:], b_comb, [16, NT], BF16)
            cbs = [k.sb(st, [128, 512], BF16, "cbs") for _ in range(2)]
            sl = [k.sb(st, [128, 512], F32, "esl") for _ in range(2)]
            tl = [k.sb(st, [128, 512], F32, "etl") for _ in range(2)]
            aa = [k.sb(st, [128, 4, 512], BF16, "eaa") for _ in range(2)]
            ci = 0; fi = 0
            for e_ in range(16):
                wg, b_wg = wgs[e_ % 2]; wu, b_wu = wus[e_ % 2]; wd, b_wd = wds[e_ % 2]
                if e_ > 0:
                    load_expert(e_)
                for g, (c0, n) in enumerate(GROUPS):
                    if l == DEPTH - 1 and g == 0:
                        continue
                    j = jof(g)
                    cb, b_cb = cbs[ci % 2]; a_, b_a = aa[ci % 2]; ci += 1
                    bc = k.bank()
                    k.mm(bc, k.ps[bc][:, :n], selb[:, e_, :], combT[:, c0:c0 + n], True, True, [b_sel, b_comb])
                    k.copy("act", cb[:, :n], k.ps[bc][:, :n], [k.pb[bc]], [b_cb])
                    for fj in range(4):
                        bg = k.bank()
                        for kt in range(8):
                            k.mm(bg, k.ps[bg][:, :n], wg[:, kt, fj * 128:(fj + 1) * 128], fT[:, kt, c0:c0 + n], kt == 0, kt == 7, [b_wg, b_fT])
                        bu = k.bank()
                        for kt in range(8):
                            k.mm(bu, k.ps[bu][:, :n], wu[:, kt, fj * 128:(fj + 1) * 128], fT[:, kt, c0:c0 + n], kt == 0, kt == 7, [b_wu, b_fT])
                        s_, b_s = sl[fi % 2]; t_, b_t = tl[fi % 2]; fi += 1
                        k.act(s_[:, :n], k.ps[bg][:, :n], AF.Silu, [k.pb[bg]], [b_s])
                        k.tt("dve", t_[:, :n], k.ps[bu][:, :n], s_[:, :n], ALU.mult, [k.pb[bu], b_s], [b_t])
                        k.tt("dve", a_[:, fj, :n], t_[:, :n], cb[:, :n], ALU.mult, [b_t, b_cb], [b_a])
                    for co in range(8):
                        bo = k.bank()
                        for fj in range(4):
                            k.mm(bo, k.ps[bo][:, :n], wd[:, fj, co * 128:(co + 1) * 128], a_[:, fj, :n], fj == 0, fj == 3, [b_wd, b_a])
                        k.stt(xT[:, co, c0:c0 + n], k.ps[bo][:, :n], MOD[:, l, 5, co, j:j + 1], xT[:, co, c0:c0 + n], ALU.mult, ALU.add,
                              [k.pb[bo], b_MOD, b_xT], [b_xT])
            P.barrier()
        dump("xout", xT[:], b_xT, [128, 8, NT])

    TWO_PI = 2.0 * math.pi

    def sincos(ang, b_ang, N, sc4, sin_out, cos_out, b_out):
        (kf, b_kf), (r_, b_r), (mk, b_mk), (sh, b_sh) = sc4
        ki = kf[:, :N].bitcast(I32)
        k.ts("dve", r_[:, :N], ang, 1.0 / TWO_PI, None, ALU.mult, None, [b_ang], [b_r])
        k.copy("dve", ki, r_[:, :N], [b_r], [b_kf])
        k.copy("dve", mk[:, :N], ki, [b_kf], [b_mk])
        k.stt(r_[:, :N], mk[:, :N], -TWO_PI, ang, ALU.mult, ALU.add, [b_mk, b_ang], [b_r])
        k.ts("dve", mk[:, :N], r_[:, :N], math.pi, -TWO_PI, ALU.is_gt, ALU.mult, [b_r], [b_mk])
        k.tt("dve", r_[:, :N], r_[:, :N], mk[:, :N], ALU.add, [b_r, b_mk], [b_r])
        k.ts("dve", mk[:, :N], r_[:, :N], -math.pi, TWO_PI, ALU.is_lt, ALU.mult, [b_r], [b_mk])
        k.tt("dve", r_[:, :N], r_[:, :N], mk[:, :N], ALU.add, [b_r, b_mk], [b_r])
        k.ts("dve", r_[:, :N], r_[:, :N], math.pi, -math.pi, ALU.min, ALU.max, [b_r], [b_r])
        k.act(sin_out, r_[:, :N], AF.Sin, [b_r], [b_out])
        k.act(sh[:, :N], r_[:, :N], AF.Sin, [b_r], [b_sh], scale=0.5)
        k.tt("dve", sh[:, :N], sh[:, :N], sh[:, :N], ALU.mult, [b_sh], [b_sh])
        k.ts("dve", cos_out, sh[:, :N], -2.0, 1.0, ALU.mult, ALU.add, [b_sh], [b_out])

    def s5_mixer(l):
        ydst, b_ydst = yT[1]
        with ExitStack() as st:
            uT, b_u = k.sb(st, [128, 2, NT], BF16, "s5u")
            yacc, b_yacc = k.sb(st, [128, 2, NT], F32, "s5y")
            dcol, b_dcol = k.sb(st, [128, 2], F32, "s5d")
            k.load("sp", dcol[:], s5d_d[l], [b_dcol])
            with ExitStack() as s2:
                ws, b_ws = load_w("pool", s2, w_s5_d[l], 256, "ws5")
                hgs = [k.sb(s2, [128, 8, 512], BF16, "hg") for _ in range(2)]
                tmp, tmpb = k.sb(s2, [128, 512], F32, "htmp")
                for g, (c0, n) in enumerate(GROUPS):
                    hg, hb = hgs[g % 2]
                    h_group(l, 0, g, hg, hb, tmp, tmpb)
                    for ti in range(2):
                        bk = k.bank()
                        proj(bk, ws, b_ws, ti * 128, hg, hb, n)
                        k.copy("act", uT[:, ti, c0:c0 + n], k.ps[bk][:, :n], [k.pb[bk]], [b_u])
                P.barrier()
            for dr in range(2):
                with ExitStack() as sd:
                    pr, b_pr = k.sb(sd, [128, 1024], F32, "pr"); pi_, b_pi = k.sb(sd, [128, 1024], F32, "pi")
                    qr, b_qr = k.sb(sd, [128, 1024], F32, "qr"); qi, b_qi = k.sb(sd, [128, 1024], F32, "qi")
                    Bb, b_Bb = k.sb(sd, [128, 2, 1024], BF16, "Bb")
                    Cb, b_Cb = k.sb(sd, [128, 3, 8, 128], BF16, "Cb")
                    triT, b_tri = k.sb(sd, [128, 2, 128], BF16, "triT")
                    trif, b_trif = k.sb(sd, [128, 128], F32, "trif")
                    posc, b_posc = k.sb(sd, [128, 2], F32, "posc")
                    posr, b_posr = k.sb(sd, [128, 128], F32, "posr")
                    fm, b_fm = k.sb(sd, [128, 3, 8], F32, "fm")
                    cc, b_cc = k.sb(sd, [128, 16, 8], F32, "cc")
                    A128, b_A128 = k.sb(sd, [128, 2, 8], F32, "A128")
                    k.load("pool", Bb[:], s5B_d[l, dr].rearrange("kt p n -> p kt n"), [b_Bb])
                    with ExitStack() as sc_:
                        Cf, b_Cf = k.sb(sc_, [128, 2, 8, 128], F32, "Cf")
                        k.load("sp", Cf[:, 0], s5C_d[l, dr, 0], [b_Cf]); k.load("sp", Cf[:, 1], s5C_d[l, dr, 1], [b_Cf])
                        k.copy("dve", Cb[:, 0], Cf[:, 0], [b_Cf], [b_Cb])
                        k.ts("dve", Cb[:, 1], Cf[:, 0], -1.0, None, ALU.mult, None, [b_Cf], [b_Cb])
                        k.ts("dve", Cb[:, 2], Cf[:, 1], -1.0, None, ALU.mult, None, [b_Cf], [b_Cb])
                        P.barrier()
                    k.load("sp", trif[:], tri_d[dr], [b_trif])
                    k.copy("dve", triT[:, 0, :], trif[:], [b_trif], [b_tri])
                    k.ts("dve", triT[:, 1, :], trif[:], -1.0, None, ALU.mult, None, [b_trif], [b_tri])
                    k.load("sp", posc[:], posc_d[dr], [b_posc]); k.load("sp", posr[:], posr_d[dr], [b_posr])
                    k.load("sp", fm[:], s5fm_d[l, dr], [b_fm])
                    with ExitStack() as sx:
                        sc4 = [k.sb(sx, [128, 1024], F32, "sc4") for _ in range(4)]
                        rho, b_rho = k.sb(sx, [128, 1024], F32, "rho"); th, b_th = k.sb(sx, [128, 1024], F32, "th")
                        ang, b_angb = k.sb(sx, [128, 1024], F32, "ang")
                        k.load("sp", rho[:], s5tm_d[l, dr, 0], [b_rho]); k.load("sp", th[:], s5tm_d[l, dr, 1], [b_th])
                        k.load("sp", ang[:], s5tm_d[l, dr, 2], [b_angb])
                        k.act(ang[:], ang[:], AF.Exp, [b_angb], [b_angb])
                        k.tt("dve", rho[:], rho[:], ang[:], ALU.mult, [b_rho, b_angb], [b_rho])
                        k.tt("dve", th[:], th[:], ang[:], ALU.mult, [b_th, b_angb], [b_th])
                        k.ts("dve", ang[:], th[:], posc[:, 0:1], None, ALU.mult, None, [b_th, b_posc], [b_angb])
                        sincos(ang[:], b_angb, 1024, sc4, pi_[:], pr[:], b_pr)
                        b_pi.w = b_pr.w
                        P.op("act", lambda e: e.activation(out=ang[:], in_=rho[:], func=AF.Exp, scale=posc[:, 0:1]), reads=[b_rho, b_posc], writes=[b_angb])
                        k.tt("dve", pr[:], pr[:], ang[:], ALU.mult, [b_pr, b_angb], [b_pr])
                        k.tt("dve", pi_[:], pi_[:], ang[:], ALU.mult, [b_pr, b_angb], [b_pr])
                        dtc = cc[:, 0, :]; rc_ = cc[:, 1, :]; tc_ = cc[:, 2, :]
                        k.act(dtc, fm[:, 2, :], AF.Exp, [b_fm], [b_cc])
                        k.tt("dve", rc_, fm[:, 0, :], dtc, ALU.mult, [b_fm, b_cc], [b_cc])
                        k.tt("dve", tc_, fm[:, 1, :], dtc, ALU.mult, [b_fm, b_cc], [b_cc])
                        k.copy("dve", ang[:, 0:8], tc_, [b_cc], [b_angb])
                        k.ts("dve", ang[:, 8:16], tc_, 128.0, None, ALU.mult, None, [b_cc], [b_angb])
                        sincos(ang[:, 0:16], b_angb, 16, sc4, th[:, 0:16], th[:, 16:32], b_th)
                        er = cc[:, 3, :]; e128 = cc[:, 4, :]
                        k.act(er, rc_, AF.Exp, [b_cc], [b_cc])
                        k.act(e128, rc_, AF.Exp, [b_cc], [b_cc], scale=128.0)
                        k.tt("dve", A128[:, 0, :], e128, th[:, 24:32], ALU.mult, [b_cc, b_th], [b_A128])
                        k.tt("dve", A128[:, 1, :], e128, th[:, 8:16], ALU.mult, [b_cc, b_th], [b_A128])
                        ar1 = cc[:, 5, :]; ai = cc[:, 6, :]; den = cc[:, 7, :]; cr = cc[:, 8, :]; ci = cc[:, 9, :]; t1 = cc[:, 10, :]
                        k.tt("dve", ar1, er, th[:, 16:24], ALU.mult, [b_cc, b_th], [b_cc])
                        k.ts("dve", ar1, ar1, -1.0, None, ALU.add, None, [b_cc], [b_cc])
                        k.tt("dve", ai, er, th[:, 0:8], ALU.mult, [b_cc, b_th], [b_cc])
                        k.tt("dve", den, fm[:, 0, :], fm[:, 0, :], ALU.mult, [b_fm], [b_cc])
                        k.tt("dve", t1, fm[:, 1, :], fm[:, 1, :], ALU.mult, [b_fm], [b_cc])
                        k.tt("dve", den, den, t1, ALU.add, [b_cc], [b_cc])
                        P.op("dve", lambda e: e.reciprocal(out=den, in_=den), reads=[b_cc], writes=[b_cc])
                        k.tt("dve", cr, ar1, fm[:, 0, :], ALU.mult, [b_cc, b_fm], [b_cc])
                        k.tt("dve", t1, ai, fm[:, 1, :], ALU.mult, [b_cc, b_fm], [b_cc])
                        k.tt("dve", cr, cr, t1, ALU.add, [b_cc], [b_cc])
                        k.tt("dve", cr, cr, den, ALU.mult, [b_cc], [b_cc])
                        k.tt("dve", ci, ai, fm[:, 0, :], ALU.mult, [b_cc, b_fm], [b_cc])
                        k.tt("dve", t1, ar1, fm[:, 1, :], ALU.mult, [b_cc, b_fm], [b_cc])
                        k.tt("dve", ci, ci, t1, ALU.subtract, [b_cc], [b_cc])
                        k.tt("dve", ci, ci, den, ALU.mult, [b_cc], [b_cc])
                        for i in range(8):
                            k.ts("dve", ang[:, i * 128:(i + 1) * 128], posr[:], cc[:, 2, i:i + 1], None, ALU.mult, None, [b_posr, b_cc], [b_angb])
                            P.op("act", lambda e: e.activation(out=rho[:, i * 128:(i + 1) * 128], in_=posr[:], func=AF.Exp, scale=cc[:, 1, i:i + 1]),
                                 reads=[b_posr, b_cc], writes=[b_rho])
                        sincos(ang[:], b_angb, 1024, sc4, qi[:], qr[:], b_qr)
                        k.tt("dve", qr[:], qr[:], rho[:], ALU.mult, [b_qr, b_rho], [b_qr])
                        k.tt("dve", qi[:], qi[:], rho[:], ALU.mult, [b_qr, b_rho], [b_qr])
                        for i in range(8):
                            sl_ = slice(i * 128, (i + 1) * 128)
                            k.ts("dve", th[:, sl_], qi[:, sl_], cc[:, 9, i:i + 1], None, ALU.mult, None, [b_qr, b_cc], [b_th])
                            k.ts("dve", ang[:, sl_], qr[:, sl_], cc[:, 9, i:i + 1], None, ALU.mult, None, [b_qr, b_cc], [b_angb])
                            k.stt(qr[:, sl_], qr[:, sl_], cc[:, 8, i:i + 1], th[:, sl_], ALU.mult, ALU.subtract, [b_qr, b_cc, b_th], [b_qr])
                            k.stt(qi[:, sl_], qi[:, sl_], cc[:, 8, i:i + 1], ang[:, sl_], ALU.mult, ALU.add, [b_qr, b_cc, b_angb], [b_qr])
                        P.barrier()
                    if l == 0 and dr == 0:
                        dump("s5pr", pr[:], b_pr, [128, 1024]); dump("s5pi", pi_[:], b_pr, [128, 1024])
                        dump("s5qr", qr[:], b_qr, [128, 1024]); dump("s5qi", qi[:], b_qr, [128, 1024])
                        dump("s5A128", A128[:], b_A128, [128, 2, 8])
                    with ExitStack() as sp_:
                        zp, _ = k.sb(sp_, [128, 4, 1024], BF16, "zp")
                        hp, _ = k.sb(sp_, [128, 4, 1024], BF16, "hp")
                        Dg, _ = k.sb(sp_, [128, 16, 128], BF16, "Dg")
                        zl, _ = k.sb(sp_, [128, 16], F32, "zl")
                        car, _ = k.sb(sp_, [128, 16], F32, "car")
                        ct_, _ = k.sb(sp_, [128, 4, 8], F32, "ctmp")
                        hb2 = lambda nm: [Buf(nm + "0"), Buf(nm + "1")]
                        b_zp, b_hp, b_DgT, b_zl, b_car, b_ct = hb2("zp"), hb2("hp"), hb2("Dg"), hb2("zl"), hb2("car"), hb2("ct")
                        k.memset("pool", Dg[:], 0.0, b_DgT)
                        order = [0, 1] + list(range(2, NTT)) if dr == 0 else [1, 0] + list(range(NTT - 1, 1, -1))
                        tl = 127 if dr == 0 else 0

                        def half_gen(kt, banks):
                            bre, bim, zre, zim = banks
                            cs = slice(kt * 512, (kt + 1) * 512)
                            i4 = slice(kt * 4, kt * 4 + 4); i4m = slice(8 + kt * 4, 8 + kt * 4 + 4)
                            for tt in order:
                                cols = slice(tt * 128, (tt + 1) * 128)
                                k.mm(bre, k.ps[bre][:, :], uT[:, kt, cols], Bb[:, kt, 0:512], True, True, [b_u, b_Bb])
                                k.mm(bim, k.ps[bim][:, :], uT[:, kt, cols], Bb[:, kt, 512:1024], True, True, [b_u, b_Bb])
                                yield
                                k.tt("dve", zp[:, 0, cs], k.ps[bre][:, :], pr[:, cs], ALU.mult, [k.pb[bre], b_pr], [b_zp[kt]])
                                k.tt("dve", zp[:, 1, cs], k.ps[bim][:, :], pi_[:, cs], ALU.mult, [k.pb[bim], b_pr], [b_zp[kt]])
                                k.tt("dve", zp[:, 2, cs], k.ps[bim][:, :], pr[:, cs], ALU.mult, [k.pb[bim], b_pr], [b_zp[kt]])
                                k.tt("dve", zp[:, 3, cs], k.ps[bre][:, :], pi_[:, cs], ALU.mult, [k.pb[bre], b_pr], [b_zp[kt]])
                                yield
                                for ii in range(4):
                                    i = kt * 4 + ii
                                    sl_ = slice(i * 128, (i + 1) * 128)
                                    oc = slice(ii * 128, (ii + 1) * 128)
                                    k.mm(zre, k.ps[zre][:, oc], zp[:, 0, sl_], triT[:, 0, :], True, False, [b_zp[kt], b_tri])
                                    k.mm(zre, k.ps[zre][:, oc], zp[:, 1, sl_], triT[:, 1, :], False, False, [b_zp[kt], b_tri])
                                    k.mm(zre, k.ps[zre][:, oc], Dg[:, i, :], onesb[:], False, True, [b_DgT[kt], b_ones])
                                    k.mm(zim, k.ps[zim][:, oc], zp[:, 2, sl_], triT[:, 0, :], True, False, [b_zp[kt], b_tri])
                                    k.mm(zim, k.ps[zim][:, oc], zp[:, 3, sl_], triT[:, 0, :], False, False, [b_zp[kt], b_tri])
                                    k.mm(zim, k.ps[zim][:, oc], Dg[:, 8 + i, :], onesb[:], False, True, [b_DgT[kt], b_ones])
                                yield
                                k.copy("act", zl[:, i4], k.ps[zre][:, :].rearrange("p (i t) -> p i t", t=128)[:, :, tl], [k.pb[zre]], [b_zl[kt]])
                                k.copy("act", zl[:, i4m], k.ps[zim][:, :].rearrange("p (i t) -> p i t", t=128)[:, :, tl], [k.pb[zim]], [b_zl[kt]])
                                zr_, zi_ = zl[:, i4], zl[:, i4m]
                                k.tt("dve", ct_[:, 0, i4], A128[:, 0, i4], zr_, ALU.mult, [b_A128, b_zl[kt]], [b_ct[kt]])
                                k.tt("dve", ct_[:, 1, i4], A128[:, 1, i4], zi_, ALU.mult, [b_A128, b_zl[kt]], [b_ct[kt]])
                                k.tt("dve", ct_[:, 2, i4], A128[:, 0, i4], zi_, ALU.mult, [b_A128, b_zl[kt]], [b_ct[kt]])
                                k.tt("dve", ct_[:, 3, i4], A128[:, 1, i4], zr_, ALU.mult, [b_A128, b_zl[kt]], [b_ct[kt]])
                                k.tt("dve", car[:, i4], ct_[:, 0, i4], ct_[:, 1, i4], ALU.subtract, [b_ct[kt]], [b_car[kt]])
                                k.tt("dve", car[:, i4m], ct_[:, 2, i4], ct_[:, 3, i4], ALU.add, [b_ct[kt]], [b_car[kt]])
                                for isl in (i4, i4m):
                                    k.tt("pool", Dg[:, isl, :], identf.unsqueeze(1).to_broadcast([128, 4, 128]),
                                         car[:, isl].unsqueeze(2).to_broadcast([128, 4, 128]), ALU.mult, [b_cstf, b_car[kt]], [b_DgT[kt]])
                                yield
                                k.tt("dve", hp[:, 0, cs], k.ps[zre][:, :], qr[:, cs], ALU.mult, [k.pb[zre], b_qr], [b_hp[kt]])
                                k.tt("dve", hp[:, 1, cs], k.ps[zim][:, :], qi[:, cs], ALU.mult, [k.pb[zim], b_qr], [b_hp[kt]])
                                k.tt("dve", hp[:, 2, cs], k.ps[zim][:, :], qr[:, cs], ALU.mult, [k.pb[zim], b_qr], [b_hp[kt]])
                                k.tt("dve", hp[:, 3, cs], k.ps[zre][:, :], qi[:, cs], ALU.mult, [k.pb[zre], b_qr], [b_hp[kt]])
                                yield
                                bk = bre
                                first = True
                                for ii in range(4):
                                    i = kt * 4 + ii
                                    sl_ = slice(i * 128, (i + 1) * 128)
                                    for (pi_x, ci_x) in ((0, 0), (1, 1), (2, 2), (3, 2)):
                                        last = (ii == 3 and pi_x == 3)
                                        k.mm(bk, k.ps[bk][:, 0:128], Cb[:, ci_x, i, :], hp[:, pi_x, sl_], first, last, [b_Cb, b_hp[kt]])
                                        first = False
                                if dr == 0:
                                    k.stt(yacc[:, kt, cols], uT[:, kt, cols], dcol[:, kt:kt + 1], k.ps[bk][:, 0:128], ALU.mult, ALU.add,
                                          [b_u, b_dcol, k.pb[bk]], [b_yacc])
                                else:
                                    k.tt("dve", yacc[:, kt, cols], k.ps[bk][:, 0:128], yacc[:, kt, cols], ALU.add, [k.pb[bk], b_yacc], [b_yacc])
                                yield

                        live = [half_gen(0, [0, 1, 2, 3]), half_gen(1, [4, 5, 6, 7])]
                        for _ in range(S5SKEW):
                            next(live[0])
                        while live:
                            for g_ in list(live):
                                try:
                                    next(g_)
                                except StopIteration:
                                    live.remove(g_)
                        P.barrier()
            dump("s5yacc", yacc[:], b_yacc, [128, 2, NT])
            with ExitStack() as s4:
                wgf, b_wgf = k.sb(s4, [128, 2, 256], F32, "wgf"); wgb, b_wgb = k.sb(s4, [128, 2, 256], BF16, "wgb")
                bg, b_bg = k.sb(s4, [128, 2], F32, "bglu")
                k.load("sp", wgf[:], w_glu_d[l].rearrange("(kt p) n -> p kt n", p=128), [b_wgf])
                k.copy("dve", wgb[:], wgf[:], [b_wgf], [b_wgb])
                k.load("sp", bg[:], s5bg_d[l], [b_bg])
                zT, b_zT = k.sb(s4, [128, 2, 512], BF16, "zT")
                x2, b_x2 = k.sb(s4, [128, 512], F32, "x2"); thh, b_thh = k.sb(s4, [128, 512], F32, "thh")
                sg, b_sg = k.sb(s4, [128, 512], F32, "sg")
                for g, (c0, n) in enumerate(GROUPS):
                    for kt in range(2):
                        x_ = yacc[:, kt, c0:c0 + n]
                        k.tt("dve", x2[:, :n], x_, x_, ALU.mult, [b_yacc], [b_x2])
                        k.ts("dve", x2[:, :n], x2[:, :n], 0.044715, 1.0, ALU.mult, ALU.add, [b_x2], [b_x2])
                        k.tt("dve", x2[:, :n], x2[:, :n], x_, ALU.mult, [b_x2, b_yacc], [b_x2])
                        k.act(thh[:, :n], x2[:, :n], AF.Tanh, [b_x2], [b_thh], scale=math.sqrt(2.0 / math.pi))
                        k.stt(thh[:, :n], thh[:, :n], 1.0, x_, ALU.add, ALU.mult, [b_thh, b_yacc], [b_thh])
                        k.ts("dve", zT[:, kt, :n], thh[:, :n], 0.5, None, ALU.mult, None, [b_thh], [b_zT])
                    for ct in range(2):
                        bk = k.bank()
                        for kt in range(2):
                            k.mm(bk, k.ps[bk][:, :n], wgb[:, kt, ct * 128:(ct + 1) * 128], zT[:, kt, :n], kt == 0, kt == 1, [b_wgb, b_zT])
                        P.op("act", lambda e: e.activation(out=sg[:, :n], in_=k.ps[bk][:, :n], func=AF.Sigmoid, bias=bg[:, ct:ct + 1]),
                             reads=[k.pb[bk], b_bg], writes=[b_sg])
                        k.tt("dve", ydst[:, ct, c0:c0 + n], zT[:, ct, :n], sg[:, :n], ALU.mult, [b_zT, b_sg], [b_ydst])
                P.barrier()
        dump("yb", ydst[:], b_ydst, [128, 2, NT], BF16)

    def rwkv_mixer(l):
        ydst, b_ydst = yT[3]
        xd, b_xd = ydst, Buf("xd")
        with ExitStack() as st:
            rS, b_r = yall[:, 0], Buf("rw_r")
            kS, b_k = yall[:, 1], Buf("rw_k")
            vS, b_v = yall[:, 2], Buf("rw_v")
            kkS, b_kk = k.sb(st, [128, 2, NT], BF16, "rw_kk")
            cols, b_cols = k.sb(st, [128, 2, 9], F32, "rw_cols")
            k.load("sp", cols[:], rw_cols_d[l], [b_cols])
            C_KK, C_KA, C_LNG, C_LNB, C_RK, C_W0, C_A0 = 0, 1, 2, 3, 4, 5, 7
            for half in range(2):
              with ExitStack() as s2:
                WA, b_WA = load_w("pool", s2, w_rw_d[l, :, half * 512:(half + 1) * 512], 512, "rwWA")
                WB, b_WB = k.sb(s2, [128, 8, 512], BF16, "rwWB")
                with ExitStack() as s3:
                    mu, b_mu = k.sb(s3, [128, 512], F32, "rwmu")
                    k.load("sp", mu[:], rw_mu_d[l, :, half * 512:(half + 1) * 512], [b_mu])
                    for kt in range(8):
                        k.stt(WB[:, kt, :], WA[:, kt, :], 0.5, mu[:], ALU.mult, ALU.mult, [b_WA, b_mu], [b_WB])
                    k.ts("dve", mu[:], mu[:], -1.0, 1.0, ALU.mult, ALU.add, [b_mu], [b_mu])
                    for kt in range(8):
                        k.tt("pool", WA[:, kt, :], WA[:, kt, :], mu[:], ALU.mult, [b_WA, b_mu], [b_WA])
                    P.barrier()
                hgxs = [k.sb(s2, [128, 8, 516], BF16, "hgx") for _ in range(2)]
                hsxs = [k.sb(s2, [128, 8, 512], BF16, "hsx") for _ in range(2)]
                tmps = [k.sb(s2, [128, 514], F32, "htmp") for _ in range(2)]
                sq, b_sq = k.sb(s2, [128, 512], BF16, "rwsq"); kq, b_kq = k.sb(s2, [128, 512], F32, "rwkq")
                rs_, b_rs = k.sb(s2, [128, 512], F32, "rwrs"); lnt, b_lnt = k.sb(s2, [128, 512], F32, "rwlnt")
                for g, (c0, n) in enumerate(GROUPS if RW_SUB >= 2 else []):
                    j = jof(g)
                    hgx, b_hgx = hgxs[g % 2]; hsx, b_hsx = hsxs[g % 2]
                    s_lo, s_hi = (0, NCTX) if g == 0 else (NCTX, NT)
                    lo, hi = max(s_lo, c0 - 1), min(s_hi, c0 + n + 1)
                    o0 = lo - (c0 - 1)
                    if lo > c0 - 1:
                        k.memset("dve", hgx[:, :, 0:3], 0.0, [b_hgx])
                    if hi < c0 + n + 1:
                        k.memset("dve", hgx[:, :, n + 1:n + 3], 0.0, [b_hgx])
                    w_ = hi - lo
                    for kt in range(8):
                        tmp, tmpb = tmps[kt % 2]
                        k.tt("dve", tmp[:, :w_], xT[:, kt, lo:hi], rstd_b[:, lo:hi], ALU.mult, [b_xT, b_rstd], [tmpb])
                        P.op("act", lambda e: e.activation(out=hgx[:, kt, 1 + o0:1 + o0 + w_], in_=tmp[:, :w_], func=AF.Identity,
                                                           scale=AB[:, l, 0, 0, kt, j:j + 1], bias=AB[:, l, 0, 1, kt, j:j + 1]),
                             reads=[tmpb, b_AB], writes=[b_hgx])
                    for kt in range(8):
                        k.tt("pool", hsx[:, kt, :n], hgx[:, kt, 1:n + 1], hgx[:, kt, 3:n + 3], ALU.add, [b_hgx], [b_hsx])
                    for tl_ in range(4 if RW_SUB >= 3 else 0):
                        ti = half * 4 + tl_
                        bk = k.bank()
                        for kt in range(8):
                            k.mm(bk, k.ps[bk][:, :n], WA[:, kt, tl_ * 128:(tl_ + 1) * 128], hgx[:, kt, 2:n + 2], kt == 0, False, [b_WA, b_hgx])
                        for kt in range(8):
                            k.mm(bk, k.ps[bk][:, :n], WB[:, kt, tl_ * 128:(tl_ + 1) * 128], hsx[:, kt, :n], False, kt == 7, [b_WB, b_hsx])
                        dst, dstb = [(rS, b_r), (kS, b_k), (vS, b_v), (xd, b_xd)][ti // 2]
                        k.copy("act", dst[:, ti % 2, c0:c0 + n], k.ps[bk][:, :n], [k.pb[bk]], [dstb])
                        if ti // 2 == 1 and RW_SUB >= 4:
                            ci = ti % 2
                            k.ts("dve", kq[:, :n], k.ps[bk][:, :n], cols[:, ci, C_KK:C_KK + 1], None, ALU.mult, None, [k.pb[bk], b_cols], [b_kq])
                            k.act(sq[:, :n], kq[:, :n], AF.Square, [b_kq], [b_sq])
                            b2 = k.bank()
                            k.mm(b2, k.ps[b2][:, :n], bo64, sq[:, :n], True, True, [b_sq, b_cstb])
                            k.rstd_from_ss(rs_[:, :n], k.ps[b2][:, :n], b2, 1.0, lnt[:, :n], b_lnt, [b_rs])
                            k.tt("dve", kkS[:, ci, c0:c0 + n], kq[:, :n], rs_[:, :n], ALU.mult, [b_kq, b_rs], [b_kk])
                P.barrier()
            dump("rw_r", rS[:], b_r, [128, 2, NT], BF16); dump("rw_kk", kkS[:], b_kk, [128, 2, NT], BF16)
            dump("rw_xd", xd[:], b_xd, [128, 2, NT], BF16)
            yacc, _ = k.sb(st, [128, 2, NT], BF16, "rw_yacc")
            sw = {}
            def small(name, shape, src, dt=BF16, q="pool"):
                t, b = k.sb(st, shape, dt, name)
                k.load(q if dt == BF16 else "sp", t[:], src, [b])
                sw[name] = (t, b)
            for dr in range(2 if RW_LEVEL >= 1 else 0):
                small(f"w1_{dr}", [128, 2, 32], rw_w1_d[l, dr].rearrange("(kt p) n -> p kt n", p=128))
                small(f"w2_{dr}", [32, 256], rw_w2_d[l, dr])
                small(f"a1_{dr}", [128, 2, 32], rw_a1_d[l, dr].rearrange("(kt p) n -> p kt n", p=128))
                small(f"a2_{dr}", [32, 256], rw_a2_d[l, dr])
                small(f"w0r_{dr}", [1, 256], rw_w0row_d[l, dr], F32)
                small(f"tri_{dr}", [128, 2, 128], rw_tri_d[dr], F32)
                small(f"m1_{dr}", [128, 256], rw_m1_d[dr])
                small(f"mnt_{dr}", [128, 128], rw_mnt_d[dr])
            if RW_LEVEL >= 0:
                small("g1", [128, 2, 64], rw_g1_d[l].rearrange("(kt p) n -> p kt n", p=128))
                small("g2", [64, 256], rw_g2_d[l])
                small("ind", [128, 2], rw_ind_d, F32)
            ones1, b_ones1 = k.sb(st, [1, 128], F32, "ones1")
            k.memset("dve", ones1[:], 1.0, [b_ones1])

            def a_of(dr, xsrc, n, a_out, b_aout, x1t, b_x1t):
                a1, b_a1 = sw[f"a1_{dr}"]; a2, b_a2 = sw[f"a2_{dr}"]
                bk = k.bank()
                for kt in range(2):
                    k.mm(bk, k.ps[bk][0:32, :n], a1[:, kt, :], xsrc[:, kt, :], kt == 0, kt == 1, [b_a1, b_xd])
                k.copy("act", x1t[0:32, :n], k.ps[bk][0:32, :n], [k.pb[bk]], [b_x1t])
                for ci in range(2):
                    b2 = k.bank()
                    k.mm(b2, k.ps[b2][:, :n], a2[:, ci * 128:(ci + 1) * 128], x1t[0:32, :n], True, True, [b_a2, b_x1t])
                    P.op("act", lambda e: e.activation(out=a_out[:, ci, :n], in_=k.ps[b2][:, :n], func=AF.Sigmoid,
                                                       bias=cols[:, ci, C_A0 + dr:C_A0 + dr + 1]),
                         reads=[k.pb[b2], b_cols], writes=[b_aout])

            def kmod_of(a_in, b_ain, ksrc, n, out, b_out_, tmpf, b_tmpf):
                for ci in range(2):
                    k.ts("dve", tmpf[:, ci, :n], a_in[:, ci, :n], -1.0, cols[:, ci, C_KA:C_KA + 1], ALU.add, ALU.mult, [b_ain, b_cols], [b_tmpf])
                    k.stt(out[:, ci, :n], tmpf[:, ci, :n], 1.0, ksrc[:, ci, :], ALU.add, ALU.mult, [b_tmpf, b_k], [b_out_])

            b_yt = [Buf(f"yacc{t}") for t in range(NTT)]
            for (c0_, n_) in GROUPS:
                k.memset("dve", yacc[:, :, c0_:c0_ + n_], 0.0, b_yt[c0_ // 128:(c0_ + n_) // 128])

            def scan_gen(dr, sd, banks):
                    w1, b_w1 = sw[f"w1_{dr}"]; w2, b_w2 = sw[f"w2_{dr}"]; w0r, b_w0r = sw[f"w0r_{dr}"]
                    tri, b_tri = sw[f"tri_{dr}"]; m1, b_m1 = sw[f"m1_{dr}"]; mnt, b_mnt = sw[f"mnt_{dr}"]; ind, b_ind = sw["ind"]
                    M32, b_M32 = k.sb(sd, [128, 2, 64], F32, "M32"); Mbf, b_Mbf = k.sb(sd, [128, 2, 128], BF16, "Mblk")
                    k.memset("dve", M32[:], 0.0, [b_M32]); k.memset("dve", Mbf[:], 0.0, [b_Mbf])
                    x1t, b_x1t = k.sb(sd, [32, 128], BF16, "x1t")
                    tnh, b_tnh = k.sb(sd, [32, 128], BF16, "tnh")
                    lwT, b_lwT = k.sb(sd, [128, 256], F32, "lwT")
                    lam, b_lam = k.sb(sd, [128, 2, 3, 128], F32, "lam")
                    gam, b_gam = k.sb(sd, [128, 2, 2], F32, "gam")
                    aF, b_aF = k.sb(sd, [128, 2, 128], F32, "aF")
                    tF, b_tF = k.sb(sd, [128, 2, 128], F32, "tF")
                    kmod, b_kmod = k.sb(sd, [128, 2, 128], F32, "kmod")
                    AR, b_AR = k.sb(sd, [128, 2, 2, 128], BF16, "AR")
                    BK, b_BK = k.sb(sd, [128, 2, 2, 128], BF16, "BK")
                    BKt2, b_BKt = k.sb(sd, [128, 2, 2, 2, 128], BF16, "BKt")
                    k.memset("pool", BKt2[:], 0.0, [b_BKt])
                    Vt, b_Vt = k.sb(sd, [128, 2, 128], BF16, "Vt")
                    SCb, b_SCb = k.sb(sd, [128, 4, 256], BF16, "SCb"); SCk, b_SCk = k.sb(sd, [128, 4, 256], BF16, "SCk")
                    Nb = [k.sb(sd, [128, 4, 128], BF16, f"Nb{i}") for i in range(2)]
                    Tb = [k.sb(sd, [128, 4, 128], BF16, f"Tb{i}") for i in range(2)]
                    R, b_R = k.sb(sd, [128, 4, 128], BF16, "Rinv")
                    Wsb, b_Wsb = k.sb(sd, [128, 256], BF16, "Wsb"); Usb, b_Usb = k.sb(sd, [128, 256], BF16, "Usb")
                    mt, b_mt = k.sb(sd, [128, 2, 64], F32, "mt")
                    v4h = lambda ap: ap.rearrange("p (h t) -> p h t", h=4)
                    order = list(range(NTT)) if dr == 0 else [1, 0] + list(range(NTT - 1, 1, -1))
                    yield
                    for tt in order:
                        tc_ = slice(tt * 128, (tt + 1) * 128)
                        bk = k.bank()
                        for kt in range(2):
                            k.mm(bk, k.ps[bk][0:32, 0:128], w1[:, kt, :], xd[:, kt, tc_], kt == 0, kt == 1, [b_w1, b_xd])
                        k.act(tnh[:], k.ps[bk][0:32, 0:128], AF.Tanh, [k.pb[bk]], [b_tnh])
                        a1, b_a1 = sw[f"a1_{dr}"]; a2, b_a2 = sw[f"a2_{dr}"]
                        bk = k.bank()
                        for kt in range(2):
                            k.mm(bk, k.ps[bk][0:32, 0:128], a1[:, kt, :], xd[:, kt, tc_], kt == 0, kt == 1, [b_a1, b_xd])
                        k.copy("dve", x1t[0:32, 0:128], k.ps[bk][0:32, 0:128], [k.pb[bk]], [b_x1t])
                        yield
                        bk = k.bank()
                        k.mm(bk, k.ps[bk][:, 0:256], tnh[:], w2[:], True, False, [b_tnh, b_w2])
                        k.mm(bk, k.ps[bk][:, 0:256], ones1[:], w0r[:], False, True, [b_ones1, b_w0r])
                        k.act(lwT[:], k.ps[bk][:, 0:256], AF.Sigmoid, [k.pb[bk]], [b_lwT])
                        k.ts("dve", lwT[:], lwT[:], -math.exp(-0.5), None, ALU.mult, None, [b_lwT], [b_lwT])
                        b2 = k.bank()
                        for ci in range(2):
                            k.mm(b2, k.ps[b2][:, ci * 128:(ci + 1) * 128], a2[:, ci * 128:(ci + 1) * 128], x1t[0:32, 0:128], True, True, [b_a2, b_x1t],
                                 inc=(ci == 1))
                        for ci in range(2):
                            P.op("act", lambda e: e.activation(out=aF[:, ci, :], in_=k.ps[b2][:, ci * 128:(ci + 1) * 128], func=AF.Sigmoid,
                                                               bias=cols[:, ci, C_A0 + dr:C_A0 + dr + 1]),
                                 reads=[k.pb[b2], b_cols], writes=[b_aF])
                        yield
                        kmod_of(aF, b_aF, kS[:, :, tc_], 128, kmod, b_kmod, tF, b_tF)
                        for ci in range(2):
                            k.tt("pool", tF[:, ci, :], kkS[:, ci, tc_], aF[:, ci, :], ALU.mult, [b_kk, b_aF], [b_tF])
                        lbk = []
                        for ci in range(2):
                            bk = k.bank(); lbk.append(bk)
                            k.mm(bk, k.ps[bk][:, 0:128], lwT[:, ci * 128:(ci + 1) * 128], tri[:, 0, :], True, True, [b_lwT, b_tri], inc=False)
                            k.mm(bk, k.ps[bk][:, 128:256], lwT[:, ci * 128:(ci + 1) * 128], tri[:, 1, :], True, True, [b_lwT, b_tri], inc=False)
                            k.mm(bk, k.ps[bk][:, 256:258], lwT[:, ci * 128:(ci + 1) * 128], ind[:], True, True, [b_lwT, b_ind])
                        yield
                        for ci in range(2):
                            bk = lbk[ci]
                            k.act(lam[:, ci, 0, :], k.ps[bk][:, 0:128], AF.Exp, [k.pb[bk]], [b_lam])
                            k.act(lam[:, ci, 1, :], k.ps[bk][:, 0:128], AF.Exp, [k.pb[bk]], [b_lam], scale=-1.0)
                            k.act(lam[:, ci, 2, :], k.ps[bk][:, 128:256], AF.Exp, [k.pb[bk]], [b_lam])
                            k.act(gam[:, ci, :], k.ps[bk][:, 256:258], AF.Exp, [k.pb[bk]], [b_gam])
                        yield
                        if RWSTOP <= 1:
                            continue
                        for ci in range(2):
                            k.tt("dve", AR[:, ci, 1, :], rS[:, ci, tc_], lam[:, ci, 0, :], ALU.mult, [b_r, b_lam], [b_AR])
                            k.stt(AR[:, ci, 0, :], kkS[:, ci, tc_], -1.0, lam[:, ci, 2, :], ALU.mult, ALU.mult, [b_kk, b_lam], [b_AR])
                            k.tt("dve", BK[:, ci, 1, :], kmod[:, ci, :], lam[:, ci, 1, :], ALU.mult, [b_kmod, b_lam], [b_BK])
                            k.tt("pool", BK[:, ci, 0, :], tF[:, ci, :], lam[:, ci, 1, :], ALU.mult, [b_tF, b_lam], [b_BK])
                        yield
                        if RWSTOP <= 2:
                            continue
                        bk = k.bank()
                        pv = k.ps[bk][:].bitcast(BF16)
                        for ci in range(2):
                            for x_ in range(2):
                                P.op("pe", lambda e: e.transpose(pv[:, (ci * 2 + x_) * 128:(ci * 2 + x_ + 1) * 128], BK[:, ci, x_, :], identb),
                                     reads=[b_BK, b_cstb], writes=[k.pb[bk]], inc=False)
                            P.op("pe", lambda e: e.transpose(pv[:, (4 + ci) * 128:(5 + ci) * 128], vS[:, ci, tc_], identb),
                                 reads=[b_v, b_cstb], writes=[k.pb[bk]], inc=(ci == 1))
                        k.copy("act", BKt2[0:64, 0].rearrange("p a b c -> p (a b c)"), pv[0:64, 0:512], [k.pb[bk]], [b_BKt])
                        k.copy("act", BKt2[64:128, 1].rearrange("p a b c -> p (a b c)"), pv[64:128, 0:512], [k.pb[bk]], [b_BKt])
                        k.copy("dve", Vt[:].rearrange("p a c -> p (a c)"), pv[:, 512:768], [k.pb[bk]], [b_Vt])
                        if RWSTOP <= 3:
                            continue
                        m1b = m1[:].unsqueeze(1).to_broadcast([128, 2, 256])
                        for hh in range(2):
                            pb = 64 * hh
                            bA = k.bank(); bB = k.bank()
                            for x_, bx in ((0, bA), (1, bB)):
                                for ci in range(2):
                                    arv = AR[pb:pb + 64, ci, :, :].rearrange("p a t -> p (a t)")
                                    k.mm(bx, k.ps[bx][:, ci * 256:(ci + 1) * 256], BK[pb:pb + 64, ci, x_, :], arv, True, True, [b_BK, b_AR], inc=(ci == 1))
                            k.tt("dve", SCb[:, hh * 2:hh * 2 + 2, :], k.ps[bA][:, 0:512].rearrange("p (h t) -> p h t", h=2), m1b, ALU.mult,
                                 [k.pb[bA], b_m1], [b_SCb])
                            k.tt("dve", SCk[:, hh * 2:hh * 2 + 2, :], k.ps[bB][:, 0:512].rearrange("p (h t) -> p h t", h=2), m1b, ALU.mult,
                                 [k.pb[bB], b_m1], [b_SCk])
                            yield
                        Tc, b_Tc = Tb[0]
                        for hh in range(2):
                            pb = 64 * hh
                            bC = k.bank()
                            for ci in range(2):
                                k.mm(bC, k.ps[bC][:, ci * 128:(ci + 1) * 128], AR[pb:pb + 64, ci, 0, :], BK[pb:pb + 64, ci, 0, :], True, True, [b_AR, b_BK],
                                     inc=(ci == 1))
                            k.tt("dve", Tc[:, hh * 2:hh * 2 + 2, :], k.ps[bC][:, 0:256].rearrange("p (h t) -> p h t", h=2),
                                 mnt[:].unsqueeze(1).to_broadcast([128, 2, 128]), ALU.mult, [k.pb[bC], b_mnt], [b_Tc])
                        k.tt("pool", R[:], SCb[:, :, 0:128], identb.unsqueeze(1).to_broadcast([128, 4, 128]), ALU.add, [b_SCb, b_cstb], [b_R])
                        yield
                        if RWSTOP <= 4:
                            continue
                        Nc, b_Nc = SCb[:, :, 0:128], b_SCb
                        for i in range(1, 6):
                            Nn, b_Nn = Nb[i % 2]; Tn, b_Tn = Tb[i % 2]
                            bt_ = k.bank()
                            for h in range(4):
                                k.mm(bt_, k.ps[bt_][:, h * 128:(h + 1) * 128], Nc[:, h, :], Tc[:, h, :], True, True, [b_Nc, b_Tc], inc=(h == 3))
                            k.copy("dve", Tn[:], v4h(k.ps[bt_][:, 0:512]), [k.pb[bt_]], [b_Tn])
                            if i < 5:
                                bn = k.bank()
                                for h in range(4):
                                    k.mm(bn, k.ps[bn][:, h * 128:(h + 1) * 128], Tc[:, h, :], Nc[:, h, :], True, True, [b_Tc, b_Nc], inc=(h == 3))
                                k.copy("act", Nn[:], v4h(k.ps[bn][:, 0:512]), [k.pb[bn]], [b_Nn])
                            yield
                            br_ = k.bank()
                            for h in range(4):
                                k.mm(br_, k.ps[br_][:, h * 128:(h + 1) * 128], Tn[:, h, :], R[:, h, :], True, True, [b_Tn, b_R], inc=(h == 3))
                            k.tt("dve", R[:], v4h(k.ps[br_][:, 0:512]), R[:], ALU.add, [k.pb[br_], b_R], [b_R])
                            yield
                            Nc, b_Nc = Nn[:], b_Nn
                            Tc, b_Tc = Tn, b_Tn
                        if RWSTOP <= 5:
                            continue
                        bY, bW, bU, bM = banks
                        for jj in ([0, 1] if dr == 0 else [1, 0]):
                            pj = 64 * jj
                            for ci in range(2):
                                k.mm(bW, k.ps[bW][:, ci * 128:(ci + 1) * 128], AR[:, ci, 0, :], Mbf[:, ci, :], True, False, [b_AR, b_Mbf], inc=False)
                                for hh in range(2):
                                    h = ci * 2 + hh
                                    pb = 64 * hh
                                    k.mm(bW, k.ps[bW][:, h * 64:(h + 1) * 64], SCk[:, hh * 2 + ci, 0:128], Vt[:, ci, pb:pb + 64], False, True, [b_SCk, b_Vt],
                                         inc=(h == 3))
                            k.copy("act", Wsb[:], k.ps[bW][:, 0:256], [k.pb[bW]], [b_Wsb])
                            yield
                            for h in range(4):
                                oc = slice(h * 64, (h + 1) * 64)
                                k.mm(bU, k.ps[bU][:, oc], R[:, (h % 2) * 2 + h // 2, :], Wsb[:, oc], True, True, [b_R, b_Wsb], inc=(h == 3))
                            k.copy("dve", Usb[:], k.ps[bU][:, 0:256], [k.pb[bU]], [b_Usb])
                            yield
                            for ci in range(2):
                                for hh in range(2):
                                    h = ci * 2 + hh
                                    pb = 64 * hh
                                    oc = slice(h * 64, (h + 1) * 64)
                                    mo = k.ps[bM][pb:pb + 64, ci * 64:(ci + 1) * 64]
                                    k.mm(bM, mo, BKt2[:, jj, ci, 0, pb:pb + 64], Usb[:, oc], True, False, [b_BKt, b_Usb], inc=False)
                                    k.mm(bM, mo, BKt2[:, jj, ci, 1, pb:pb + 64], Vt[:, ci, pb:pb + 64], False, True, [b_BKt, b_Vt], inc=(h == 3))
                            for ci in range(2):
                                ycol = slice(ci * 128 + pj, ci * 128 + pj + 64)
                                k.mm(bY, k.ps[bY][:, ycol], Mbf[:, ci, :], AR[:, ci, 1, pj:pj + 64], True, False, [b_Mbf, b_AR], inc=False)
                                for hh in range(2):
                                    h = ci * 2 + hh
                                    pb = 64 * hh
                                    oc = slice(h * 64, (h + 1) * 64)
                                    k.mm(bY, k.ps[bY][pb:pb + 64, ycol], Usb[:, oc], SCb[:, hh * 2 + ci, 128 + pj:128 + pj + 64], False, False, [b_Usb, b_SCb], inc=False)
                                    k.mm(bY, k.ps[bY][pb:pb + 64, ycol], Vt[:, ci, pb:pb + 64], SCk[:, hh * 2 + ci, 128 + pj:128 + pj + 64], False, True, [b_Vt, b_SCk],
                                         inc=(h == 3))
                            k.tt("dve", mt[:], k.ps[bM][:, 0:128].rearrange("p (c n) -> p c n", c=2), M32[:], ALU.add, [k.pb[bM], b_M32], [b_mt])
                            k.tt("dve", Mbf[0:64, :, 0:64], mt[0:64, :, :], gam[0:64, :, jj:jj + 1].to_broadcast([64, 2, 64]), ALU.mult,
                                 [b_mt, b_gam], [b_Mbf])
                            k.tt("dve", Mbf[64:128, :, 64:128], mt[64:128, :, :], gam[64:128, :, jj:jj + 1].to_broadcast([64, 2, 64]), ALU.mult,
                                 [b_mt, b_gam], [b_Mbf])
                            k.tt("dve", M32[:], mt[:], gam[:, :, jj:jj + 1].to_broadcast([128, 2, 64]), ALU.mult, [b_mt, b_gam], [b_M32])
                            yield
                        k.tt("dve", yacc[:, :, tc_], k.ps[bY][:, 0:256].rearrange("p (c t) -> p c t", c=2), yacc[:, :, tc_], ALU.add,
                             [k.pb[bY], b_yt[tt]], [b_yt[tt]])
                        yield

            with ExitStack() as sd:
                threads = [[scan_gen(dr, sd, [4 * dr + i for i in range(4)]), [4 * dr + i for i in range(4)], 0] for dr in range(2)]
                live = list(threads)
                k.rrset, k.rr = threads[0][1], threads[0][2]
                for _ in range(RWSKEW):
                    next(threads[0][0])
                threads[0][2] = k.rr
                if os.environ.get("RWSEQ"):
                    for th in threads:
                        k.rrset, k.rr = th[1], th[2]
                        for _ in th[0]:
                            pass
                    live = []
                while live:
                    for th in list(live):
                        k.rrset, k.rr = th[1], th[2]
                        try:
                            next(th[0])
                        except StopIteration:
                            live.remove(th)
                        th[2] = k.rr
                k.rrset = list(range(8)); k.rr = 0
                P.barrier()
            b_yacc = Buf("rw_yacc_all")
            dump("rw_yacc", yacc[:], b_yacc, [128, 2, NT], BF16)
            with ExitStack() as s5_:
              if RW_LEVEL >= 6:
                    g1, b_g1 = sw["g1"]; g2, b_g2 = sw["g2"]
                    x1t, b_x1t = k.sb(s5_, [32, 512], BF16, "ex1t")
                    aF, b_aF = k.sb(s5_, [128, 2, 512], F32, "eaF"); tF, b_tF = k.sb(s5_, [128, 2, 512], F32, "etF")
                    kmod, b_kmod = k.sb(s5_, [128, 2, 512], F32, "ekm")
                    bon, b_bon = k.sb(s5_, [128, 2, 512], F32, "ebon")
                    ybf, b_ybf = k.sb(s5_, [128, 512], BF16, "eybf"); yc, b_yc = k.sb(s5_, [128, 512], F32, "eyc")
                    rs_, b_rs = k.sb(s5_, [128, 512], F32, "ers"); lnt, b_lnt = k.sb(s5_, [128, 512], F32, "elnt")
                    gh, b_gh = k.sb(s5_, [64, 512], BF16, "egh"); gg, b_gg = k.sb(s5_, [128, 2, 512], F32, "egg")
                    rkb, b_rkb = k.sb(s5_, [128, 512], BF16, "erkb")
                    lneps, b_lneps = k.sb(s5_, [128, 1], F32, "lneps")
                    k.memset("dve", lneps[:], 64e-5, [b_lneps])
                    for g, (c0, n) in enumerate(GROUPS):
                        gc_ = slice(c0, c0 + n)
                        for dr in range(2):
                            a_of(dr, xd[:, :, gc_], n, aF, b_aF, x1t, b_x1t)
                            kmod_of(aF, b_aF, kS[:, :, gc_], n, kmod, b_kmod, tF, b_tF)
                            for ci in range(2):
                                k.stt(rkb[:, :n], kmod[:, ci, :n], cols[:, ci, C_RK:C_RK + 1], rS[:, ci, gc_], ALU.mult, ALU.mult,
                                      [b_kmod, b_cols, b_r], [b_rkb])
                                bk = k.bank()
                                k.mm(bk, k.ps[bk][:, :n], bo64, rkb[:, :n], True, True, [b_rkb, b_cstb])
                                if dr == 0:
                                    k.tt("dve", bon[:, ci, :n], k.ps[bk][:, :n], vS[:, ci, gc_], ALU.mult, [k.pb[bk], b_v], [b_bon])
                                else:
                                    k.tt("dve", tF[:, ci, :n], k.ps[bk][:, :n], vS[:, ci, gc_], ALU.mult, [k.pb[bk], b_v], [b_tF])
                                    k.tt("pool", bon[:, ci, :n], bon[:, ci, :n], tF[:, ci, :n], ALU.add, [b_bon, b_tF], [b_bon])
                        bk = k.bank()
                        for kt in range(2):
                            k.mm(bk, k.ps[bk][0:64, :n], g1[:, kt, :], xd[:, kt, gc_], kt == 0, kt == 1, [b_g1, b_xd])
                        k.act(gh[:, :n], k.ps[bk][0:64, :n], AF.Sigmoid, [k.pb[bk]], [b_gh])
                        for ci in range(2):
                            b2 = k.bank()
                            k.mm(b2, k.ps[b2][:, :n], g2[:, ci * 128:(ci + 1) * 128], gh[:, :n], True, True, [b_g2, b_gh])
                            k.copy("act", gg[:, ci, :n], k.ps[b2][:, :n], [k.pb[b2]], [b_gg])
                        for ci in range(2):
                            bk = k.bank()
                            k.mm(bk, k.ps[bk][:, :n], bo64, yacc[:, ci, gc_], True, True, [b_yacc, b_cstb])
                            k.stt(yc[:, :n], k.ps[bk][:, :n], -1.0 / 64, yacc[:, ci, gc_], ALU.mult, ALU.add, [k.pb[bk], b_yacc], [b_yc])
                            k.act(ybf[:, :n], yc[:, :n], AF.Square, [b_yc], [b_ybf])
                            b2 = k.bank()
                            k.mm(b2, k.ps[b2][:, :n], bo64, ybf[:, :n], True, True, [b_ybf, b_cstb])
                            P.op("act", lambda e: e.activation(out=lnt[:, :n], in_=k.ps[b2][:, :n], func=AF.Ln, scale=1.0 / 64, bias=lneps[:]),
                                 reads=[k.pb[b2], b_lneps], writes=[b_lnt])
                            k.act(rs_[:, :n], lnt[:, :n], AF.Exp, [b_lnt], [b_rs], scale=-0.5)
                            k.stt(yc[:, :n], yc[:, :n], cols[:, ci, C_LNG:C_LNG + 1], rs_[:, :n], ALU.mult, ALU.mult, [b_yc, b_cols, b_rs], [b_yc])
                            k.stt(yc[:, :n], yc[:, :n], cols[:, ci, C_LNB:C_LNB + 1], bon[:, ci, :n], ALU.add, ALU.add, [b_yc, b_cols, b_bon], [b_yc])
                            k.tt("dve", ydst[:, ci, gc_], yc[:, :n], gg[:, ci, :n], ALU.mult, [b_yc, b_gg, b_xd], [b_ydst, b_xd])
                    P.barrier()
        dump("yd", ydst[:], b_ydst, [128, 2, NT], BF16)

    def final_out():
        with ExitStack() as st:
            ot = [k.sb(st, [128, D], F32, "ostage") for _ in range(2)]
            for tt in range(NLAT // 128):
                o, ob = ot[tt % 2]
                c0 = NCTX + tt * 128
                for half in range(2):
                    bk = k.bank()
                    for j in range(4):
                        kt = half * 4 + j
                        P.op("pe", lambda e: e.transpose(k.ps[bk][:, j * 128:(j + 1) * 128], xT[:, kt, c0:c0 + 128], identf),
                             reads=[b_xT, b_cstf], writes=[k.pb[bk]], inc=(j == 3))
                    k.copy("dve" if half == 0 else "act", o[:, half * 512:(half + 1) * 512], k.ps[bk][:], [k.pb[bk]], [ob])
                P.dma("sp", out_d[tt * 128:(tt + 1) * 128, :], o[:], reads=[ob])
            P.finish([b for _, b in ot])
            P.barrier()

    for l in range(DEPTH):
        if ("L%d" % l) not in stages:
            continue
        with ExitStack() as st:
            rms_stats(st)
            P.barrier()
        if "rw" in stages:
            rwkv_mixer(l)
        if "da" in stages:
            da_mixer(l)
        if "mla" in stages:
            mla_mixer(l)
        if "s5" in stages:
            s5_mixer(l)
        if "merge" in stages:
            merge(l)
        if "moe" in stages:
            moe(l)
    final_out()
    for e in ("sp",):
        toks = [(kk, v) for kk, v in P.cnt.items() if v > 0 and isinstance(kk, tuple)]
        for tok in toks:
            P._wait(e, tok)
    top.close()
    return nc, k, dbg_out


_CONSTS = None


def prep_shared(inp):
    g = {}
    g.update(host_consts())
    f32 = np.float32
    w_in = np.asarray(inp["w_in"], f32)
    offs = np.cumsum([0, 256, 256, 256, 256, 192, 128, 16, 256, 256, 256, 256, 4096])
    seg = lambda i: w_in[:, :, offs[i]:offs[i + 1]]
    g["w_ada"] = np.ascontiguousarray(inp["w_ada"], f32)
    ba = np.asarray(inp["b_ada"], f32)
    g["bada"] = np.ascontiguousarray(ba.reshape(DEPTH, 48, 128).transpose(0, 2, 1))
    g["gmix"] = np.stack([fm_cols(inp["norm_mix_g"][l], 8) for l in range(DEPTH)])
    g["gffn"] = np.stack([fm_cols(inp["norm_ffn_g"][l], 8) for l in range(DEPTH)])
    def pad3(w):
        o = np.zeros((DEPTH, D, 384), f32)
        for j in range(8):
            o[:, :, (j // 3) * 128 + (j % 3) * 32:(j // 3) * 128 + (j % 3) * 32 + 32] = w[:, :, j * 32:(j + 1) * 32]
        return o
    g["w_da_qk"] = np.ascontiguousarray(np.concatenate([pad3(seg(0)), pad3(seg(1))], axis=2))
    g["w_da_v"] = np.ascontiguousarray(seg(2))
    dg = np.asarray(inp["da_qk_norm_g"], f32)
    g["da_g"] = np.ascontiguousarray(np.stack([np.tile(dg[:, 0, :], (1, 4)), np.tile(dg[:, 1, :], (1, 4))], axis=2))
    g["da_lam"] = np.ascontiguousarray(np.broadcast_to(np.asarray(inp["da_lambda"], f32).reshape(DEPTH, 1, 128), (DEPTH, 128, 128)))
    g["da_sub"] = np.ascontiguousarray(np.broadcast_to(np.asarray(inp["da_subln_g"], f32).reshape(DEPTH, 1, 64), (DEPTH, 128, 64)))
    g["w_mla_c"] = np.zeros((DEPTH, D, 384), f32)
    g["w_mla_c"][:, :, 0:192] = seg(4)
    g["w_mla_c"][:, :, 256:384] = seg(5)
    g["w_mla_kr"] = np.zeros((DEPTH, D, 128), f32)
    g["w_mla_kr"][:, :, 32:48] = seg(6)
    g["w_mla_kr"][:, :, 96:112] = seg(6)
    wuq = np.asarray(inp["mla_w_uq"], f32)
    wuqp = np.zeros((DEPTH, 256, 256), f32)
    for h in range(4):
        wuqp[:, 0:192, 64 * h:64 * h + 48] = wuq[:, :, 48 * h:48 * h + 48]
    g["w_uq"] = np.ascontiguousarray(wuqp.reshape(DEPTH, 2, 128, 256).transpose(0, 2, 1, 3))
    wukv = np.asarray(inp["mla_w_ukv"], f32)
    g["w_ukvk"] = np.zeros((DEPTH, 128, 256), f32)
    g["w_ukvv"] = np.zeros((DEPTH, 128, 256), f32)
    for h in range(4):
        g["w_ukvk"][:, :, 64 * h:64 * h + 32] = wukv[:, :, 96 * h:96 * h + 32]
        g["w_ukvv"][:, :, 64 * h:64 * h + 64] = wukv[:, :, 96 * h + 32:96 * h + 96]
    gcq = np.zeros((DEPTH, 256), f32); gcq[:, 0:192] = np.asarray(inp["mla_cq_norm_g"], f32)
    gkv = np.asarray(inp["mla_ckv_norm_g"], f32)
    g["mla_gc"] = np.ascontiguousarray(np.stack([gcq[:, 0:128], gcq[:, 128:256], gkv], axis=2))
    mg = np.asarray(inp["mla_qk_norm_g"], f32)
    mgp = np.zeros((DEPTH, 2, 128), f32)
    for h in range(2):
        mgp[:, :, 64 * h:64 * h + 48] = mg
    g["mla_g"] = np.ascontiguousarray(mgp.transpose(0, 2, 1))
    g["w_s5"] = np.ascontiguousarray(seg(3))
    Bre = np.asarray(inp["s5_b_re"], f32); Bim = np.asarray(inp["s5_b_im"], f32)
    Cre = np.asarray(inp["s5_c_re"], f32); Cim = np.asarray(inp["s5_c_im"], f32)
    s5B = np.zeros((DEPTH, 2, 2, 128, 1024), f32)
    s5C = np.zeros((DEPTH, 2, 2, 128, 8, 128), f32)
    for gi in range(16):
        kt, gl = gi // 8, gi % 8
        s5B[:, :, kt, gl * 16:(gl + 1) * 16, gl * 64:(gl + 1) * 64] = Bre[:, :, gi].transpose(0, 1, 3, 2)
        s5B[:, :, kt, gl * 16:(gl + 1) * 16, 512 + gl * 64:512 + (gl + 1) * 64] = Bim[:, :, gi].transpose(0, 1, 3, 2)
        i, po = gi // 2, (gi % 2) * 64
        s5C[:, :, 0, po:po + 64, i, gl * 16:(gl + 1) * 16] = Cre[:, :, gi].transpose(0, 1, 3, 2)
        s5C[:, :, 1, po:po + 64, i, gl * 16:(gl + 1) * 16] = Cim[:, :, gi].transpose(0, 1, 3, 2)
    g["s5B"] = s5B; g["s5C"] = s5C
    lre = np.asarray(inp["s5_lam_re"], f32).reshape(DEPTH, 2, 1024)
    lim = np.asarray(inp["s5_lam_im"], f32).reshape(DEPTH, 2, 1024)
    ldt = np.repeat(np.asarray(inp["s5_log_dt"], f32), 64, axis=2)
    tm = np.stack([lre, lim, ldt], axis=2)
    g["s5tm"] = np.ascontiguousarray(np.broadcast_to(tm[:, :, :, None, :], (DEPTH, 2, 3, 128, 1024)))
    g["s5fm"] = np.ascontiguousarray(tm.reshape(DEPTH, 2, 3, 8, 128).transpose(0, 1, 4, 2, 3))
    g["s5d"] = np.stack([fm_cols(np.asarray(inp["s5_d"], f32)[l].reshape(256), 2) for l in range(DEPTH)])
    g["s5bg"] = np.stack([fm_cols(np.asarray(inp["s5_b_glu"], f32)[l], 2) for l in range(DEPTH)])
    g["w_glu"] = np.ascontiguousarray(inp["s5_w_glu"], f32)
    pidx = np.arange(128, dtype=f32)
    posc = np.zeros((2, 128, 2), f32); posc[0, :, 0] = -pidx; posc[1, :, 0] = -(127 - pidx)
    posr = np.zeros((2, 128, 128), f32); posr[0] = pidx[None, :]; posr[1] = (127 - pidx)[None, :]
    tri = np.zeros((2, 128, 128), f32)
    tri[0] = (pidx[:, None] <= pidx[None, :]).astype(f32)
    tri[1] = (pidx[:, None] >= pidx[None, :]).astype(f32)
    g["posc"] = posc; g["posr"] = posr; g["tri"] = tri
    g["w_rw"] = np.ascontiguousarray(np.concatenate([seg(7), seg(8), seg(9), seg(10)], axis=2))
    mu = np.asarray(inp["rw_mu"], f32).reshape(DEPTH, 1, 1024)
    g["rw_mu_row"] = np.ascontiguousarray(np.broadcast_to(mu, (DEPTH, 128, 1024)))
    colsl = []
    for l in range(DEPTH):
        cs = [inp["rw_k_k"][l], inp["rw_k_a"][l], inp["rw_ln_g"][l], inp["rw_ln_b"][l], np.asarray(inp["rw_r_k"][l]).reshape(256),
              inp["rw_w0"][l, 0], inp["rw_w0"][l, 1], inp["rw_a0"][l, 0], inp["rw_a0"][l, 1]]
        colsl.append(np.stack([fm_cols(c, 2) for c in cs], axis=2))
    g["rw_cols"] = np.ascontiguousarray(np.stack(colsl))
    g["rw_w0row"] = np.ascontiguousarray(np.asarray(inp["rw_w0"], f32).reshape(DEPTH, 2, 1, 256))
    for nm in ("rw_w1", "rw_w2", "rw_a1", "rw_a2", "rw_g1", "rw_g2"):
        g[nm] = np.ascontiguousarray(inp[nm], f32)
    t = np.arange(128)
    same = (t[:, None] // 64) == (t[None, :] // 64)
    tri = np.zeros((2, 128, 2, 128), f32)
    tri[0, :, 0, :] = (same & (t[:, None] <= t[None, :])); tri[0, :, 1, :] = (same & (t[:, None] < t[None, :]))
    tri[1, :, 0, :] = (same & (t[:, None] >= t[None, :])); tri[1, :, 1, :] = (same & (t[:, None] > t[None, :]))
    g["rw_tri"] = tri
    ind = np.zeros((128, 2), f32); ind[:64, 0] = 1; ind[64:, 1] = 1
    g["rw_ind"] = ind
    m1 = np.zeros((2, 128, 256), f32)
    m1[0, :, :128] = (same & (t[:, None] < t[None, :])); m1[0, :, 128:] = (same & (t[:, None] <= t[None, :]))
    m1[1, :, :128] = (same & (t[:, None] > t[None, :])); m1[1, :, 128:] = (same & (t[:, None] >= t[None, :]))
    g["rw_m1"] = m1
    mnt = np.zeros((2, 128, 128), f32)
    mnt[0] = (same & (t[None, :] < t[:, None])); mnt[1] = (same & (t[None, :] > t[:, None]))
    g["rw_mnt"] = mnt
    g["w_gates"] = np.ascontiguousarray(seg(11).reshape(DEPTH, 8, 128, 4, 8, 128).transpose(0, 4, 2, 1, 3, 5)).reshape(DEPTH, 8, 128, 4096)
    g["w_branch"] = np.ascontiguousarray(np.asarray(inp["w_branch"], f32).reshape(DEPTH, 4, 2, 128, D).transpose(0, 3, 2, 1, 4)).reshape(DEPTH, 128, 8192)
    g["w_out"] = np.ascontiguousarray(inp["w_out"], f32)
    g["router_w"] = np.ascontiguousarray(inp["router_w"], f32)
    g["router_b"] = np.ascontiguousarray(np.broadcast_to(np.asarray(inp["router_bias"], f32).reshape(1, 16), (128, 16)))
    sel = np.zeros((16, 16, 128), f32)
    for e in range(16):
        sel[e, e, :] = 1.0
    g["sel"] = sel
    g["exp_w_gate"] = np.ascontiguousarray(inp["exp_w_gate"], f32)
    g["exp_w_up"] = np.ascontiguousarray(inp["exp_w_up"], f32)
    g["exp_w_down"] = np.ascontiguousarray(inp["exp_w_down"], f32)
    return g


def prep_core(inp, b):
    x = np.asarray(inp["x"], np.float32)[b]
    ctx = np.asarray(inp["ctx"], np.float32)[b]
    xin = np.ascontiguousarray(np.concatenate([ctx, x], axis=0))
    c2 = np.stack([np.asarray(inp["c"], np.float32)[b], np.asarray(inp["c_ctx"], np.float32)], axis=0)
    c2T = np.ascontiguousarray(c2.reshape(2, 8, 128).transpose(2, 1, 0))
    return {"xin": xin, "c2T": c2T}


RW_LEVEL = int(os.environ.get('RW_LEVEL', '9'))
RWSTOP = int(os.environ.get('RWSTOP', '9'))
S5SKEW = int(os.environ.get('S5SKEW', '1'))
RWSKEW = int(os.environ.get('RWSKEW', '0'))
RW_SUB = int(os.environ.get('RW_SUB', '9'))
STAGES_ALL = ("L0", "L1", "da", "mla", "s5", "rw", "merge", "moe")


def kernel(**inputs):
    nc, k, _ = build(STAGES_ALL)
    shared = prep_shared(inputs)
    in_maps = []
    for b in range(8):
        m = dict(shared)
        m.update(prep_core(inputs, b))
        in_maps.append({kk: m[kk] for kk in k.ins})
    res = run_bass_kernel_spmd(nc, in_maps, core_ids=list(range(8)))
    return np.stack([np.asarray(r["out"]) for r in res.results], axis=0).astype(np.float32)
```

```python
import math
import os
import numpy as np
import concourse.bass as bass
import concourse.mybir as mybir
from concourse.bass_utils import run_bass_kernel_spmd

F32 = mybir.dt.float32
BF16 = mybir.dt.bfloat16
I32 = mybir.dt.int32
AF = mybir.ActivationFunctionType
ALU = mybir.AluOpType
AX = mybir.AxisListType

D = 1024
NT = 2304
NCTX = 256
NLAT = 2048
DEPTH = 2
EPS = 1e-6
GROUPS = [(0, 256)] + [(256 + 512 * i, 512) for i in range(4)]
NTT = 18


class Buf:
    __slots__ = ("name", "w", "r", "excl")

    def __init__(self, name="", excl=False):
        self.name = name
        self.w = None
        self.r = {}
        self.excl = excl


class Prog:
    NDMA = 8

    def __init__(self, nc):
        self.nc = nc
        self.eng = {"pe": nc.tensor, "act": nc.scalar, "dve": nc.vector, "pool": nc.gpsimd, "sp": nc.sync}
        self.sems = {}
        self.cnt = {}
        self.seen = {e: {} for e in self.eng}
        self.pend = {e: ([], []) for e in self.eng}
        for e in self.eng:
            self.sems[e] = nc.alloc_semaphore("s_" + e)
            self.cnt[e] = 0
        self.dq = {}
        for q in ("sp", "pool"):
            lst = []
            for i in range(self.NDMA):
                key = ("d", q, i)
                self.sems[key] = nc.alloc_semaphore(f"d_{q}{i}")
                self.cnt[key] = 0
                lst.append(key)
            self.dq[q] = [lst, 0, [None] * self.NDMA]
        self.ninst = 0

    def _wait(self, e, tok):
        key, val = tok
        if key == e:
            if e == "pe":
                return
            if val < self.cnt[e] - 1:
                return
        if self.seen[e].get(key, 0) >= val:
            return
        self.eng[e].wait_ge(self.sems[key], val)
        self.seen[e][key] = val
        self.ninst += 1

    def _deps(self, e, reads, writes):
        for b in reads:
            if b.w is not None:
                self._wait(e, b.w)
            if b.excl:
                for tok in b.r.values():
                    if tok[0] != e:
                        self._wait(e, tok)
        for b in writes:
            if b.w is not None:
                self._wait(e, b.w)
            for tok in b.r.values():
                if tok[0] != e:
                    self._wait(e, tok)

    def op(self, e, fn, reads=(), writes=(), inc=True):
        self._deps(e, reads, writes)
        inst = fn(self.eng[e])
        self.ninst += 1
        pr, pw = self.pend[e]
        pr.extend(reads)
        pw.extend(writes)
        if inc:
            self.cnt[e] += 1
            tok = (e, self.cnt[e])
            inst.then_inc(self.sems[e], 1)
            for b in pr:
                b.r[e] = tok
            for b in pw:
                b.w = tok
                b.r = {}
            self.pend[e] = ([], [])
        return inst

    def dma(self, q, out, in_, reads=(), writes=(), **kw):
        lst, rr, last = self.dq[q]
        k = rr
        self.dq[q][1] = (rr + 1) % self.NDMA
        if last[k] is not None:
            self._wait(q, last[k])
        self._deps(q, reads, writes)
        inst = self.eng[q].dma_start(out=out, in_=in_, **kw)
        self.ninst += 1
        key = lst[k]
        self.cnt[key] += 16
        tok = (key, self.cnt[key])
        inst.then_inc(self.sems[key], 16)
        last[k] = tok
        for b in reads:
            b.r[key] = tok
        for b in writes:
            b.w = tok
            b.r = {}
        return tok

    def finish(self, bufs, e="sp"):
        for b in bufs:
            if b.w is not None:
                self._wait(e, b.w)
            for tok in b.r.values():
                self._wait(e, tok)

    def barrier(self):
        toks = [(k, v) for k, v in self.cnt.items() if v > 0]
        for e in self.eng:
            for tok in toks:
                if tok[0] != e:
                    self._wait(e, tok)


from contextlib import ExitStack


class K:
    def __init__(self, nc, dbg=None):
        self.nc = nc
        self.P = Prog(nc)
        self.dbg = dbg or {}
        self.ins = {}
        self.uid = 0
        self.ps = []
        self.pb = []
        for i in range(8):
            self.ps.append(nc.alloc_psum_tensor(f"ps{i}", [128, 512], F32))
            self.pb.append(Buf(f"ps{i}", excl=True))
        self.rr = 0
        self.rrset = list(range(8))

    def din(self, name, shape, dtype=F32):
        t = self.nc.dram_tensor(name, list(shape), dtype, kind="ExternalInput").ap()
        self.ins[name] = t
        return t

    def sb(self, stack, shape, dtype, name=None):
        self.uid += 1
        nm = f"{name or 't'}_{self.uid}"
        t = stack.enter_context(self.nc.sbuf_tensor(nm, list(shape), dtype))
        nb = int(np.prod(shape[1:])) * (2 if dtype == BF16 else 4)
        self.live = getattr(self, "live", 0) + nb
        if self.live > getattr(self, "peak", 0):
            self.peak = self.live; self.peak_at = nm
        def _dec(nb=nb):
            self.live -= nb
        stack.callback(_dec)
        return t, Buf(nm)

    def bank(self):
        i = self.rrset[self.rr % len(self.rrset)]
        self.rr += 1
        return i

    def mm(self, bank, out, lhsT, rhs, start, stop, reads, inc=None):
        self.P.op("pe", lambda e: e.matmul(out, lhsT=lhsT, rhs=rhs, start=start, stop=stop, skip_group_check=True),
                  reads=reads, writes=[self.pb[bank]], inc=(stop if inc is None else inc))

    def act(self, out, in_, func, reads, writes, scale=None, bias=None, eng="act"):
        kw = {}
        if scale is not None:
            kw["scale"] = scale
        if bias is not None:
            kw["bias"] = bias
        self.P.op("act", lambda e: e.activation(out=out, in_=in_, func=func, **kw), reads=reads, writes=writes)

    def tt(self, eng, out, in0, in1, op, reads, writes):
        self.P.op(eng, lambda e: e.tensor_tensor(out=out, in0=in0, in1=in1, op=op), reads=reads, writes=writes)

    def ts(self, eng, out, in0, s1, s2, op0, op1, reads, writes):
        if op1 is None:
            self.P.op(eng, lambda e: e.tensor_scalar(out=out, in0=in0, scalar1=s1, scalar2=None, op0=op0),
                      reads=reads, writes=writes)
        else:
            self.P.op(eng, lambda e: e.tensor_scalar(out=out, in0=in0, scalar1=s1, scalar2=s2, op0=op0, op1=op1),
                      reads=reads, writes=writes)

    def stt(self, out, in0, scalar, in1, op0, op1, reads, writes):
        self.P.op("dve", lambda e: e.scalar_tensor_tensor(out=out, in0=in0, scalar=scalar, in1=in1, op0=op0, op1=op1),
                  reads=reads, writes=writes)

    def copy(self, eng, out, in_, reads, writes):
        if eng == "act":
            self.P.op("act", lambda e: e.copy(out=out, in_=in_), reads=reads, writes=writes)
        else:
            self.P.op(eng, lambda e: e.tensor_copy(out=out, in_=in_), reads=reads, writes=writes)

    def memset(self, eng, ap, val, writes):
        self.P.op(eng, lambda e: e.memset(ap, val), writes=writes)

    def load(self, q, out, in_, writes, reads=()):
        self.P.dma(q, out, in_, reads=reads, writes=writes)

    def rstd_from_ss(self, out, ss_ps, bank, inv_n, tmp, tmpb, writes):
        self.P.op("act", lambda e: e.activation(out=tmp, in_=ss_ps, func=AF.Ln, scale=inv_n, bias=self.eps_col[:]),
                  reads=[self.pb[bank], self.b_const], writes=[tmpb])
        self.P.op("act", lambda e: e.activation(out=out, in_=tmp, func=AF.Exp, scale=-0.5), reads=[tmpb], writes=writes)


def host_consts():
    ident = np.eye(128, dtype=np.float32)
    bo32 = np.kron(np.eye(4, dtype=np.float32), np.ones((32, 32), np.float32))
    bo64 = np.kron(np.eye(2, dtype=np.float32), np.ones((64, 64), np.float32))
    Rda = np.zeros((128, 128), np.float32)
    for h in range(4):
        for d in range(16):
            Rda[32 * h + d, 32 * h + d + 16] = -1.0
            Rda[32 * h + d + 16, 32 * h + d] = 1.0
    Rmla = np.zeros((128, 128), np.float32)
    for h in range(2):
        for d in range(8):
            Rmla[64 * h + 32 + d, 64 * h + 40 + d] = -1.0
            Rmla[64 * h + 40 + d, 64 * h + 32 + d] = 1.0
    cst = np.concatenate([ident, bo32, bo64, Rda.T.copy(), Rmla.T.copy()], axis=1)
    def tables(rot_dim):
        rows = NLAT // 64
        row = np.repeat(np.arange(rows, dtype=np.float32), 64)
        col = np.tile(np.arange(64, dtype=np.float32), rows)
        n_freq = rot_dim // 4
        inv = (10000.0 ** (-np.arange(n_freq, dtype=np.float32) / n_freq)).astype(np.float32)
        ang = np.concatenate([row[:, None] * inv, col[:, None] * inv], axis=-1)
        return np.cos(ang).astype(np.float32), np.sin(ang).astype(np.float32)
    c, s = tables(32)
    da_cos = np.ones((128, NT), np.float32)
    da_sin = np.zeros((128, NT), np.float32)
    for h in range(4):
        for d in range(32):
            da_cos[32 * h + d, NCTX:] = c[:, d % 16]
            da_sin[32 * h + d, NCTX:] = s[:, d % 16]
    c, s = tables(16)
    ml_cos = np.ones((128, NT), np.float32)
    ml_sin = np.zeros((128, NT), np.float32)
    for h in range(2):
        for d in range(16):
            ml_cos[64 * h + 32 + d, NCTX:] = c[:, d % 8]
            ml_sin[64 * h + 32 + d, NCTX:] = s[:, d % 8]
    return dict(cst=cst, da_cos=da_cos, da_sin=da_sin, ml_cos=ml_cos, ml_sin=ml_sin)


def fm_cols(v, ntile):
    return np.ascontiguousarray(np.asarray(v, np.float32).reshape(ntile, 128).T)


def build(stages, dbg_names=()):
    nc = bass.Bass("TRN2", target_bir_lowering=False)
    k = K(nc)
    P = k.P
    top = ExitStack()
    xin = k.din("xin", [NT, D])
    c2T = k.din("c2T", [128, 8, 2])
    cst_d = k.din("cst", [128, 640])
    da_cos_d = k.din("da_cos", [128, NT]); da_sin_d = k.din("da_sin", [128, NT])
    ml_cos_d = k.din("ml_cos", [128, NT]); ml_sin_d = k.din("ml_sin", [128, NT])
    w_ada = k.din("w_ada", [DEPTH, D, 6 * D])
    bada_d = k.din("bada", [DEPTH, 128, 48])
    gmix_d = k.din("gmix", [DEPTH, 128, 8]); gffn_d = k.din("gffn", [DEPTH, 128, 8])
    w_da_qk = k.din("w_da_qk", [DEPTH, D, 768]); w_da_v = k.din("w_da_v", [DEPTH, D, 256])
    da_g_d = k.din("da_g", [DEPTH, 128, 2])
    da_lam_d = k.din("da_lam", [DEPTH, 128, 128])
    da_sub_d = k.din("da_sub", [DEPTH, 128, 64])
    w_mla_c = k.din("w_mla_c", [DEPTH, D, 384]); w_mla_kr = k.din("w_mla_kr", [DEPTH, D, 128])
    w_uq_d = k.din("w_uq", [DEPTH, 128, 2, 256]); w_ukvk_d = k.din("w_ukvk", [DEPTH, 128, 256]); w_ukvv_d = k.din("w_ukvv", [DEPTH, 128, 256])
    mla_gc_d = k.din("mla_gc", [DEPTH, 128, 3])
    mla_g_d = k.din("mla_g", [DEPTH, 128, 2])
    w_gates_d = k.din("w_gates", [DEPTH, 8, 128, 4096]); w_branch_d = k.din("w_branch", [DEPTH, 128, 8192]); w_out_d = k.din("w_out", [DEPTH, D, D])
    router_w_d = k.din("router_w", [D, 16]); router_b_d = k.din("router_b", [128, 16]); sel_d = k.din("sel", [16, 16, 128])
    exp_g_d = k.din("exp_w_gate", [DEPTH, 16, D, 512]); exp_u_d = k.din("exp_w_up", [DEPTH, 16, D, 512]); exp_d_d = k.din("exp_w_down", [DEPTH, 16, 512, D])
    w_s5_d = k.din("w_s5", [DEPTH, D, 256])
    s5B_d = k.din("s5B", [DEPTH, 2, 2, 128, 1024])
    s5C_d = k.din("s5C", [DEPTH, 2, 2, 128, 8, 128])
    s5tm_d = k.din("s5tm", [DEPTH, 2, 3, 128, 1024])
    s5fm_d = k.din("s5fm", [DEPTH, 2, 128, 3, 8])
    s5d_d = k.din("s5d", [DEPTH, 128, 2]); s5bg_d = k.din("s5bg", [DEPTH, 128, 2])
    w_glu_d = k.din("w_glu", [DEPTH, 256, 256])
    posc_d = k.din("posc", [2, 128, 2]); posr_d = k.din("posr", [2, 128, 128]); tri_d = k.din("tri", [2, 128, 128])
    w_rw_d = k.din("w_rw", [DEPTH, D, 1024]); rw_mu_d = k.din("rw_mu_row", [DEPTH, 128, 1024])
    rw_cols_d = k.din("rw_cols", [DEPTH, 128, 2, 9])
    rw_w0row_d = k.din("rw_w0row", [DEPTH, 2, 1, 256])
    rw_w1_d = k.din("rw_w1", [DEPTH, 2, 256, 32]); rw_w2_d = k.din("rw_w2", [DEPTH, 2, 32, 256])
    rw_a1_d = k.din("rw_a1", [DEPTH, 2, 256, 32]); rw_a2_d = k.din("rw_a2", [DEPTH, 2, 32, 256])
    rw_g1_d = k.din("rw_g1", [DEPTH, 256, 64]); rw_g2_d = k.din("rw_g2", [DEPTH, 64, 256])
    rw_tri_d = k.din("rw_tri", [2, 128, 2, 128]); rw_ind_d = k.din("rw_ind", [128, 2])
    rw_m1_d = k.din("rw_m1", [2, 128, 256]); rw_mnt_d = k.din("rw_mnt", [2, 128, 128])
    out_d = nc.dram_tensor("out", [NLAT, D], F32, kind="ExternalOutput").ap()
    dbg_out = {}

    xT, b_xT = k.sb(top, [128, 8, NT], F32, "xT")
    cstf, b_cstf = k.sb(top, [128, 640], F32, "cstf")
    cstb, b_cstb = k.sb(top, [128, 640], BF16, "cstb")
    onesb, b_ones = k.sb(top, [128, 128], BF16, "ones")
    k.eps_col, k.b_const = k.sb(top, [128, 1], F32, "eps")
    MOD, b_MOD = k.sb(top, [128, DEPTH, 6, 8, 2], F32, "MOD")
    AB, b_AB = k.sb(top, [128, DEPTH, 2, 2, 8, 2], F32, "AB")
    rstd_b, b_rstd = k.sb(top, [128, NT], F32, "rstd")
    yall, b_yall = k.sb(top, [128, 4, 2, NT], BF16, "yall")
    yT = []
    for i in range(4):
        yT.append((yall[:, i], Buf(f"y{i}")))
    k.memset("dve", k.eps_col[:], EPS, [k.b_const])
    k.memset("dve", onesb[:], 1.0, [b_ones])
    k.load("sp", cstf[:], cst_d, [b_cstf])
    k.copy("dve", cstb[:], cstf[:], [b_cstf], [b_cstb])
    identf = cstf[:, 0:128]
    identb = cstb[:, 0:128]
    bo32 = cstb[:, 128:256]; bo64 = cstb[:, 256:384]; Rda = cstb[:, 384:512]; Rmla = cstb[:, 512:640]

    def dump(name, ap_sb, buf, shape, dtype=F32):
        if name in dbg_names:
            d = nc.dram_tensor("dbg_" + name, list(shape), dtype, kind="ExternalOutput").ap()
            dbg_out[name] = d
            P.dma("sp", d, ap_sb, reads=[buf])
            P.barrier()

    with ExitStack() as st:
        xs = [k.sb(st, [128, D], F32, "xstage") for _ in range(2)]
        for tt in range(NTT):
            xt, xb = xs[tt % 2]
            k.load("sp", xt[:], xin[tt * 128:(tt + 1) * 128, :], [xb])
            for half in range(2):
                bk = k.bank()
                for j in range(4):
                    kt = half * 4 + j
                    P.op("pe", lambda e: e.transpose(k.ps[bk][:, j * 128:(j + 1) * 128], xt[:, kt * 128:(kt + 1) * 128], identf),
                         reads=[xb, b_cstf], writes=[k.pb[bk]], inc=(j == 3))
                k.copy("dve" if half == 0 else "act", xT[:, half * 4:half * 4 + 4, tt * 128:(tt + 1) * 128],
                       k.ps[bk][:].rearrange("p (j t) -> p j t", j=4), [k.pb[bk]], [b_xT])
        scT, b_scT = k.sb(st, [128, 8, 2], F32, "scT")
        scb, b_scb = k.sb(st, [128, 8, 2], BF16, "scb")
        bada, b_bada = k.sb(st, [128, DEPTH, 48], F32, "bada")
        gm, b_gm = k.sb(st, [128, DEPTH, 2, 8], F32, "gm")
        k.load("sp", scT[:], c2T, [b_scT])
        k.act(scb[:], scT[:], AF.Silu, [b_scT], [b_scb])
        for l in range(DEPTH):
            k.load("sp", bada[:, l, :], bada_d[l], [b_bada])
            k.load("sp", gm[:, l, 0, :], gmix_d[l], [b_gm])
            k.load("sp", gm[:, l, 1, :], gffn_d[l], [b_gm])
        wa = [k.sb(st, [128, 8, 1024], BF16, "wada") for _ in range(2)]
        ci = 0
        for l in range(DEPTH):
            for ch in range(6):
                wt, wb = wa[ci % 2]; ci += 1
                k.load("pool", wt[:], w_ada[l, :, ch * 1024:(ch + 1) * 1024].rearrange("(kt p) n -> p kt n", p=128), [wb])
                bk = k.bank()
                for ft in range(8):
                    for kt in range(8):
                        k.mm(bk, k.ps[bk][:, ft * 2:ft * 2 + 2], wt[:, kt, ft * 128:(ft + 1) * 128], scb[:, kt, :],
                             kt == 0, kt == 7, [wb, b_scb])
                k.tt("dve", MOD[:, l, ch, :, :], k.ps[bk][:, 0:16].rearrange("p (f j) -> p f j", j=2),
                     bada[:, l, ch * 8:(ch + 1) * 8].unsqueeze(2).to_broadcast([128, 8, 2]), ALU.add,
                     [k.pb[bk], b_bada], [b_MOD])
        for l in range(DEPTH):
            for m in range(2):
                sh, sc = (0, 1) if m == 0 else (3, 4)
                P.op("dve", lambda e: e.scalar_tensor_tensor(out=AB[:, l, m, 0, :, :], in0=MOD[:, l, sc, :, :], scalar=1.0,
                                                             in1=gm[:, l, m, :].unsqueeze(2).to_broadcast([128, 8, 2]),
                                                             op0=ALU.add, op1=ALU.mult),
                     reads=[b_MOD, b_gm], writes=[b_AB])
                k.copy("dve", AB[:, l, m, 1, :, :], MOD[:, l, sh, :, :], [b_MOD], [b_AB])
        P.barrier()
    dump("xT", xT[:], b_xT, [128, 8, NT])
    dump("MOD", MOD[:], b_MOD, [128, DEPTH, 6, 8, 2])

    def jof(g):
        return 1 if g == 0 else 0

    def rms_stats(st):
        sq = [k.sb(st, [128, 512], BF16, "sq") for _ in range(2)]
        lnt, b_lnt = k.sb(st, [128, 512], F32, "lnt")
        i = 0
        for (c0, n) in GROUPS:
            bk = k.bank()
            for kt in range(8):
                s, sbf = sq[i % 2]; i += 1
                k.act(s[:, :n], xT[:, kt, c0:c0 + n], AF.Square, [b_xT], [sbf])
                k.mm(bk, k.ps[bk][:, :n], onesb[:], s[:, :n], kt == 0, kt == 7, [sbf, b_ones])
            k.rstd_from_ss(rstd_b[:, c0:c0 + n], k.ps[bk][:, :n], bk, 1.0 / D, lnt[:, :n], b_lnt, [b_rstd])

    def h_group(l, m, g, hg, hb, tmp, tmpb):
        c0, n = GROUPS[g]
        j = jof(g)
        for kt in range(8):
            k.tt("dve", tmp[:, :n], xT[:, kt, c0:c0 + n], rstd_b[:, c0:c0 + n], ALU.mult, [b_xT, b_rstd], [tmpb])
            P.op("act", lambda e: e.activation(out=hg[:, kt, :n], in_=tmp[:, :n], func=AF.Identity,
                                               scale=AB[:, l, m, 0, kt, j:j + 1], bias=AB[:, l, m, 1, kt, j:j + 1]),
                 reads=[tmpb, b_AB], writes=[hb])

    def load_w(q, st, src, ncols, name):
        wt, wb = k.sb(st, [128, 8, ncols], BF16, name)
        k.load(q, wt[:], src.rearrange("(kt p) n -> p kt n", p=128), [wb])
        return wt, wb

    def proj(bk, wt, wb, col0, hg, hb, n, start=True, stop=True):
        for kt in range(8):
            k.mm(bk, k.ps[bk][:, :n], wt[:, kt, col0:col0 + 128], hg[:, kt, :n], start and kt == 0, stop and kt == 7, [wb, hb])

    def headnorm_rope(st, src_bk, n, c0, blockones, inv_dim, gcol, gbuf, Rm, cos_d, sin_d, dst, dstb, scr):
        sq, b_sq, rs, b_rs, lnt, b_lnt, qn, b_qn, ct, b_ct, sn, b_sn, t1, b_t1, t2, b_t2 = scr
        src = k.ps[src_bk][:, :n]
        k.act(sq[:, :n], src, AF.Square, [k.pb[src_bk]], [b_sq])
        b2 = k.bank()
        k.mm(b2, k.ps[b2][:, :n], blockones, sq[:, :n], True, True, [b_sq, b_cstb])
        k.rstd_from_ss(rs[:, :n], k.ps[b2][:, :n], b2, inv_dim, lnt[:, :n], b_lnt, [b_rs])
        k.stt(qn[:, :n], src, gcol, rs[:, :n], ALU.mult, ALU.mult, [k.pb[src_bk], gbuf, b_rs], [b_qn])
        k.load("sp", ct[:, :n], cos_d[:, c0:c0 + n], [b_ct])
        k.load("sp", sn[:, :n], sin_d[:, c0:c0 + n], [b_sn])
        b3 = k.bank()
        k.mm(b3, k.ps[b3][:, :n], Rm, qn[:, :n], True, True, [b_qn, b_cstb])
        k.tt("pool", t1[:, :n], qn[:, :n], ct[:, :n], ALU.mult, [b_qn, b_ct], [b_t1])
        k.tt("dve", t2[:, :n], k.ps[b3][:, :n], sn[:, :n], ALU.mult, [k.pb[b3], b_sn], [b_t2])
        k.tt("pool", dst, t1[:, :n], t2[:, :n], ALU.add, [b_t1, b_t2], [dstb])

    def norm_scratch(st):
        out = []
        for nm, dt in (("sq", BF16), ("rs", F32), ("lnt", F32), ("qn", BF16), ("ct", F32), ("sn", F32), ("t1", F32), ("t2", F32)):
            t, b = k.sb(st, [128, 512], dt, nm)
            out += [t, b]
        return out

    def attention(st, qT, b_q, kT, b_k, V, b_V, heads, scale, epilogue, skip_ctx=False):
        Et = [k.sb(st, [128, 512], BF16, "E") for _ in range(4)]
        sbanks = [0, 1, 2, 3]
        obanks = [4, 5, 6, 7]
        assert len(heads) % 2 == 0
        steps = []
        for g, (c0, n) in enumerate(GROUPS):
            if skip_ctx and g == 0:
                continue
            ktiles = [0, 1] if g == 0 else list(range(NTT))
            for hp in range(len(heads) // 2):
                for ki, kt in enumerate(ktiles):
                    steps.append((g, c0, n, hp, ki, kt, len(ktiles)))

        def qk_exp(si):
            g, c0, n, hp, ki, kt, nk = steps[si]
            outs = []
            for m in range(2):
                tile, pb, Kd, vh = heads[2 * hp + m]
                sbk = sbanks[(2 * si + m) % 4]
                k.mm(sbk, k.ps[sbk][:, :n], kT[pb:pb + Kd, tile, kt * 128:(kt + 1) * 128], qT[pb:pb + Kd, tile, c0:c0 + n],
                     True, True, [b_k, b_q])
                outs.append(sbk)
            res = []
            for m in range(2):
                sbk = outs[m]
                E, Eb = Et[(2 * si + m) % 4]
                k.act(E[:, :n], k.ps[sbk][:, :n], AF.Exp, [k.pb[sbk]], [Eb], scale=scale)
                res.append((E, Eb))
            return res

        oi = 0
        obs = None
        nxt = qk_exp(0)
        for si, (g, c0, n, hp, ki, kt, nk) in enumerate(steps):
            cur = nxt
            if si + 1 < len(steps):
                nxt = qk_exp(si + 1)
            nq = n // 128
            if ki == 0:
                obs = [obanks[(2 * oi) % 4], obanks[(2 * oi + 1) % 4]]; oi += 1
            for m in range(2):
                E, Eb = cur[m]
                vh = heads[2 * hp + m][3]
                ob = obs[m]
                for qi in range(nq):
                    P.op("pe", lambda e: e.matmul(k.ps[ob][:, qi * 65:(qi + 1) * 65], lhsT=E[:, qi * 128:(qi + 1) * 128],
                                                  rhs=V[:, kt, vh, :], start=(ki == 0 and qi == 0), stop=(ki == nk - 1),
                                                  skip_group_check=True),
                         reads=[Eb, b_V], writes=[k.pb[ob]], inc=(qi == nq - 1))
            if ki == nk - 1:
                for m in range(2):
                    epilogue(g, c0, nq, 2 * hp + m, obs[m])

    def transpose_out(ytok, b_ytok, nq, c0, ydst, b_ydst):
        for qi in range(nq):
            bk = k.bank()
            pv = k.ps[bk][:].bitcast(BF16)
            for tile in range(2):
                P.op("pe", lambda e: e.transpose(pv[:, tile * 128:(tile + 1) * 128], ytok[:, qi, tile * 128:(tile + 1) * 128], identb),
                     reads=[b_ytok, b_cstb], writes=[k.pb[bk]], inc=(tile == 1))
            k.copy("dve", ydst[:, :, c0 + qi * 128:c0 + (qi + 1) * 128], pv[:, 0:256].rearrange("p (j t) -> p j t", j=2),
                   [k.pb[bk]], [b_ydst])

    def da_mixer(l):
        lam_init = 0.8 - 0.6 * math.exp(-0.3 * l)
        ydst, b_ydst = yT[0]
        with ExitStack() as st:
            qT, b_q = k.sb(st, [128, 3, NT], BF16, "daq")
            kT, b_k = k.sb(st, [128, 3, NT], BF16, "dak")
            V, b_V = k.sb(st, [128, NTT, 4, 65], BF16, "dav")
            gcol, b_g = k.sb(st, [128, 2], F32, "dag")
            lam, b_lam = k.sb(st, [128, 128], F32, "dalam")
            lt, b_lt = k.sb(st, [128, 8], F32, "dalt")
            gsub, b_gsub = k.sb(st, [128, 64], F32, "dagsub")
            k.load("sp", gcol[:], da_g_d[l], [b_g])
            k.load("sp", lam[:], da_lam_d[l], [b_lam])
            k.load("sp", gsub[:], da_sub_d[l], [b_gsub])
            k.ts("dve", gsub[:], gsub[:], 1.0 - lam_init, None, ALU.mult, None, [b_gsub], [b_gsub])
            k.tt("dve", lam[:, 0:32], lam[:, 0:32], lam[:, 32:64], ALU.mult, [b_lam], [b_lam])
            k.tt("dve", lam[:, 64:96], lam[:, 64:96], lam[:, 96:128], ALU.mult, [b_lam], [b_lam])
            P.op("dve", lambda e: e.tensor_reduce(out=lt[:, 0:1], in_=lam[:, 0:32], axis=AX.X, op=ALU.add), reads=[b_lam], writes=[b_lt])
            P.op("dve", lambda e: e.tensor_reduce(out=lt[:, 1:2], in_=lam[:, 64:96], axis=AX.X, op=ALU.add), reads=[b_lam], writes=[b_lt])
            k.act(lt[:, 2:4], lt[:, 0:2], AF.Exp, [b_lt], [b_lt])
            k.tt("dve", lt[:, 4:5], lt[:, 3:4], lt[:, 2:3], ALU.subtract, [b_lt], [b_lt])
            k.ts("dve", lt[:, 4:5], lt[:, 4:5], -lam_init, None, ALU.add, None, [b_lt], [b_lt])
            k.memset("pool", V[:, :, :, 64:65], 1.0, [b_V])
            with ExitStack() as s2:
                wqk, b_wqk = load_w("pool", s2, w_da_qk[l], 768, "wqk")
                wv, b_wv = load_w("pool", s2, w_da_v[l], 256, "wv")
                hgs = [k.sb(s2, [128, 8, 512], BF16, "hg") for _ in range(2)]
                tmp, tmpb = k.sb(s2, [128, 512], F32, "htmp")
                scr = norm_scratch(s2)
                for g, (c0, n) in enumerate(GROUPS):
                    hg, hb = hgs[g % 2]
                    h_group(l, 0, g, hg, hb, tmp, tmpb)
                    for ti in range(6):
                        bk = k.bank()
                        proj(bk, wqk, b_wqk, ti * 128, hg, hb, n)
                        dst = (qT if ti < 3 else kT)
                        dstb = (b_q if ti < 3 else b_k)
                        headnorm_rope(s2, bk, n, c0, bo32, 1.0 / 32, gcol[:, (ti // 3):(ti // 3) + 1], b_g, Rda, da_cos_d, da_sin_d,
                                      dst[:, ti % 3, c0:c0 + n], dstb, scr)
                    for qi in range(n // 128):
                        tt_ = (c0 + qi * 128) // 128
                        bk = k.bank()
                        for kt in range(8):
                            k.mm(bk, k.ps[bk][:, 0:256], hg[:, kt, qi * 128:(qi + 1) * 128], wv[:, kt, :], kt == 0, kt == 7, [hb, b_wv])
                        k.copy("act", V[:, tt_, :, 0:64], k.ps[bk][:, 0:256].rearrange("p (h d) -> p h d", h=4), [k.pb[bk]], [b_V])
                P.barrier()
            dump("da_q", qT[:], b_q, [128, 3, NT], BF16)
            dump("da_k", kT[:], b_k, [128, 3, NT], BF16)
            dump("da_v", V[:], b_V, [128, NTT, 4, 65], BF16)
            with ExitStack() as s3:
                ytok, b_ytok = k.sb(s3, [128, 4, 256], BF16, "ytok")
                o0, b_o0 = k.sb(s3, [128, 4, 64], F32, "o0")
                dd, b_dd = k.sb(s3, [128, 4, 64], F32, "dd")
                junk, b_junk = k.sb(s3, [128, 64], F32, "junk")
                rc, b_rc = k.sb(s3, [128, 4, 4], F32, "rc")
                lnt, b_lnt = k.sb(s3, [128, 4], F32, "lnt2")
                state = {}

                def epi(g, c0, nq, hidx, ob):
                    h, m = hidx // 2, hidx % 2
                    if m == 0:
                        state["ob0"] = ob
                        return
                    ob0 = state["ob0"]
                    O0 = k.ps[ob0][:, 0:nq * 65].rearrange("p (q c) -> p q c", c=65)
                    O1 = k.ps[ob][:, 0:nq * 65].rearrange("p (q c) -> p q c", c=65)
                    P.op("dve", lambda e: e.reciprocal(out=rc[:, 0, 0:nq], in_=O0[:, :, 64]), reads=[k.pb[ob0]], writes=[b_rc])
                    P.op("dve", lambda e: e.reciprocal(out=rc[:, 1, 0:nq], in_=O1[:, :, 64]), reads=[k.pb[ob]], writes=[b_rc])
                    k.ts("dve", rc[:, 1, 0:nq], rc[:, 1, 0:nq], lt[:, 4:5], None, ALU.mult, None, [b_rc, b_lt], [b_rc])
                    for qi in range(nq):
                        k.ts("dve", o0[:, qi, :], O0[:, qi, 0:64], rc[:, 0, qi:qi + 1], None, ALU.mult, None, [k.pb[ob0], b_rc], [b_o0])
                        k.stt(dd[:, qi, :], O1[:, qi, 0:64], rc[:, 1, qi:qi + 1], o0[:, qi, :], ALU.mult, ALU.add,
                              [k.pb[ob], b_rc, b_o0], [b_dd])
                        P.op("act", lambda e: e.activation(out=junk[:], in_=dd[:, qi, :], func=AF.Square, accum_out=rc[:, 2, qi:qi + 1]),
                             reads=[b_dd], writes=[b_junk, b_rc])
                    P.op("act", lambda e: e.activation(out=lnt[:, 0:nq], in_=rc[:, 2, 0:nq], func=AF.Ln, scale=1.0 / 64, bias=k.eps_col[:]),
                         reads=[b_rc, k.b_const], writes=[b_lnt])
                    k.act(rc[:, 3, 0:nq], lnt[:, 0:nq], AF.Exp, [b_lnt], [b_rc], scale=-0.5)
                    for qi in range(nq):
                        k.stt(ytok[:, qi, h * 64:(h + 1) * 64], dd[:, qi, :], rc[:, 3, qi:qi + 1], gsub[:], ALU.mult, ALU.mult,
                              [b_dd, b_rc, b_gsub], [b_ytok])
                    if h == 3:
                        k.rrset = [0, 1, 2, 3]
                        transpose_out(ytok, b_ytok, nq, c0, ydst, b_ydst)
                        k.rrset = list(range(8))

                heads = [(j // 3, 32 * (j % 3), 32, j // 2) for j in range(8)]
                attention(s3, qT, b_q, kT, b_k, V, b_V, heads, 32 ** -0.5, epi, skip_ctx=(l == DEPTH - 1))
                P.barrier()
        dump("ya", ydst[:], b_ydst, [128, 2, NT], BF16)

    def mla_mixer(l):
        ydst, b_ydst = yT[2]
        with ExitStack() as st:
            qT, b_q = k.sb(st, [128, 2, NT], BF16, "mlq")
            kT, b_k = k.sb(st, [128, 2, NT], BF16, "mlk")
            V, b_V = k.sb(st, [128, NTT, 4, 65], BF16, "mlv")
            gcol, b_g = k.sb(st, [128, 2], F32, "mlg")
            gc, b_gc = k.sb(st, [128, 3], F32, "mlgc")
            k.load("sp", gcol[:], mla_g_d[l], [b_g])
            k.load("sp", gc[:], mla_gc_d[l], [b_gc])
            k.memset("pool", V[:, :, :, 64:65], 1.0, [b_V])
            with ExitStack() as s2:
                wc, b_wc = load_w("pool", s2, w_mla_c[l], 384, "wc")
                wkr, b_wkr = load_w("pool", s2, w_mla_kr[l], 128, "wkr")
                wf, b_wf = k.sb(s2, [128, 4, 256], F32, "wf")
                wuq, b_wuq = k.sb(s2, [128, 2, 256], BF16, "wuq")
                wkk, b_wkk = k.sb(s2, [128, 256], BF16, "wkk")
                wvv, b_wvv = k.sb(s2, [128, 256], BF16, "wvv")
                k.load("sp", wf[:, 0:2, :], w_uq_d[l], [b_wf])
                k.load("sp", wf[:, 2, :], w_ukvk_d[l], [b_wf])
                k.load("sp", wf[:, 3, :], w_ukvv_d[l], [b_wf])
                for kt in range(2):
                    k.ts("dve", wuq[:, kt, :], wf[:, kt, :], gc[:, kt:kt + 1], None, ALU.mult, None, [b_wf, b_gc], [b_wuq])
                k.ts("dve", wkk[:], wf[:, 2, :], gc[:, 2:3], None, ALU.mult, None, [b_wf, b_gc], [b_wkk])
                k.ts("dve", wvv[:], wf[:, 3, :], gc[:, 2:3], None, ALU.mult, None, [b_wf, b_gc], [b_wvv])
                hgs = [k.sb(s2, [128, 8, 512], BF16, "hg") for _ in range(2)]
                tmp, tmpb = k.sb(s2, [128, 512], F32, "htmp")
                scr = norm_scratch(s2)
                sq2, b_sq2 = k.sb(s2, [128, 2, 512], BF16, "sq2")
                rsq, b_rsq = k.sb(s2, [128, 512], F32, "rsq")
                lnq, b_lnq = k.sb(s2, [128, 512], F32, "lnq")
                cqn, b_cqn = k.sb(s2, [128, 2, 512], BF16, "cqn")
                ckvn, b_ckvn = k.sb(s2, [128, 512], BF16, "ckvn")
                for g, (c0, n) in enumerate(GROUPS):
                    hg, hb = hgs[g % 2]
                    h_group(l, 0, g, hg, hb, tmp, tmpb)
                    bA = k.bank(); proj(bA, wc, b_wc, 0, hg, hb, n)
                    bB = k.bank(); proj(bB, wc, b_wc, 128, hg, hb, n)
                    k.act(sq2[:, 0, :n], k.ps[bA][:, :n], AF.Square, [k.pb[bA]], [b_sq2])
                    k.act(sq2[:, 1, :n], k.ps[bB][:, :n], AF.Square, [k.pb[bB]], [b_sq2])
                    bS = k.bank()
                    k.mm(bS, k.ps[bS][:, :n], onesb[:], sq2[:, 0, :n], True, False, [b_sq2, b_ones])
                    k.mm(bS, k.ps[bS][:, :n], onesb[:], sq2[:, 1, :n], False, True, [b_sq2, b_ones])
                    k.rstd_from_ss(rsq[:, :n], k.ps[bS][:, :n], bS, 1.0 / 192, lnq[:, :n], b_lnq, [b_rsq])
                    k.tt("dve", cqn[:, 0, :n], k.ps[bA][:, :n], rsq[:, :n], ALU.mult, [k.pb[bA], b_rsq], [b_cqn])
                    k.tt("dve", cqn[:, 1, :n], k.ps[bB][:, :n], rsq[:, :n], ALU.mult, [k.pb[bB], b_rsq], [b_cqn])
                    bC = k.bank(); proj(bC, wc, b_wc, 256, hg, hb, n)
                    k.act(sq2[:, 0, :n], k.ps[bC][:, :n], AF.Square, [k.pb[bC]], [b_sq2])
                    bS = k.bank()
                    k.mm(bS, k.ps[bS][:, :n], onesb[:], sq2[:, 0, :n], True, True, [b_sq2, b_ones])
                    k.rstd_from_ss(rsq[:, :n], k.ps[bS][:, :n], bS, 1.0 / 128, lnq[:, :n], b_lnq, [b_rsq])
                    k.tt("dve", ckvn[:, :n], k.ps[bC][:, :n], rsq[:, :n], ALU.mult, [k.pb[bC], b_rsq], [b_ckvn])
                    for tile in range(2):
                        bk = k.bank()
                        k.mm(bk, k.ps[bk][:, :n], wuq[:, 0, tile * 128:(tile + 1) * 128], cqn[:, 0, :n], True, False, [b_wuq, b_cqn])
                        k.mm(bk, k.ps[bk][:, :n], wuq[0:64, 1, tile * 128:(tile + 1) * 128], cqn[0:64, 1, :n], False, True, [b_wuq, b_cqn])
                        headnorm_rope(s2, bk, n, c0, bo64, 1.0 / 48, gcol[:, 0:1], b_g, Rmla, ml_cos_d, ml_sin_d,
                                      qT[:, tile, c0:c0 + n], b_q, scr)
                    for tile in range(2):
                        bk = k.bank()
                        k.mm(bk, k.ps[bk][:, :n], wkk[:, tile * 128:(tile + 1) * 128], ckvn[:, :n], True, False, [b_wkk, b_ckvn])
                        for kt in range(8):
                            k.mm(bk, k.ps[bk][:, :n], wkr[:, kt, :], hg[:, kt, :n], False, kt == 7, [b_wkr, hb])
                        headnorm_rope(s2, bk, n, c0, bo64, 1.0 / 48, gcol[:, 1:2], b_g, Rmla, ml_cos_d, ml_sin_d,
                                      kT[:, tile, c0:c0 + n], b_k, scr)
                    for qi in range(n // 128):
                        tt_ = (c0 + qi * 128) // 128
                        bk = k.bank()
                        k.mm(bk, k.ps[bk][:, 0:256], ckvn[:, qi * 128:(qi + 1) * 128], wvv[:], True, True, [b_ckvn, b_wvv])
                        k.copy("act", V[:, tt_, :, 0:64], k.ps[bk][:, 0:256].rearrange("p (h d) -> p h d", h=4), [k.pb[bk]], [b_V])
                P.barrier()
            dump("ml_q", qT[:], b_q, [128, 2, NT], BF16)
            dump("ml_k", kT[:], b_k, [128, 2, NT], BF16)
            with ExitStack() as s3:
                ytok, b_ytok = k.sb(s3, [128, 4, 256], BF16, "ytok")
                rc, b_rc = k.sb(s3, [128, 4], F32, "rc")

                def epi(g, c0, nq, h, ob):
                    O = k.ps[ob][:, 0:nq * 65].rearrange("p (q c) -> p q c", c=65)
                    P.op("dve", lambda e: e.reciprocal(out=rc[:, 0:nq], in_=O[:, :, 64]), reads=[k.pb[ob]], writes=[b_rc])
                    for qi in range(nq):
                        k.ts("dve", ytok[:, qi, h * 64:(h + 1) * 64], O[:, qi, 0:64], rc[:, qi:qi + 1], None, ALU.mult, None,
                             [k.pb[ob], b_rc], [b_ytok])
                    if h == 3:
                        k.rrset = [0, 1, 2, 3]
                        transpose_out(ytok, b_ytok, nq, c0, ydst, b_ydst)
                        k.rrset = list(range(8))

                heads = [(h // 2, 64 * (h % 2), 48, h) for h in range(4)]
                attention(s3, qT, b_q, kT, b_k, V, b_V, heads, 48 ** -0.5, epi, skip_ctx=(l == DEPTH - 1))
                P.barrier()
        dump("yc", ydst[:], b_ydst, [128, 2, NT], BF16)

    def merge(l):
        with ExitStack() as st:
            wbr, b_wbr = k.sb(st, [128, 2, 4, D], BF16, "wbr")
            wout, b_wout = k.sb(st, [128, 8, D], BF16, "wout")
            wgs = [k.sb(st, [128, 8, 4, 128], BF16, "wg") for _ in range(2)]
            hgs = [k.sb(st, [128, 8, 512], BF16, "hg") for _ in range(2)]
            tmp, tmpb = k.sb(st, [128, 512], F32, "htmp")
            sig = [k.sb(st, [128, 512], F32, "sig") for _ in range(2)]
            prod = [k.sb(st, [128, 512], F32, "prod") for _ in range(2)]
            macc, b_macc = k.sb(st, [128, 512], F32, "macc")
            mbf, b_mbf = k.sb(st, [128, 8, 512], BF16, "mbf")
            wi = 0; si = 0
            for g, (c0, n) in enumerate(GROUPS):
                if l == DEPTH - 1 and g == 0:
                    continue
                j = jof(g)
                hg, hb = hgs[g % 2]
                h_group(l, 0, g, hg, hb, tmp, tmpb)
                for ct in range(8):
                    wg, b_wg = wgs[wi % 2]; wi += 1
                    k.load("pool", wg[:].rearrange("p a b c -> p (a b c)"), w_gates_d[l, ct], [b_wg])
                    if wi == 1:
                        k.load("pool", wbr[:].rearrange("p a b c -> p (a b c)"), w_branch_d[l], [b_wbr])
                    if wi == 3:
                        k.load("pool", wout[:], w_out_d[l].rearrange("(kt p) n -> p kt n", p=128), [b_wout])
                    for i in range(4):
                        bg = k.bank()
                        for kt in range(8):
                            k.mm(bg, k.ps[bg][:, :n], wg[:, kt, i, :], hg[:, kt, :n], kt == 0, kt == 7, [b_wg, hb])
                        sg, b_sg = sig[si % 2]; pr, b_pr = prod[si % 2]; si += 1
                        k.act(sg[:, :n], k.ps[bg][:, :n], AF.Sigmoid, [k.pb[bg]], [b_sg])
                        bp = k.bank()
                        for kt2 in range(2):
                            k.mm(bp, k.ps[bp][:, :n], wbr[:, kt2, i, ct * 128:(ct + 1) * 128], yT[i][0][:, kt2, c0:c0 + n],
                                 kt2 == 0, kt2 == 1, [b_wbr, yT[i][1]])
                        if i == 0:
                            k.tt("dve", macc[:, :n], k.ps[bp][:, :n], sg[:, :n], ALU.mult, [k.pb[bp], b_sg], [b_macc])
                        else:
                            k.tt("dve", pr[:, :n], k.ps[bp][:, :n], sg[:, :n], ALU.mult, [k.pb[bp], b_sg], [b_pr])
                            if i < 3:
                                k.tt("dve", macc[:, :n], macc[:, :n], pr[:, :n], ALU.add, [b_macc, b_pr], [b_macc])
                            else:
                                k.tt("dve", mbf[:, ct, :n], macc[:, :n], pr[:, :n], ALU.add, [b_macc, b_pr], [b_mbf])
                for co in range(8):
                    bo = k.bank()
                    for ct in range(8):
                        k.mm(bo, k.ps[bo][:, :n], wout[:, ct, co * 128:(co + 1) * 128], mbf[:, ct, :n], ct == 0, ct == 7, [b_wout, b_mbf])
                    k.stt(xT[:, co, c0:c0 + n], k.ps[bo][:, :n], MOD[:, l, 2, co, j:j + 1], xT[:, co, c0:c0 + n], ALU.mult, ALU.add,
                          [k.pb[bo], b_MOD, b_xT], [b_xT])
            P.barrier()
        dump("xmix", xT[:], b_xT, [128, 8, NT])

    def moe(l):
        fT = yall[:].rearrange("p i k t -> p (i k) t")
        b_fT = Buf("fT")
        with ExitStack() as st:
            rms_stats(st)
            P.barrier()
        with ExitStack() as st:
            combT, b_comb = k.sb(st, [16, NT], BF16, "combT")
            selb, b_sel = k.sb(st, [16, 16, 128], BF16, "selb")
            k.load("pool", selb[:], sel_d, [b_sel])
            wgs = [k.sb(st, [128, 8, 512], BF16, "ewg") for _ in range(2)]
            wus = [k.sb(st, [128, 8, 512], BF16, "ewu") for _ in range(2)]
            wds = [k.sb(st, [128, 4, D], BF16, "ewd") for _ in range(2)]

            def load_expert(e_):
                wg, b_wg = wgs[e_ % 2]; wu, b_wu = wus[e_ % 2]; wd, b_wd = wds[e_ % 2]
                k.load("pool", wg[:], exp_g_d[l, e_].rearrange("(kt p) n -> p kt n", p=128), [b_wg])
                k.load("pool", wu[:], exp_u_d[l, e_].rearrange("(kt p) n -> p kt n", p=128), [b_wu])
                k.load("pool", wd[:], exp_d_d[l, e_].rearrange("(kt p) n -> p kt n", p=128), [b_wd])
            load_expert(0)
            with ExitStack() as s2:
                tmp, tmpb = k.sb(s2, [128, 512], F32, "htmp")
                f32s = [k.sb(s2, [128, 512], F32, "f32") for _ in range(2)]
                rw, b_rw = k.sb(s2, [128, 8, 16], F32, "rw")
                k.load("sp", rw[:], router_w_d.rearrange("(kt p) n -> p kt n", p=128), [b_rw])
                rb, b_rb = k.sb(s2, [128, 16], F32, "rb")
                k.load("sp", rb[:], router_b_d, [b_rb])
                lg, b_lg = k.sb(s2, [128, NTT, 16], F32, "lg")
                fi_ = 0
                skip0 = (l == DEPTH - 1)
                if skip0:
                    k.memset("dve", lg[:, 0:2, :], 0.0, [b_lg])
                for g in range(5):
                    if skip0 and g == 0:
                        continue
                    c0, n = GROUPS[g]
                    j = jof(g)
                    nq = n // 128
                    bl = k.bank()
                    for kt in range(8):
                        k.tt("dve", tmp[:, :n], xT[:, kt, c0:c0 + n], rstd_b[:, c0:c0 + n], ALU.mult, [b_xT, b_rstd], [tmpb])
                        P.op("act", lambda e: e.activation(out=fT[:, kt, c0:c0 + n], in_=tmp[:, :n], func=AF.Identity,
                                                           scale=AB[:, l, 1, 0, kt, j:j + 1], bias=AB[:, l, 1, 1, kt, j:j + 1]),
                             reads=[tmpb, b_AB], writes=[b_fT])
                        f32, b_f32 = f32s[fi_ % 2]; fi_ += 1
                        k.ts("pool", f32[:, :n], tmp[:, :n], AB[:, l, 1, 0, kt, j:j + 1], AB[:, l, 1, 1, kt, j:j + 1], ALU.mult, ALU.add,
                             [tmpb, b_AB], [b_f32])
                        for qi in range(nq):
                            P.op("pe", lambda e: e.matmul(k.ps[bl][:, qi * 16:(qi + 1) * 16], lhsT=f32[:, qi * 128:(qi + 1) * 128], rhs=rw[:, kt, :],
                                                          start=(kt == 0 and qi == 0), stop=(kt == 7), skip_group_check=True),
                                 reads=[b_f32, b_rw], writes=[k.pb[bl]], inc=(qi == nq - 1))
                    tt0 = c0 // 128
                    k.copy("dve", lg[:, tt0:tt0 + nq, :], k.ps[bl][:, 0:nq * 16].rearrange("p (q e) -> p q e", e=16), [k.pb[bl]], [b_lg])
                r = {}
                NR = NTT * 16
                for nm, wd_ in (("sc", NR), ("bi", NR), ("m1", NR // 4), ("eq", NR), ("bi2", NR), ("m2", NR // 4),
                                ("gs", NR // 4), ("gm", NTT), ("gsel", NR // 4), ("sel", NR), ("w", NR), ("ws", NTT), ("cmb", NR)):
                    r[nm] = k.sb(s2, [128, wd_], F32, "r_" + nm)
                v4 = lambda ap: ap.rearrange("p (g e) -> p g e", e=4)
                b4 = lambda ap: ap.unsqueeze(2).to_broadcast([128, NR // 4, 4])
                sc, b_sc = r["sc"]; bi, b_bi = r["bi"]; m1, b_m1 = r["m1"]; eq, b_eq = r["eq"]; bi2, b_bi2 = r["bi2"]
                m2, b_m2 = r["m2"]; gs, b_gs = r["gs"]; gm_, b_gm_ = r["gm"]; gsel, b_gsel = r["gsel"]; sel, b_sl = r["sel"]
                w_, b_w = r["w"]; ws, b_ws = r["ws"]; cmb, b_cmb = r["cmb"]
                k.act(sc[:], lg[:].rearrange("p t e -> p (t e)"), AF.Sigmoid, [b_lg], [b_sc])
                k.tt("dve", sc[:].rearrange("p (t e) -> p t e", e=16) if False else bi[:].rearrange("p (t e) -> p t e", e=16),
                     sc[:].rearrange("p (t e) -> p t e", e=16), rb[:].unsqueeze(1).to_broadcast([128, NTT, 16]), ALU.add, [b_sc, b_rb], [b_bi])
                P.op("dve", lambda e: e.tensor_reduce(out=m1[:], in_=v4(bi[:]), axis=AX.X, op=ALU.max), reads=[b_bi], writes=[b_m1])
                k.tt("dve", v4(eq[:]), v4(bi[:]), b4(m1[:]), ALU.is_equal, [b_bi, b_m1], [b_eq])
                k.stt(bi2[:], eq[:], -1e9, bi[:], ALU.mult, ALU.add, [b_eq, b_bi], [b_bi2])
                P.op("dve", lambda e: e.tensor_reduce(out=m2[:], in_=v4(bi2[:]), axis=AX.X, op=ALU.max), reads=[b_bi2], writes=[b_m2])
                k.tt("dve", gs[:], m1[:], m2[:], ALU.add, [b_m1, b_m2], [b_gs])
                P.op("dve", lambda e: e.tensor_reduce(out=gm_[:], in_=v4(gs[:]), axis=AX.X, op=ALU.max), reads=[b_gs], writes=[b_gm_])
                k.tt("dve", v4(gsel[:]), v4(gs[:]), gm_[:].unsqueeze(2).to_broadcast([128, NTT, 4]), ALU.is_equal, [b_gs, b_gm_], [b_gsel])
                k.tt("dve", v4(sel[:]), v4(bi[:]), b4(m2[:]), ALU.is_ge, [b_bi, b_m2], [b_sl])
                k.tt("dve", v4(sel[:]), v4(sel[:]), b4(gsel[:]), ALU.mult, [b_sl, b_gsel], [b_sl])
                k.tt("dve", w_[:], sc[:], sel[:], ALU.mult, [b_sc, b_sl], [b_w])
                P.op("dve", lambda e: e.tensor_reduce(out=ws[:], in_=w_[:].rearrange("p (t e) -> p t e", e=16), axis=AX.X, op=ALU.add),
                     reads=[b_w], writes=[b_ws])
                P.op("dve", lambda e: e.reciprocal(out=ws[:], in_=ws[:]), reads=[b_ws], writes=[b_ws])
                k.tt("dve", cmb[:].rearrange("p (t e) -> p t e", e=16), w_[:].rearrange("p (t e) -> p t e", e=16),
                     ws[:].unsqueeze(2).to_broadcast([128, NTT, 16]), ALU.mult, [b_w, b_ws], [b_cmb])
                for tq in range(0, NTT, 4):
                    nn = min(4, NTT - tq)
                    bt = k.bank()
                    for i_ in range(nn):
                        P.op("pe", lambda e: e.transpose(k.ps[bt][0:16, i_ * 128:(i_ + 1) * 128], cmb[:, (tq + i_) * 16:(tq + i_ + 1) * 16], identf),
                             reads=[b_cmb, b_cstf], writes=[k.pb[bt]], inc=(i_ == nn - 1))
                    k.copy("act", combT[:, tq * 128:(tq + nn) * 128], k.ps[bt][0:16, 0:nn * 128], [k.pb[bt]], [b_comb])
                P.barrier()
            dump("combT", combT[:], b_comb, [16, NT], BF16)
            cbs = [k.sb(st, [128, 512], BF16, "cbs") for _ in range(2)]
            sl = [k.sb(st, [128, 512], F32, "esl") for _ in range(2)]
            tl = [k.sb(st, [128, 512], F32, "etl") for _ in range(2)]
            aa = [k.sb(st, [128, 4, 512], BF16, "eaa") for _ in range(2)]
            ci = 0; fi = 0
            for e_ in range(16):
                wg, b_wg = wgs[e_ % 2]; wu, b_wu = wus[e_ % 2]; wd, b_wd = wds[e_ % 2]
                if e_ > 0:
                    load_expert(e_)
                for g, (c0, n) in enumerate(GROUPS):
                    if l == DEPTH - 1 and g == 0:
                        continue
                    j = jof(g)
                    cb, b_cb = cbs[ci % 2]; a_, b_a = aa[ci % 2]; ci += 1
                    bc = k.bank()
                    k.mm(bc, k.ps[bc][:, :n], selb[:, e_, :], combT[:, c0:c0 + n], True, True, [b_sel, b_comb])
                    k.copy("act", cb[:, :n], k.ps[bc][:, :n], [k.pb[bc]], [b_cb])
                    for fj in range(4):
                        bg = k.bank()
                        for kt in range(8):
                            k.mm(bg, k.ps[bg][:, :n], wg[:, kt, fj * 128:(fj + 1) * 128], fT[:, kt, c0:c0 + n], kt == 0, kt == 7, [b_wg, b_fT])
                        bu = k.bank()
                        for kt in range(8):
                            k.mm(bu, k.ps[bu][:, :n], wu[:, kt, fj * 128:(fj + 1) * 128], fT[:, kt, c0:c0 + n], kt == 0, kt == 7, [b_wu, b_fT])
                        s_, b_s = sl[fi % 2]; t_, b_t = tl[fi % 2]; fi += 1
                        k.act(s_[:, :n], k.ps[bg][:, :n], AF.Silu, [k.pb[bg]], [b_s])
                        k.tt("dve", t_[:, :n], k.ps[bu][:, :n], s_[:, :n], ALU.mult, [k.pb[bu], b_s], [b_t])
                        k.tt("dve", a_[:, fj, :n], t_[:, :n], cb[:, :n], ALU.mult, [b_t, b_cb], [b_a])
                    for co in range(8):
                        bo = k.bank()
                        for fj in range(4):
                            k.mm(bo, k.ps[bo][:, :n], wd[:, fj, co * 128:(co + 1) * 128], a_[:, fj, :n], fj == 0, fj == 3, [b_wd, b_a])
                        k.stt(xT[:, co, c0:c0 + n], k.ps[bo][:, :n], MOD[:, l, 5, co, j:j + 1], xT[:, co, c0:c0 + n], ALU.mult, ALU.add,
                              [k.pb[bo], b_MOD, b_xT], [b_xT])
            P.barrier()
        dump("xout", xT[:], b_xT, [128, 8, NT])

    TWO_PI = 2.0 * math.pi

    def sincos(ang, b_ang, N, sc4, sin_out, cos_out, b_out):
        (kf, b_kf), (r_, b_r), (mk, b_mk), (sh, b_sh) = sc4
        ki = kf[:, :N].bitcast(I32)
        k.ts("dve", r_[:, :N], ang, 1.0 / TWO_PI, None, ALU.mult, None, [b_ang], [b_r])
        k.copy("dve", ki, r_[:, :N], [b_r], [b_kf])
        k.copy("dve", mk[:, :N], ki, [b_kf], [b_mk])
        k.stt(r_[:, :N], mk[:, :N], -TWO_PI, ang, ALU.mult, ALU.add, [b_mk, b_ang], [b_r])
        k.ts("dve", mk[:, :N], r_[:, :N], math.pi, -TWO_PI, ALU.is_gt, ALU.mult, [b_r], [b_mk])
        k.tt("dve", r_[:, :N], r_[:, :N], mk[:, :N], ALU.add, [b_r, b_mk], [b_r])
        k.ts("dve", mk[:, :N], r_[:, :N], -math.pi, TWO_PI, ALU.is_lt, ALU.mult, [b_r], [b_mk])
        k.tt("dve", r_[:, :N], r_[:, :N], mk[:, :N], ALU.add, [b_r, b_mk], [b_r])
        k.ts("dve", r_[:, :N], r_[:, :N], math.pi, -math.pi, ALU.min, ALU.max, [b_r], [b_r])
        k.act(sin_out, r_[:, :N], AF.Sin, [b_r], [b_out])
        k.act(sh[:, :N], r_[:, :N], AF.Sin, [b_r], [b_sh], scale=0.5)
        k.tt("dve", sh[:, :N], sh[:, :N], sh[:, :N], ALU.mult, [b_sh], [b_sh])
        k.ts("dve", cos_out, sh[:, :N], -2.0, 1.0, ALU.mult, ALU.add, [b_sh], [b_out])

    def s5_mixer(l):
        ydst, b_ydst = yT[1]
        with ExitStack() as st:
            uT, b_u = k.sb(st, [128, 2, NT], BF16, "s5u")
            yacc, b_yacc = k.sb(st, [128, 2, NT], F32, "s5y")
            dcol, b_dcol = k.sb(st, [128, 2], F32, "s5d")
            k.load("sp", dcol[:], s5d_d[l], [b_dcol])
            with ExitStack() as s2:
                ws, b_ws = load_w("pool", s2, w_s5_d[l], 256, "ws5")
                hgs = [k.sb(s2, [128, 8, 512], BF16, "hg") for _ in range(2)]
                tmp, tmpb = k.sb(s2, [128, 512], F32, "htmp")
                for g, (c0, n) in enumerate(GROUPS):
                    hg, hb = hgs[g % 2]
                    h_group(l, 0, g, hg, hb, tmp, tmpb)
                    for ti in range(2):
                        bk = k.bank()
                        proj(bk, ws, b_ws, ti * 128, hg, hb, n)
                        k.copy("act", uT[:, ti, c0:c0 + n], k.ps[bk][:, :n], [k.pb[bk]], [b_u])
                P.barrier()
            for dr in range(2):
                with ExitStack() as sd:
                    pr, b_pr = k.sb(sd, [128, 1024], F32, "pr"); pi_, b_pi = k.sb(sd, [128, 1024], F32, "pi")
                    qr, b_qr = k.sb(sd, [128, 1024], F32, "qr"); qi, b_qi = k.sb(sd, [128, 1024], F32, "qi")
                    Bb, b_Bb = k.sb(sd, [128, 2, 1024], BF16, "Bb")
                    Cb, b_Cb = k.sb(sd, [128, 3, 8, 128], BF16, "Cb")
                    triT, b_tri = k.sb(sd, [128, 2, 128], BF16, "triT")
                    trif, b_trif = k.sb(sd, [128, 128], F32, "trif")
                    posc, b_posc = k.sb(sd, [128, 2], F32, "posc")
                    posr, b_posr = k.sb(sd, [128, 128], F32, "posr")
                    fm, b_fm = k.sb(sd, [128, 3, 8], F32, "fm")
                    cc, b_cc = k.sb(sd, [128, 16, 8], F32, "cc")
                    A128, b_A128 = k.sb(sd, [128, 2, 8], F32, "A128")
                    k.load("pool", Bb[:], s5B_d[l, dr].rearrange("kt p n -> p kt n"), [b_Bb])
                    with ExitStack() as sc_:
                        Cf, b_Cf = k.sb(sc_, [128, 2, 8, 128], F32, "Cf")
                        k.load("sp", Cf[:, 0], s5C_d[l, dr, 0], [b_Cf]); k.load("sp", Cf[:, 1], s5C_d[l, dr, 1], [b_Cf])
                        k.copy("dve", Cb[:, 0], Cf[:, 0], [b_Cf], [b_Cb])
                        k.ts("dve", Cb[:, 1], Cf[:, 0], -1.0, None, ALU.mult, None, [b_Cf], [b_Cb])
                        k.ts("dve", Cb[:, 2], Cf[:, 1], -1.0, None, ALU.mult, None, [b_Cf], [b_Cb])
                        P.barrier()
                    k.load("sp", trif[:], tri_d[dr], [b_trif])
                    k.copy("dve", triT[:, 0, :], trif[:], [b_trif], [b_tri])
                    k.ts("dve", triT[:, 1, :], trif[:], -1.0, None, ALU.mult, None, [b_trif], [b_tri])
                    k.load("sp", posc[:], posc_d[dr], [b_posc]); k.load("sp", posr[:], posr_d[dr], [b_posr])
                    k.load("sp", fm[:], s5fm_d[l, dr], [b_fm])
                    with ExitStack() as sx:
                        sc4 = [k.sb(sx, [128, 1024], F32, "sc4") for _ in range(4)]
                        rho, b_rho = k.sb(sx, [128, 1024], F32, "rho"); th, b_th = k.sb(sx, [128, 1024], F32, "th")
                        ang, b_angb = k.sb(sx, [128, 1024], F32, "ang")
                        k.load("sp", rho[:], s5tm_d[l, dr, 0], [b_rho]); k.load("sp", th[:], s5tm_d[l, dr, 1], [b_th])
                        k.load("sp", ang[:], s5tm_d[l, dr, 2], [b_angb])
                        k.act(ang[:], ang[:], AF.Exp, [b_angb], [b_angb])
                        k.tt("dve", rho[:], rho[:], ang[:], ALU.mult, [b_rho, b_angb], [b_rho])
                        k.tt("dve", th[:], th[:], ang[:], ALU.mult, [b_th, b_angb], [b_th])
                        k.ts("dve", ang[:], th[:], posc[:, 0:1], None, ALU.mult, None, [b_th, b_posc], [b_angb])
                        sincos(ang[:], b_angb, 1024, sc4, pi_[:], pr[:], b_pr)
                        b_pi.w = b_pr.w
                        P.op("act", lambda e: e.activation(out=ang[:], in_=rho[:], func=AF.Exp, scale=posc[:, 0:1]), reads=[b_rho, b_posc], writes=[b_angb])
                        k.tt("dve", pr[:], pr[:], ang[:], ALU.mult, [b_pr, b_angb], [b_pr])
                        k.tt("dve", pi_[:], pi_[:], ang[:], ALU.mult, [b_pr, b_angb], [b_pr])
                        dtc = cc[:, 0, :]; rc_ = cc[:, 1, :]; tc_ = cc[:, 2, :]
                        k.act(dtc, fm[:, 2, :], AF.Exp, [b_fm], [b_cc])
                        k.tt("dve", rc_, fm[:, 0, :], dtc, ALU.mult, [b_fm, b_cc], [b_cc])
                        k.tt("dve", tc_, fm[:, 1, :], dtc, ALU.mult, [b_fm, b_cc], [b_cc])
                        k.copy("dve", ang[:, 0:8], tc_, [b_cc], [b_angb])
                        k.ts("dve", ang[:, 8:16], tc_, 128.0, None, ALU.mult, None, [b_cc], [b_angb])
                        sincos(ang[:, 0:16], b_angb, 16, sc4, th[:, 0:16], th[:, 16:32], b_th)
                        er = cc[:, 3, :]; e128 = cc[:, 4, :]
                        k.act(er, rc_, AF.Exp, [b_cc], [b_cc])
                        k.act(e128, rc_, AF.Exp, [b_cc], [b_cc], scale=128.0)
                        k.tt("dve", A128[:, 0, :], e128, th[:, 24:32], ALU.mult, [b_cc, b_th], [b_A128])
                        k.tt("dve", A128[:, 1, :], e128, th[:, 8:16], ALU.mult, [b_cc, b_th], [b_A128])
                        ar1 = cc[:, 5, :]; ai = cc[:, 6, :]; den = cc[:, 7, :]; cr = cc[:, 8, :]; ci = cc[:, 9, :]; t1 = cc[:, 10, :]
                        k.tt("dve", ar1, er, th[:, 16:24], ALU.mult, [b_cc, b_th], [b_cc])
                        k.ts("dve", ar1, ar1, -1.0, None, ALU.add, None, [b_cc], [b_cc])
                        k.tt("dve", ai, er, th[:, 0:8], ALU.mult, [b_cc, b_th], [b_cc])
                        k.tt("dve", den, fm[:, 0, :], fm[:, 0, :], ALU.mult, [b_fm], [b_cc])
                        k.tt("dve", t1, fm[:, 1, :], fm[:, 1, :], ALU.mult, [b_fm], [b_cc])
                        k.tt("dve", den, den, t1, ALU.add, [b_cc], [b_cc])
                        P.op("dve", lambda e: e.reciprocal(out=den, in_=den), reads=[b_cc], writes=[b_cc])
                        k.tt("dve", cr, ar1, fm[:, 0, :], ALU.mult, [b_cc, b_fm], [b_cc])
                        k.tt("dve", t1, ai, fm[:, 1, :], ALU.mult, [b_cc, b_fm], [b_cc])
                        k.tt("dve", cr, cr, t1, ALU.add, [b_cc], [b_cc])
                        k.tt("dve", cr, cr, den, ALU.mult, [b_cc], [b_cc])
                        k.tt("dve", ci, ai, fm[:, 0, :], ALU.mult, [b_cc, b_fm], [b_cc])
                        k.tt("dve", t1, ar1, fm[:, 1, :], ALU.mult, [b_cc, b_fm], [b_cc])
                        k.tt("dve", ci, ci, t1, ALU.subtract, [b_cc], [b_cc])
                        k.tt("dve", ci, ci, den, ALU.mult, [b_cc], [b_cc])
                        for i in range(8):
                            k.ts("dve", ang[:, i * 128:(i + 1) * 128], posr[:], cc[:, 2, i:i + 1], None, ALU.mult, None, [b_posr, b_cc], [b_angb])
                            P.op("act", lambda e: e.activation(out=rho[:, i * 128:(i + 1) * 128], in_=posr[:], func=AF.Exp, scale=cc[:, 1, i:i + 1]),
                                 reads=[b_posr, b_cc], writes=[b_rho])
                        sincos(ang[:], b_angb, 1024, sc4, qi[:], qr[:], b_qr)
                        k.tt("dve", qr[:], qr[:], rho[:], ALU.mult, [b_qr, b_rho], [b_qr])
                        k.tt("dve", qi[:], qi[:], rho[:], ALU.mult, [b_qr, b_rho], [b_qr])
                        for i in range(8):
                            sl_ = slice(i * 128, (i + 1) * 128)
                            k.ts("dve", th[:, sl_], qi[:, sl_], cc[:, 9, i:i + 1], None, ALU.mult, None, [b_qr, b_cc], [b_th])
                            k.ts("dve", ang[:, sl_], qr[:, sl_], cc[:, 9, i:i + 1], None, ALU.mult, None, [b_qr, b_cc], [b_angb])
                            k.stt(qr[:, sl_], qr[:, sl_], cc[:, 8, i:i + 1], th[:, sl_], ALU.mult, ALU.subtract, [b_qr, b_cc, b_th], [b_qr])
                            k.stt(qi[:, sl_], qi[:, sl_], cc[:, 8, i:i + 1], ang[:, sl_], ALU.mult, ALU.add, [b_qr, b_cc, b_angb], [b_qr])
                        P.barrier()
                    if l == 0 and dr == 0:
                        dump("s5pr", pr[:], b_pr, [128, 1024]); dump("s5pi", pi_[:], b_pr, [128, 1024])
                        dump("s5qr", qr[:], b_qr, [128, 1024]); dump("s5qi", qi[:], b_qr, [128, 1024])
                        dump("s5A128", A128[:], b_A128, [128, 2, 8])
                    with ExitStack() as sp_:
                        zp, _ = k.sb(sp_, [128, 4, 1024], BF16, "zp")
                        hp, _ = k.sb(sp_, [128, 4, 1024], BF16, "hp")
                        Dg, _ = k.sb(sp_, [128, 16, 128], BF16, "Dg")
                        zl, _ = k.sb(sp_, [128, 16], F32, "zl")
                        car, _ = k.sb(sp_, [128, 16], F32, "car")
                        ct_, _ = k.sb(sp_, [128, 4, 8], F32, "ctmp")
                        hb2 = lambda nm: [Buf(nm + "0"), Buf(nm + "1")]
                        b_zp, b_hp, b_DgT, b_zl, b_car, b_ct = hb2("zp"), hb2("hp"), hb2("Dg"), hb2("zl"), hb2("car"), hb2("ct")
                        k.memset("pool", Dg[:], 0.0, b_DgT)
                        order = [0, 1] + list(range(2, NTT)) if dr == 0 else [1, 0] + list(range(NTT - 1, 1, -1))
                        tl = 127 if dr == 0 else 0

                        def half_gen(kt, banks):
                            bre, bim, zre, zim = banks
                            cs = slice(kt * 512, (kt + 1) * 512)
                            i4 = slice(kt * 4, kt * 4 + 4); i4m = slice(8 + kt * 4, 8 + kt * 4 + 4)
                            for tt in order:
                                cols = slice(tt * 128, (tt + 1) * 128)
                                k.mm(bre, k.ps[bre][:, :], uT[:, kt, cols], Bb[:, kt, 0:512], True, True, [b_u, b_Bb])
                                k.mm(bim, k.ps[bim][:, :], uT[:, kt, cols], Bb[:, kt, 512:1024], True, True, [b_u, b_Bb])
                                yield
                                k.tt("dve", zp[:, 0, cs], k.ps[bre][:, :], pr[:, cs], ALU.mult, [k.pb[bre], b_pr], [b_zp[kt]])
                                k.tt("dve", zp[:, 1, cs], k.ps[bim][:, :], pi_[:, cs], ALU.mult, [k.pb[bim], b_pr], [b_zp[kt]])
                                k.tt("dve", zp[:, 2, cs], k.ps[bim][:, :], pr[:, cs], ALU.mult, [k.pb[bim], b_pr], [b_zp[kt]])
                                k.tt("dve", zp[:, 3, cs], k.ps[bre][:, :], pi_[:, cs], ALU.mult, [k.pb[bre], b_pr], [b_zp[kt]])
                                yield
                                for ii in range(4):
                                    i = kt * 4 + ii
                                    sl_ = slice(i * 128, (i + 1) * 128)
                                    oc = slice(ii * 128, (ii + 1) * 128)
                                    k.mm(zre, k.ps[zre][:, oc], zp[:, 0, sl_], triT[:, 0, :], True, False, [b_zp[kt], b_tri])
                                    k.mm(zre, k.ps[zre][:, oc], zp[:, 1, sl_], triT[:, 1, :], False, False, [b_zp[kt], b_tri])
                                    k.mm(zre, k.ps[zre][:, oc], Dg[:, i, :], onesb[:], False, True, [b_DgT[kt], b_ones])
                                    k.mm(zim, k.ps[zim][:, oc], zp[:, 2, sl_], triT[:, 0, :], True, False, [b_zp[kt], b_tri])
                                    k.mm(zim, k.ps[zim][:, oc], zp[:, 3, sl_], triT[:, 0, :], False, False, [b_zp[kt], b_tri])
                                    k.mm(zim, k.ps[zim][:, oc], Dg[:, 8 + i, :], onesb[:], False, True, [b_DgT[kt], b_ones])
                                yield
                                k.copy("act", zl[:, i4], k.ps[zre][:, :].rearrange("p (i t) -> p i t", t=128)[:, :, tl], [k.pb[zre]], [b_zl[kt]])
                                k.copy("act", zl[:, i4m], k.ps[zim][:, :].rearrange("p (i t) -> p i t", t=128)[:, :, tl], [k.pb[zim]], [b_zl[kt]])
                                zr_, zi_ = zl[:, i4], zl[:, i4m]
                                k.tt("dve", ct_[:, 0, i4], A128[:, 0, i4], zr_, ALU.mult, [b_A128, b_zl[kt]], [b_ct[kt]])
                                k.tt("dve", ct_[:, 1, i4], A128[:, 1, i4], zi_, ALU.mult, [b_A128, b_zl[kt]], [b_ct[kt]])
                                k.tt("dve", ct_[:, 2, i4], A128[:, 0, i4], zi_, ALU.mult, [b_A128, b_zl[kt]], [b_ct[kt]])
                                k.tt("dve", ct_[:, 3, i4], A128[:, 1, i4], zr_, ALU.mult, [b_A128, b_zl[kt]], [b_ct[kt]])
                                k.tt("dve", car[:, i4], ct_[:, 0, i4], ct_[:, 1, i4], ALU.subtract, [b_ct[kt]], [b_car[kt]])
                                k.tt("dve", car[:, i4m], ct_[:, 2, i4], ct_[:, 3, i4], ALU.add, [b_ct[kt]], [b_car[kt]])
                                for isl in (i4, i4m):
                                    k.tt("pool", Dg[:, isl, :], identf.unsqueeze(1).to_broadcast([128, 4, 128]),
                                         car[:, isl].unsqueeze(2).to_broadcast([128, 4, 128]), ALU.mult, [b_cstf, b_car[kt]], [b_DgT[kt]])
                                yield
                                k.tt("dve", hp[:, 0, cs], k.ps[zre][:, :], qr[:, cs], ALU.mult, [k.pb[zre], b_qr], [b_hp[kt]])
                                k.tt("dve", hp[:, 1, cs], k.ps[zim][:, :], qi[:, cs], ALU.mult, [k.pb[zim], b_qr], [b_hp[kt]])
                                k.tt("dve", hp[:, 2, cs], k.ps[zim][:, :], qr[:, cs], ALU.mult, [k.pb[zim], b_qr], [b_hp[kt]])
                                k.tt("dve", hp[:, 3, cs], k.ps[zre][:, :], qi[:, cs], ALU.mult, [k.pb[zre], b_qr], [b_hp[kt]])
                                yield
                                bk = bre
                                first = True
                                for ii in range(4):
                                    i = kt * 4 + ii
                                    sl_ = slice(i * 128, (i + 1) * 128)
                                    for (pi_x, ci_x) in ((0, 0), (1, 1), (2, 2), (3, 2)):
                                        last = (ii == 3 and pi_x == 3)
                                        k.mm(bk, k.ps[bk][:, 0:128], Cb[:, ci_x, i, :], hp[:, pi_x, sl_], first, last, [b_Cb, b_hp[kt]])
                                        first = False
                                if dr == 0:
                                    k.stt(yacc[:, kt, cols], uT[:, kt, cols], dcol[:, kt:kt + 1], k.ps[bk][:, 0:128], ALU.mult, ALU.add,
                                          [b_u, b_dcol, k.pb[bk]], [b_yacc])
                                else:
                                    k.tt("dve", yacc[:, kt, cols], k.ps[bk][:, 0:128], yacc[:, kt, cols], ALU.add, [k.pb[bk], b_yacc], [b_yacc])
                                yield

                        live = [half_gen(0, [0, 1, 2, 3]), half_gen(1, [4, 5, 6, 7])]
                        for _ in range(S5SKEW):
                            next(live[0])
                        while live:
                            for g_ in list(live):
                                try:
                                    next(g_)
                                except StopIteration:
                                    live.remove(g_)
                        P.barrier()
            dump("s5yacc", yacc[:], b_yacc, [128, 2, NT])
            with ExitStack() as s4:
                wgf, b_wgf = k.sb(s4, [128, 2, 256], F32, "wgf"); wgb, b_wgb = k.sb(s4, [128, 2, 256], BF16, "wgb")
                bg, b_bg = k.sb(s4, [128, 2], F32, "bglu")
                k.load("sp", wgf[:], w_glu_d[l].rearrange("(kt p) n -> p kt n", p=128), [b_wgf])
                k.copy("dve", wgb[:], wgf[:], [b_wgf], [b_wgb])
                k.load("sp", bg[:], s5bg_d[l], [b_bg])
                zT, b_zT = k.sb(s4, [128, 2, 512], BF16, "zT")
                x2, b_x2 = k.sb(s4, [128, 512], F32, "x2"); thh, b_thh = k.sb(s4, [128, 512], F32, "thh")
                sg, b_sg = k.sb(s4, [128, 512], F32, "sg")
                for g, (c0, n) in enumerate(GROUPS):
                    for kt in range(2):
                        x_ = yacc[:, kt, c0:c0 + n]
                        k.tt("dve", x2[:, :n], x_, x_, ALU.mult, [b_yacc], [b_x2])
                        k.ts("dve", x2[:, :n], x2[:, :n], 0.044715, 1.0, ALU.mult, ALU.add, [b_x2], [b_x2])
                        k.tt("dve", x2[:, :n], x2[:, :n], x_, ALU.mult, [b_x2, b_yacc], [b_x2])
                        k.act(thh[:, :n], x2[:, :n], AF.Tanh, [b_x2], [b_thh], scale=math.sqrt(2.0 / math.pi))
                        k.stt(thh[:, :n], thh[:, :n], 1.0, x_, ALU.add, ALU.mult, [b_thh, b_yacc], [b_thh])
                        k.ts("dve", zT[:, kt, :n], thh[:, :n], 0.5, None, ALU.mult, None, [b_thh], [b_zT])
                    for ct in range(2):
                        bk = k.bank()
                        for kt in range(2):
                            k.mm(bk, k.ps[bk][:, :n], wgb[:, kt, ct * 128:(ct + 1) * 128], zT[:, kt, :n], kt == 0, kt == 1, [b_wgb, b_zT])
                        P.op("act", lambda e: e.activation(out=sg[:, :n], in_=k.ps[bk][:, :n], func=AF.Sigmoid, bias=bg[:, ct:ct + 1]),
                             reads=[k.pb[bk], b_bg], writes=[b_sg])
                        k.tt("dve", ydst[:, ct, c0:c0 + n], zT[:, ct, :n], sg[:, :n], ALU.mult, [b_zT, b_sg], [b_ydst])
                P.barrier()
        dump("yb", ydst[:], b_ydst, [128, 2, NT], BF16)

    def rwkv_mixer(l):
        ydst, b_ydst = yT[3]
        xd, b_xd = ydst, Buf("xd")
        with ExitStack() as st:
            rS, b_r = yall[:, 0], Buf("rw_r")
            kS, b_k = yall[:, 1], Buf("rw_k")
            vS, b_v = yall[:, 2], Buf("rw_v")
            kkS, b_kk = k.sb(st, [128, 2, NT], BF16, "rw_kk")
            cols, b_cols = k.sb(st, [128, 2, 9], F32, "rw_cols")
            k.load("sp", cols[:], rw_cols_d[l], [b_cols])
            C_KK, C_KA, C_LNG, C_LNB, C_RK, C_W0, C_A0 = 0, 1, 2, 3, 4, 5, 7
            for half in range(2):
              with ExitStack() as s2:
                WA, b_WA = load_w("pool", s2, w_rw_d[l, :, half * 512:(half + 1) * 512], 512, "rwWA")
                WB, b_WB = k.sb(s2, [128, 8, 512], BF16, "rwWB")
                with ExitStack() as s3:
                    mu, b_mu = k.sb(s3, [128, 512], F32, "rwmu")
                    k.load("sp", mu[:], rw_mu_d[l, :, half * 512:(half + 1) * 512], [b_mu])
                    for kt in range(8):
                        k.stt(WB[:, kt, :], WA[:, kt, :], 0.5, mu[:], ALU.mult, ALU.mult, [b_WA, b_mu], [b_WB])
                    k.ts("dve", mu[:], mu[:], -1.0, 1.0, ALU.mult, ALU.add, [b_mu], [b_mu])
                    for kt in range(8):
                        k.tt("pool", WA[:, kt, :], WA[:, kt, :], mu[:], ALU.mult, [b_WA, b_mu], [b_WA])
                    P.barrier()
                hgxs = [k.sb(s2, [128, 8, 516], BF16, "hgx") for _ in range(2)]
                hsxs = [k.sb(s2, [128, 8, 512], BF16, "hsx") for _ in range(2)]
                tmps = [k.sb(s2, [128, 514], F32, "htmp") for _ in range(2)]
                sq, b_sq = k.sb(s2, [128, 512], BF16, "rwsq"); kq, b_kq = k.sb(s2, [128, 512], F32, "rwkq")
                rs_, b_rs = k.sb(s2, [128, 512], F32, "rwrs"); lnt, b_lnt = k.sb(s2, [128, 512], F32, "rwlnt")
                for g, (c0, n) in enumerate(GROUPS if RW_SUB >= 2 else []):
                    j = jof(g)
                    hgx, b_hgx = hgxs[g % 2]; hsx, b_hsx = hsxs[g % 2]
                    s_lo, s_hi = (0, NCTX) if g == 0 else (NCTX, NT)
                    lo, hi = max(s_lo, c0 - 1), min(s_hi, c0 + n + 1)
                    o0 = lo - (c0 - 1)
                    if lo > c0 - 1:
                        k.memset("dve", hgx[:, :, 0:3], 0.0, [b_hgx])
                    if hi < c0 + n + 1:
                        k.memset("dve", hgx[:, :, n + 1:n + 3], 0.0, [b_hgx])
                    w_ = hi - lo
                    for kt in range(8):
                        tmp, tmpb = tmps[kt % 2]
                        k.tt("dve", tmp[:, :w_], xT[:, kt, lo:hi], rstd_b[:, lo:hi], ALU.mult, [b_xT, b_rstd], [tmpb])
                        P.op("act", lambda e: e.activation(out=hgx[:, kt, 1 + o0:1 + o0 + w_], in_=tmp[:, :w_], func=AF.Identity,
                                                           scale=AB[:, l, 0, 0, kt, j:j + 1], bias=AB[:, l, 0, 1, kt, j:j + 1]),
                             reads=[tmpb, b_AB], writes=[b_hgx])
                    for kt in range(8):
                        k.tt("pool", hsx[:, kt, :n], hgx[:, kt, 1:n + 1], hgx[:, kt, 3:n + 3], ALU.add, [b_hgx], [b_hsx])
                    for tl_ in range(4 if RW_SUB >= 3 else 0):
                        ti = half * 4 + tl_
                        bk = k.bank()
                        for kt in range(8):
                            k.mm(bk, k.ps[bk][:, :n], WA[:, kt, tl_ * 128:(tl_ + 1) * 128], hgx[:, kt, 2:n + 2], kt == 0, False, [b_WA, b_hgx])
                        for kt in range(8):
                            k.mm(bk, k.ps[bk][:, :n], WB[:, kt, tl_ * 128:(tl_ + 1) * 128], hsx[:, kt, :n], False, kt == 7, [b_WB, b_hsx])
                        dst, dstb = [(rS, b_r), (kS, b_k), (vS, b_v), (xd, b_xd)][ti // 2]
                        k.copy("act", dst[:, ti % 2, c0:c0 + n], k.ps[bk][:, :n], [k.pb[bk]], [dstb])
                        if ti // 2 == 1 and RW_SUB >= 4:
                            ci = ti % 2
                            k.ts("dve", kq[:, :n], k.ps[bk][:, :n], cols[:, ci, C_KK:C_KK + 1], None, ALU.mult, None, [k.pb[bk], b_cols], [b_kq])
                            k.act(sq[:, :n], kq[:, :n], AF.Square, [b_kq], [b_sq])
                            b2 = k.bank()
                            k.mm(b2, k.ps[b2][:, :n], bo64, sq[:, :n], True, True, [b_sq, b_cstb])
                            k.rstd_from_ss(rs_[:, :n], k.ps[b2][:, :n], b2, 1.0, lnt[:, :n], b_lnt, [b_rs])
                            k.tt("dve", kkS[:, ci, c0:c0 + n], kq[:, :n], rs_[:, :n], ALU.mult, [b_kq, b_rs], [b_kk])
                P.barrier()
            dump("rw_r", rS[:], b_r, [128, 2, NT], BF16); dump("rw_kk", kkS[:], b_kk, [128, 2, NT], BF16)
            dump("rw_xd", xd[:], b_xd, [128, 2, NT], BF16)
            yacc, _ = k.sb(st, [128, 2, NT], BF16, "rw_yacc")
            sw = {}
            def small(name, shape, src, dt=BF16, q="pool"):
                t, b = k.sb(st, shape, dt, name)
                k.load(q if dt == BF16 else "sp", t[:], src, [b])
                sw[name] = (t, b)
            for dr in range(2 if RW_LEVEL >= 1 else 0):
                small(f"w1_{dr}", [128, 2, 32], rw_w1_d[l, dr].rearrange("(kt p) n -> p kt n", p=128))
                small(f"w2_{dr}", [32, 256], rw_w2_d[l, dr])
                small(f"a1_{dr}", [128, 2, 32], rw_a1_d[l, dr].rearrange("(kt p) n -> p kt n", p=128))
                small(f"a2_{dr}", [32, 256], rw_a2_d[l, dr])
                small(f"w0r_{dr}", [1, 256], rw_w0row_d[l, dr], F32)
                small(f"tri_{dr}", [128, 2, 128], rw_tri_d[dr], F32)
                small(f"m1_{dr}", [128, 256], rw_m1_d[dr])
                small(f"mnt_{dr}", [128, 128], rw_mnt_d[dr])
            if RW_LEVEL >= 0:
                small("g1", [128, 2, 64], rw_g1_d[l].rearrange("(kt p) n -> p kt n", p=128))
                small("g2", [64, 256], rw_g2_d[l])
                small("ind", [128, 2], rw_ind_d, F32)
            ones1, b_ones1 = k.sb(st, [1, 128], F32, "ones1")
            k.memset("dve", ones1[:], 1.0, [b_ones1])

            def a_of(dr, xsrc, n, a_out, b_aout, x1t, b_x1t):
                a1, b_a1 = sw[f"a1_{dr}"]; a2, b_a2 = sw[f"a2_{dr}"]
                bk = k.bank()
                for kt in range(2):
                    k.mm(bk, k.ps[bk][0:32, :n], a1[:, kt, :], xsrc[:, kt, :], kt == 0, kt == 1, [b_a1, b_xd])
                k.copy("act", x1t[0:32, :n], k.ps[bk][0:32, :n], [k.pb[bk]], [b_x1t])
                for ci in range(2):
                    b2 = k.bank()
                    k.mm(b2, k.ps[b2][:, :n], a2[:, ci * 128:(ci + 1) * 128], x1t[0:32, :n], True, True, [b_a2, b_x1t])
                    P.op("act", lambda e: e.activation(out=a_out[:, ci, :n], in_=k.ps[b2][:, :n], func=AF.Sigmoid,
                                                       bias=cols[:, ci, C_A0 + dr:C_A0 + dr + 1]),
                         reads=[k.pb[b2], b_cols], writes=[b_aout])

            def kmod_of(a_in, b_ain, ksrc, n, out, b_out_, tmpf, b_tmpf):
                for ci in range(2):
                    k.ts("dve", tmpf[:, ci, :n], a_in[:, ci, :n], -1.0, cols[:, ci, C_KA:C_KA + 1], ALU.add, ALU.mult, [b_ain, b_cols], [b_tmpf])
                    k.stt(out[:, ci, :n], tmpf[:, ci, :n], 1.0, ksrc[:, ci, :], ALU.add, ALU.mult, [b_tmpf, b_k], [b_out_])

            b_yt = [Buf(f"yacc{t}") for t in range(NTT)]
            for (c0_, n_) in GROUPS:
                k.memset("dve", yacc[:, :, c0_:c0_ + n_], 0.0, b_yt[c0_ // 128:(c0_ + n_) // 128])

            def scan_gen(dr, sd, banks):
                    w1, b_w1 = sw[f"w1_{dr}"]; w2, b_w2 = sw[f"w2_{dr}"]; w0r, b_w0r = sw[f"w0r_{dr}"]
                    tri, b_tri = sw[f"tri_{dr}"]; m1, b_m1 = sw[f"m1_{dr}"]; mnt, b_mnt = sw[f"mnt_{dr}"]; ind, b_ind = sw["ind"]
                    M32, b_M32 = k.sb(sd, [128, 2, 64], F32, "M32"); Mbf, b_Mbf = k.sb(sd, [128, 2, 128], BF16, "Mblk")
                    k.memset("dve", M32[:], 0.0, [b_M32]); k.memset("dve", Mbf[:], 0.0, [b_Mbf])
                    x1t, b_x1t = k.sb(sd, [32, 128], BF16, "x1t")
                    tnh, b_tnh = k.sb(sd, [32, 128], BF16, "tnh")
                    lwT, b_lwT = k.sb(sd, [128, 256], F32, "lwT")
                    lam, b_lam = k.sb(sd, [128, 2, 3, 128], F32, "lam")
                    gam, b_gam = k.sb(sd, [128, 2, 2], F32, "gam")
                    aF, b_aF = k.sb(sd, [128, 2, 128], F32, "aF")
                    tF, b_tF = k.sb(sd, [128, 2, 128], F32, "tF")
                    kmod, b_kmod = k.sb(sd, [128, 2, 128], F32, "kmod")
                    AR, b_AR = k.sb(sd, [128, 2, 2, 128], BF16, "AR")
                    BK, b_BK = k.sb(sd, [128, 2, 2, 128], BF16, "BK")
                    BKt2, b_BKt = k.sb(sd, [128, 2, 2, 2, 128], BF16, "BKt")
                    k.memset("pool", BKt2[:], 0.0, [b_BKt])
                    Vt, b_Vt = k.sb(sd, [128, 2, 128], BF16, "Vt")
                    SCb, b_SCb = k.sb(sd, [128, 4, 256], BF16, "SCb"); SCk, b_SCk = k.sb(sd, [128, 4, 256], BF16, "SCk")
                    Nb = [k.sb(sd, [128, 4, 128], BF16, f"Nb{i}") for i in range(2)]
                    Tb = [k.sb(sd, [128, 4, 128], BF16, f"Tb{i}") for i in range(2)]
                    R, b_R = k.sb(sd, [128, 4, 128], BF16, "Rinv")
                    Wsb, b_Wsb = k.sb(sd, [128, 256], BF16, "Wsb"); Usb, b_Usb = k.sb(sd, [128, 256], BF16, "Usb")
                    mt, b_mt = k.sb(sd, [128, 2, 64], F32, "mt")
                    v4h = lambda ap: ap.rearrange("p (h t) -> p h t", h=4)
                    order = list(range(NTT)) if dr == 0 else [1, 0] + list(range(NTT - 1, 1, -1))
                    yield
                    for tt in order:
                        tc_ = slice(tt * 128, (tt + 1) * 128)
                        bk = k.bank()
                        for kt in range(2):
                            k.mm(bk, k.ps[bk][0:32, 0:128], w1[:, kt, :], xd[:, kt, tc_], kt == 0, kt == 1, [b_w1, b_xd])
                        k.act(tnh[:], k.ps[bk][0:32, 0:128], AF.Tanh, [k.pb[bk]], [b_tnh])
                        a1, b_a1 = sw[f"a1_{dr}"]; a2, b_a2 = sw[f"a2_{dr}"]
                        bk = k.bank()
                        for kt in range(2):
                            k.mm(bk, k.ps[bk][0:32, 0:128], a1[:, kt, :], xd[:, kt, tc_], kt == 0, kt == 1, [b_a1, b_xd])
                        k.copy("dve", x1t[0:32, 0:128], k.ps[bk][0:32, 0:128], [k.pb[bk]], [b_x1t])
                        yield
                        bk = k.bank()
                        k.mm(bk, k.ps[bk][:, 0:256], tnh[:], w2[:], True, False, [b_tnh, b_w2])
                        k.mm(bk, k.ps[bk][:, 0:256], ones1[:], w0r[:], False, True, [b_ones1, b_w0r])
                        k.act(lwT[:], k.ps[bk][:, 0:256], AF.Sigmoid, [k.pb[bk]], [b_lwT])
                        k.ts("dve", lwT[:], lwT[:], -math.exp(-0.5), None, ALU.mult, None, [b_lwT], [b_lwT])
                        b2 = k.bank()
                        for ci in range(2):
                            k.mm(b2, k.ps[b2][:, ci * 128:(ci + 1) * 128], a2[:, ci * 128:(ci + 1) * 128], x1t[0:32, 0:128], True, True, [b_a2, b_x1t],
                                 inc=(ci == 1))
                        for ci in range(2):
                            P.op("act", lambda e: e.activation(out=aF[:, ci, :], in_=k.ps[b2][:, ci * 128:(ci + 1) * 128], func=AF.Sigmoid,
                                                               bias=cols[:, ci, C_A0 + dr:C_A0 + dr + 1]),
                                 reads=[k.pb[b2], b_cols], writes=[b_aF])
                        yield
                        kmod_of(aF, b_aF, kS[:, :, tc_], 128, kmod, b_kmod, tF, b_tF)
                        for ci in range(2):
                            k.tt("pool", tF[:, ci, :], kkS[:, ci, tc_], aF[:, ci, :], ALU.mult, [b_kk, b_aF], [b_tF])
                        lbk = []
                        for ci in range(2):
                            bk = k.bank(); lbk.append(bk)
                            k.mm(bk, k.ps[bk][:, 0:128], lwT[:, ci * 128:(ci + 1) * 128], tri[:, 0, :], True, True, [b_lwT, b_tri], inc=False)
                            k.mm(bk, k.ps[bk][:, 128:256], lwT[:, ci * 128:(ci + 1) * 128], tri[:, 1, :], True, True, [b_lwT, b_tri], inc=False)
                            k.mm(bk, k.ps[bk][:, 256:258], lwT[:, ci * 128:(ci + 1) * 128], ind[:], True, True, [b_lwT, b_ind])
                        yield
                        for ci in range(2):
                            bk = lbk[ci]
                            k.act(lam[:, ci, 0, :], k.ps[bk][:, 0:128], AF.Exp, [k.pb[bk]], [b_lam])
                            k.act(lam[:, ci, 1, :], k.ps[bk][:, 0:128], AF.Exp, [k.pb[bk]], [b_lam], scale=-1.0)
                            k.act(lam[:, ci, 2, :], k.ps[bk][:, 128:256], AF.Exp, [k.pb[bk]], [b_lam])
                            k.act(gam[:, ci, :], k.ps[bk][:, 256:258], AF.Exp, [k.pb[bk]], [b_gam])
                        yield
                        if RWSTOP <= 1:
                            continue
                        for ci in range(2):
                            k.tt("dve", AR[:, ci, 1, :], rS[:, ci, tc_], lam[:, ci, 0, :], ALU.mult, [b_r, b_lam], [b_AR])
                            k.stt(AR[:, ci, 0, :], kkS[:, ci, tc_], -1.0, lam[:, ci, 2, :], ALU.mult, ALU.mult, [b_kk, b_lam], [b_AR])
                            k.tt("dve", BK[:, ci, 1, :], kmod[:, ci, :], lam[:, ci, 1, :], ALU.mult, [b_kmod, b_lam], [b_BK])
                            k.tt("pool", BK[:, ci, 0, :], tF[:, ci, :], lam[:, ci, 1, :], ALU.mult, [b_tF, b_lam], [b_BK])
                        yield
                        if RWSTOP <= 2:
                            continue
                        bk = k.bank()
                        pv = k.ps[bk][:].bitcast(BF16)
                        for ci in range(2):
                            for x_ in range(2):
                                P.op("pe", lambda e: e.transpose(pv[:, (ci * 2 + x_) * 128:(ci * 2 + x_ + 1) * 128], BK[:, ci, x_, :], identb),
                                     reads=[b_BK, b_cstb], writes=[k.pb[bk]], inc=False)
                            P.op("pe", lambda e: e.transpose(pv[:, (4 + ci) * 128:(5 + ci) * 128], vS[:, ci, tc_], identb),
                                 reads=[b_v, b_cstb], writes=[k.pb[bk]], inc=(ci == 1))
                        k.copy("act", BKt2[0:64, 0].rearrange("p a b c -> p (a b c)"), pv[0:64, 0:512], [k.pb[bk]], [b_BKt])
                        k.copy("act", BKt2[64:128, 1].rearrange("p a b c -> p (a b c)"), pv[64:128, 0:512], [k.pb[bk]], [b_BKt])
                        k.copy("dve", Vt[:].rearrange("p a c -> p (a c)"), pv[:, 512:768], [k.pb[bk]], [b_Vt])
                        if RWSTOP <= 3:
                            continue
                        m1b = m1[:].unsqueeze(1).to_broadcast([128, 2, 256])
                        for hh in range(2):
                            pb = 64 * hh
                            bA = k.bank(); bB = k.bank()
                            for x_, bx in ((0, bA), (1, bB)):
                                for ci in range(2):
                                    arv = AR[pb:pb + 64, ci, :, :].rearrange("p a t -> p (a t)")
                                    k.mm(bx, k.ps[bx][:, ci * 256:(ci + 1) * 256], BK[pb:pb + 64, ci, x_, :], arv, True, True, [b_BK, b_AR], inc=(ci == 1))
                            k.tt("dve", SCb[:, hh * 2:hh * 2 + 2, :], k.ps[bA][:, 0:512].rearrange("p (h t) -> p h t", h=2), m1b, ALU.mult,
                                 [k.pb[bA], b_m1], [b_SCb])
                            k.tt("dve", SCk[:, hh * 2:hh * 2 + 2, :], k.ps[bB][:, 0:512].rearrange("p (h t) -> p h t", h=2), m1b, ALU.mult,
                                 [k.pb[bB], b_m1], [b_SCk])
                            yield
                        Tc, b_Tc = Tb[0]
                        for hh in range(2):
                            pb = 64 * hh
                            bC = k.bank()
                            for ci in range(2):
                                k.mm(bC, k.ps[bC][:, ci * 128:(ci + 1) * 128], AR[pb:pb + 64, ci, 0, :], BK[pb:pb + 64, ci, 0, :], True, True, [b_AR, b_BK],
                                     inc=(ci == 1))
                            k.tt("dve", Tc[:, hh * 2:hh * 2 + 2, :], k.ps[bC][:, 0:256].rearrange("p (h t) -> p h t", h=2),
                                 mnt[:].unsqueeze(1).to_broadcast([128, 2, 128]), ALU.mult, [k.pb[bC], b_mnt], [b_Tc])
                        k.tt("pool", R[:], SCb[:, :, 0:128], identb.unsqueeze(1).to_broadcast([128, 4, 128]), ALU.add, [b_SCb, b_cstb], [b_R])
                        yield
                        if RWSTOP <= 4:
                            continue
                        Nc, b_Nc = SCb[:, :, 0:128], b_SCb
                        for i in range(1, 6):
                            Nn, b_Nn = Nb[i % 2]; Tn, b_Tn = Tb[i % 2]
                            bt_ = k.bank()
                            for h in range(4):
                                k.mm(bt_, k.ps[bt_][:, h * 128:(h + 1) * 128], Nc[:, h, :], Tc[:, h, :], True, True, [b_Nc, b_Tc], inc=(h == 3))
                            k.copy("dve", Tn[:], v4h(k.ps[bt_][:, 0:512]), [k.pb[bt_]], [b_Tn])
                            if i < 5:
                                bn = k.bank()
                                for h in range(4):
                                    k.mm(bn, k.ps[bn][:, h * 128:(h + 1) * 128], Tc[:, h, :], Nc[:, h, :], True, True, [b_Tc, b_Nc], inc=(h == 3))
                                k.copy("act", Nn[:], v4h(k.ps[bn][:, 0:512]), [k.pb[bn]], [b_Nn])
                            yield
                            br_ = k.bank()
                            for h in range(4):
                                k.mm(br_, k.ps[br_][:, h * 128:(h + 1) * 128], Tn[:, h, :], R[:, h, :], True, True, [b_Tn, b_R], inc=(h == 3))
                            k.tt("dve", R[:], v4h(k.ps[br_][:, 0:512]), R[:], ALU.add, [k.pb[br_], b_R], [b_R])
                            yield
                            Nc, b_Nc = Nn[:], b_Nn
                            Tc, b_Tc = Tn, b_Tn
                        if RWSTOP <= 5:
                            continue
                        bY, bW, bU, bM = banks
                        for jj in ([0, 1] if dr == 0 else [1, 0]):
                            pj = 64 * jj
                            for ci in range(2):
                                k.mm(bW, k.ps[bW][:, ci * 128:(ci + 1) * 128], AR[:, ci, 0, :], Mbf[:, ci, :], True, False, [b_AR, b_Mbf], inc=False)
                                for hh in range(2):
                                    h = ci * 2 + hh
                                    pb = 64 * hh
                                    k.mm(bW, k.ps[bW][:, h * 64:(h + 1) * 64], SCk[:, hh * 2 + ci, 0:128], Vt[:, ci, pb:pb + 64], False, True, [b_SCk, b_Vt],
                                         inc=(h == 3))
                            k.copy("act", Wsb[:], k.ps[bW][:, 0:256], [k.pb[bW]], [b_Wsb])
                            yield
                            for h in range(4):
                                oc = slice(h * 64, (h + 1) * 64)
                                k.mm(bU, k.ps[bU][:, oc], R[:, (h % 2) * 2 + h // 2, :], Wsb[:, oc], True, True, [b_R, b_Wsb], inc=(h == 3))
                            k.copy("dve", Usb[:], k.ps[bU][:, 0:256], [k.pb[bU]], [b_Usb])
                            yield
                            for ci in range(2):
                                for hh in range(2):
                                    h = ci * 2 + hh
                                    pb = 64 * hh
                                    oc = slice(h * 64, (h + 1) * 64)
                                    mo = k.ps[bM][pb:pb + 64, ci * 64:(ci + 1) * 64]
                                    k.mm(bM, mo, BKt2[:, jj, ci, 0, pb:pb + 64], Usb[:, oc], True, False, [b_BKt, b_Usb], inc=False)
                                    k.mm(bM, mo, BKt2[:, jj, ci, 1, pb:pb + 64], Vt[:, ci, pb:pb + 64], False, True, [b_BKt, b_Vt], inc=(h == 3))
                            for ci in range(2):
                                ycol = slice(ci * 128 + pj, ci * 128 + pj + 64)
                                k.mm(bY, k.ps[bY][:, ycol], Mbf[:, ci, :], AR[:, ci, 1, pj:pj + 64], True, False, [b_Mbf, b_AR], inc=False)
                                for hh in range(2):
                                    h = ci * 2 + hh
                                    pb = 64 * hh
                                    oc = slice(h * 64, (h + 1) * 64)
                                    k.mm(bY, k.ps[bY][pb:pb + 64, ycol], Usb[:, oc], SCb[:, hh * 2 + ci, 128 + pj:128 + pj + 64], False, False, [b_Usb, b_SCb], inc=False)
                                    k.mm(bY, k.ps[bY][pb:pb + 64, ycol], Vt[:, ci, pb:pb + 64], SCk[:, hh * 2 + ci, 128 + pj:128 + pj + 64], False, True, [b_Vt, b_SCk],
                                         inc=(h == 3))
                            k.tt("dve", mt[:], k.ps[bM][:, 0:128].rearrange("p (c n) -> p c n", c=2), M32[:], ALU.add, [k.pb[bM], b_M32], [b_mt])
                            k.tt("dve", Mbf[0:64, :, 0:64], mt[0:64, :, :], gam[0:64, :, jj:jj + 1].to_broadcast([64, 2, 64]), ALU.mult,
                                 [b_mt, b_gam], [b_Mbf])
                            k.tt("dve", Mbf[64:128, :, 64:128], mt[64:128, :, :], gam[64:128, :, jj:jj + 1].to_broadcast([64, 2, 64]), ALU.mult,
                                 [b_mt, b_gam], [b_Mbf])
                            k.tt("dve", M32[:], mt[:], gam[:, :, jj:jj + 1].to_broadcast([128, 2, 64]), ALU.mult, [b_mt, b_gam], [b_M32])
                            yield
                        k.tt("dve", yacc[:, :, tc_], k.ps[bY][:, 0:256].rearrange("p (c t) -> p c t", c=2), yacc[:, :, tc_], ALU.add,
                             [k.pb[bY], b_yt[tt]], [b_yt[tt]])
                        yield

            with ExitStack() as sd:
                threads = [[scan_gen(dr, sd, [4 * dr + i for i in range(4)]), [4 * dr + i for i in range(4)], 0] for dr in range(2)]
                live = list(threads)
                k.rrset, k.rr = threads[0][1], threads[0][2]
                for _ in range(RWSKEW):
                    next(threads[0][0])
                threads[0][2] = k.rr
                if os.environ.get("RWSEQ"):
                    for th in threads:
                        k.rrset, k.rr = th[1], th[2]
                        for _ in th[0]:
                            pass
                    live = []
                while live:
                    for th in list(live):
                        k.rrset, k.rr = th[1], th[2]
                        try:
                            next(th[0])
                        except StopIteration:
                            live.remove(th)
                        th[2] = k.rr
                k.rrset = list(range(8)); k.rr = 0
                P.barrier()
            b_yacc = Buf("rw_yacc_all")
            dump("rw_yacc", yacc[:], b_yacc, [128, 2, NT], BF16)
            with ExitStack() as s5_:
              if RW_LEVEL >= 6:
                    g1, b_g1 = sw["g1"]; g2, b_g2 = sw["g2"]
                    x1t, b_x1t = k.sb(s5_, [32, 512], BF16, "ex1t")
                    aF, b_aF = k.sb(s5_, [128, 2, 512], F32, "eaF"); tF, b_tF = k.sb(s5_, [128, 2, 512], F32, "etF")
                    kmod, b_kmod = k.sb(s5_, [128, 2, 512], F32, "ekm")
                    bon, b_bon = k.sb(s5_, [128, 2, 512], F32, "ebon")
                    ybf, b_ybf = k.sb(s5_, [128, 512], BF16, "eybf"); yc, b_yc = k.sb(s5_, [128, 512], F32, "eyc")
                    rs_, b_rs = k.sb(s5_, [128, 512], F32, "ers"); lnt, b_lnt = k.sb(s5_, [128, 512], F32, "elnt")
                    gh, b_gh = k.sb(s5_, [64, 512], BF16, "egh"); gg, b_gg = k.sb(s5_, [128, 2, 512], F32, "egg")
                    rkb, b_rkb = k.sb(s5_, [128, 512], BF16, "erkb")
                    lneps, b_lneps = k.sb(s5_, [128, 1], F32, "lneps")
                    k.memset("dve", lneps[:], 64e-5, [b_lneps])
                    for g, (c0, n) in enumerate(GROUPS):
                        gc_ = slice(c0, c0 + n)
                        for dr in range(2):
                            a_of(dr, xd[:, :, gc_], n, aF, b_aF, x1t, b_x1t)
                            kmod_of(aF, b_aF, kS[:, :, gc_], n, kmod, b_kmod, tF, b_tF)
                            for ci in range(2):
                                k.stt(rkb[:, :n], kmod[:, ci, :n], cols[:, ci, C_RK:C_RK + 1], rS[:, ci, gc_], ALU.mult, ALU.mult,
                                      [b_kmod, b_cols, b_r], [b_rkb])
                                bk = k.bank()
                                k.mm(bk, k.ps[bk][:, :n], bo64, rkb[:, :n], True, True, [b_rkb, b_cstb])
                                if dr == 0:
                                    k.tt("dve", bon[:, ci, :n], k.ps[bk][:, :n], vS[:, ci, gc_], ALU.mult, [k.pb[bk], b_v], [b_bon])
                                else:
                                    k.tt("dve", tF[:, ci, :n], k.ps[bk][:, :n], vS[:, ci, gc_], ALU.mult, [k.pb[bk], b_v], [b_tF])
                                    k.tt("pool", bon[:, ci, :n], bon[:, ci, :n], tF[:, ci, :n], ALU.add, [b_bon, b_tF], [b_bon])
                        bk = k.bank()
                        for kt in range(2):
                            k.mm(bk, k.ps[bk][0:64, :n], g1[:, kt, :], xd[:, kt, gc_], kt == 0, kt == 1, [b_g1, b_xd])
                        k.act(gh[:, :n], k.ps[bk][0:64, :n], AF.Sigmoid, [k.pb[bk]], [b_gh])
                        for ci in range(2):
                            b2 = k.bank()
                            k.mm(b2, k.ps[b2][:, :n], g2[:, ci * 128:(ci + 1) * 128], gh[:, :n], True, True, [b_g2, b_gh])
                            k.copy("act", gg[:, ci, :n], k.ps[b2][:, :n], [k.pb[b2]], [b_gg])
                        for ci in range(2):
                            bk = k.bank()
                            k.mm(bk, k.ps[bk][:, :n], bo64, yacc[:, ci, gc_], True, True, [b_yacc, b_cstb])
                            k.stt(yc[:, :n], k.ps[bk][:, :n], -1.0 / 64, yacc[:, ci, gc_], ALU.mult, ALU.add, [k.pb[bk], b_yacc], [b_yc])
                            k.act(ybf[:, :n], yc[:, :n], AF.Square, [b_yc], [b_ybf])
                            b2 = k.bank()
                            k.mm(b2, k.ps[b2][:, :n], bo64, ybf[:, :n], True, True, [b_ybf, b_cstb])
                            P.op("act", lambda e: e.activation(out=lnt[:, :n], in_=k.ps[b2][:, :n], func=AF.Ln, scale=1.0 / 64, bias=lneps[:]),
                                 reads=[k.pb[b2], b_lneps], writes=[b_lnt])
                            k.act(rs_[:, :n], lnt[:, :n], AF.Exp, [b_lnt], [b_rs], scale=-0.5)
                            k.stt(yc[:, :n], yc[:, :n], cols[:, ci, C_LNG:C_LNG + 1], rs_[:, :n], ALU.mult, ALU.mult, [b_yc, b_cols, b_rs], [b_yc])
                            k.stt(yc[:, :n], yc[:, :n], cols[:, ci, C_LNB:C_LNB + 1], bon[:, ci, :n], ALU.add, ALU.add, [b_yc, b_cols, b_bon], [b_yc])
                            k.tt("dve", ydst[:, ci, gc_], yc[:, :n], gg[:, ci, :n], ALU.mult, [b_yc, b_gg, b_xd], [b_ydst, b_xd])
                    P.barrier()
        dump("yd", ydst[:], b_ydst, [128, 2, NT], BF16)

    def final_out():
        with ExitStack() as st:
            ot = [k.sb(st, [128, D], F32, "ostage") for _ in range(2)]
            for tt in range(NLAT // 128):
                o, ob = ot[tt % 2]
                c0 = NCTX + tt * 128
                for half in range(2):
                    bk = k.bank()
                    for j in range(4):
                        kt = half * 4 + j
                        P.op("pe", lambda e: e.transpose(k.ps[bk][:, j * 128:(j + 1) * 128], xT[:, kt, c0:c0 + 128], identf),
                             reads=[b_xT, b_cstf], writes=[k.pb[bk]], inc=(j == 3))
                    k.copy("dve" if half == 0 else "act", o[:, half * 512:(half + 1) * 512], k.ps[bk][:], [k.pb[bk]], [ob])
                P.dma("sp", out_d[tt * 128:(tt + 1) * 128, :], o[:], reads=[ob])
            P.finish([b for _, b in ot])
            P.barrier()

    for l in range(DEPTH):
        if ("L%d" % l) not in stages:
            continue
        with ExitStack() as st:
            rms_stats(st)
            P.barrier()
        if "rw" in stages:
            rwkv_mixer(l)
        if "da" in stages:
            da_mixer(l)
        if "mla" in stages:
            mla_mixer(l)
        if "s5" in stages:
            s5_mixer(l)
        if "merge" in stages:
            merge(l)
        if "moe" in stages:
            moe(l)
    final_out()
    for e in ("sp",):
        toks = [(kk, v) for kk, v in P.cnt.items() if v > 0 and isinstance(kk, tuple)]
        for tok in toks:
            P._wait(e, tok)
    top.close()
    return nc, k, dbg_out


_CONSTS = None


def prep_shared(inp):
    g = {}
    g.update(host_consts())
    f32 = np.float32
    w_in = np.asarray(inp["w_in"], f32)
    offs = np.cumsum([0, 256, 256, 256, 256, 192, 128, 16, 256, 256, 256, 256, 4096])
    seg = lambda i: w_in[:, :, offs[i]:offs[i + 1]]
    g["w_ada"] = np.ascontiguousarray(inp["w_ada"], f32)
    ba = np.asarray(inp["b_ada"], f32)
    g["bada"] = np.ascontiguousarray(ba.reshape(DEPTH, 48, 128).transpose(0, 2, 1))
    g["gmix"] = np.stack([fm_cols(inp["norm_mix_g"][l], 8) for l in range(DEPTH)])
    g["gffn"] = np.stack([fm_cols(inp["norm_ffn_g"][l], 8) for l in range(DEPTH)])
    def pad3(w):
        o = np.zeros((DEPTH, D, 384), f32)
        for j in range(8):
            o[:, :, (j // 3) * 128 + (j % 3) * 32:(j // 3) * 128 + (j % 3) * 32 + 32] = w[:, :, j * 32:(j + 1) * 32]
        return o
    g["w_da_qk"] = np.ascontiguousarray(np.concatenate([pad3(seg(0)), pad3(seg(1))], axis=2))
    g["w_da_v"] = np.ascontiguousarray(seg(2))
    dg = np.asarray(inp["da_qk_norm_g"], f32)
    g["da_g"] = np.ascontiguousarray(np.stack([np.tile(dg[:, 0, :], (1, 4)), np.tile(dg[:, 1, :], (1, 4))], axis=2))
    g["da_lam"] = np.ascontiguousarray(np.broadcast_to(np.asarray(inp["da_lambda"], f32).reshape(DEPTH, 1, 128), (DEPTH, 128, 128)))
    g["da_sub"] = np.ascontiguousarray(np.broadcast_to(np.asarray(inp["da_subln_g"], f32).reshape(DEPTH, 1, 64), (DEPTH, 128, 64)))
    g["w_mla_c"] = np.zeros((DEPTH, D, 384), f32)
    g["w_mla_c"][:, :, 0:192] = seg(4)
    g["w_mla_c"][:, :, 256:384] = seg(5)
    g["w_mla_kr"] = np.zeros((DEPTH, D, 128), f32)
    g["w_mla_kr"][:, :, 32:48] = seg(6)
    g["w_mla_kr"][:, :, 96:112] = seg(6)
    wuq = np.asarray(inp["mla_w_uq"], f32)
    wuqp = np.zeros((DEPTH, 256, 256), f32)
    for h in range(4):
        wuqp[:, 0:192, 64 * h:64 * h + 48] = wuq[:, :, 48 * h:48 * h + 48]
    g["w_uq"] = np.ascontiguousarray(wuqp.reshape(DEPTH, 2, 128, 256).transpose(0, 2, 1, 3))
    wukv = np.asarray(inp["mla_w_ukv"], f32)
    g["w_ukvk"] = np.zeros((DEPTH, 128, 256), f32)
    g["w_ukvv"] = np.zeros((DEPTH, 128, 256), f32)
    for h in range(4):
        g["w_ukvk"][:, :, 64 * h:64 * h + 32] = wukv[:, :, 96 * h:96 * h + 32]
        g["w_ukvv"][:, :, 64 * h:64 * h + 64] = wukv[:, :, 96 * h + 32:96 * h + 96]
    gcq = np.zeros((DEPTH, 256), f32); gcq[:, 0:192] = np.asarray(inp["mla_cq_norm_g"], f32)
    gkv = np.asarray(inp["mla_ckv_norm_g"], f32)
    g["mla_gc"] = np.ascontiguousarray(np.stack([gcq[:, 0:128], gcq[:, 128:256], gkv], axis=2))
    mg = np.asarray(inp["mla_qk_norm_g"], f32)
    mgp = np.zeros((DEPTH, 2, 128), f32)
    for h in range(2):
        mgp[:, :, 64 * h:64 * h + 48] = mg
    g["mla_g"] = np.ascontiguousarray(mgp.transpose(0, 2, 1))
    g["w_s5"] = np.ascontiguousarray(seg(3))
    Bre = np.asarray(inp["s5_b_re"], f32); Bim = np.asarray(inp["s5_b_im"], f32)
    Cre = np.asarray(inp["s5_c_re"], f32); Cim = np.asarray(inp["s5_c_im"], f32)
    s5B = np.zeros((DEPTH, 2, 2, 128, 1024), f32)
    s5C = np.zeros((DEPTH, 2, 2, 128, 8, 128), f32)
    for gi in range(16):
        kt, gl = gi // 8, gi % 8
        s5B[:, :, kt, gl * 16:(gl + 1) * 16, gl * 64:(gl + 1) * 64] = Bre[:, :, gi].transpose(0, 1, 3, 2)
        s5B[:, :, kt, gl * 16:(gl + 1) * 16, 512 + gl * 64:512 + (gl + 1) * 64] = Bim[:, :, gi].transpose(0, 1, 3, 2)
        i, po = gi // 2, (gi % 2) * 64
        s5C[:, :, 0, po:po + 64, i, gl * 16:(gl + 1) * 16] = Cre[:, :, gi].transpose(0, 1, 3, 2)
        s5C[:, :, 1, po:po + 64, i, gl * 16:(gl + 1) * 16] = Cim[:, :, gi].transpose(0, 1, 3, 2)
    g["s5B"] = s5B; g["s5C"] = s5C
    lre = np.asarray(inp["s5_lam_re"], f32).reshape(DEPTH, 2, 1024)
    lim = np.asarray(inp["s5_lam_im"], f32).reshape(DEPTH, 2, 1024)
    ldt = np.repeat(np.asarray(inp["s5_log_dt"], f32), 64, axis=2)
    tm = np.stack([lre, lim, ldt], axis=2)
    g["s5tm"] = np.ascontiguousarray(np.broadcast_to(tm[:, :, :, None, :], (DEPTH, 2, 3, 128, 1024)))
    g["s5fm"] = np.ascontiguousarray(tm.reshape(DEPTH, 2, 3, 8, 128).transpose(0, 1, 4, 2, 3))
    g["s5d"] = np.stack([fm_cols(np.asarray(inp["s5_d"], f32)[l].reshape(256), 2) for l in range(DEPTH)])
    g["s5bg"] = np.stack([fm_cols(np.asarray(inp["s5_b_glu"], f32)[l], 2) for l in range(DEPTH)])
    g["w_glu"] = np.ascontiguousarray(inp["s5_w_glu"], f32)
    pidx = np.arange(128, dtype=f32)
    posc = np.zeros((2, 128, 2), f32); posc[0, :, 0] = -pidx; posc[1, :, 0] = -(127 - pidx)
    posr = np.zeros((2, 128, 128), f32); posr[0] = pidx[None, :]; posr[1] = (127 - pidx)[None, :]
    tri = np.zeros((2, 128, 128), f32)
    tri[0] = (pidx[:, None] <= pidx[None, :]).astype(f32)
    tri[1] = (pidx[:, None] >= pidx[None, :]).astype(f32)
    g["posc"] = posc; g["posr"] = posr; g["tri"] = tri
    g["w_rw"] = np.ascontiguousarray(np.concatenate([seg(7), seg(8), seg(9), seg(10)], axis=2))
    mu = np.asarray(inp["rw_mu"], f32).reshape(DEPTH, 1, 1024)
    g["rw_mu_row"] = np.ascontiguousarray(np.broadcast_to(mu, (DEPTH, 128, 1024)))
    colsl = []
    for l in range(DEPTH):
        cs = [inp["rw_k_k"][l], inp["rw_k_a"][l], inp["rw_ln_g"][l], inp["rw_ln_b"][l], np.asarray(inp["rw_r_k"][l]).reshape(256),
              inp["rw_w0"][l, 0], inp["rw_w0"][l, 1], inp["rw_a0"][l, 0], inp["rw_a0"][l, 1]]
        colsl.append(np.stack([fm_cols(c, 2) for c in cs], axis=2))
    g["rw_cols"] = np.ascontiguousarray(np.stack(colsl))
    g["rw_w0row"] = np.ascontiguousarray(np.asarray(inp["rw_w0"], f32).reshape(DEPTH, 2, 1, 256))
    for nm in ("rw_w1", "rw_w2", "rw_a1", "rw_a2", "rw_g1", "rw_g2"):
        g[nm] = np.ascontiguousarray(inp[nm], f32)
    t = np.arange(128)
    same = (t[:, None] // 64) == (t[None, :] // 64)
    tri = np.zeros((2, 128, 2, 128), f32)
    tri[0, :, 0, :] = (same & (t[:, None] <= t[None, :])); tri[0, :, 1, :] = (same & (t[:, None] < t[None, :]))
    tri[1, :, 0, :] = (same & (t[:, None] >= t[None, :])); tri[1, :, 1, :] = (same & (t[:, None] > t[None, :]))
    g["rw_tri"] = tri
    ind = np.zeros((128, 2), f32); ind[:64, 0] = 1; ind[64:, 1] = 1
    g["rw_ind"] = ind
    m1 = np.zeros((2, 128, 256), f32)
    m1[0, :, :128] = (same & (t[:, None] < t[None, :])); m1[0, :, 128:] = (same & (t[:, None] <= t[None, :]))
    m1[1, :, :128] = (same & (t[:, None] > t[None, :])); m1[1, :, 128:] = (same & (t[:, None] >= t[None, :]))
    g["rw_m1"] = m1
    mnt = np.zeros((2, 128, 128), f32)
    mnt[0] = (same & (t[None, :] < t[:, None])); mnt[1] = (same & (t[None, :] > t[:, None]))
    g["rw_mnt"] = mnt
    g["w_gates"] = np.ascontiguousarray(seg(11).reshape(DEPTH, 8, 128, 4, 8, 128).transpose(0, 4, 2, 1, 3, 5)).reshape(DEPTH, 8, 128, 4096)
    g["w_branch"] = np.ascontiguousarray(np.asarray(inp["w_branch"], f32).reshape(DEPTH, 4, 2, 128, D).transpose(0, 3, 2, 1, 4)).reshape(DEPTH, 128, 8192)
    g["w_out"] = np.ascontiguousarray(inp["w_out"], f32)
    g["router_w"] = np.ascontiguousarray(inp["router_w"], f32)
    g["router_b"] = np.ascontiguousarray(np.broadcast_to(np.asarray(inp["router_bias"], f32).reshape(1, 16), (128, 16)))
    sel = np.zeros((16, 16, 128), f32)
    for e in range(16):
        sel[e, e, :] = 1.0
    g["sel"] = sel
    g["exp_w_gate"] = np.ascontiguousarray(inp["exp_w_gate"], f32)
    g["exp_w_up"] = np.ascontiguousarray(inp["exp_w_up"], f32)
    g["exp_w_down"] = np.ascontiguousarray(inp["exp_w_down"], f32)
    return g


def prep_core(inp, b):
    x = np.asarray(inp["x"], np.float32)[b]
    ctx = np.asarray(inp["ctx"], np.float32)[b]
    xin = np.ascontiguousarray(np.concatenate([ctx, x], axis=0))
    c2 = np.stack([np.asarray(inp["c"], np.float32)[b], np.asarray(inp["c_ctx"], np.float32)], axis=0)
    c2T = np.ascontiguousarray(c2.reshape(2, 8, 128).transpose(2, 1, 0))
    return {"xin": xin, "c2T": c2T}


RW_LEVEL = int(os.environ.get('RW_LEVEL', '9'))
RWSTOP = int(os.environ.get('RWSTOP', '9'))
S5SKEW = int(os.environ.get('S5SKEW', '1'))
RWSKEW = int(os.environ.get('RWSKEW', '0'))
RW_SUB = int(os.environ.get('RW_SUB', '9'))
STAGES_ALL = ("L0", "L1", "da", "mla", "s5", "rw", "merge", "moe")


def kernel(**inputs):
    nc, k, _ = build(STAGES_ALL)
    shared = prep_shared(inputs)
    in_maps = []
    for b in range(8):
        m = dict(shared)
        m.update(prep_core(inputs, b))
        in_maps.append({kk: m[kk] for kk in k.ins})
    res = run_bass_kernel_spmd(nc, in_maps, core_ids=list(range(8)))
    return np.stack([np.asarray(r["out"]) for r in res.results], axis=0).astype(np.float32)
```

```python
import math
import os
import numpy as np
import concourse.bass as bass
import concourse.mybir as mybir
from concourse.bass_utils import run_bass_kernel_spmd

F32 = mybir.dt.float32
BF16 = mybir.dt.bfloat16
I32 = mybir.dt.int32
AF = mybir.ActivationFunctionType
ALU = mybir.AluOpType
AX = mybir.AxisListType

D = 1024
NT = 2304
NCTX = 256
NLAT = 2048
DEPTH = 2
EPS = 1e-6
GROUPS = [(0, 256)] + [(256 + 512 * i, 512) for i in range(4)]
NTT = 18


class Buf:
    __slots__ = ("name", "w", "r", "excl")

    def __init__(self, name="", excl=False):
        self.name = name
        self.w = None
        self.r = {}
        self.excl = excl


class Prog:
    NDMA = 8

    def __init__(self, nc):
        self.nc = nc
        self.eng = {"pe": nc.tensor, "act": nc.scalar, "dve": nc.vector, "pool": nc.gpsimd, "sp": nc.sync}
        self.sems = {}
        self.cnt = {}
        self.seen = {e: {} for e in self.eng}
        self.pend = {e: ([], []) for e in self.eng}
        for e in self.eng:
            self.sems[e] = nc.alloc_semaphore("s_" + e)
            self.cnt[e] = 0
        self.dq = {}
        for q in ("sp", "pool"):
            lst = []
            for i in range(self.NDMA):
                key = ("d", q, i)
                self.sems[key] = nc.alloc_semaphore(f"d_{q}{i}")
                self.cnt[key] = 0
                lst.append(key)
            self.dq[q] = [lst, 0, [None] * self.NDMA]
        self.ninst = 0

    def _wait(self, e, tok):
        key, val = tok
        if key == e:
            if e == "pe":
                return
            if val < self.cnt[e] - 1:
                return
        if self.seen[e].get(key, 0) >= val:
            return
        self.eng[e].wait_ge(self.sems[key], val)
        self.seen[e][key] = val
        self.ninst += 1

    def _deps(self, e, reads, writes):
        for b in reads:
            if b.w is not None:
                self._wait(e, b.w)
            if b.excl:
                for tok in b.r.values():
                    if tok[0] != e:
                        self._wait(e, tok)
        for b in writes:
            if b.w is not None:
                self._wait(e, b.w)
            for tok in b.r.values():
                if tok[0] != e:
                    self._wait(e, tok)

    def op(self, e, fn, reads=(), writes=(), inc=True):
        self._deps(e, reads, writes)
        inst = fn(self.eng[e])
        self.ninst += 1
        pr, pw = self.pend[e]
        pr.extend(reads)
        pw.extend(writes)
        if inc:
            self.cnt[e] += 1
            tok = (e, self.cnt[e])
            inst.then_inc(self.sems[e], 1)
            for b in pr:
                b.r[e] = tok
            for b in pw:
                b.w = tok
                b.r = {}
            self.pend[e] = ([], [])
        return inst

    def dma(self, q, out, in_, reads=(), writes=(), **kw):
        lst, rr, last = self.dq[q]
        k = rr
        self.dq[q][1] = (rr + 1) % self.NDMA
        if last[k] is not None:
            self._wait(q, last[k])
        self._deps(q, reads, writes)
        inst = self.eng[q].dma_start(out=out, in_=in_, **kw)
        self.ninst += 1
        key = lst[k]
        self.cnt[key] += 16
        tok = (key, self.cnt[key])
        inst.then_inc(self.sems[key], 16)
        last[k] = tok
        for b in reads:
            b.r[key] = tok
        for b in writes:
            b.w = tok
            b.r = {}
        return tok

    def finish(self, bufs, e="sp"):
        for b in bufs:
            if b.w is not None:
                self._wait(e, b.w)
            for tok in b.r.values():
                self._wait(e, tok)

    def barrier(self):
        toks = [(k, v) for k, v in self.cnt.items() if v > 0]
        for e in self.eng:
            for tok in toks:
                if tok[0] != e:
                    self._wait(e, tok)


from contextlib import ExitStack


class K:
    def __init__(self, nc, dbg=None):
        self.nc = nc
        self.P = Prog(nc)
        self.dbg = dbg or {}
        self.ins = {}
        self.uid = 0
        self.ps = []
        self.pb = []
        for i in range(8):
            self.ps.append(nc.alloc_psum_tensor(f"ps{i}", [128, 512], F32))
            self.pb.append(Buf(f"ps{i}", excl=True))
        self.rr = 0
        self.rrset = list(range(8))

    def din(self, name, shape, dtype=F32):
        t = self.nc.dram_tensor(name, list(shape), dtype, kind="ExternalInput").ap()
        self.ins[name] = t
        return t

    def sb(self, stack, shape, dtype, name=None):
        self.uid += 1
        nm = f"{name or 't'}_{self.uid}"
        t = stack.enter_context(self.nc.sbuf_tensor(nm, list(shape), dtype))
        nb = int(np.prod(shape[1:])) * (2 if dtype == BF16 else 4)
        self.live = getattr(self, "live", 0) + nb
        if self.live > getattr(self, "peak", 0):
            self.peak = self.live; self.peak_at = nm
        def _dec(nb=nb):
            self.live -= nb
        stack.callback(_dec)
        return t, Buf(nm)

    def bank(self):
        i = self.rrset[self.rr % len(self.rrset)]
        self.rr += 1
        return i

    def mm(self, bank, out, lhsT, rhs, start, stop, reads, inc=None):
        self.P.op("pe", lambda e: e.matmul(out, lhsT=lhsT, rhs=rhs, start=start, stop=stop, skip_group_check=True),
                  reads=reads, writes=[self.pb[bank]], inc=(stop if inc is None else inc))

    def act(self, out, in_, func, reads, writes, scale=None, bias=None, eng="act"):
        kw = {}
        if scale is not None:
            kw["scale"] = scale
        if bias is not None:
            kw["bias"] = bias
        self.P.op("act", lambda e: e.activation(out=out, in_=in_, func=func, **kw), reads=reads, writes=writes)

    def tt(self, eng, out, in0, in1, op, reads, writes):
        self.P.op(eng, lambda e: e.tensor_tensor(out=out, in0=in0, in1=in1, op=op), reads=reads, writes=writes)

    def ts(self, eng, out, in0, s1, s2, op0, op1, reads, writes):
        if op1 is None:
            self.P.op(eng, lambda e: e.tensor_scalar(out=out, in0=in0, scalar1=s1, scalar2=None, op0=op0),
                      reads=reads, writes=writes)
        else:
            self.P.op(eng, lambda e: e.tensor_scalar(out=out, in0=in0, scalar1=s1, scalar2=s2, op0=op0, op1=op1),
                      reads=reads, writes=writes)

    def stt(self, out, in0, scalar, in1, op0, op1, reads, writes):
        self.P.op("dve", lambda e: e.scalar_tensor_tensor(out=out, in0=in0, scalar=scalar, in1=in1, op0=op0, op1=op1),
                  reads=reads, writes=writes)

    def copy(self, eng, out, in_, reads, writes):
        if eng == "act":
            self.P.op("act", lambda e: e.copy(out=out, in_=in_), reads=reads, writes=writes)
        else:
            self.P.op(eng, lambda e: e.tensor_copy(out=out, in_=in_), reads=reads, writes=writes)

    def memset(self, eng, ap, val, writes):
        self.P.op(eng, lambda e: e.memset(ap, val), writes=writes)

    def load(self, q, out, in_, writes, reads=()):
        self.P.dma(q, out, in_, reads=reads, writes=writes)

    def rstd_from_ss(self, out, ss_ps, bank, inv_n, tmp, tmpb, writes):
        self.P.op("act", lambda e: e.activation(out=tmp, in_=ss_ps, func=AF.Ln, scale=inv_n, bias=self.eps_col[:]),
                  reads=[self.pb[bank], self.b_const], writes=[tmpb])
        self.P.op("act", lambda e: e.activation(out=out, in_=tmp, func=AF.Exp, scale=-0.5), reads=[tmpb], writes=writes)


def host_consts():
    ident = np.eye(128, dtype=np.float32)
    bo32 = np.kron(np.eye(4, dtype=np.float32), np.ones((32, 32), np.float32))
    bo64 = np.kron(np.eye(2, dtype=np.float32), np.ones((64, 64), np.float32))
    Rda = np.zeros((128, 128), np.float32)
    for h in range(4):
        for d in range(16):
            Rda[32 * h + d, 32 * h + d + 16] = -1.0
            Rda[32 * h + d + 16, 32 * h + d] = 1.0
    Rmla = np.zeros((128, 128), np.float32)
    for h in range(2):
        for d in range(8):
            Rmla[64 * h + 32 + d, 64 * h + 40 + d] = -1.0
            Rmla[64 * h + 40 + d, 64 * h + 32 + d] = 1.0
    cst = np.concatenate([ident, bo32, bo64, Rda.T.copy(), Rmla.T.copy()], axis=1)
    def tables(rot_dim):
        rows = NLAT // 64
        row = np.repeat(np.arange(rows, dtype=np.float32), 64)
        col = np.tile(np.arange(64, dtype=np.float32), rows)
        n_freq = rot_dim // 4
        inv = (10000.0 ** (-np.arange(n_freq, dtype=np.float32) / n_freq)).astype(np.float32)
        ang = np.concatenate([row[:, None] * inv, col[:, None] * inv], axis=-1)
        return np.cos(ang).astype(np.float32), np.sin(ang).astype(np.float32)
    c, s = tables(32)
    da_cos = np.ones((128, NT), np.float32)
    da_sin = np.zeros((128, NT), np.float32)
    for h in range(4):
        for d in range(32):
            da_cos[32 * h + d, NCTX:] = c[:, d % 16]
            da_sin[32 * h + d, NCTX:] = s[:, d % 16]
    c, s = tables(16)
    ml_cos = np.ones((128, NT), np.float32)
    ml_sin = np.zeros((128, NT), np.float32)
    for h in range(2):
        for d in range(16):
            ml_cos[64 * h + 32 + d, NCTX:] = c[:, d % 8]
            ml_sin[64 * h + 32 + d, NCTX:] = s[:, d % 8]
    return dict(cst=cst, da_cos=da_cos, da_sin=da_sin, ml_cos=ml_cos, ml_sin=ml_sin)


def fm_cols(v, ntile):
    return np.ascontiguousarray(np.asarray(v, np.float32).reshape(ntile, 128).T)


def build(stages, dbg_names=()):
    nc = bass.Bass("TRN2", target_bir_lowering=False)
    k = K(nc)
    P = k.P
    top = ExitStack()
    xin = k.din("xin", [NT, D])
    c2T = k.din("c2T", [128, 8, 2])
    cst_d = k.din("cst", [128, 640])
    da_cos_d = k.din("da_cos", [128, NT]); da_sin_d = k.din("da_sin", [128, NT])
    ml_cos_d = k.din("ml_cos", [128, NT]); ml_sin_d = k.din("ml_sin", [128, NT])
    w_ada = k.din("w_ada", [DEPTH, D, 6 * D])
    bada_d = k.din("bada", [DEPTH, 128, 48])
    gmix_d = k.din("gmix", [DEPTH, 128, 8]); gffn_d = k.din("gffn", [DEPTH, 128, 8])
    w_da_qk = k.din("w_da_qk", [DEPTH, D, 768]); w_da_v = k.din("w_da_v", [DEPTH, D, 256])
    da_g_d = k.din("da_g", [DEPTH, 128, 2])
    da_lam_d = k.din("da_lam", [DEPTH, 128, 128])
    da_sub_d = k.din("da_sub", [DEPTH, 128, 64])
    w_mla_c = k.din("w_mla_c", [DEPTH, D, 384]); w_mla_kr = k.din("w_mla_kr", [DEPTH, D, 128])
    w_uq_d = k.din("w_uq", [DEPTH, 128, 2, 256]); w_ukvk_d = k.din("w_ukvk", [DEPTH, 128, 256]); w_ukvv_d = k.din("w_ukvv", [DEPTH, 128, 256])
    mla_gc_d = k.din("mla_gc", [DEPTH, 128, 3])
    mla_g_d = k.din("mla_g", [DEPTH, 128, 2])
    w_gates_d = k.din("w_gates", [DEPTH, 8, 128, 4096]); w_branch_d = k.din("w_branch", [DEPTH, 128, 8192]); w_out_d = k.din("w_out", [DEPTH, D, D])
    router_w_d = k.din("router_w", [D, 16]); router_b_d = k.din("router_b", [128, 16]); sel_d = k.din("sel", [16, 16, 128])
    exp_g_d = k.din("exp_w_gate", [DEPTH, 16, D, 512]); exp_u_d = k.din("exp_w_up", [DEPTH, 16, D, 512]); exp_d_d = k.din("exp_w_down", [DEPTH, 16, 512, D])
    w_s5_d = k.din("w_s5", [DEPTH, D, 256])
    s5B_d = k.din("s5B", [DEPTH, 2, 2, 128, 1024])
    s5C_d = k.din("s5C", [DEPTH, 2, 2, 128, 8, 128])
    s5tm_d = k.din("s5tm", [DEPTH, 2, 3, 128, 1024])
    s5fm_d = k.din("s5fm", [DEPTH, 2, 128, 3, 8])
    s5d_d = k.din("s5d", [DEPTH, 128, 2]); s5bg_d = k.din("s5bg", [DEPTH, 128, 2])
    w_glu_d = k.din("w_glu", [DEPTH, 256, 256])
    posc_d = k.din("posc", [2, 128, 2]); posr_d = k.din("posr", [2, 128, 128]); tri_d = k.din("tri", [2, 128, 128])
    w_rw_d = k.din("w_rw", [DEPTH, D, 1024]); rw_mu_d = k.din("rw_mu_row", [DEPTH, 128, 1024])
    rw_cols_d = k.din("rw_cols", [DEPTH, 128, 2, 9])
    rw_w0row_d = k.din("rw_w0row", [DEPTH, 2, 1, 256])
    rw_w1_d = k.din("rw_w1", [DEPTH, 2, 256, 32]); rw_w2_d = k.din("rw_w2", [DEPTH, 2, 32, 256])
    rw_a1_d = k.din("rw_a1", [DEPTH, 2, 256, 32]); rw_a2_d = k.din("rw_a2", [DEPTH, 2, 32, 256])
    rw_g1_d = k.din("rw_g1", [DEPTH, 256, 64]); rw_g2_d = k.din("rw_g2", [DEPTH, 64, 256])
    rw_tri_d = k.din("rw_tri", [2, 128, 2, 128]); rw_ind_d = k.din("rw_ind", [128, 2])
    rw_m1_d = k.din("rw_m1", [2, 128, 256]); rw_mnt_d = k.din("rw_mnt", [2, 128, 128])
    out_d = nc.dram_tensor("out", [NLAT, D], F32, kind="ExternalOutput").ap()
    dbg_out = {}

    xT, b_xT = k.sb(top, [128, 8, NT], F32, "xT")
    cstf, b_cstf = k.sb(top, [128, 640], F32, "cstf")
    cstb, b_cstb = k.sb(top, [128, 640], BF16, "cstb")
    onesb, b_ones = k.sb(top, [128, 128], BF16, "ones")
    k.eps_col, k.b_const = k.sb(top, [128, 1], F32, "eps")
    MOD, b_MOD = k.sb(top, [128, DEPTH, 6, 8, 2], F32, "MOD")
    AB, b_AB = k.sb(top, [128, DEPTH, 2, 2, 8, 2], F32, "AB")
    rstd_b, b_rstd = k.sb(top, [128, NT], F32, "rstd")
    yall, b_yall = k.sb(top, [128, 4, 2, NT], BF16, "yall")
    yT = []
    for i in range(4):
        yT.append((yall[:, i], Buf(f"y{i}")))
    k.memset("dve", k.eps_col[:], EPS, [k.b_const])
    k.memset("dve", onesb[:], 1.0, [b_ones])
    k.load("sp", cstf[:], cst_d, [b_cstf])
    k.copy("dve", cstb[:], cstf[:], [b_cstf], [b_cstb])
    identf = cstf[:, 0:128]
    identb = cstb[:, 0:128]
    bo32 = cstb[:, 128:256]; bo64 = cstb[:, 256:384]; Rda = cstb[:, 384:512]; Rmla = cstb[:, 512:640]

    def dump(name, ap_sb, buf, shape, dtype=F32):
        if name in dbg_names:
            d = nc.dram_tensor("dbg_" + name, list(shape), dtype, kind="ExternalOutput").ap()
            dbg_out[name] = d
            P.dma("sp", d, ap_sb, reads=[buf])
            P.barrier()

    with ExitStack() as st:
        xs = [k.sb(st, [128, D], F32, "xstage") for _ in range(2)]
        for tt in range(NTT):
            xt, xb = xs[tt % 2]
            k.load("sp", xt[:], xin[tt * 128:(tt + 1) * 128, :], [xb])
            for half in range(2):
                bk = k.bank()
                for j in range(4):
                    kt = half * 4 + j
                    P.op("pe", lambda e: e.transpose(k.ps[bk][:, j * 128:(j + 1) * 128], xt[:, kt * 128:(kt + 1) * 128], identf),
                         reads=[xb, b_cstf], writes=[k.pb[bk]], inc=(j == 3))
                k.copy("dve" if half == 0 else "act", xT[:, half * 4:half * 4 + 4, tt * 128:(tt + 1) * 128],
                       k.ps[bk][:].rearrange("p (j t) -> p j t", j=4), [k.pb[bk]], [b_xT])
        scT, b_scT = k.sb(st, [128, 8, 2], F32, "scT")
        scb, b_scb = k.sb(st, [128, 8, 2], BF16, "scb")
        bada, b_bada = k.sb(st, [128, DEPTH, 48], F32, "bada")
        gm, b_gm = k.sb(st, [128, DEPTH, 2, 8], F32, "gm")
        k.load("sp", scT[:], c2T, [b_scT])
        k.act(scb[:], scT[:], AF.Silu, [b_scT], [b_scb])
        for l in range(DEPTH):
            k.load("sp", bada[:, l, :], bada_d[l], [b_bada])
            k.load("sp", gm[:, l, 0, :], gmix_d[l], [b_gm])
            k.load("sp", gm[:, l, 1, :], gffn_d[l], [b_gm])
        wa = [k.sb(st, [128, 8, 1024], BF16, "wada") for _ in range(2)]
        ci = 0
        for l in range(DEPTH):
            for ch in range(6):
                wt, wb = wa[ci % 2]; ci += 1
                k.load("pool", wt[:], w_ada[l, :, ch * 1024:(ch + 1) * 1024].rearrange("(kt p) n -> p kt n", p=128), [wb])
                bk = k.bank()
                for ft in range(8):
                    for kt in range(8):
                        k.mm(bk, k.ps[bk][:, ft * 2:ft * 2 + 2], wt[:, kt, ft * 128:(ft + 1) * 128], scb[:, kt, :],
                             kt == 0, kt == 7, [wb, b_scb])
                k.tt("dve", MOD[:, l, ch, :, :], k.ps[bk][:, 0:16].rearrange("p (f j) -> p f j", j=2),
                     bada[:, l, ch * 8:(ch + 1) * 8].unsqueeze(2).to_broadcast([128, 8, 2]), ALU.add,
                     [k.pb[bk], b_bada], [b_MOD])
        for l in range(DEPTH):
            for m in range(2):
                sh, sc = (0, 1) if m == 0 else (3, 4)
                P.op("dve", lambda e: e.scalar_tensor_tensor(out=AB[:, l, m, 0, :, :], in0=MOD[:, l, sc, :, :], scalar=1.0,
                                                             in1=gm[:, l, m, :].unsqueeze(2).to_broadcast([128, 8, 2]),
                                                             op0=ALU.add, op1=ALU.mult),
                     reads=[b_MOD, b_gm], writes=[b_AB])
                k.copy("dve", AB[:, l, m, 1, :, :], MOD[:, l, sh, :, :], [b_MOD], [b_AB])
        P.barrier()
    dump("xT", xT[:], b_xT, [128, 8, NT])
    dump("MOD", MOD[:], b_MOD, [128, DEPTH, 6, 8, 2])

    def jof(g):
        return 1 if g == 0 else 0

    def rms_stats(st):
        sq = [k.sb(st, [128, 512], BF16, "sq") for _ in range(2)]
        lnt, b_lnt = k.sb(st, [128, 512], F32, "lnt")
        i = 0
        for (c0, n) in GROUPS:
            bk = k.bank()
            for kt in range(8):
                s, sbf = sq[i % 2]; i += 1
                k.act(s[:, :n], xT[:, kt, c0:c0 + n], AF.Square, [b_xT], [sbf])
                k.mm(bk, k.ps[bk][:, :n], onesb[:], s[:, :n], kt == 0, kt == 7, [sbf, b_ones])
            k.rstd_from_ss(rstd_b[:, c0:c0 + n], k.ps[bk][:, :n], bk, 1.0 / D, lnt[:, :n], b_lnt, [b_rstd])

    def h_group(l, m, g, hg, hb, tmp_, tmpb_):
        c0, n = GROUPS[g]
        j = jof(g)
        for kt in range(8):
            if isinstance(tmp_, list):
                tmp, tmpb = tmp_[kt % 2]
            else:
                tmp, tmpb = tmp_, tmpb_
            k.tt("dve", tmp[:, :n], xT[:, kt, c0:c0 + n], rstd_b[:, c0:c0 + n], ALU.mult, [b_xT, b_rstd], [tmpb])
            P.op("act", lambda e: e.activation(out=hg[:, kt, :n], in_=tmp[:, :n], func=AF.Identity,
                                               scale=AB[:, l, m, 0, kt, j:j + 1], bias=AB[:, l, m, 1, kt, j:j + 1]),
                 reads=[tmpb, b_AB], writes=[hb])

    def load_w(q, st, src, ncols, name):
        wt, wb = k.sb(st, [128, 8, ncols], BF16, name)
        k.load(q, wt[:], src.rearrange("(kt p) n -> p kt n", p=128), [wb])
        return wt, wb

    def proj(bk, wt, wb, col0, hg, hb, n, start=True, stop=True):
        for kt in range(8):
            k.mm(bk, k.ps[bk][:, :n], wt[:, kt, col0:col0 + 128], hg[:, kt, :n], start and kt == 0, stop and kt == 7, [wb, hb])

    def headnorm_rope(st, src_bk, n, c0, blockones, inv_dim, gcol, gbuf, Rm, cos_d, sin_d, dst, dstb, scr):
        sq, b_sq, rs, b_rs, lnt, b_lnt, qn, b_qn, ct, b_ct, sn, b_sn, t1, b_t1, t2, b_t2 = scr
        src = k.ps[src_bk][:, :n]
        k.act(sq[:, :n], src, AF.Square, [k.pb[src_bk]], [b_sq])
        b2 = k.bank()
        k.mm(b2, k.ps[b2][:, :n], blockones, sq[:, :n], True, True, [b_sq, b_cstb])
        k.rstd_from_ss(rs[:, :n], k.ps[b2][:, :n], b2, inv_dim, lnt[:, :n], b_lnt, [b_rs])
        k.stt(qn[:, :n], src, gcol, rs[:, :n], ALU.mult, ALU.mult, [k.pb[src_bk], gbuf, b_rs], [b_qn])
        k.load("sp", ct[:, :n], cos_d[:, c0:c0 + n], [b_ct])
        k.load("sp", sn[:, :n], sin_d[:, c0:c0 + n], [b_sn])
        b3 = k.bank()
        k.mm(b3, k.ps[b3][:, :n], Rm, qn[:, :n], True, True, [b_qn, b_cstb])
        k.tt("pool", t1[:, :n], qn[:, :n], ct[:, :n], ALU.mult, [b_qn, b_ct], [b_t1])
        k.tt("dve", t2[:, :n], k.ps[b3][:, :n], sn[:, :n], ALU.mult, [k.pb[b3], b_sn], [b_t2])
        k.tt("pool", dst, t1[:, :n], t2[:, :n], ALU.add, [b_t1, b_t2], [dstb])

    def norm_scratch(st):
        out = []
        for nm, dt in (("sq", BF16), ("rs", F32), ("lnt", F32), ("qn", BF16), ("ct", F32), ("sn", F32), ("t1", F32), ("t2", F32)):
            t, b = k.sb(st, [128, 512], dt, nm)
            out += [t, b]
        return out

    def attention(st, qT, b_q, kT, b_k, V, b_V, heads, scale, epilogue, skip_ctx=False):
        Et = [k.sb(st, [128, 512], BF16, "E") for _ in range(4)]
        sbanks = [0, 1, 2, 3]
        obanks = [4, 5, 6, 7]
        assert len(heads) % 2 == 0
        steps = []
        for g, (c0, n) in enumerate(GROUPS):
            if skip_ctx and g == 0:
                continue
            ktiles = [0, 1] if g == 0 else list(range(NTT))
            for hp in range(len(heads) // 2):
                for ki, kt in enumerate(ktiles):
                    steps.append((g, c0, n, hp, ki, kt, len(ktiles)))

        def qk_exp(si):
            g, c0, n, hp, ki, kt, nk = steps[si]
            outs = []
            for m in range(2):
                tile, pb, Kd, vh = heads[2 * hp + m]
                sbk = sbanks[(2 * si + m) % 4]
                k.mm(sbk, k.ps[sbk][:, :n], kT[pb:pb + Kd, tile, kt * 128:(kt + 1) * 128], qT[pb:pb + Kd, tile, c0:c0 + n],
                     True, True, [b_k, b_q])
                outs.append(sbk)
            res = []
            for m in range(2):
                sbk = outs[m]
                E, Eb = Et[(2 * si + m) % 4]
                k.act(E[:, :n], k.ps[sbk][:, :n], AF.Exp, [k.pb[sbk]], [Eb], scale=scale)
                res.append((E, Eb))
            return res

        oi = 0
        obs = None
        nxt = qk_exp(0)
        for si, (g, c0, n, hp, ki, kt, nk) in enumerate(steps):
            cur = nxt
            if si + 1 < len(steps):
                nxt = qk_exp(si + 1)
            nq = n // 128
            if ki == 0:
                obs = [obanks[(2 * oi) % 4], obanks[(2 * oi + 1) % 4]]; oi += 1
            for m in range(2):
                E, Eb = cur[m]
                vh = heads[2 * hp + m][3]
                ob = obs[m]
                for qi in range(nq):
                    P.op("pe", lambda e: e.matmul(k.ps[ob][:, qi * 65:(qi + 1) * 65], lhsT=E[:, qi * 128:(qi + 1) * 128],
                                                  rhs=V[:, kt, vh, :], start=(ki == 0 and qi == 0), stop=(ki == nk - 1),
                                                  skip_group_check=True),
                         reads=[Eb, b_V], writes=[k.pb[ob]], inc=(qi == nq - 1))
            if ki == nk - 1:
                for m in range(2):
                    epilogue(g, c0, nq, 2 * hp + m, obs[m])

    def transpose_out(ytok, b_ytok, nq, c0, ydst, b_ydst):
        for qi in range(nq):
            bk = k.bank()
            pv = k.ps[bk][:].bitcast(BF16)
            for tile in range(2):
                P.op("pe", lambda e: e.transpose(pv[:, tile * 128:(tile + 1) * 128], ytok[:, qi, tile * 128:(tile + 1) * 128], identb),
                     reads=[b_ytok, b_cstb], writes=[k.pb[bk]], inc=(tile == 1))
            k.copy("dve", ydst[:, :, c0 + qi * 128:c0 + (qi + 1) * 128], pv[:, 0:256].rearrange("p (j t) -> p j t", j=2),
                   [k.pb[bk]], [b_ydst])

    def da_mixer(l):
        lam_init = 0.8 - 0.6 * math.exp(-0.3 * l)
        ydst, b_ydst = yT[0]
        with ExitStack() as st:
            qT, b_q = k.sb(st, [128, 3, NT], BF16, "daq")
            kT, b_k = k.sb(st, [128, 3, NT], BF16, "dak")
            V, b_V = k.sb(st, [128, NTT, 4, 65], BF16, "dav")
            gcol, b_g = k.sb(st, [128, 2], F32, "dag")
            lam, b_lam = k.sb(st, [128, 128], F32, "dalam")
            lt, b_lt = k.sb(st, [128, 8], F32, "dalt")
            gsub, b_gsub = k.sb(st, [128, 64], F32, "dagsub")
            k.load("sp", gcol[:], da_g_d[l], [b_g])
            k.load("sp", lam[:], da_lam_d[l], [b_lam])
            k.load("sp", gsub[:], da_sub_d[l], [b_gsub])
            k.ts("dve", gsub[:], gsub[:], 1.0 - lam_init, None, ALU.mult, None, [b_gsub], [b_gsub])
            k.tt("dve", lam[:, 0:32], lam[:, 0:32], lam[:, 32:64], ALU.mult, [b_lam], [b_lam])
            k.tt("dve", lam[:, 64:96], lam[:, 64:96], lam[:, 96:128], ALU.mult, [b_lam], [b_lam])
            P.op("dve", lambda e: e.tensor_reduce(out=lt[:, 0:1], in_=lam[:, 0:32], axis=AX.X, op=ALU.add), reads=[b_lam], writes=[b_lt])
            P.op("dve", lambda e: e.tensor_reduce(out=lt[:, 1:2], in_=lam[:, 64:96], axis=AX.X, op=ALU.add), reads=[b_lam], writes=[b_lt])
            k.act(lt[:, 2:4], lt[:, 0:2], AF.Exp, [b_lt], [b_lt])
            k.tt("dve", lt[:, 4:5], lt[:, 3:4], lt[:, 2:3], ALU.subtract, [b_lt], [b_lt])
            k.ts("dve", lt[:, 4:5], lt[:, 4:5], -lam_init, None, ALU.add, None, [b_lt], [b_lt])
            k.memset("pool", V[:, :, :, 64:65], 1.0, [b_V])
            with ExitStack() as s2:
                wqk, b_wqk = load_w("pool", s2, w_da_qk[l], 768, "wqk")
                wv, b_wv = load_w("pool", s2, w_da_v[l], 256, "wv")
                hgs = [k.sb(s2, [128, 8, 512], BF16, "hg") for _ in range(2)]
                tmp, tmpb = k.sb(s2, [128, 512], F32, "htmp")
                scr = norm_scratch(s2)
                for g, (c0, n) in enumerate(GROUPS):
                    hg, hb = hgs[g % 2]
                    h_group(l, 0, g, hg, hb, tmp, tmpb)
                    for ti in range(6):
                        bk = k.bank()
                        proj(bk, wqk, b_wqk, ti * 128, hg, hb, n)
                        dst = (qT if ti < 3 else kT)
                        dstb = (b_q if ti < 3 else b_k)
                        headnorm_rope(s2, bk, n, c0, bo32, 1.0 / 32, gcol[:, (ti // 3):(ti // 3) + 1], b_g, Rda, da_cos_d, da_sin_d,
                                      dst[:, ti % 3, c0:c0 + n], dstb, scr)
                    for qi in range(n // 128):
                        tt_ = (c0 + qi * 128) // 128
                        bk = k.bank()
                        for kt in range(8):
                            k.mm(bk, k.ps[bk][:, 0:256], hg[:, kt, qi * 128:(qi + 1) * 128], wv[:, kt, :], kt == 0, kt == 7, [hb, b_wv])
                        k.copy("act", V[:, tt_, :, 0:64], k.ps[bk][:, 0:256].rearrange("p (h d) -> p h d", h=4), [k.pb[bk]], [b_V])
                P.barrier()
            dump("da_q", qT[:], b_q, [128, 3, NT], BF16)
            dump("da_k", kT[:], b_k, [128, 3, NT], BF16)
            dump("da_v", V[:], b_V, [128, NTT, 4, 65], BF16)
            with ExitStack() as s3:
                ytok, b_ytok = k.sb(s3, [128, 4, 256], BF16, "ytok")
                o0, b_o0 = k.sb(s3, [128, 4, 64], F32, "o0")
                dd, b_dd = k.sb(s3, [128, 4, 64], F32, "dd")
                junk, b_junk = k.sb(s3, [128, 64], F32, "junk")
                rc, b_rc = k.sb(s3, [128, 4, 4], F32, "rc")
                lnt, b_lnt = k.sb(s3, [128, 4], F32, "lnt2")
                state = {}

                def epi(g, c0, nq, hidx, ob):
                    h, m = hidx // 2, hidx % 2
                    if m == 0:
                        state["ob0"] = ob
                        return
                    ob0 = state["ob0"]
                    O0 = k.ps[ob0][:, 0:nq * 65].rearrange("p (q c) -> p q c", c=65)
                    O1 = k.ps[ob][:, 0:nq * 65].rearrange("p (q c) -> p q c", c=65)
                    P.op("dve", lambda e: e.reciprocal(out=rc[:, 0, 0:nq], in_=O0[:, :, 64]), reads=[k.pb[ob0]], writes=[b_rc])
                    P.op("dve", lambda e: e.reciprocal(out=rc[:, 1, 0:nq], in_=O1[:, :, 64]), reads=[k.pb[ob]], writes=[b_rc])
                    k.ts("dve", rc[:, 1, 0:nq], rc[:, 1, 0:nq], lt[:, 4:5], None, ALU.mult, None, [b_rc, b_lt], [b_rc])
                    for qi in range(nq):
                        k.ts("dve", o0[:, qi, :], O0[:, qi, 0:64], rc[:, 0, qi:qi + 1], None, ALU.mult, None, [k.pb[ob0], b_rc], [b_o0])
                        k.stt(dd[:, qi, :], O1[:, qi, 0:64], rc[:, 1, qi:qi + 1], o0[:, qi, :], ALU.mult, ALU.add,
                              [k.pb[ob], b_rc, b_o0], [b_dd])
                        P.op("act", lambda e: e.activation(out=junk[:], in_=dd[:, qi, :], func=AF.Square, accum_out=rc[:, 2, qi:qi + 1]),
                             reads=[b_dd], writes=[b_junk, b_rc])
                    P.op("act", lambda e: e.activation(out=lnt[:, 0:nq], in_=rc[:, 2, 0:nq], func=AF.Ln, scale=1.0 / 64, bias=k.eps_col[:]),
                         reads=[b_rc, k.b_const], writes=[b_lnt])
                    k.act(rc[:, 3, 0:nq], lnt[:, 0:nq], AF.Exp, [b_lnt], [b_rc], scale=-0.5)
                    for qi in range(nq):
                        k.stt(ytok[:, qi, h * 64:(h + 1) * 64], dd[:, qi, :], rc[:, 3, qi:qi + 1], gsub[:], ALU.mult, ALU.mult,
                              [b_dd, b_rc, b_gsub], [b_ytok])
                    if h == 3:
                        k.rrset = [0, 1, 2, 3]
                        transpose_out(ytok, b_ytok, nq, c0, ydst, b_ydst)
                        k.rrset = list(range(8))

                heads = [(j // 3, 32 * (j % 3), 32, j // 2) for j in range(8)]
                attention(s3, qT, b_q, kT, b_k, V, b_V, heads, 32 ** -0.5, epi, skip_ctx=(l == DEPTH - 1))
                P.barrier()
        dump("ya", ydst[:], b_ydst, [128, 2, NT], BF16)

    def mla_mixer(l):
        ydst, b_ydst = yT[2]
        with ExitStack() as st:
            qT, b_q = k.sb(st, [128, 2, NT], BF16, "mlq")
            kT, b_k = k.sb(st, [128, 2, NT], BF16, "mlk")
            V, b_V = k.sb(st, [128, NTT, 4, 65], BF16, "mlv")
            gcol, b_g = k.sb(st, [128, 2], F32, "mlg")
            gc, b_gc = k.sb(st, [128, 3], F32, "mlgc")
            k.load("sp", gcol[:], mla_g_d[l], [b_g])
            k.load("sp", gc[:], mla_gc_d[l], [b_gc])
            k.memset("pool", V[:, :, :, 64:65], 1.0, [b_V])
            with ExitStack() as s2:
                wc, b_wc = load_w("pool", s2, w_mla_c[l], 384, "wc")
                wkr, b_wkr = load_w("pool", s2, w_mla_kr[l], 128, "wkr")
                wf, b_wf = k.sb(s2, [128, 4, 256], F32, "wf")
                wuq, b_wuq = k.sb(s2, [128, 2, 256], BF16, "wuq")
                wkk, b_wkk = k.sb(s2, [128, 256], BF16, "wkk")
                wvv, b_wvv = k.sb(s2, [128, 256], BF16, "wvv")
                k.load("sp", wf[:, 0:2, :], w_uq_d[l], [b_wf])
                k.load("sp", wf[:, 2, :], w_ukvk_d[l], [b_wf])
                k.load("sp", wf[:, 3, :], w_ukvv_d[l], [b_wf])
                for kt in range(2):
                    k.ts("dve", wuq[:, kt, :], wf[:, kt, :], gc[:, kt:kt + 1], None, ALU.mult, None, [b_wf, b_gc], [b_wuq])
                k.ts("dve", wkk[:], wf[:, 2, :], gc[:, 2:3], None, ALU.mult, None, [b_wf, b_gc], [b_wkk])
                k.ts("dve", wvv[:], wf[:, 3, :], gc[:, 2:3], None, ALU.mult, None, [b_wf, b_gc], [b_wvv])
                hgs = [k.sb(s2, [128, 8, 512], BF16, "hg") for _ in range(2)]
                tmp, tmpb = [k.sb(s2, [128, 512], F32, "htmp") for _ in range(2)], None
                scr = norm_scratch(s2)
                sq2, b_sq2 = k.sb(s2, [128, 2, 512], BF16, "sq2")
                rsq, b_rsq = k.sb(s2, [128, 512], F32, "rsq")
                lnq, b_lnq = k.sb(s2, [128, 512], F32, "lnq")
                cqn, b_cqn = k.sb(s2, [128, 2, 512], BF16, "cqn")
                ckvn, b_ckvn = k.sb(s2, [128, 512], BF16, "ckvn")
                for g, (c0, n) in enumerate(GROUPS):
                    hg, hb = hgs[g % 2]
                    h_group(l, 0, g, hg, hb, tmp, tmpb)
                    bA = k.bank(); proj(bA, wc, b_wc, 0, hg, hb, n)
                    bB = k.bank(); proj(bB, wc, b_wc, 128, hg, hb, n)
                    k.act(sq2[:, 0, :n], k.ps[bA][:, :n], AF.Square, [k.pb[bA]], [b_sq2])
                    k.act(sq2[:, 1, :n], k.ps[bB][:, :n], AF.Square, [k.pb[bB]], [b_sq2])
                    bS = k.bank()
                    k.mm(bS, k.ps[bS][:, :n], onesb[:], sq2[:, 0, :n], True, False, [b_sq2, b_ones])
                    k.mm(bS, k.ps[bS][:, :n], onesb[:], sq2[:, 1, :n], False, True, [b_sq2, b_ones])
                    k.rstd_from_ss(rsq[:, :n], k.ps[bS][:, :n], bS, 1.0 / 192, lnq[:, :n], b_lnq, [b_rsq])
                    k.tt("dve", cqn[:, 0, :n], k.ps[bA][:, :n], rsq[:, :n], ALU.mult, [k.pb[bA], b_rsq], [b_cqn])
                    k.tt("dve", cqn[:, 1, :n], k.ps[bB][:, :n], rsq[:, :n], ALU.mult, [k.pb[bB], b_rsq], [b_cqn])
                    bC = k.bank(); proj(bC, wc, b_wc, 256, hg, hb, n)
                    k.act(sq2[:, 0, :n], k.ps[bC][:, :n], AF.Square, [k.pb[bC]], [b_sq2])
                    bS = k.bank()
                    k.mm(bS, k.ps[bS][:, :n], onesb[:], sq2[:, 0, :n], True, True, [b_sq2, b_ones])
                    k.rstd_from_ss(rsq[:, :n], k.ps[bS][:, :n], bS, 1.0 / 128, lnq[:, :n], b_lnq, [b_rsq])
                    k.tt("dve", ckvn[:, :n], k.ps[bC][:, :n], rsq[:, :n], ALU.mult, [k.pb[bC], b_rsq], [b_ckvn])
                    for tile in range(2):
                        bk = k.bank()
                        k.mm(bk, k.ps[bk][:, :n], wuq[:, 0, tile * 128:(tile + 1) * 128], cqn[:, 0, :n], True, False, [b_wuq, b_cqn])
                        k.mm(bk, k.ps[bk][:, :n], wuq[0:64, 1, tile * 128:(tile + 1) * 128], cqn[0:64, 1, :n], False, True, [b_wuq, b_cqn])
                        headnorm_rope(s2, bk, n, c0, bo64, 1.0 / 48, gcol[:, 0:1], b_g, Rmla, ml_cos_d, ml_sin_d,
                                      qT[:, tile, c0:c0 + n], b_q, scr)
                    for tile in range(2):
                        bk = k.bank()
                        k.mm(bk, k.ps[bk][:, :n], wkk[:, tile * 128:(tile + 1) * 128], ckvn[:, :n], True, False, [b_wkk, b_ckvn])
                        for kt in range(8):
                            k.mm(bk, k.ps[bk][:, :n], wkr[:, kt, :], hg[:, kt, :n], False, kt == 7, [b_wkr, hb])
                        headnorm_rope(s2, bk, n, c0, bo64, 1.0 / 48, gcol[:, 1:2], b_g, Rmla, ml_cos_d, ml_sin_d,
                                      kT[:, tile, c0:c0 + n], b_k, scr)
                    for qi in range(n // 128):
                        tt_ = (c0 + qi * 128) // 128
                        bk = k.bank()
                        k.mm(bk, k.ps[bk][:, 0:256], ckvn[:, qi * 128:(qi + 1) * 128], wvv[:], True, True, [b_ckvn, b_wvv])
                        k.copy("act", V[:, tt_, :, 0:64], k.ps[bk][:, 0:256].rearrange("p (h d) -> p h d", h=4), [k.pb[bk]], [b_V])
                P.barrier()
            dump("ml_q", qT[:], b_q, [128, 2, NT], BF16)
            dump("ml_k", kT[:], b_k, [128, 2, NT], BF16)
            with ExitStack() as s3:
                ytok, b_ytok = k.sb(s3, [128, 4, 256], BF16, "ytok")
                rc, b_rc = k.sb(s3, [128, 4], F32, "rc")

                def epi(g, c0, nq, h, ob):
                    O = k.ps[ob][:, 0:nq * 65].rearrange("p (q c) -> p q c", c=65)
                    P.op("dve", lambda e: e.reciprocal(out=rc[:, 0:nq], in_=O[:, :, 64]), reads=[k.pb[ob]], writes=[b_rc])
                    for qi in range(nq):
                        k.ts("dve", ytok[:, qi, h * 64:(h + 1) * 64], O[:, qi, 0:64], rc[:, qi:qi + 1], None, ALU.mult, None,
                             [k.pb[ob], b_rc], [b_ytok])
                    if h == 3:
                        k.rrset = [0, 1, 2, 3]
                        transpose_out(ytok, b_ytok, nq, c0, ydst, b_ydst)
                        k.rrset = list(range(8))

                heads = [(h // 2, 64 * (h % 2), 48, h) for h in range(4)]
                attention(s3, qT, b_q, kT, b_k, V, b_V, heads, 48 ** -0.5, epi, skip_ctx=(l == DEPTH - 1))
                P.barrier()
        dump("yc", ydst[:], b_ydst, [128, 2, NT], BF16)

    def merge(l):
        with ExitStack() as st:
            wbr, b_wbr = k.sb(st, [128, 2, 4, D], BF16, "wbr")
            wout, b_wout = k.sb(st, [128, 8, D], BF16, "wout")
            wgs = [k.sb(st, [128, 8, 4, 128], BF16, "wg") for _ in range(2)]
            hgs = [k.sb(st, [128, 8, 512], BF16, "hg") for _ in range(2)]
            tmp, tmpb = k.sb(st, [128, 512], F32, "htmp")
            sig = [k.sb(st, [128, 512], F32, "sig") for _ in range(2)]
            prod = [k.sb(st, [128, 512], F32, "prod") for _ in range(2)]
            macc, b_macc = k.sb(st, [128, 512], F32, "macc")
            mbf, b_mbf = k.sb(st, [128, 8, 512], BF16, "mbf")
            wi = 0; si = 0
            for g, (c0, n) in enumerate(GROUPS):
                if l == DEPTH - 1 and g == 0:
                    continue
                j = jof(g)
                hg, hb = hgs[g % 2]
                h_group(l, 0, g, hg, hb, tmp, tmpb)
                for ct in range(8):
                    wg, b_wg = wgs[wi % 2]; wi += 1
                    k.load("pool", wg[:].rearrange("p a b c -> p (a b c)"), w_gates_d[l, ct], [b_wg])
                    if wi == 1:
                        k.load("pool", wbr[:].rearrange("p a b c -> p (a b c)"), w_branch_d[l], [b_wbr])
                    if wi == 3:
                        k.load("pool", wout[:], w_out_d[l].rearrange("(kt p) n -> p kt n", p=128), [b_wout])
                    for i in range(4):
                        bg = k.bank()
                        for kt in range(8):
                            k.mm(bg, k.ps[bg][:, :n], wg[:, kt, i, :], hg[:, kt, :n], kt == 0, kt == 7, [b_wg, hb])
                        sg, b_sg = sig[si % 2]; pr, b_pr = prod[si % 2]; si += 1
                        k.act(sg[:, :n], k.ps[bg][:, :n], AF.Sigmoid, [k.pb[bg]], [b_sg])
                        bp = k.bank()
                        for kt2 in range(2):
                            k.mm(bp, k.ps[bp][:, :n], wbr[:, kt2, i, ct * 128:(ct + 1) * 128], yT[i][0][:, kt2, c0:c0 + n],
                                 kt2 == 0, kt2 == 1, [b_wbr, yT[i][1]])
                        if i == 0:
                            k.tt("dve", macc[:, :n], k.ps[bp][:, :n], sg[:, :n], ALU.mult, [k.pb[bp], b_sg], [b_macc])
                        else:
                            k.tt("dve", pr[:, :n], k.ps[bp][:, :n], sg[:, :n], ALU.mult, [k.pb[bp], b_sg], [b_pr])
                            if i < 3:
                                k.tt("dve", macc[:, :n], macc[:, :n], pr[:, :n], ALU.add, [b_macc, b_pr], [b_macc])
                            else:
                                k.tt("dve", mbf[:, ct, :n], macc[:, :n], pr[:, :n], ALU.add, [b_macc, b_pr], [b_mbf])
                for co in range(8):
                    bo = k.bank()
                    for ct in range(8):
                        k.mm(bo, k.ps[bo][:, :n], wout[:, ct, co * 128:(co + 1) * 128], mbf[:, ct, :n], ct == 0, ct == 7, [b_wout, b_mbf])
                    k.stt(xT[:, co, c0:c0 + n], k.ps[bo][:, :n], MOD[:, l, 2, co, j:j + 1], xT[:, co, c0:c0 + n], ALU.mult, ALU.add,
                          [k.pb[bo], b_MOD, b_xT], [b_xT])
            P.barrier()
        dump("xmix", xT[:], b_xT, [128, 8, NT])

    def moe(l):
        fT = yall[:].rearrange("p i k t -> p (i k) t")
        b_fT = Buf("fT")
        with ExitStack() as st:
            rms_stats(st)
            P.barrier()
        with ExitStack() as st:
            combT, b_comb = k.sb(st, [16, NT], BF16, "combT")
            selb, b_sel = k.sb(st, [16, 16, 128], BF16, "selb")
            k.load("pool", selb[:], sel_d, [b_sel])
            wgs = [k.sb(st, [128, 8, 512], BF16, "ewg") for _ in range(2)]
            wus = [k.sb(st, [128, 8, 512], BF16, "ewu") for _ in range(2)]
            wds = [k.sb(st, [128, 4, D], BF16, "ewd") for _ in range(2)]

            def load_expert(e_):
                wg, b_wg = wgs[e_ % 2]; wu, b_wu = wus[e_ % 2]; wd, b_wd = wds[e_ % 2]
                k.load("pool", wg[:], exp_g_d[l, e_].rearrange("(kt p) n -> p kt n", p=128), [b_wg])
                k.load("pool", wu[:], exp_u_d[l, e_].rearrange("(kt p) n -> p kt n", p=128), [b_wu])
                k.load("pool", wd[:], exp_d_d[l, e_].rearrange("(kt p) n -> p kt n", p=128), [b_wd])
            load_expert(0)
            with ExitStack() as s2:
                tmp, tmpb = k.sb(s2, [128, 512], F32, "htmp")
                f32s = [k.sb(s2, [128, 512], F32, "f32") for _ in range(2)]
                rw, b_rw = k.sb(s2, [128, 8, 16], F32, "rw")
                k.load("sp", rw[:], router_w_d.rearrange("(kt p) n -> p kt n", p=128), [b_rw])
                rb, b_rb = k.sb(s2, [128, 16], F32, "rb")
                k.load("sp", rb[:], router_b_d, [b_rb])
                lg, b_lg = k.sb(s2, [128, NTT, 16], F32, "lg")
                fi_ = 0
                skip0 = (l == DEPTH - 1)
                if skip0:
                    k.memset("dve", lg[:, 0:2, :], 0.0, [b_lg])
                for g in range(5):
                    if skip0 and g == 0:
                        continue
                    c0, n = GROUPS[g]
                    j = jof(g)
                    nq = n // 128
                    bl = k.bank()
                    for kt in range(8):
                        k.tt("dve", tmp[:, :n], xT[:, kt, c0:c0 + n], rstd_b[:, c0:c0 + n], ALU.mult, [b_xT, b_rstd], [tmpb])
                        P.op("act", lambda e: e.activation(out=fT[:, kt, c0:c0 + n], in_=tmp[:, :n], func=AF.Identity,
                                                           scale=AB[:, l, 1, 0, kt, j:j + 1], bias=AB[:, l, 1, 1, kt, j:j + 1]),
                             reads=[tmpb, b_AB], writes=[b_fT])
                        f32, b_f32 = f32s[fi_ % 2]; fi_ += 1
                        k.ts("pool", f32[:, :n], tmp[:, :n], AB[:, l, 1, 0, kt, j:j + 1], AB[:, l, 1, 1, kt, j:j + 1], ALU.mult, ALU.add,
                             [tmpb, b_AB], [b_f32])
                        for qi in range(nq):
                            P.op("pe", lambda e: e.matmul(k.ps[bl][:, qi * 16:(qi + 1) * 16], lhsT=f32[:, qi * 128:(qi + 1) * 128], rhs=rw[:, kt, :],
                                                          start=(kt == 0 and qi == 0), stop=(kt == 7), skip_group_check=True),
                                 reads=[b_f32, b_rw], writes=[k.pb[bl]], inc=(qi == nq - 1))
                    tt0 = c0 // 128
                    k.copy("dve", lg[:, tt0:tt0 + nq, :], k.ps[bl][:, 0:nq * 16].rearrange("p (q e) -> p q e", e=16), [k.pb[bl]], [b_lg])
                r = {}
                NR = NTT * 16
                for nm, wd_ in (("sc", NR), ("bi", NR), ("m1", NR // 4), ("eq", NR), ("bi2", NR), ("m2", NR // 4),
                                ("gs", NR // 4), ("gm", NTT), ("gsel", NR // 4), ("sel", NR), ("w", NR), ("ws", NTT), ("cmb", NR)):
                    r[nm] = k.sb(s2, [128, wd_], F32, "r_" + nm)
                v4 = lambda ap: ap.rearrange("p (g e) -> p g e", e=4)
                b4 = lambda ap: ap.unsqueeze(2).to_broadcast([128, NR // 4, 4])
                sc, b_sc = r["sc"]; bi, b_bi = r["bi"]; m1, b_m1 = r["m1"]; eq, b_eq = r["eq"]; bi2, b_bi2 = r["bi2"]
                m2, b_m2 = r["m2"]; gs, b_gs = r["gs"]; gm_, b_gm_ = r["gm"]; gsel, b_gsel = r["gsel"]; sel, b_sl = r["sel"]
                w_, b_w = r["w"]; ws, b_ws = r["ws"]; cmb, b_cmb = r["cmb"]
                k.act(sc[:], lg[:].rearrange("p t e -> p (t e)"), AF.Sigmoid, [b_lg], [b_sc])
                k.tt("dve", sc[:].rearrange("p (t e) -> p t e", e=16) if False else bi[:].rearrange("p (t e) -> p t e", e=16),
                     sc[:].rearrange("p (t e) -> p t e", e=16), rb[:].unsqueeze(1).to_broadcast([128, NTT, 16]), ALU.add, [b_sc, b_rb], [b_bi])
                P.op("dve", lambda e: e.tensor_reduce(out=m1[:], in_=v4(bi[:]), axis=AX.X, op=ALU.max), reads=[b_bi], writes=[b_m1])
                k.tt("dve", v4(eq[:]), v4(bi[:]), b4(m1[:]), ALU.is_equal, [b_bi, b_m1], [b_eq])
                k.stt(bi2[:], eq[:], -1e9, bi[:], ALU.mult, ALU.add, [b_eq, b_bi], [b_bi2])
                P.op("dve", lambda e: e.tensor_reduce(out=m2[:], in_=v4(bi2[:]), axis=AX.X, op=ALU.max), reads=[b_bi2], writes=[b_m2])
                k.tt("dve", gs[:], m1[:], m2[:], ALU.add, [b_m1, b_m2], [b_gs])
                P.op("dve", lambda e: e.tensor_reduce(out=gm_[:], in_=v4(gs[:]), axis=AX.X, op=ALU.max), reads=[b_gs], writes=[b_gm_])
                k.tt("dve", v4(gsel[:]), v4(gs[:]), gm_[:].unsqueeze(2).to_broadcast([128, NTT, 4]), ALU.is_equal, [b_gs, b_gm_], [b_gsel])
                k.tt("dve", v4(sel[:]), v4(bi[:]), b4(m2[:]), ALU.is_ge, [b_bi, b_m2], [b_sl])
                k.tt("dve", v4(sel[:]), v4(sel[:]), b4(gsel[:]), ALU.mult, [b_sl, b_gsel], [b_sl])
                k.tt("dve", w_[:], sc[:], sel[:], ALU.mult, [b_sc, b_sl], [b_w])
                P.op("dve", lambda e: e.tensor_reduce(out=ws[:], in_=w_[:].rearrange("p (t e) -> p t e", e=16), axis=AX.X, op=ALU.add),
                     reads=[b_w], writes=[b_ws])
                P.op("dve", lambda e: e.reciprocal(out=ws[:], in_=ws[:]), reads=[b_ws], writes=[b_ws])
                k.tt("dve", cmb[:].rearrange("p (t e) -> p t e", e=16), w_[:].rearrange("p (t e) -> p t e", e=16),
                     ws[:].unsqueeze(2).to_broadcast([128, NTT, 16]), ALU.mult, [b_w, b_ws], [b_cmb])
                for tq in range(0, NTT, 4):
                    nn = min(4, NTT - tq)
                    bt = k.bank()
                    for i_ in range(nn):
                        P.op("pe", lambda e: e.transpose(k.ps[bt][0:16, i_ * 128:(i_ + 1) * 128], cmb[:, (tq + i_) * 16:(tq + i_ + 1) * 16], identf),
                             reads=[b_cmb, b_cstf], writes=[k.pb[bt]], inc=(i_ == nn - 1))
                    k.copy("act", combT[:, tq * 128:(tq + nn) * 128], k.ps[bt][0:16, 0:nn * 128], [k.pb[bt]], [b_comb])
                P.barrier()
            dump("combT", combT[:], b_comb, [16, NT], BF16)
            cbs = [k.sb(st, [128, 512], BF16, "cbs") for _ in range(2)]
            sl = [k.sb(st, [128, 512], F32, "esl") for _ in range(2)]
            tl = [k.sb(st, [128, 512], F32, "etl") for _ in range(2)]
            aa = [k.sb(st, [128, 4, 512], BF16, "eaa") for _ in range(2)]
            ci = 0; fi = 0
            for e_ in range(16):
                wg, b_wg = wgs[e_ % 2]; wu, b_wu = wus[e_ % 2]; wd, b_wd = wds[e_ % 2]
                if e_ > 0:
                    load_expert(e_)
                for g, (c0, n) in enumerate(GROUPS):
                    if l == DEPTH - 1 and g == 0:
                        continue
                    j = jof(g)
                    cb, b_cb = cbs[ci % 2]; a_, b_a = aa[ci % 2]; ci += 1
                    bc = k.bank()
                    k.mm(bc, k.ps[bc][:, :n], selb[:, e_, :], combT[:, c0:c0 + n], True, True, [b_sel, b_comb])
                    k.copy("act", cb[:, :n], k.ps[bc][:, :n], [k.pb[bc]], [b_cb])
                    for fj in range(4):
                        bg = k.bank()
                        for kt in range(8):
                            k.mm(bg, k.ps[bg][:, :n], wg[:, kt, fj * 128:(fj + 1) * 128], fT[:, kt, c0:c0 + n], kt == 0, kt == 7, [b_wg, b_fT])
                        bu = k.bank()
                        for kt in range(8):
                            k.mm(bu, k.ps[bu][:, :n], wu[:, kt, fj * 128:(fj + 1) * 128], fT[:, kt, c0:c0 + n], kt == 0, kt == 7, [b_wu, b_fT])
                        s_, b_s = sl[fi % 2]; t_, b_t = tl[fi % 2]; fi += 1
                        k.act(s_[:, :n], k.ps[bg][:, :n], AF.Silu, [k.pb[bg]], [b_s])
                        k.tt("dve", t_[:, :n], k.ps[bu][:, :n], s_[:, :n], ALU.mult, [k.pb[bu], b_s], [b_t])
                        k.tt("dve", a_[:, fj, :n], t_[:, :n], cb[:, :n], ALU.mult, [b_t, b_cb], [b_a])
                    for co in range(8):
                        bo = k.bank()
                        for fj in range(4):
                            k.mm(bo, k.ps[bo][:, :n], wd[:, fj, co * 128:(co + 1) * 128], a_[:, fj, :n], fj == 0, fj == 3, [b_wd, b_a])
                        k.stt(xT[:, co, c0:c0 + n], k.ps[bo][:, :n], MOD[:, l, 5, co, j:j + 1], xT[:, co, c0:c0 + n], ALU.mult, ALU.add,
                              [k.pb[bo], b_MOD, b_xT], [b_xT])
            P.barrier()
        dump("xout", xT[:], b_xT, [128, 8, NT])

    TWO_PI = 2.0 * math.pi

    def sincos(ang, b_ang, N, sc4, sin_out, cos_out, b_out):
        (kf, b_kf), (r_, b_r), (mk, b_mk), (sh, b_sh) = sc4
        ki = kf[:, :N].bitcast(I32)
        k.ts("dve", r_[:, :N], ang, 1.0 / TWO_PI, None, ALU.mult, None, [b_ang], [b_r])
        k.copy("dve", ki, r_[:, :N], [b_r], [b_kf])
        k.copy("dve", mk[:, :N], ki, [b_kf], [b_mk])
        k.stt(r_[:, :N], mk[:, :N], -TWO_PI, ang, ALU.mult, ALU.add, [b_mk, b_ang], [b_r])
        k.ts("dve", mk[:, :N], r_[:, :N], math.pi, -TWO_PI, ALU.is_gt, ALU.mult, [b_r], [b_mk])
        k.tt("dve", r_[:, :N], r_[:, :N], mk[:, :N], ALU.add, [b_r, b_mk], [b_r])
        k.ts("dve", mk[:, :N], r_[:, :N], -math.pi, TWO_PI, ALU.is_lt, ALU.mult, [b_r], [b_mk])
        k.tt("dve", r_[:, :N], r_[:, :N], mk[:, :N], ALU.add, [b_r, b_mk], [b_r])
        k.ts("dve", r_[:, :N], r_[:, :N], math.pi, -math.pi, ALU.min, ALU.max, [b_r], [b_r])
        k.act(sin_out, r_[:, :N], AF.Sin, [b_r], [b_out])
        k.act(sh[:, :N], r_[:, :N], AF.Sin, [b_r], [b_sh], scale=0.5)
        k.tt("dve", sh[:, :N], sh[:, :N], sh[:, :N], ALU.mult, [b_sh], [b_sh])
        k.ts("dve", cos_out, sh[:, :N], -2.0, 1.0, ALU.mult, ALU.add, [b_sh], [b_out])

    def s5_mixer(l):
        ydst, b_ydst = yT[1]
        with ExitStack() as st:
            uT, b_u = k.sb(st, [128, 2, NT], BF16, "s5u")
            yacc, b_yacc = k.sb(st, [128, 2, NT], F32, "s5y")
            dcol, b_dcol = k.sb(st, [128, 2], F32, "s5d")
            k.load("sp", dcol[:], s5d_d[l], [b_dcol])
            with ExitStack() as s2:
                ws, b_ws = load_w("pool", s2, w_s5_d[l], 256, "ws5")
                hgs = [k.sb(s2, [128, 8, 512], BF16, "hg") for _ in range(2)]
                tmp, tmpb = [k.sb(s2, [128, 512], F32, "htmp") for _ in range(2)], None
                for g, (c0, n) in enumerate(GROUPS):
                    hg, hb = hgs[g % 2]
                    h_group(l, 0, g, hg, hb, tmp, tmpb)
                    for ti in range(2):
                        bk = k.bank()
                        proj(bk, ws, b_ws, ti * 128, hg, hb, n)
                        k.copy("act", uT[:, ti, c0:c0 + n], k.ps[bk][:, :n], [k.pb[bk]], [b_u])
                P.barrier()
            for dr in range(2):
                with ExitStack() as sd:
                    pr, b_pr = k.sb(sd, [128, 1024], F32, "pr"); pi_, b_pi = k.sb(sd, [128, 1024], F32, "pi")
                    qr, b_qr = k.sb(sd, [128, 1024], F32, "qr"); qi, b_qi = k.sb(sd, [128, 1024], F32, "qi")
                    Bb, b_Bb = k.sb(sd, [128, 2, 1024], BF16, "Bb")
                    Cb, b_Cb = k.sb(sd, [128, 3, 8, 128], BF16, "Cb")
                    triT, b_tri = k.sb(sd, [128, 2, 128], BF16, "triT")
                    trif, b_trif = k.sb(sd, [128, 128], F32, "trif")
                    posc, b_posc = k.sb(sd, [128, 2], F32, "posc")
                    posr, b_posr = k.sb(sd, [128, 128], F32, "posr")
                    fm, b_fm = k.sb(sd, [128, 3, 8], F32, "fm")
                    cc, b_cc = k.sb(sd, [128, 16, 8], F32, "cc")
                    A128, b_A128 = k.sb(sd, [128, 2, 8], F32, "A128")
                    k.load("pool", Bb[:], s5B_d[l, dr].rearrange("kt p n -> p kt n"), [b_Bb])
                    with ExitStack() as sc_:
                        Cf, b_Cf = k.sb(sc_, [128, 2, 8, 128], F32, "Cf")
                        k.load("sp", Cf[:, 0], s5C_d[l, dr, 0], [b_Cf]); k.load("sp", Cf[:, 1], s5C_d[l, dr, 1], [b_Cf])
                        k.copy("dve", Cb[:, 0], Cf[:, 0], [b_Cf], [b_Cb])
                        k.ts("dve", Cb[:, 1], Cf[:, 0], -1.0, None, ALU.mult, None, [b_Cf], [b_Cb])
                        k.ts("dve", Cb[:, 2], Cf[:, 1], -1.0, None, ALU.mult, None, [b_Cf], [b_Cb])
                        P.barrier()
                    k.load("sp", trif[:], tri_d[dr], [b_trif])
                    k.copy("dve", triT[:, 0, :], trif[:], [b_trif], [b_tri])
                    k.ts("dve", triT[:, 1, :], trif[:], -1.0, None, ALU.mult, None, [b_trif], [b_tri])
                    k.load("sp", posc[:], posc_d[dr], [b_posc]); k.load("sp", posr[:], posr_d[dr], [b_posr])
                    k.load("sp", fm[:], s5fm_d[l, dr], [b_fm])
                    with ExitStack() as sx:
                        sc4 = [k.sb(sx, [128, 1024], F32, "sc4") for _ in range(4)]
                        rho, b_rho = k.sb(sx, [128, 1024], F32, "rho"); th, b_th = k.sb(sx, [128, 1024], F32, "th")
                        ang, b_angb = k.sb(sx, [128, 1024], F32, "ang")
                        k.load("sp", rho[:], s5tm_d[l, dr, 0], [b_rho]); k.load("sp", th[:], s5tm_d[l, dr, 1], [b_th])
                        k.load("sp", ang[:], s5tm_d[l, dr, 2], [b_angb])
                        k.act(ang[:], ang[:], AF.Exp, [b_angb], [b_angb])
                        k.tt("dve", rho[:], rho[:], ang[:], ALU.mult, [b_rho, b_angb], [b_rho])
                        k.tt("dve", th[:], th[:], ang[:], ALU.mult, [b_th, b_angb], [b_th])
                        k.ts("dve", ang[:], th[:], posc[:, 0:1], None, ALU.mult, None, [b_th, b_posc], [b_angb])
                        sincos(ang[:], b_angb, 1024, sc4, pi_[:], pr[:], b_pr)
                        b_pi.w = b_pr.w
                        P.op("act", lambda e: e.activation(out=ang[:], in_=rho[:], func=AF.Exp, scale=posc[:, 0:1]), reads=[b_rho, b_posc], writes=[b_angb])
                        k.tt("dve", pr[:], pr[:], ang[:], ALU.mult, [b_pr, b_angb], [b_pr])
                        k.tt("dve", pi_[:], pi_[:], ang[:], ALU.mult, [b_pr, b_angb], [b_pr])
                        dtc = cc[:, 0, :]; rc_ = cc[:, 1, :]; tc_ = cc[:, 2, :]
                        k.act(dtc, fm[:, 2, :], AF.Exp, [b_fm], [b_cc])
                        k.tt("dve", rc_, fm[:, 0, :], dtc, ALU.mult, [b_fm, b_cc], [b_cc])
                        k.tt("dve", tc_, fm[:, 1, :], dtc, ALU.mult, [b_fm, b_cc], [b_cc])
                        k.copy("dve", ang[:, 0:8], tc_, [b_cc], [b_angb])
                        k.ts("dve", ang[:, 8:16], tc_, 128.0, None, ALU.mult, None, [b_cc], [b_angb])
                        sincos(ang[:, 0:16], b_angb, 16, sc4, th[:, 0:16], th[:, 16:32], b_th)
                        er = cc[:, 3, :]; e128 = cc[:, 4, :]
                        k.act(er, rc_, AF.Exp, [b_cc], [b_cc])
                        k.act(e128, rc_, AF.Exp, [b_cc], [b_cc], scale=128.0)
                        k.tt("dve", A128[:, 0, :], e128, th[:, 24:32], ALU.mult, [b_cc, b_th], [b_A128])
                        k.tt("dve", A128[:, 1, :], e128, th[:, 8:16], ALU.mult, [b_cc, b_th], [b_A128])
                        ar1 = cc[:, 5, :]; ai = cc[:, 6, :]; den = cc[:, 7, :]; cr = cc[:, 8, :]; ci = cc[:, 9, :]; t1 = cc[:, 10, :]
                        k.tt("dve", ar1, er, th[:, 16:24], ALU.mult, [b_cc, b_th], [b_cc])
                        k.ts("dve", ar1, ar1, -1.0, None, ALU.add, None, [b_cc], [b_cc])
                        k.tt("dve", ai, er, th[:, 0:8], ALU.mult, [b_cc, b_th], [b_cc])
                        k.tt("dve", den, fm[:, 0, :], fm[:, 0, :], ALU.mult, [b_fm], [b_cc])
                        k.tt("dve", t1, fm[:, 1, :], fm[:, 1, :], ALU.mult, [b_fm], [b_cc])
                        k.tt("dve", den, den, t1, ALU.add, [b_cc], [b_cc])
                        P.op("dve", lambda e: e.reciprocal(out=den, in_=den), reads=[b_cc], writes=[b_cc])
                        k.tt("dve", cr, ar1, fm[:, 0, :], ALU.mult, [b_cc, b_fm], [b_cc])
                        k.tt("dve", t1, ai, fm[:, 1, :], ALU.mult, [b_cc, b_fm], [b_cc])
                        k.tt("dve", cr, cr, t1, ALU.add, [b_cc], [b_cc])
                        k.tt("dve", cr, cr, den, ALU.mult, [b_cc], [b_cc])
                        k.tt("dve", ci, ai, fm[:, 0, :], ALU.mult, [b_cc, b_fm], [b_cc])
                        k.tt("dve", t1, ar1, fm[:, 1, :], ALU.mult, [b_cc, b_fm], [b_cc])
                        k.tt("dve", ci, ci, t1, ALU.subtract, [b_cc], [b_cc])
                        k.tt("dve", ci, ci, den, ALU.mult, [b_cc], [b_cc])
                        for i in range(8):
                            k.ts("dve", ang[:, i * 128:(i + 1) * 128], posr[:], cc[:, 2, i:i + 1], None, ALU.mult, None, [b_posr, b_cc], [b_angb])
                            P.op("act", lambda e: e.activation(out=rho[:, i * 128:(i + 1) * 128], in_=posr[:], func=AF.Exp, scale=cc[:, 1, i:i + 1]),
                                 reads=[b_posr, b_cc], writes=[b_rho])
                        sincos(ang[:], b_angb, 1024, sc4, qi[:], qr[:], b_qr)
                        k.tt("dve", qr[:], qr[:], rho[:], ALU.mult, [b_qr, b_rho], [b_qr])
                        k.tt("dve", qi[:], qi[:], rho[:], ALU.mult, [b_qr, b_rho], [b_qr])
                        for i in range(8):
                            sl_ = slice(i * 128, (i + 1) * 128)
                            k.ts("dve", th[:, sl_], qi[:, sl_], cc[:, 9, i:i + 1], None, ALU.mult, None, [b_qr, b_cc], [b_th])
                            k.ts("dve", ang[:, sl_], qr[:, sl_], cc[:, 9, i:i + 1], None, ALU.mult, None, [b_qr, b_cc], [b_angb])
                            k.stt(qr[:, sl_], qr[:, sl_], cc[:, 8, i:i + 1], th[:, sl_], ALU.mult, ALU.subtract, [b_qr, b_cc, b_th], [b_qr])
                            k.stt(qi[:, sl_], qi[:, sl_], cc[:, 8, i:i + 1], ang[:, sl_], ALU.mult, ALU.add, [b_qr, b_cc, b_angb], [b_qr])
                        P.barrier()
                    if l == 0 and dr == 0:
                        dump("s5pr", pr[:], b_pr, [128, 1024]); dump("s5pi", pi_[:], b_pr, [128, 1024])
                        dump("s5qr", qr[:], b_qr, [128, 1024]); dump("s5qi", qi[:], b_qr, [128, 1024])
                        dump("s5A128", A128[:], b_A128, [128, 2, 8])
                    with ExitStack() as sp_:
                        zp, _ = k.sb(sp_, [128, 4, 1024], BF16, "zp")
                        hp, _ = k.sb(sp_, [128, 4, 1024], BF16, "hp")
                        Dg, _ = k.sb(sp_, [128, 16, 128], BF16, "Dg")
                        zl, _ = k.sb(sp_, [128, 16], F32, "zl")
                        car, _ = k.sb(sp_, [128, 16], F32, "car")
                        ct_, _ = k.sb(sp_, [128, 4, 8], F32, "ctmp")
                        hb2 = lambda nm: [Buf(nm + "0"), Buf(nm + "1")]
                        b_zp, b_hp, b_DgT, b_zl, b_car, b_ct = hb2("zp"), hb2("hp"), hb2("Dg"), hb2("zl"), hb2("car"), hb2("ct")
                        k.memset("pool", Dg[:], 0.0, b_DgT)
                        order = [0, 1] + list(range(2, NTT)) if dr == 0 else [1, 0] + list(range(NTT - 1, 1, -1))
                        tl = 127 if dr == 0 else 0

                        def half_gen(kt, banks):
                            bre, bim, zre, zim = banks
                            cs = slice(kt * 512, (kt + 1) * 512)
                            i4 = slice(kt * 4, kt * 4 + 4); i4m = slice(8 + kt * 4, 8 + kt * 4 + 4)
                            for tt in order:
                                cols = slice(tt * 128, (tt + 1) * 128)
                                k.mm(bre, k.ps[bre][:, :], uT[:, kt, cols], Bb[:, kt, 0:512], True, True, [b_u, b_Bb])
                                k.mm(bim, k.ps[bim][:, :], uT[:, kt, cols], Bb[:, kt, 512:1024], True, True, [b_u, b_Bb])
                                yield
                                k.tt("dve", zp[:, 0, cs], k.ps[bre][:, :], pr[:, cs], ALU.mult, [k.pb[bre], b_pr], [b_zp[kt]])
                                k.tt("dve", zp[:, 1, cs], k.ps[bim][:, :], pi_[:, cs], ALU.mult, [k.pb[bim], b_pr], [b_zp[kt]])
                                k.tt("dve", zp[:, 2, cs], k.ps[bim][:, :], pr[:, cs], ALU.mult, [k.pb[bim], b_pr], [b_zp[kt]])
                                k.tt("dve", zp[:, 3, cs], k.ps[bre][:, :], pi_[:, cs], ALU.mult, [k.pb[bre], b_pr], [b_zp[kt]])
                                yield
                                for ii in range(4):
                                    i = kt * 4 + ii
                                    sl_ = slice(i * 128, (i + 1) * 128)
                                    oc = slice(ii * 128, (ii + 1) * 128)
                                    k.mm(zre, k.ps[zre][:, oc], zp[:, 0, sl_], triT[:, 0, :], True, False, [b_zp[kt], b_tri])
                                    k.mm(zre, k.ps[zre][:, oc], zp[:, 1, sl_], triT[:, 1, :], False, False, [b_zp[kt], b_tri])
                                    k.mm(zre, k.ps[zre][:, oc], Dg[:, i, :], onesb[:], False, True, [b_DgT[kt], b_ones])
                                    k.mm(zim, k.ps[zim][:, oc], zp[:, 2, sl_], triT[:, 0, :], True, False, [b_zp[kt], b_tri])
                                    k.mm(zim, k.ps[zim][:, oc], zp[:, 3, sl_], triT[:, 0, :], False, False, [b_zp[kt], b_tri])
                                    k.mm(zim, k.ps[zim][:, oc], Dg[:, 8 + i, :], onesb[:], False, True, [b_DgT[kt], b_ones])
                                yield
                                k.copy("act", zl[:, i4], k.ps[zre][:, :].rearrange("p (i t) -> p i t", t=128)[:, :, tl], [k.pb[zre]], [b_zl[kt]])
                                k.copy("act", zl[:, i4m], k.ps[zim][:, :].rearrange("p (i t) -> p i t", t=128)[:, :, tl], [k.pb[zim]], [b_zl[kt]])
                                zr_, zi_ = zl[:, i4], zl[:, i4m]
                                k.tt("dve", ct_[:, 0, i4], A128[:, 0, i4], zr_, ALU.mult, [b_A128, b_zl[kt]], [b_ct[kt]])
                                k.tt("dve", ct_[:, 1, i4], A128[:, 1, i4], zi_, ALU.mult, [b_A128, b_zl[kt]], [b_ct[kt]])
                                k.tt("dve", ct_[:, 2, i4], A128[:, 0, i4], zi_, ALU.mult, [b_A128, b_zl[kt]], [b_ct[kt]])
                                k.tt("dve", ct_[:, 3, i4], A128[:, 1, i4], zr_, ALU.mult, [b_A128, b_zl[kt]], [b_ct[kt]])
                                k.tt("dve", car[:, i4], ct_[:, 0, i4], ct_[:, 1, i4], ALU.subtract, [b_ct[kt]], [b_car[kt]])
                                k.tt("dve", car[:, i4m], ct_[:, 2, i4], ct_[:, 3, i4], ALU.add, [b_ct[kt]], [b_car[kt]])
                                for isl in (i4, i4m):
                                    k.tt("pool", Dg[:, isl, :], identf.unsqueeze(1).to_broadcast([128, 4, 128]),
                                         car[:, isl].unsqueeze(2).to_broadcast([128, 4, 128]), ALU.mult, [b_cstf, b_car[kt]], [b_DgT[kt]])
                                yield
                                k.tt("dve", hp[:, 0, cs], k.ps[zre][:, :], qr[:, cs], ALU.mult, [k.pb[zre], b_qr], [b_hp[kt]])
                                k.tt("dve", hp[:, 1, cs], k.ps[zim][:, :], qi[:, cs], ALU.mult, [k.pb[zim], b_qr], [b_hp[kt]])
                                k.tt("dve", hp[:, 2, cs], k.ps[zim][:, :], qr[:, cs], ALU.mult, [k.pb[zim], b_qr], [b_hp[kt]])
                                k.tt("dve", hp[:, 3, cs], k.ps[zre][:, :], qi[:, cs], ALU.mult, [k.pb[zre], b_qr], [b_hp[kt]])
                                yield
                                bk = bre
                                first = True
                                for ii in range(4):
                                    i = kt * 4 + ii
                                    sl_ = slice(i * 128, (i + 1) * 128)
                                    for (pi_x, ci_x) in ((0, 0), (1, 1), (2, 2), (3, 2)):
                                        last = (ii == 3 and pi_x == 3)
                                        k.mm(bk, k.ps[bk][:, 0:128], Cb[:, ci_x, i, :], hp[:, pi_x, sl_], first, last, [b_Cb, b_hp[kt]])
                                        first = False
                                if dr == 0:
                                    k.stt(yacc[:, kt, cols], uT[:, kt, cols], dcol[:, kt:kt + 1], k.ps[bk][:, 0:128], ALU.mult, ALU.add,
                                          [b_u, b_dcol, k.pb[bk]], [b_yacc])
                                else:
                                    k.tt("dve", yacc[:, kt, cols], k.ps[bk][:, 0:128], yacc[:, kt, cols], ALU.add, [k.pb[bk], b_yacc], [b_yacc])
                                yield

                        live = [half_gen(0, [0, 1, 2, 3]), half_gen(1, [4, 5, 6, 7])]
                        for _ in range(S5SKEW):
                            next(live[0])
                        while live:
                            for g_ in list(live):
                                try:
                                    next(g_)
                                except StopIteration:
                                    live.remove(g_)
                        P.barrier()
            dump("s5yacc", yacc[:], b_yacc, [128, 2, NT])
            with ExitStack() as s4:
                wgf, b_wgf = k.sb(s4, [128, 2, 256], F32, "wgf"); wgb, b_wgb = k.sb(s4, [128, 2, 256], BF16, "wgb")
                bg, b_bg = k.sb(s4, [128, 2], F32, "bglu")
                k.load("sp", wgf[:], w_glu_d[l].rearrange("(kt p) n -> p kt n", p=128), [b_wgf])
                k.copy("dve", wgb[:], wgf[:], [b_wgf], [b_wgb])
                k.load("sp", bg[:], s5bg_d[l], [b_bg])
                zT, b_zT = k.sb(s4, [128, 2, 512], BF16, "zT")
                x2, b_x2 = k.sb(s4, [128, 512], F32, "x2"); thh, b_thh = k.sb(s4, [128, 512], F32, "thh")
                sg, b_sg = k.sb(s4, [128, 512], F32, "sg")
                for g, (c0, n) in enumerate(GROUPS):
                    for kt in range(2):
                        x_ = yacc[:, kt, c0:c0 + n]
                        k.tt("dve", x2[:, :n], x_, x_, ALU.mult, [b_yacc], [b_x2])
                        k.ts("dve", x2[:, :n], x2[:, :n], 0.044715, 1.0, ALU.mult, ALU.add, [b_x2], [b_x2])
                        k.tt("dve", x2[:, :n], x2[:, :n], x_, ALU.mult, [b_x2, b_yacc], [b_x2])
                        k.act(thh[:, :n], x2[:, :n], AF.Tanh, [b_x2], [b_thh], scale=math.sqrt(2.0 / math.pi))
                        k.stt(thh[:, :n], thh[:, :n], 1.0, x_, ALU.add, ALU.mult, [b_thh, b_yacc], [b_thh])
                        k.ts("dve", zT[:, kt, :n], thh[:, :n], 0.5, None, ALU.mult, None, [b_thh], [b_zT])
                    for ct in range(2):
                        bk = k.bank()
                        for kt in range(2):
                            k.mm(bk, k.ps[bk][:, :n], wgb[:, kt, ct * 128:(ct + 1) * 128], zT[:, kt, :n], kt == 0, kt == 1, [b_wgb, b_zT])
                        P.op("act", lambda e: e.activation(out=sg[:, :n], in_=k.ps[bk][:, :n], func=AF.Sigmoid, bias=bg[:, ct:ct + 1]),
                             reads=[k.pb[bk], b_bg], writes=[b_sg])
                        k.tt("dve", ydst[:, ct, c0:c0 + n], zT[:, ct, :n], sg[:, :n], ALU.mult, [b_zT, b_sg], [b_ydst])
                P.barrier()
        dump("yb", ydst[:], b_ydst, [128, 2, NT], BF16)

    def rwkv_mixer(l):
        ydst, b_ydst = yT[3]
        xd, b_xd = ydst, Buf("xd")
        with ExitStack() as st:
            rS, b_r = yall[:, 0], Buf("rw_r")
            kS, b_k = yall[:, 1], Buf("rw_k")
            vS, b_v = yall[:, 2], Buf("rw_v")
            kkS, b_kk = k.sb(st, [128, 2, NT], BF16, "rw_kk")
            cols, b_cols = k.sb(st, [128, 2, 9], F32, "rw_cols")
            k.load("sp", cols[:], rw_cols_d[l], [b_cols])
            C_KK, C_KA, C_LNG, C_LNB, C_RK, C_W0, C_A0 = 0, 1, 2, 3, 4, 5, 7
            for half in range(2):
              with ExitStack() as s2:
                WA, b_WA = load_w("pool", s2, w_rw_d[l, :, half * 512:(half + 1) * 512], 512, "rwWA")
                WB, b_WB = k.sb(s2, [128, 8, 512], BF16, "rwWB")
                with ExitStack() as s3:
                    mu, b_mu = k.sb(s3, [128, 512], F32, "rwmu")
                    k.load("sp", mu[:], rw_mu_d[l, :, half * 512:(half + 1) * 512], [b_mu])
                    for kt in range(8):
                        k.stt(WB[:, kt, :], WA[:, kt, :], 0.5, mu[:], ALU.mult, ALU.mult, [b_WA, b_mu], [b_WB])
                    k.ts("dve", mu[:], mu[:], -1.0, 1.0, ALU.mult, ALU.add, [b_mu], [b_mu])
                    for kt in range(8):
                        k.tt("pool", WA[:, kt, :], WA[:, kt, :], mu[:], ALU.mult, [b_WA, b_mu], [b_WA])
                    P.barrier()
                hgxs = [k.sb(s2, [128, 8, 516], BF16, "hgx") for _ in range(2)]
                hsxs = [k.sb(s2, [128, 8, 512], BF16, "hsx") for _ in range(2)]
                tmps = [k.sb(s2, [128, 514], F32, "htmp") for _ in range(2)]
                sq, b_sq = k.sb(s2, [128, 512], BF16, "rwsq"); kq, b_kq = k.sb(s2, [128, 512], F32, "rwkq")
                rs_, b_rs = k.sb(s2, [128, 512], F32, "rwrs"); lnt, b_lnt = k.sb(s2, [128, 512], F32, "rwlnt")
                for g, (c0, n) in enumerate(GROUPS if RW_SUB >= 2 else []):
                    j = jof(g)
                    hgx, b_hgx = hgxs[g % 2]; hsx, b_hsx = hsxs[g % 2]
                    s_lo, s_hi = (0, NCTX) if g == 0 else (NCTX, NT)
                    lo, hi = max(s_lo, c0 - 1), min(s_hi, c0 + n + 1)
                    o0 = lo - (c0 - 1)
                    if lo > c0 - 1:
                        k.memset("dve", hgx[:, :, 0:3], 0.0, [b_hgx])
                    if hi < c0 + n + 1:
                        k.memset("dve", hgx[:, :, n + 1:n + 3], 0.0, [b_hgx])
                    w_ = hi - lo
                    for kt in range(8):
                        tmp, tmpb = tmps[kt % 2]
                        k.tt("dve", tmp[:, :w_], xT[:, kt, lo:hi], rstd_b[:, lo:hi], ALU.mult, [b_xT, b_rstd], [tmpb])
                        P.op("act", lambda e: e.activation(out=hgx[:, kt, 1 + o0:1 + o0 + w_], in_=tmp[:, :w_], func=AF.Identity,
                                                           scale=AB[:, l, 0, 0, kt, j:j + 1], bias=AB[:, l, 0, 1, kt, j:j + 1]),
                             reads=[tmpb, b_AB], writes=[b_hgx])
                    for kt in range(8):
                        k.tt("pool", hsx[:, kt, :n], hgx[:, kt, 1:n + 1], hgx[:, kt, 3:n + 3], ALU.add, [b_hgx], [b_hsx])
                    for tl_ in range(4 if RW_SUB >= 3 else 0):
                        ti = half * 4 + tl_
                        bk = k.bank()
                        for kt in range(8):
                            k.mm(bk, k.ps[bk][:, :n], WA[:, kt, tl_ * 128:(tl_ + 1) * 128], hgx[:, kt, 2:n + 2], kt == 0, False, [b_WA, b_hgx])
                        for kt in range(8):
                            k.mm(bk, k.ps[bk][:, :n], WB[:, kt, tl_ * 128:(tl_ + 1) * 128], hsx[:, kt, :n], False, kt == 7, [b_WB, b_hsx])
                        dst, dstb = [(rS, b_r), (kS, b_k), (vS, b_v), (xd, b_xd)][ti // 2]
                        k.copy("act", dst[:, ti % 2, c0:c0 + n], k.ps[bk][:, :n], [k.pb[bk]], [dstb])
                        if ti // 2 == 1 and RW_SUB >= 4:
                            ci = ti % 2
                            k.ts("dve", kq[:, :n], k.ps[bk][:, :n], cols[:, ci, C_KK:C_KK + 1], None, ALU.mult, None, [k.pb[bk], b_cols], [b_kq])
                            k.act(sq[:, :n], kq[:, :n], AF.Square, [b_kq], [b_sq])
                            b2 = k.bank()
                            k.mm(b2, k.ps[b2][:, :n], bo64, sq[:, :n], True, True, [b_sq, b_cstb])
                            k.rstd_from_ss(rs_[:, :n], k.ps[b2][:, :n], b2, 1.0, lnt[:, :n], b_lnt, [b_rs])
                            k.tt("dve", kkS[:, ci, c0:c0 + n], kq[:, :n], rs_[:, :n], ALU.mult, [b_kq, b_rs], [b_kk])
                P.barrier()
            dump("rw_r", rS[:], b_r, [128, 2, NT], BF16); dump("rw_kk", kkS[:], b_kk, [128, 2, NT], BF16)
            dump("rw_xd", xd[:], b_xd, [128, 2, NT], BF16)
            yacc, _ = k.sb(st, [128, 2, NT], BF16, "rw_yacc")
            sw = {}
            def small(name, shape, src, dt=BF16, q="pool"):
                t, b = k.sb(st, shape, dt, name)
                k.load(q if dt == BF16 else "sp", t[:], src, [b])
                sw[name] = (t, b)
            for dr in range(2 if RW_LEVEL >= 1 else 0):
                small(f"w1_{dr}", [128, 2, 32], rw_w1_d[l, dr].rearrange("(kt p) n -> p kt n", p=128))
                small(f"w2_{dr}", [32, 256], rw_w2_d[l, dr])
                small(f"a1_{dr}", [128, 2, 32], rw_a1_d[l, dr].rearrange("(kt p) n -> p kt n", p=128))
                small(f"a2_{dr}", [32, 256], rw_a2_d[l, dr])
                small(f"w0r_{dr}", [1, 256], rw_w0row_d[l, dr], F32)
                small(f"tri_{dr}", [128, 2, 128], rw_tri_d[dr], F32)
                small(f"m1_{dr}", [128, 256], rw_m1_d[dr])
                small(f"mnt_{dr}", [128, 128], rw_mnt_d[dr])
            if RW_LEVEL >= 0:
                small("g1", [128, 2, 64], rw_g1_d[l].rearrange("(kt p) n -> p kt n", p=128))
                small("g2", [64, 256], rw_g2_d[l])
                small("ind", [128, 2], rw_ind_d, F32)
            ones1, b_ones1 = k.sb(st, [1, 128], F32, "ones1")
            k.memset("dve", ones1[:], 1.0, [b_ones1])

            def a_of(dr, xsrc, n, a_out, b_aout, x1t, b_x1t):
                a1, b_a1 = sw[f"a1_{dr}"]; a2, b_a2 = sw[f"a2_{dr}"]
                bk = k.bank()
                for kt in range(2):
                    k.mm(bk, k.ps[bk][0:32, :n], a1[:, kt, :], xsrc[:, kt, :], kt == 0, kt == 1, [b_a1, b_xd])
                k.copy("act", x1t[0:32, :n], k.ps[bk][0:32, :n], [k.pb[bk]], [b_x1t])
                for ci in range(2):
                    b2 = k.bank()
                    k.mm(b2, k.ps[b2][:, :n], a2[:, ci * 128:(ci + 1) * 128], x1t[0:32, :n], True, True, [b_a2, b_x1t])
                    P.op("act", lambda e: e.activation(out=a_out[:, ci, :n], in_=k.ps[b2][:, :n], func=AF.Sigmoid,
                                                       bias=cols[:, ci, C_A0 + dr:C_A0 + dr + 1]),
                         reads=[k.pb[b2], b_cols], writes=[b_aout])

            def kmod_of(a_in, b_ain, ksrc, n, out, b_out_, tmpf, b_tmpf):
                for ci in range(2):
                    k.ts("dve", tmpf[:, ci, :n], a_in[:, ci, :n], -1.0, cols[:, ci, C_KA:C_KA + 1], ALU.add, ALU.mult, [b_ain, b_cols], [b_tmpf])
                    k.stt(out[:, ci, :n], tmpf[:, ci, :n], 1.0, ksrc[:, ci, :], ALU.add, ALU.mult, [b_tmpf, b_k], [b_out_])

            b_yt = [Buf(f"yacc{t}") for t in range(NTT)]
            for (c0_, n_) in GROUPS:
                k.memset("dve", yacc[:, :, c0_:c0_ + n_], 0.0, b_yt[c0_ // 128:(c0_ + n_) // 128])

            def scan_gen(dr, sd, banks):
                    w1, b_w1 = sw[f"w1_{dr}"]; w2, b_w2 = sw[f"w2_{dr}"]; w0r, b_w0r = sw[f"w0r_{dr}"]
                    tri, b_tri = sw[f"tri_{dr}"]; m1, b_m1 = sw[f"m1_{dr}"]; mnt, b_mnt = sw[f"mnt_{dr}"]; ind, b_ind = sw["ind"]
                    M32, b_M32 = k.sb(sd, [128, 2, 64], F32, "M32"); Mbf, b_Mbf = k.sb(sd, [128, 2, 128], BF16, "Mblk")
                    k.memset("dve", M32[:], 0.0, [b_M32]); k.memset("dve", Mbf[:], 0.0, [b_Mbf])
                    x1t, b_x1t = k.sb(sd, [32, 128], BF16, "x1t")
                    tnh, b_tnh = k.sb(sd, [32, 128], BF16, "tnh")
                    lwT, b_lwT = k.sb(sd, [128, 256], F32, "lwT")
                    lam, b_lam = k.sb(sd, [128, 2, 3, 128], F32, "lam")
                    gam, b_gam = k.sb(sd, [128, 2, 2], F32, "gam")
                    aF, b_aF = k.sb(sd, [128, 2, 128], F32, "aF")
                    tF, b_tF = k.sb(sd, [128, 2, 128], F32, "tF")
                    kmod, b_kmod = k.sb(sd, [128, 2, 128], F32, "kmod")
                    AR, b_AR = k.sb(sd, [128, 2, 2, 128], BF16, "AR")
                    BK, b_BK = k.sb(sd, [128, 2, 2, 128], BF16, "BK")
                    BKt2, b_BKt = k.sb(sd, [128, 2, 2, 2, 128], BF16, "BKt")
                    k.memset("pool", BKt2[:], 0.0, [b_BKt])
                    Vt, b_Vt = k.sb(sd, [128, 2, 128], BF16, "Vt")
                    SCb, b_SCb = k.sb(sd, [128, 4, 256], BF16, "SCb"); SCk, b_SCk = k.sb(sd, [128, 4, 256], BF16, "SCk")
                    Nb = [k.sb(sd, [128, 4, 128], BF16, f"Nb{i}") for i in range(2)]
                    Tb = [k.sb(sd, [128, 4, 128], BF16, f"Tb{i}") for i in range(2)]
                    R, b_R = k.sb(sd, [128, 4, 128], BF16, "Rinv")
                    Wsb, b_Wsb = k.sb(sd, [128, 256], BF16, "Wsb"); Usb, b_Usb = k.sb(sd, [128, 256], BF16, "Usb")
                    mt, b_mt = k.sb(sd, [128, 2, 64], F32, "mt")
                    v4h = lambda ap: ap.rearrange("p (h t) -> p h t", h=4)
                    order = list(range(NTT)) if dr == 0 else [1, 0] + list(range(NTT - 1, 1, -1))
                    yield
                    for tt in order:
                        tc_ = slice(tt * 128, (tt + 1) * 128)
                        bk = k.bank()
                        for kt in range(2):
                            k.mm(bk, k.ps[bk][0:32, 0:128], w1[:, kt, :], xd[:, kt, tc_], kt == 0, kt == 1, [b_w1, b_xd])
                        k.act(tnh[:], k.ps[bk][0:32, 0:128], AF.Tanh, [k.pb[bk]], [b_tnh])
                        a1, b_a1 = sw[f"a1_{dr}"]; a2, b_a2 = sw[f"a2_{dr}"]
                        bk = k.bank()
                        for kt in range(2):
                            k.mm(bk, k.ps[bk][0:32, 0:128], a1[:, kt, :], xd[:, kt, tc_], kt == 0, kt == 1, [b_a1, b_xd])
                        k.copy("dve", x1t[0:32, 0:128], k.ps[bk][0:32, 0:128], [k.pb[bk]], [b_x1t])
                        yield
                        bk = k.bank()
                        k.mm(bk, k.ps[bk][:, 0:256], tnh[:], w2[:], True, False, [b_tnh, b_w2])
                        k.mm(bk, k.ps[bk][:, 0:256], ones1[:], w0r[:], False, True, [b_ones1, b_w0r])
                        k.act(lwT[:], k.ps[bk][:, 0:256], AF.Sigmoid, [k.pb[bk]], [b_lwT])
                        k.ts("dve", lwT[:], lwT[:], -math.exp(-0.5), None, ALU.mult, None, [b_lwT], [b_lwT])
                        b2 = k.bank()
                        for ci in range(2):
                            k.mm(b2, k.ps[b2][:, ci * 128:(ci + 1) * 128], a2[:, ci * 128:(ci + 1) * 128], x1t[0:32, 0:128], True, True, [b_a2, b_x1t],
                                 inc=(ci == 1))
                        for ci in range(2):
                            P.op("act", lambda e: e.activation(out=aF[:, ci, :], in_=k.ps[b2][:, ci * 128:(ci + 1) * 128], func=AF.Sigmoid,
                                                               bias=cols[:, ci, C_A0 + dr:C_A0 + dr + 1]),
                                 reads=[k.pb[b2], b_cols], writes=[b_aF])
                        yield
                        kmod_of(aF, b_aF, kS[:, :, tc_], 128, kmod, b_kmod, tF, b_tF)
                        for ci in range(2):
                            k.tt("pool", tF[:, ci, :], kkS[:, ci, tc_], aF[:, ci, :], ALU.mult, [b_kk, b_aF], [b_tF])
                        lbk = []
                        for ci in range(2):
                            bk = k.bank(); lbk.append(bk)
                            k.mm(bk, k.ps[bk][:, 0:128], lwT[:, ci * 128:(ci + 1) * 128], tri[:, 0, :], True, True, [b_lwT, b_tri], inc=False)
                            k.mm(bk, k.ps[bk][:, 128:256], lwT[:, ci * 128:(ci + 1) * 128], tri[:, 1, :], True, True, [b_lwT, b_tri], inc=False)
                            k.mm(bk, k.ps[bk][:, 256:258], lwT[:, ci * 128:(ci + 1) * 128], ind[:], True, True, [b_lwT, b_ind])
                        yield
                        for ci in range(2):
                            bk = lbk[ci]
                            k.act(lam[:, ci, 0, :], k.ps[bk][:, 0:128], AF.Exp, [k.pb[bk]], [b_lam])
                            k.act(lam[:, ci, 1, :], k.ps[bk][:, 0:128], AF.Exp, [k.pb[bk]], [b_lam], scale=-1.0)
                            k.act(lam[:, ci, 2, :], k.ps[bk][:, 128:256], AF.Exp, [k.pb[bk]], [b_lam])
                            k.act(gam[:, ci, :], k.ps[bk][:, 256:258], AF.Exp, [k.pb[bk]], [b_gam])
                        yield
                        if RWSTOP <= 1:
                            continue
                        for ci in range(2):
                            k.tt("dve", AR[:, ci, 1, :], rS[:, ci, tc_], lam[:, ci, 0, :], ALU.mult, [b_r, b_lam], [b_AR])
                            k.stt(AR[:, ci, 0, :], kkS[:, ci, tc_], -1.0, lam[:, ci, 2, :], ALU.mult, ALU.mult, [b_kk, b_lam], [b_AR])
                            k.tt("dve", BK[:, ci, 1, :], kmod[:, ci, :], lam[:, ci, 1, :], ALU.mult, [b_kmod, b_lam], [b_BK])
                            k.tt("pool", BK[:, ci, 0, :], tF[:, ci, :], lam[:, ci, 1, :], ALU.mult, [b_tF, b_lam], [b_BK])
                        yield
                        if RWSTOP <= 2:
                            continue
                        bk = k.bank()
                        pv = k.ps[bk][:].bitcast(BF16)
                        for ci in range(2):
                            for x_ in range(2):
                                P.op("pe", lambda e: e.transpose(pv[:, (ci * 2 + x_) * 128:(ci * 2 + x_ + 1) * 128], BK[:, ci, x_, :], identb),
                                     reads=[b_BK, b_cstb], writes=[k.pb[bk]], inc=False)
                            P.op("pe", lambda e: e.transpose(pv[:, (4 + ci) * 128:(5 + ci) * 128], vS[:, ci, tc_], identb),
                                 reads=[b_v, b_cstb], writes=[k.pb[bk]], inc=(ci == 1))
                        k.copy("act", BKt2[0:64, 0].rearrange("p a b c -> p (a b c)"), pv[0:64, 0:512], [k.pb[bk]], [b_BKt])
                        k.copy("act", BKt2[64:128, 1].rearrange("p a b c -> p (a b c)"), pv[64:128, 0:512], [k.pb[bk]], [b_BKt])
                        k.copy("dve", Vt[:].rearrange("p a c -> p (a c)"), pv[:, 512:768], [k.pb[bk]], [b_Vt])
                        if RWSTOP <= 3:
                            continue
                        m1b = m1[:].unsqueeze(1).to_broadcast([128, 2, 256])
                        for hh in range(2):
                            pb = 64 * hh
                            bA = k.bank(); bB = k.bank()
                            for x_, bx in ((0, bA), (1, bB)):
                                for ci in range(2):
                                    arv = AR[pb:pb + 64, ci, :, :].rearrange("p a t -> p (a t)")
                                    k.mm(bx, k.ps[bx][:, ci * 256:(ci + 1) * 256], BK[pb:pb + 64, ci, x_, :], arv, True, True, [b_BK, b_AR], inc=(ci == 1))
                            k.tt("dve", SCb[:, hh * 2:hh * 2 + 2, :], k.ps[bA][:, 0:512].rearrange("p (h t) -> p h t", h=2), m1b, ALU.mult,
                                 [k.pb[bA], b_m1], [b_SCb])
                            k.tt("dve", SCk[:, hh * 2:hh * 2 + 2, :], k.ps[bB][:, 0:512].rearrange("p (h t) -> p h t", h=2), m1b, ALU.mult,
                                 [k.pb[bB], b_m1], [b_SCk])
                            yield
                        Tc, b_Tc = Tb[0]
                        for hh in range(2):
                            pb = 64 * hh
                            bC = k.bank()
                            for ci in range(2):
                                k.mm(bC, k.ps[bC][:, ci * 128:(ci + 1) * 128], AR[pb:pb + 64, ci, 0, :], BK[pb:pb + 64, ci, 0, :], True, True, [b_AR, b_BK],
                                     inc=(ci == 1))
                            k.tt("dve", Tc[:, hh * 2:hh * 2 + 2, :], k.ps[bC][:, 0:256].rearrange("p (h t) -> p h t", h=2),
                                 mnt[:].unsqueeze(1).to_broadcast([128, 2, 128]), ALU.mult, [k.pb[bC], b_mnt], [b_Tc])
                        k.tt("pool", R[:], SCb[:, :, 0:128], identb.unsqueeze(1).to_broadcast([128, 4, 128]), ALU.add, [b_SCb, b_cstb], [b_R])
                        yield
                        if RWSTOP <= 4:
                            continue
                        Nc, b_Nc = SCb[:, :, 0:128], b_SCb
                        for i in range(1, 6):
                            Nn, b_Nn = Nb[i % 2]; Tn, b_Tn = Tb[i % 2]
                            bt_ = k.bank()
                            for h in range(4):
                                k.mm(bt_, k.ps[bt_][:, h * 128:(h + 1) * 128], Nc[:, h, :], Tc[:, h, :], True, True, [b_Nc, b_Tc], inc=(h == 3))
                            k.copy("dve", Tn[:], v4h(k.ps[bt_][:, 0:512]), [k.pb[bt_]], [b_Tn])
                            if i < 5:
                                bn = k.bank()
                                for h in range(4):
                                    k.mm(bn, k.ps[bn][:, h * 128:(h + 1) * 128], Tc[:, h, :], Nc[:, h, :], True, True, [b_Tc, b_Nc], inc=(h == 3))
                                k.copy("act", Nn[:], v4h(k.ps[bn][:, 0:512]), [k.pb[bn]], [b_Nn])
                            yield
                            br_ = k.bank()
                            for h in range(4):
                                k.mm(br_, k.ps[br_][:, h * 128:(h + 1) * 128], Tn[:, h, :], R[:, h, :], True, True, [b_Tn, b_R], inc=(h == 3))
                            k.tt("dve", R[:], v4h(k.ps[br_][:, 0:512]), R[:], ALU.add, [k.pb[br_], b_R], [b_R])
                            yield
                            Nc, b_Nc = Nn[:], b_Nn
                            Tc, b_Tc = Tn, b_Tn
                        if RWSTOP <= 5:
                            continue
                        bY, bW, bU, bM = banks
                        for jj in ([0, 1] if dr == 0 else [1, 0]):
                            pj = 64 * jj
                            for ci in range(2):
                                k.mm(bW, k.ps[bW][:, ci * 128:(ci + 1) * 128], AR[:, ci, 0, :], Mbf[:, ci, :], True, False, [b_AR, b_Mbf], inc=False)
                                for hh in range(2):
                                    h = ci * 2 + hh
                                    pb = 64 * hh
                                    k.mm(bW, k.ps[bW][:, h * 64:(h + 1) * 64], SCk[:, hh * 2 + ci, 0:128], Vt[:, ci, pb:pb + 64], False, True, [b_SCk, b_Vt],
                                         inc=(h == 3))
                            k.copy("act", Wsb[:], k.ps[bW][:, 0:256], [k.pb[bW]], [b_Wsb])
                            yield
                            for h in range(4):
                                oc = slice(h * 64, (h + 1) * 64)
                                k.mm(bU, k.ps[bU][:, oc], R[:, (h % 2) * 2 + h // 2, :], Wsb[:, oc], True, True, [b_R, b_Wsb], inc=(h == 3))
                            k.copy("dve", Usb[:], k.ps[bU][:, 0:256], [k.pb[bU]], [b_Usb])
                            yield
                            for ci in range(2):
                                for hh in range(2):
                                    h = ci * 2 + hh
                                    pb = 64 * hh
                                    oc = slice(h * 64, (h + 1) * 64)
                                    mo = k.ps[bM][pb:pb + 64, ci * 64:(ci + 1) * 64]
                                    k.mm(bM, mo, BKt2[:, jj, ci, 0, pb:pb + 64], Usb[:, oc], True, False, [b_BKt, b_Usb], inc=False)
                                    k.mm(bM, mo, BKt2[:, jj, ci, 1, pb:pb + 64], Vt[:, ci, pb:pb + 64], False, True, [b_BKt, b_Vt], inc=(h == 3))
                            for ci in range(2):
                                ycol = slice(ci * 128 + pj, ci * 128 + pj + 64)
                                k.mm(bY, k.ps[bY][:, ycol], Mbf[:, ci, :], AR[:, ci, 1, pj:pj + 64], True, False, [b_Mbf, b_AR], inc=False)
                                for hh in range(2):
                                    h = ci * 2 + hh
                                    pb = 64 * hh
                                    oc = slice(h * 64, (h + 1) * 64)
                                    k.mm(bY, k.ps[bY][pb:pb + 64, ycol], Usb[:, oc], SCb[:, hh * 2 + ci, 128 + pj:128 + pj + 64], False, False, [b_Usb, b_SCb], inc=False)
                                    k.mm(bY, k.ps[bY][pb:pb + 64, ycol], Vt[:, ci, pb:pb + 64], SCk[:, hh * 2 + ci, 128 + pj:128 + pj + 64], False, True, [b_Vt, b_SCk],
                                         inc=(h == 3))
                            k.tt("dve", mt[:], k.ps[bM][:, 0:128].rearrange("p (c n) -> p c n", c=2), M32[:], ALU.add, [k.pb[bM], b_M32], [b_mt])
                            k.tt("dve", Mbf[0:64, :, 0:64], mt[0:64, :, :], gam[0:64, :, jj:jj + 1].to_broadcast([64, 2, 64]), ALU.mult,
                                 [b_mt, b_gam], [b_Mbf])
                            k.tt("dve", Mbf[64:128, :, 64:128], mt[64:128, :, :], gam[64:128, :, jj:jj + 1].to_broadcast([64, 2, 64]), ALU.mult,
                                 [b_mt, b_gam], [b_Mbf])
                            k.tt("dve", M32[:], mt[:], gam[:, :, jj:jj + 1].to_broadcast([128, 2, 64]), ALU.mult, [b_mt, b_gam], [b_M32])
                            yield
                        k.tt("dve", yacc[:, :, tc_], k.ps[bY][:, 0:256].rearrange("p (c t) -> p c t", c=2), yacc[:, :, tc_], ALU.add,
                             [k.pb[bY], b_yt[tt]], [b_yt[tt]])
                        yield

            with ExitStack() as sd:
                threads = [[scan_gen(dr, sd, [4 * dr + i for i in range(4)]), [4 * dr + i for i in range(4)], 0] for dr in range(2)]
                live = list(threads)
                k.rrset, k.rr = threads[0][1], threads[0][2]
                for _ in range(RWSKEW):
                    next(threads[0][0])
                threads[0][2] = k.rr
                if os.environ.get("RWSEQ"):
                    for th in threads:
                        k.rrset, k.rr = th[1], th[2]
                        for _ in th[0]:
                            pass
                    live = []
                while live:
                    for th in list(live):
                        k.rrset, k.rr = th[1], th[2]
                        try:
                            next(th[0])
                        except StopIteration:
                            live.remove(th)
                        th[2] = k.rr
                k.rrset = list(range(8)); k.rr = 0
                P.barrier()
            b_yacc = Buf("rw_yacc_all")
            dump("rw_yacc", yacc[:], b_yacc, [128, 2, NT], BF16)
            with ExitStack() as s5_:
              if RW_LEVEL >= 6:
                    g1, b_g1 = sw["g1"]; g2, b_g2 = sw["g2"]
                    x1t, b_x1t = k.sb(s5_, [32, 512], BF16, "ex1t")
                    aF, b_aF = k.sb(s5_, [128, 2, 512], F32, "eaF"); tF, b_tF = k.sb(s5_, [128, 2, 512], F32, "etF")
                    kmod, b_kmod = k.sb(s5_, [128, 2, 512], F32, "ekm")
                    bon, b_bon = k.sb(s5_, [128, 2, 512], F32, "ebon")
                    ybf, b_ybf = k.sb(s5_, [128, 512], BF16, "eybf"); yc, b_yc = k.sb(s5_, [128, 512], F32, "eyc")
                    rs_, b_rs = k.sb(s5_, [128, 512], F32, "ers"); lnt, b_lnt = k.sb(s5_, [128, 512], F32, "elnt")
                    gh, b_gh = k.sb(s5_, [64, 512], BF16, "egh"); gg, b_gg = k.sb(s5_, [128, 2, 512], F32, "egg")
                    rkb, b_rkb = k.sb(s5_, [128, 512], BF16, "erkb")
                    lneps, b_lneps = k.sb(s5_, [128, 1], F32, "lneps")
                    k.memset("dve", lneps[:], 64e-5, [b_lneps])
                    for g, (c0, n) in enumerate(GROUPS):
                        gc_ = slice(c0, c0 + n)
                        for dr in range(2):
                            a_of(dr, xd[:, :, gc_], n, aF, b_aF, x1t, b_x1t)
                            kmod_of(aF, b_aF, kS[:, :, gc_], n, kmod, b_kmod, tF, b_tF)
                            for ci in range(2):
                                k.stt(rkb[:, :n], kmod[:, ci, :n], cols[:, ci, C_RK:C_RK + 1], rS[:, ci, gc_], ALU.mult, ALU.mult,
                                      [b_kmod, b_cols, b_r], [b_rkb])
                                bk = k.bank()
                                k.mm(bk, k.ps[bk][:, :n], bo64, rkb[:, :n], True, True, [b_rkb, b_cstb])
                                if dr == 0:
                                    k.tt("dve", bon[:, ci, :n], k.ps[bk][:, :n], vS[:, ci, gc_], ALU.mult, [k.pb[bk], b_v], [b_bon])
                                else:
                                    k.tt("dve", tF[:, ci, :n], k.ps[bk][:, :n], vS[:, ci, gc_], ALU.mult, [k.pb[bk], b_v], [b_tF])
                                    k.tt("pool", bon[:, ci, :n], bon[:, ci, :n], tF[:, ci, :n], ALU.add, [b_bon, b_tF], [b_bon])
                        bk = k.bank()
                        for kt in range(2):
                            k.mm(bk, k.ps[bk][0:64, :n], g1[:, kt, :], xd[:, kt, gc_], kt == 0, kt == 1, [b_g1, b_xd])
                        k.act(gh[:, :n], k.ps[bk][0:64, :n], AF.Sigmoid, [k.pb[bk]], [b_gh])
                        for ci in range(2):
                            b2 = k.bank()
                            k.mm(b2, k.ps[b2][:, :n], g2[:, ci * 128:(ci + 1) * 128], gh[:, :n], True, True, [b_g2, b_gh])
                            k.copy("act", gg[:, ci, :n], k.ps[b2][:, :n], [k.pb[b2]], [b_gg])
                        for ci in range(2):
                            bk = k.bank()
                            k.mm(bk, k.ps[bk][:, :n], bo64, yacc[:, ci, gc_], True, True, [b_yacc, b_cstb])
                            k.stt(yc[:, :n], k.ps[bk][:, :n], -1.0 / 64, yacc[:, ci, gc_], ALU.mult, ALU.add, [k.pb[bk], b_yacc], [b_yc])
                            k.act(ybf[:, :n], yc[:, :n], AF.Square, [b_yc], [b_ybf])
                            b2 = k.bank()
                            k.mm(b2, k.ps[b2][:, :n], bo64, ybf[:, :n], True, True, [b_ybf, b_cstb])
                            P.op("act", lambda e: e.activation(out=lnt[:, :n], in_=k.ps[b2][:, :n], func=AF.Ln, scale=1.0 / 64, bias=lneps[:]),
                                 reads=[k.pb[b2], b_lneps], writes=[b_lnt])
                            k.act(rs_[:, :n], lnt[:, :n], AF.Exp, [b_lnt], [b_rs], scale=-0.5)
                            k.stt(yc[:, :n], yc[:, :n], cols[:, ci, C_LNG:C_LNG + 1], rs_[:, :n], ALU.mult, ALU.mult, [b_yc, b_cols, b_rs], [b_yc])
                            k.stt(yc[:, :n], yc[:, :n], cols[:, ci, C_LNB:C_LNB + 1], bon[:, ci, :n], ALU.add, ALU.add, [b_yc, b_cols, b_bon], [b_yc])
                            k.tt("dve", ydst[:, ci, gc_], yc[:, :n], gg[:, ci, :n], ALU.mult, [b_yc, b_gg, b_xd], [b_ydst, b_xd])
                    P.barrier()
        dump("yd", ydst[:], b_ydst, [128, 2, NT], BF16)

    def final_out():
        with ExitStack() as st:
            ot = [k.sb(st, [128, D], F32, "ostage") for _ in range(2)]
            for tt in range(NLAT // 128):
                o, ob = ot[tt % 2]
                c0 = NCTX + tt * 128
                for half in range(2):
                    bk = k.bank()
                    for j in range(4):
                        kt = half * 4 + j
                        P.op("pe", lambda e: e.transpose(k.ps[bk][:, j * 128:(j + 1) * 128], xT[:, kt, c0:c0 + 128], identf),
                             reads=[b_xT, b_cstf], writes=[k.pb[bk]], inc=(j == 3))
                    k.copy("dve" if half == 0 else "act", o[:, half * 512:(half + 1) * 512], k.ps[bk][:], [k.pb[bk]], [ob])
                P.dma("sp", out_d[tt * 128:(tt + 1) * 128, :], o[:], reads=[ob])
            P.finish([b for _, b in ot])
            P.barrier()

    for l in range(DEPTH):
        if ("L%d" % l) not in stages:
            continue
        with ExitStack() as st:
            rms_stats(st)
            P.barrier()
        if "rw" in stages:
            rwkv_mixer(l)
        if "da" in stages:
            da_mixer(l)
        if "mla" in stages:
            mla_mixer(l)
        if "s5" in stages:
            s5_mixer(l)
        if "merge" in stages:
            merge(l)
        if "moe" in stages:
            moe(l)
    final_out()
    for e in ("sp",):
        toks = [(kk, v) for kk, v in P.cnt.items() if v > 0 and isinstance(kk, tuple)]
        for tok in toks:
            P._wait(e, tok)
    top.close()
    return nc, k, dbg_out


_CONSTS = None


def prep_shared(inp):
    g = {}
    g.update(host_consts())
    f32 = np.float32
    w_in = np.asarray(inp["w_in"], f32)
    offs = np.cumsum([0, 256, 256, 256, 256, 192, 128, 16, 256, 256, 256, 256, 4096])
    seg = lambda i: w_in[:, :, offs[i]:offs[i + 1]]
    g["w_ada"] = np.ascontiguousarray(inp["w_ada"], f32)
    ba = np.asarray(inp["b_ada"], f32)
    g["bada"] = np.ascontiguousarray(ba.reshape(DEPTH, 48, 128).transpose(0, 2, 1))
    g["gmix"] = np.stack([fm_cols(inp["norm_mix_g"][l], 8) for l in range(DEPTH)])
    g["gffn"] = np.stack([fm_cols(inp["norm_ffn_g"][l], 8) for l in range(DEPTH)])
    def pad3(w):
        o = np.zeros((DEPTH, D, 384), f32)
        for j in range(8):
            o[:, :, (j // 3) * 128 + (j % 3) * 32:(j // 3) * 128 + (j % 3) * 32 + 32] = w[:, :, j * 32:(j + 1) * 32]
        return o
    g["w_da_qk"] = np.ascontiguousarray(np.concatenate([pad3(seg(0)), pad3(seg(1))], axis=2))
    g["w_da_v"] = np.ascontiguousarray(seg(2))
    dg = np.asarray(inp["da_qk_norm_g"], f32)
    g["da_g"] = np.ascontiguousarray(np.stack([np.tile(dg[:, 0, :], (1, 4)), np.tile(dg[:, 1, :], (1, 4))], axis=2))
    g["da_lam"] = np.ascontiguousarray(np.broadcast_to(np.asarray(inp["da_lambda"], f32).reshape(DEPTH, 1, 128), (DEPTH, 128, 128)))
    g["da_sub"] = np.ascontiguousarray(np.broadcast_to(np.asarray(inp["da_subln_g"], f32).reshape(DEPTH, 1, 64), (DEPTH, 128, 64)))
    g["w_mla_c"] = np.zeros((DEPTH, D, 384), f32)
    g["w_mla_c"][:, :, 0:192] = seg(4)
    g["w_mla_c"][:, :, 256:384] = seg(5)
    g["w_mla_kr"] = np.zeros((DEPTH, D, 128), f32)
    g["w_mla_kr"][:, :, 32:48] = seg(6)
    g["w_mla_kr"][:, :, 96:112] = seg(6)
    wuq = np.asarray(inp["mla_w_uq"], f32)
    wuqp = np.zeros((DEPTH, 256, 256), f32)
    for h in range(4):
        wuqp[:, 0:192, 64 * h:64 * h + 48] = wuq[:, :, 48 * h:48 * h + 48]
    g["w_uq"] = np.ascontiguousarray(wuqp.reshape(DEPTH, 2, 128, 256).transpose(0, 2, 1, 3))
    wukv = np.asarray(inp["mla_w_ukv"], f32)
    g["w_ukvk"] = np.zeros((DEPTH, 128, 256), f32)
    g["w_ukvv"] = np.zeros((DEPTH, 128, 256), f32)
    for h in range(4):
        g["w_ukvk"][:, :, 64 * h:64 * h + 32] = wukv[:, :, 96 * h:96 * h + 32]
        g["w_ukvv"][:, :, 64 * h:64 * h + 64] = wukv[:, :, 96 * h + 32:96 * h + 96]
    gcq = np.zeros((DEPTH, 256), f32); gcq[:, 0:192] = np.asarray(inp["mla_cq_norm_g"], f32)
    gkv = np.asarray(inp["mla_ckv_norm_g"], f32)
    g["mla_gc"] = np.ascontiguousarray(np.stack([gcq[:, 0:128], gcq[:, 128:256], gkv], axis=2))
    mg = np.asarray(inp["mla_qk_norm_g"], f32)
    mgp = np.zeros((DEPTH, 2, 128), f32)
    for h in range(2):
        mgp[:, :, 64 * h:64 * h + 48] = mg
    g["mla_g"] = np.ascontiguousarray(mgp.transpose(0, 2, 1))
    g["w_s5"] = np.ascontiguousarray(seg(3))
    Bre = np.asarray(inp["s5_b_re"], f32); Bim = np.asarray(inp["s5_b_im"], f32)
    Cre = np.asarray(inp["s5_c_re"], f32); Cim = np.asarray(inp["s5_c_im"], f32)
    s5B = np.zeros((DEPTH, 2, 2, 128, 1024), f32)
    s5C = np.zeros((DEPTH, 2, 2, 128, 8, 128), f32)
    for gi in range(16):
        kt, gl = gi // 8, gi % 8
        s5B[:, :, kt, gl * 16:(gl + 1) * 16, gl * 64:(gl + 1) * 64] = Bre[:, :, gi].transpose(0, 1, 3, 2)
        s5B[:, :, kt, gl * 16:(gl + 1) * 16, 512 + gl * 64:512 + (gl + 1) * 64] = Bim[:, :, gi].transpose(0, 1, 3, 2)
        i, po = gi // 2, (gi % 2) * 64
        s5C[:, :, 0, po:po + 64, i, gl * 16:(gl + 1) * 16] = Cre[:, :, gi].transpose(0, 1, 3, 2)
        s5C[:, :, 1, po:po + 64, i, gl * 16:(gl + 1) * 16] = Cim[:, :, gi].transpose(0, 1, 3, 2)
    g["s5B"] = s5B; g["s5C"] = s5C
    lre = np.asarray(inp["s5_lam_re"], f32).reshape(DEPTH, 2, 1024)
    lim = np.asarray(inp["s5_lam_im"], f32).reshape(DEPTH, 2, 1024)
    ldt = np.repeat(np.asarray(inp["s5_log_dt"], f32), 64, axis=2)
    tm = np.stack([lre, lim, ldt], axis=2)
    g["s5tm"] = np.ascontiguousarray(np.broadcast_to(tm[:, :, :, None, :], (DEPTH, 2, 3, 128, 1024)))
    g["s5fm"] = np.ascontiguousarray(tm.reshape(DEPTH, 2, 3, 8, 128).transpose(0, 1, 4, 2, 3))
    g["s5d"] = np.stack([fm_cols(np.asarray(inp["s5_d"], f32)[l].reshape(256), 2) for l in range(DEPTH)])
    g["s5bg"] = np.stack([fm_cols(np.asarray(inp["s5_b_glu"], f32)[l], 2) for l in range(DEPTH)])
    g["w_glu"] = np.ascontiguousarray(inp["s5_w_glu"], f32)
    pidx = np.arange(128, dtype=f32)
    posc = np.zeros((2, 128, 2), f32); posc[0, :, 0] = -pidx; posc[1, :, 0] = -(127 - pidx)
    posr = np.zeros((2, 128, 128), f32); posr[0] = pidx[None, :]; posr[1] = (127 - pidx)[None, :]
    tri = np.zeros((2, 128, 128), f32)
    tri[0] = (pidx[:, None] <= pidx[None, :]).astype(f32)
    tri[1] = (pidx[:, None] >= pidx[None, :]).astype(f32)
    g["posc"] = posc; g["posr"] = posr; g["tri"] = tri
    g["w_rw"] = np.ascontiguousarray(np.concatenate([seg(7), seg(8), seg(9), seg(10)], axis=2))
    mu = np.asarray(inp["rw_mu"], f32).reshape(DEPTH, 1, 1024)
    g["rw_mu_row"] = np.ascontiguousarray(np.broadcast_to(mu, (DEPTH, 128, 1024)))
    colsl = []
    for l in range(DEPTH):
        cs = [inp["rw_k_k"][l], inp["rw_k_a"][l], inp["rw_ln_g"][l], inp["rw_ln_b"][l], np.asarray(inp["rw_r_k"][l]).reshape(256),
              inp["rw_w0"][l, 0], inp["rw_w0"][l, 1], inp["rw_a0"][l, 0], inp["rw_a0"][l, 1]]
        colsl.append(np.stack([fm_cols(c, 2) for c in cs], axis=2))
    g["rw_cols"] = np.ascontiguousarray(np.stack(colsl))
    g["rw_w0row"] = np.ascontiguousarray(np.asarray(inp["rw_w0"], f32).reshape(DEPTH, 2, 1, 256))
    for nm in ("rw_w1", "rw_w2", "rw_a1", "rw_a2", "rw_g1", "rw_g2"):
        g[nm] = np.ascontiguousarray(inp[nm], f32)
    t = np.arange(128)
    same = (t[:, None] // 64) == (t[None, :] // 64)
    tri = np.zeros((2, 128, 2, 128), f32)
    tri[0, :, 0, :] = (same & (t[:, None] <= t[None, :])); tri[0, :, 1, :] = (same & (t[:, None] < t[None, :]))
    tri[1, :, 0, :] = (same & (t[:, None] >= t[None, :])); tri[1, :, 1, :] = (same & (t[:, None] > t[None, :]))
    g["rw_tri"] = tri
    ind = np.zeros((128, 2), f32); ind[:64, 0] = 1; ind[64:, 1] = 1
    g["rw_ind"] = ind
    m1 = np.zeros((2, 128, 256), f32)
    m1[0, :, :128] = (same & (t[:, None] < t[None, :])); m1[0, :, 128:] = (same & (t[:, None] <= t[None, :]))
    m1[1, :, :128] = (same & (t[:, None] > t[None, :])); m1[1, :, 128:] = (same & (t[:, None] >= t[None, :]))
    g["rw_m1"] = m1
    mnt = np.zeros((2, 128, 128), f32)
    mnt[0] = (same & (t[None, :] < t[:, None])); mnt[1] = (same & (t[None, :] > t[:, None]))
    g["rw_mnt"] = mnt
    g["w_gates"] = np.ascontiguousarray(seg(11).reshape(DEPTH, 8, 128, 4, 8, 128).transpose(0, 4, 2, 1, 3, 5)).reshape(DEPTH, 8, 128, 4096)
    g["w_branch"] = np.ascontiguousarray(np.asarray(inp["w_branch"], f32).reshape(DEPTH, 4, 2, 128, D).transpose(0, 3, 2, 1, 4)).reshape(DEPTH, 128, 8192)
    g["w_out"] = np.ascontiguousarray(inp["w_out"], f32)
    g["router_w"] = np.ascontiguousarray(inp["router_w"], f32)
    g["router_b"] = np.ascontiguousarray(np.broadcast_to(np.asarray(inp["router_bias"], f32).reshape(1, 16), (128, 16)))
    sel = np.zeros((16, 16, 128), f32)
    for e in range(16):
        sel[e, e, :] = 1.0
    g["sel"] = sel
    g["exp_w_gate"] = np.ascontiguousarray(inp["exp_w_gate"], f32)
    g["exp_w_up"] = np.ascontiguousarray(inp["exp_w_up"], f32)
    g["exp_w_down"] = np.ascontiguousarray(inp["exp_w_down"], f32)
    return g


def prep_core(inp, b):
    x = np.asarray(inp["x"], np.float32)[b]
    ctx = np.asarray(inp["ctx"], np.float32)[b]
    xin = np.ascontiguousarray(np.concatenate([ctx, x], axis=0))
    c2 = np.stack([np.asarray(inp["c"], np.float32)[b], np.asarray(inp["c_ctx"], np.float32)], axis=0)
    c2T = np.ascontiguousarray(c2.reshape(2, 8, 128).transpose(2, 1, 0))
    return {"xin": xin, "c2T": c2T}


RW_LEVEL = int(os.environ.get('RW_LEVEL', '9'))
RWSTOP = int(os.environ.get('RWSTOP', '9'))
S5SKEW = int(os.environ.get('S5SKEW', '1'))
RWSKEW = int(os.environ.get('RWSKEW', '0'))
RW_SUB = int(os.environ.get('RW_SUB', '9'))
STAGES_ALL = ("L0", "L1", "da", "mla", "s5", "rw", "merge", "moe")


def kernel(**inputs):
    nc, k, _ = build(STAGES_ALL)
    shared = prep_shared(inputs)
    in_maps = []
    for b in range(8):
        m = dict(shared)
        m.update(prep_core(inputs, b))
        in_maps.append({kk: m[kk] for kk in k.ins})
    res = run_bass_kernel_spmd(nc, in_maps, core_ids=list(range(8)))
    return np.stack([np.asarray(r["out"]) for r in res.results], axis=0).astype(np.float32)
```

```python
import math
import os
import numpy as np
import concourse.bass as bass
import concourse.mybir as mybir
from concourse.bass_utils import run_bass_kernel_spmd

F32 = mybir.dt.float32
BF16 = mybir.dt.bfloat16
I32 = mybir.dt.int32
AF = mybir.ActivationFunctionType
ALU = mybir.AluOpType
AX = mybir.AxisListType

D = 1024
NT = 2304
NCTX = 256
NLAT = 2048
DEPTH = 2
EPS = 1e-6
GROUPS = [(0, 256)] + [(256 + 512 * i, 512) for i in range(4)]
NTT = 18


class Buf:
    __slots__ = ("name", "w", "r", "excl")

    def __init__(self, name="", excl=False):
        self.name = name
        self.w = None
        self.r = {}
        self.excl = excl


class Prog:
    NDMA = 8

    def __init__(self, nc):
        self.nc = nc
        self.eng = {"pe": nc.tensor, "act": nc.scalar, "dve": nc.vector, "pool": nc.gpsimd, "sp": nc.sync}
        self.sems = {}
        self.cnt = {}
        self.seen = {e: {} for e in self.eng}
        self.pend = {e: ([], []) for e in self.eng}
        for e in self.eng:
            self.sems[e] = nc.alloc_semaphore("s_" + e)
            self.cnt[e] = 0
        self.dq = {}
        for q in ("sp", "pool"):
            lst = []
            for i in range(self.NDMA):
                key = ("d", q, i)
                self.sems[key] = nc.alloc_semaphore(f"d_{q}{i}")
                self.cnt[key] = 0
                lst.append(key)
            self.dq[q] = [lst, 0, [None] * self.NDMA]
        self.ninst = 0

    def _wait(self, e, tok):
        key, val = tok
        if key == e:
            if e == "pe":
                return
            if val < self.cnt[e] - 1:
                return
        if self.seen[e].get(key, 0) >= val:
            return
        self.eng[e].wait_ge(self.sems[key], val)
        self.seen[e][key] = val
        self.ninst += 1

    def _deps(self, e, reads, writes):
        for b in reads:
            if b.w is not None:
                self._wait(e, b.w)
            if b.excl:
                for tok in b.r.values():
                    if tok[0] != e:
                        self._wait(e, tok)
        for b in writes:
            if b.w is not None:
                self._wait(e, b.w)
            for tok in b.r.values():
                if tok[0] != e:
                    self._wait(e, tok)

    def op(self, e, fn, reads=(), writes=(), inc=True):
        self._deps(e, reads, writes)
        inst = fn(self.eng[e])
        self.ninst += 1
        pr, pw = self.pend[e]
        pr.extend(reads)
        pw.extend(writes)
        if inc:
            self.cnt[e] += 1
            tok = (e, self.cnt[e])
            inst.then_inc(self.sems[e], 1)
            for b in pr:
                b.r[e] = tok
            for b in pw:
                b.w = tok
                b.r = {}
            self.pend[e] = ([], [])
        return inst

    def dma(self, q, out, in_, reads=(), writes=(), **kw):
        lst, rr, last = self.dq[q]
        k = rr
        self.dq[q][1] = (rr + 1) % self.NDMA
        if last[k] is not None:
            self._wait(q, last[k])
        self._deps(q, reads, writes)
        inst = self.eng[q].dma_start(out=out, in_=in_, **kw)
        self.ninst += 1
        key = lst[k]
        self.cnt[key] += 16
        tok = (key, self.cnt[key])
        inst.then_inc(self.sems[key], 16)
        last[k] = tok
        for b in reads:
            b.r[key] = tok
        for b in writes:
            b.w = tok
            b.r = {}
        return tok

    def finish(self, bufs, e="sp"):
        for b in bufs:
            if b.w is not None:
                self._wait(e, b.w)
            for tok in b.r.values():
                self._wait(e, tok)

    def barrier(self):
        toks = [(k, v) for k, v in self.cnt.items() if v > 0]
        for e in self.eng:
            for tok in toks:
                if tok[0] != e:
                    self._wait(e, tok)


from contextlib import ExitStack


class K:
    def __init__(self, nc, dbg=None):
        self.nc = nc
        self.P = Prog(nc)
        self.dbg = dbg or {}
        self.ins = {}
        self.uid = 0
        self.ps = []
        self.pb = []
        for i in range(8):
            self.ps.append(nc.alloc_psum_tensor(f"ps{i}", [128, 512], F32))
            self.pb.append(Buf(f"ps{i}", excl=True))
        self.rr = 0
        self.rrset = list(range(8))

    def din(self, name, shape, dtype=F32):
        t = self.nc.dram_tensor(name, list(shape), dtype, kind="ExternalInput").ap()
        self.ins[name] = t
        return t

    def sb(self, stack, shape, dtype, name=None):
        self.uid += 1
        nm = f"{name or 't'}_{self.uid}"
        t = stack.enter_context(self.nc.sbuf_tensor(nm, list(shape), dtype))
        nb = int(np.prod(shape[1:])) * (2 if dtype == BF16 else 4)
        self.live = getattr(self, "live", 0) + nb
        if self.live > getattr(self, "peak", 0):
            self.peak = self.live; self.peak_at = nm
        def _dec(nb=nb):
            self.live -= nb
        stack.callback(_dec)
        return t, Buf(nm)

    def bank(self):
        i = self.rrset[self.rr % len(self.rrset)]
        self.rr += 1
        return i

    def mm(self, bank, out, lhsT, rhs, start, stop, reads, inc=None):
        self.P.op("pe", lambda e: e.matmul(out, lhsT=lhsT, rhs=rhs, start=start, stop=stop, skip_group_check=True),
                  reads=reads, writes=[self.pb[bank]], inc=(stop if inc is None else inc))

    def act(self, out, in_, func, reads, writes, scale=None, bias=None, eng="act"):
        kw = {}
        if scale is not None:
            kw["scale"] = scale
        if bias is not None:
            kw["bias"] = bias
        self.P.op("act", lambda e: e.activation(out=out, in_=in_, func=func, **kw), reads=reads, writes=writes)

    def tt(self, eng, out, in0, in1, op, reads, writes):
        self.P.op(eng, lambda e: e.tensor_tensor(out=out, in0=in0, in1=in1, op=op), reads=reads, writes=writes)

    def ts(self, eng, out, in0, s1, s2, op0, op1, reads, writes):
        if op1 is None:
            self.P.op(eng, lambda e: e.tensor_scalar(out=out, in0=in0, scalar1=s1, scalar2=None, op0=op0),
                      reads=reads, writes=writes)
        else:
            self.P.op(eng, lambda e: e.tensor_scalar(out=out, in0=in0, scalar1=s1, scalar2=s2, op0=op0, op1=op1),
                      reads=reads, writes=writes)

    def stt(self, out, in0, scalar, in1, op0, op1, reads, writes):
        self.P.op("dve", lambda e: e.scalar_tensor_tensor(out=out, in0=in0, scalar=scalar, in1=in1, op0=op0, op1=op1),
                  reads=reads, writes=writes)

    def copy(self, eng, out, in_, reads, writes):
        if eng == "act":
            self.P.op("act", lambda e: e.copy(out=out, in_=in_), reads=reads, writes=writes)
        else:
            self.P.op(eng, lambda e: e.tensor_copy(out=out, in_=in_), reads=reads, writes=writes)

    def memset(self, eng, ap, val, writes):
        self.P.op(eng, lambda e: e.memset(ap, val), writes=writes)

    def load(self, q, out, in_, writes, reads=()):
        self.P.dma(q, out, in_, reads=reads, writes=writes)

    def rstd_from_ss(self, out, ss_ps, bank, inv_n, tmp, tmpb, writes):
        self.P.op("act", lambda e: e.activation(out=tmp, in_=ss_ps, func=AF.Ln, scale=inv_n, bias=self.eps_col[:]),
                  reads=[self.pb[bank], self.b_const], writes=[tmpb])
        self.P.op("act", lambda e: e.activation(out=out, in_=tmp, func=AF.Exp, scale=-0.5), reads=[tmpb], writes=writes)


def host_consts():
    ident = np.eye(128, dtype=np.float32)
    bo32 = np.kron(np.eye(4, dtype=np.float32), np.ones((32, 32), np.float32))
    bo64 = np.kron(np.eye(2, dtype=np.float32), np.ones((64, 64), np.float32))
    Rda = np.zeros((128, 128), np.float32)
    for h in range(4):
        for d in range(16):
            Rda[32 * h + d, 32 * h + d + 16] = -1.0
            Rda[32 * h + d + 16, 32 * h + d] = 1.0
    Rmla = np.zeros((128, 128), np.float32)
    for h in range(2):
        for d in range(8):
            Rmla[64 * h + 32 + d, 64 * h + 40 + d] = -1.0
            Rmla[64 * h + 40 + d, 64 * h + 32 + d] = 1.0
    cst = np.concatenate([ident, bo32, bo64, Rda.T.copy(), Rmla.T.copy()], axis=1)
    def tables(rot_dim):
        rows = NLAT // 64
        row = np.repeat(np.arange(rows, dtype=np.float32), 64)
        col = np.tile(np.arange(64, dtype=np.float32), rows)
        n_freq = rot_dim // 4
        inv = (10000.0 ** (-np.arange(n_freq, dtype=np.float32) / n_freq)).astype(np.float32)
        ang = np.concatenate([row[:, None] * inv, col[:, None] * inv], axis=-1)
        return np.cos(ang).astype(np.float32), np.sin(ang).astype(np.float32)
    c, s = tables(32)
    da_cos = np.ones((128, NT), np.float32)
    da_sin = np.zeros((128, NT), np.float32)
    for h in range(4):
        for d in range(32):
            da_cos[32 * h + d, NCTX:] = c[:, d % 16]
            da_sin[32 * h + d, NCTX:] = s[:, d % 16]
    c, s = tables(16)
    ml_cos = np.ones((128, NT), np.float32)
    ml_sin = np.zeros((128, NT), np.float32)
    for h in range(2):
        for d in range(16):
            ml_cos[64 * h + 32 + d, NCTX:] = c[:, d % 8]
            ml_sin[64 * h + 32 + d, NCTX:] = s[:, d % 8]
    return dict(cst=cst, da_cos=da_cos, da_sin=da_sin, ml_cos=ml_cos, ml_sin=ml_sin)


def fm_cols(v, ntile):
    return np.ascontiguousarray(np.asarray(v, np.float32).reshape(ntile, 128).T)


def build(stages, dbg_names=()):
    nc = bass.Bass("TRN2", target_bir_lowering=False)
    k = K(nc)
    P = k.P
    top = ExitStack()
    xin = k.din("xin", [NT, D])
    c2T = k.din("c2T", [128, 8, 2])
    cst_d = k.din("cst", [128, 640])
    da_cos_d = k.din("da_cos", [128, NT]); da_sin_d = k.din("da_sin", [128, NT])
    ml_cos_d = k.din("ml_cos", [128, NT]); ml_sin_d = k.din("ml_sin", [128, NT])
    w_ada = k.din("w_ada", [DEPTH, D, 6 * D])
    bada_d = k.din("bada", [DEPTH, 128, 48])
    gmix_d = k.din("gmix", [DEPTH, 128, 8]); gffn_d = k.din("gffn", [DEPTH, 128, 8])
    w_da_qk = k.din("w_da_qk", [DEPTH, D, 768]); w_da_v = k.din("w_da_v", [DEPTH, D, 256])
    da_g_d = k.din("da_g", [DEPTH, 128, 2])
    da_lam_d = k.din("da_lam", [DEPTH, 128, 128])
    da_sub_d = k.din("da_sub", [DEPTH, 128, 64])
    w_mla_c = k.din("w_mla_c", [DEPTH, D, 384]); w_mla_kr = k.din("w_mla_kr", [DEPTH, D, 128])
    w_uq_d = k.din("w_uq", [DEPTH, 128, 2, 256]); w_ukvk_d = k.din("w_ukvk", [DEPTH, 128, 256]); w_ukvv_d = k.din("w_ukvv", [DEPTH, 128, 256])
    mla_gc_d = k.din("mla_gc", [DEPTH, 128, 3])
    mla_g_d = k.din("mla_g", [DEPTH, 128, 2])
    w_gates_d = k.din("w_gates", [DEPTH, 8, 128, 4096]); w_branch_d = k.din("w_branch", [DEPTH, 128, 8192]); w_out_d = k.din("w_out", [DEPTH, D, D])
    router_w_d = k.din("router_w", [D, 16]); router_b_d = k.din("router_b", [128, 16]); sel_d = k.din("sel", [16, 16, 128])
    exp_g_d = k.din("exp_w_gate", [DEPTH, 16, D, 512]); exp_u_d = k.din("exp_w_up", [DEPTH, 16, D, 512]); exp_d_d = k.din("exp_w_down", [DEPTH, 16, 512, D])
    w_s5_d = k.din("w_s5", [DEPTH, D, 256])
    s5B_d = k.din("s5B", [DEPTH, 2, 2, 128, 1024])
    s5C_d = k.din("s5C", [DEPTH, 2, 2, 128, 8, 128])
    s5tm_d = k.din("s5tm", [DEPTH, 2, 3, 128, 1024])
    s5fm_d = k.din("s5fm", [DEPTH, 2, 128, 3, 8])
    s5d_d = k.din("s5d", [DEPTH, 128, 2]); s5bg_d = k.din("s5bg", [DEPTH, 128, 2])
    w_glu_d = k.din("w_glu", [DEPTH, 256, 256])
    posc_d = k.din("posc", [2, 128, 2]); posr_d = k.din("posr", [2, 128, 128]); tri_d = k.din("tri", [2, 128, 128])
    w_rw_d = k.din("w_rw", [DEPTH, D, 1024]); rw_mu_d = k.din("rw_mu_row", [DEPTH, 128, 1024])
    rw_cols_d = k.din("rw_cols", [DEPTH, 128, 2, 9])
    rw_w0row_d = k.din("rw_w0row", [DEPTH, 2, 1, 256])
    rw_w1_d = k.din("rw_w1", [DEPTH, 2, 256, 32]); rw_w2_d = k.din("rw_w2", [DEPTH, 2, 32, 256])
    rw_a1_d = k.din("rw_a1", [DEPTH, 2, 256, 32]); rw_a2_d = k.din("rw_a2", [DEPTH, 2, 32, 256])
    rw_g1_d = k.din("rw_g1", [DEPTH, 256, 64]); rw_g2_d = k.din("rw_g2", [DEPTH, 64, 256])
    rw_tri_d = k.din("rw_tri", [2, 128, 2, 128]); rw_ind_d = k.din("rw_ind", [128, 2])
    rw_m1_d = k.din("rw_m1", [2, 128, 256]); rw_mnt_d = k.din("rw_mnt", [2, 128, 128])
    out_d = nc.dram_tensor("out", [NLAT, D], F32, kind="ExternalOutput").ap()
    dbg_out = {}

    xT, b_xT = k.sb(top, [128, 8, NT], F32, "xT")
    cstf, b_cstf = k.sb(top, [128, 640], F32, "cstf")
    cstb, b_cstb = k.sb(top, [128, 640], BF16, "cstb")
    onesb, b_ones = k.sb(top, [128, 128], BF16, "ones")
    k.eps_col, k.b_const = k.sb(top, [128, 1], F32, "eps")
    MOD, b_MOD = k.sb(top, [128, DEPTH, 6, 8, 2], F32, "MOD")
    AB, b_AB = k.sb(top, [128, DEPTH, 2, 2, 8, 2], F32, "AB")
    rstd_b, b_rstd = k.sb(top, [128, NT], F32, "rstd")
    yall, b_yall = k.sb(top, [128, 4, 2, NT], BF16, "yall")
    yT = []
    for i in range(4):
        yT.append((yall[:, i], Buf(f"y{i}")))
    k.memset("dve", k.eps_col[:], EPS, [k.b_const])
    k.memset("dve", onesb[:], 1.0, [b_ones])
    k.load("sp", cstf[:], cst_d, [b_cstf])
    k.copy("dve", cstb[:], cstf[:], [b_cstf], [b_cstb])
    identf = cstf[:, 0:128]
    identb = cstb[:, 0:128]
    bo32 = cstb[:, 128:256]; bo64 = cstb[:, 256:384]; Rda = cstb[:, 384:512]; Rmla = cstb[:, 512:640]

    def dump(name, ap_sb, buf, shape, dtype=F32):
        if name in dbg_names:
            d = nc.dram_tensor("dbg_" + name, list(shape), dtype, kind="ExternalOutput").ap()
            dbg_out[name] = d
            P.dma("sp", d, ap_sb, reads=[buf])
            P.barrier()

    with ExitStack() as st:
        xs = [k.sb(st, [128, D], F32, "xstage") for _ in range(2)]
        for tt in range(NTT):
            xt, xb = xs[tt % 2]
            k.load("sp", xt[:], xin[tt * 128:(tt + 1) * 128, :], [xb])
            for half in range(2):
                bk = k.bank()
                for j in range(4):
                    kt = half * 4 + j
                    P.op("pe", lambda e: e.transpose(k.ps[bk][:, j * 128:(j + 1) * 128], xt[:, kt * 128:(kt + 1) * 128], identf),
                         reads=[xb, b_cstf], writes=[k.pb[bk]], inc=(j == 3))
                k.copy("dve" if half == 0 else "act", xT[:, half * 4:half * 4 + 4, tt * 128:(tt + 1) * 128],
                       k.ps[bk][:].rearrange("p (j t) -> p j t", j=4), [k.pb[bk]], [b_xT])
        scT, b_scT = k.sb(st, [128, 8, 2], F32, "scT")
        scb, b_scb = k.sb(st, [128, 8, 2], BF16, "scb")
        bada, b_bada = k.sb(st, [128, DEPTH, 48], F32, "bada")
        gm, b_gm = k.sb(st, [128, DEPTH, 2, 8], F32, "gm")
        k.load("sp", scT[:], c2T, [b_scT])
        k.act(scb[:], scT[:], AF.Silu, [b_scT], [b_scb])
        for l in range(DEPTH):
            k.load("sp", bada[:, l, :], bada_d[l], [b_bada])
            k.load("sp", gm[:, l, 0, :], gmix_d[l], [b_gm])
            k.load("sp", gm[:, l, 1, :], gffn_d[l], [b_gm])
        wa = [k.sb(st, [128, 8, 1024], BF16, "wada") for _ in range(2)]
        ci = 0
        for l in range(DEPTH):
            for ch in range(6):
                wt, wb = wa[ci % 2]; ci += 1
                k.load("pool", wt[:], w_ada[l, :, ch * 1024:(ch + 1) * 1024].rearrange("(kt p) n -> p kt n", p=128), [wb])
                bk = k.bank()
                for ft in range(8):
                    for kt in range(8):
                        k.mm(bk, k.ps[bk][:, ft * 2:ft * 2 + 2], wt[:, kt, ft * 128:(ft + 1) * 128], scb[:, kt, :],
                             kt == 0, kt == 7, [wb, b_scb])
                k.tt("dve", MOD[:, l, ch, :, :], k.ps[bk][:, 0:16].rearrange("p (f j) -> p f j", j=2),
                     bada[:, l, ch * 8:(ch + 1) * 8].unsqueeze(2).to_broadcast([128, 8, 2]), ALU.add,
                     [k.pb[bk], b_bada], [b_MOD])
        for l in range(DEPTH):
            for m in range(2):
                sh, sc = (0, 1) if m == 0 else (3, 4)
                P.op("dve", lambda e: e.scalar_tensor_tensor(out=AB[:, l, m, 0, :, :], in0=MOD[:, l, sc, :, :], scalar=1.0,
                                                             in1=gm[:, l, m, :].unsqueeze(2).to_broadcast([128, 8, 2]),
                                                             op0=ALU.add, op1=ALU.mult),
                     reads=[b_MOD, b_gm], writes=[b_AB])
                k.copy("dve", AB[:, l, m, 1, :, :], MOD[:, l, sh, :, :], [b_MOD], [b_AB])
        P.barrier()
    dump("xT", xT[:], b_xT, [128, 8, NT])
    dump("MOD", MOD[:], b_MOD, [128, DEPTH, 6, 8, 2])

    def jof(g):
        return 1 if g == 0 else 0

    def rms_stats(st):
        sq = [k.sb(st, [128, 512], BF16, "sq") for _ in range(2)]
        lnt, b_lnt = k.sb(st, [128, 512], F32, "lnt")
        i = 0
        for (c0, n) in GROUPS:
            bk = k.bank()
            for kt in range(8):
                s, sbf = sq[i % 2]; i += 1
                k.act(s[:, :n], xT[:, kt, c0:c0 + n], AF.Square, [b_xT], [sbf])
                k.mm(bk, k.ps[bk][:, :n], onesb[:], s[:, :n], kt == 0, kt == 7, [sbf, b_ones])
            k.rstd_from_ss(rstd_b[:, c0:c0 + n], k.ps[bk][:, :n], bk, 1.0 / D, lnt[:, :n], b_lnt, [b_rstd])

    def h_group(l, m, g, hg, hb, tmp_, tmpb_):
        c0, n = GROUPS[g]
        j = jof(g)
        for kt in range(8):
            if isinstance(tmp_, list):
                tmp, tmpb = tmp_[kt % 2]
            else:
                tmp, tmpb = tmp_, tmpb_
            k.tt("dve", tmp[:, :n], xT[:, kt, c0:c0 + n], rstd_b[:, c0:c0 + n], ALU.mult, [b_xT, b_rstd], [tmpb])
            P.op("act", lambda e: e.activation(out=hg[:, kt, :n], in_=tmp[:, :n], func=AF.Identity,
                                               scale=AB[:, l, m, 0, kt, j:j + 1], bias=AB[:, l, m, 1, kt, j:j + 1]),
                 reads=[tmpb, b_AB], writes=[hb])

    def load_w(q, st, src, ncols, name):
        wt, wb = k.sb(st, [128, 8, ncols], BF16, name)
        k.load(q, wt[:], src.rearrange("(kt p) n -> p kt n", p=128), [wb])
        return wt, wb

    def proj(bk, wt, wb, col0, hg, hb, n, start=True, stop=True):
        for kt in range(8):
            k.mm(bk, k.ps[bk][:, :n], wt[:, kt, col0:col0 + 128], hg[:, kt, :n], start and kt == 0, stop and kt == 7, [wb, hb])

    def headnorm_rope(st, src_bk, n, c0, blockones, inv_dim, gcol, gbuf, Rm, cos_d, sin_d, dst, dstb, scr):
        sq, b_sq, rs, b_rs, lnt, b_lnt, qn, b_qn, ct, b_ct, sn, b_sn, t1, b_t1, t2, b_t2 = scr
        src = k.ps[src_bk][:, :n]
        k.act(sq[:, :n], src, AF.Square, [k.pb[src_bk]], [b_sq])
        b2 = k.bank()
        k.mm(b2, k.ps[b2][:, :n], blockones, sq[:, :n], True, True, [b_sq, b_cstb])
        k.rstd_from_ss(rs[:, :n], k.ps[b2][:, :n], b2, inv_dim, lnt[:, :n], b_lnt, [b_rs])
        k.stt(qn[:, :n], src, gcol, rs[:, :n], ALU.mult, ALU.mult, [k.pb[src_bk], gbuf, b_rs], [b_qn])
        k.load("sp", ct[:, :n], cos_d[:, c0:c0 + n], [b_ct])
        k.load("sp", sn[:, :n], sin_d[:, c0:c0 + n], [b_sn])
        b3 = k.bank()
        k.mm(b3, k.ps[b3][:, :n], Rm, qn[:, :n], True, True, [b_qn, b_cstb])
        k.tt("pool", t1[:, :n], qn[:, :n], ct[:, :n], ALU.mult, [b_qn, b_ct], [b_t1])
        k.tt("dve", t2[:, :n], k.ps[b3][:, :n], sn[:, :n], ALU.mult, [k.pb[b3], b_sn], [b_t2])
        k.tt("pool", dst, t1[:, :n], t2[:, :n], ALU.add, [b_t1, b_t2], [dstb])

    def norm_scratch(st):
        out = []
        for nm, dt in (("sq", BF16), ("rs", F32), ("lnt", F32), ("qn", BF16), ("ct", F32), ("sn", F32), ("t1", F32), ("t2", F32)):
            t, b = k.sb(st, [128, 512], dt, nm)
            out += [t, b]
        return out

    def attention(st, qT, b_q, kT, b_k, V, b_V, heads, scale, epilogue, skip_ctx=False):
        Et = [k.sb(st, [128, 512], BF16, "E") for _ in range(4)]
        sbanks = [0, 1, 2, 3]
        obanks = [4, 5, 6, 7]
        assert len(heads) % 2 == 0
        steps = []
        for g, (c0, n) in enumerate(GROUPS):
            if skip_ctx and g == 0:
                continue
            ktiles = [0, 1] if g == 0 else list(range(NTT))
            for hp in range(len(heads) // 2):
                for ki, kt in enumerate(ktiles):
                    steps.append((g, c0, n, hp, ki, kt, len(ktiles)))

        def qk_exp(si):
            g, c0, n, hp, ki, kt, nk = steps[si]
            outs = []
            for m in range(2):
                tile, pb, Kd, vh = heads[2 * hp + m]
                sbk = sbanks[(2 * si + m) % 4]
                k.mm(sbk, k.ps[sbk][:, :n], kT[pb:pb + Kd, tile, kt * 128:(kt + 1) * 128], qT[pb:pb + Kd, tile, c0:c0 + n],
                     True, True, [b_k, b_q])
                outs.append(sbk)
            res = []
            for m in range(2):
                sbk = outs[m]
                E, Eb = Et[(2 * si + m) % 4]
                k.act(E[:, :n], k.ps[sbk][:, :n], AF.Exp, [k.pb[sbk]], [Eb], scale=scale)
                res.append((E, Eb))
            return res

        oi = 0
        obs = None
        nxt = qk_exp(0)
        for si, (g, c0, n, hp, ki, kt, nk) in enumerate(steps):
            cur = nxt
            if si + 1 < len(steps):
                nxt = qk_exp(si + 1)
            nq = n // 128
            if ki == 0:
                obs = [obanks[(2 * oi) % 4], obanks[(2 * oi + 1) % 4]]; oi += 1
            for m in range(2):
                E, Eb = cur[m]
                vh = heads[2 * hp + m][3]
                ob = obs[m]
                for qi in range(nq):
                    P.op("pe", lambda e: e.matmul(k.ps[ob][:, qi * 65:(qi + 1) * 65], lhsT=E[:, qi * 128:(qi + 1) * 128],
                                                  rhs=V[:, kt, vh, :], start=(ki == 0 and qi == 0), stop=(ki == nk - 1),
                                                  skip_group_check=True),
                         reads=[Eb, b_V], writes=[k.pb[ob]], inc=(qi == nq - 1))
            if ki == nk - 1:
                for m in range(2):
                    epilogue(g, c0, nq, 2 * hp + m, obs[m])

    def transpose_out(ytok, b_ytok, nq, c0, ydst, b_ydst):
        for qi in range(nq):
            bk = k.bank()
            pv = k.ps[bk][:].bitcast(BF16)
            for tile in range(2):
                P.op("pe", lambda e: e.transpose(pv[:, tile * 128:(tile + 1) * 128], ytok[:, qi, tile * 128:(tile + 1) * 128], identb),
                     reads=[b_ytok, b_cstb], writes=[k.pb[bk]], inc=(tile == 1))
            k.copy("dve", ydst[:, :, c0 + qi * 128:c0 + (qi + 1) * 128], pv[:, 0:256].rearrange("p (j t) -> p j t", j=2),
                   [k.pb[bk]], [b_ydst])

    def da_mixer(l):
        lam_init = 0.8 - 0.6 * math.exp(-0.3 * l)
        ydst, b_ydst = yT[0]
        with ExitStack() as st:
            qT, b_q = k.sb(st, [128, 3, NT], BF16, "daq")
            kT, b_k = k.sb(st, [128, 3, NT], BF16, "dak")
            V, b_V = k.sb(st, [128, NTT, 4, 65], BF16, "dav")
            gcol, b_g = k.sb(st, [128, 2], F32, "dag")
            lam, b_lam = k.sb(st, [128, 128], F32, "dalam")
            lt, b_lt = k.sb(st, [128, 8], F32, "dalt")
            gsub, b_gsub = k.sb(st, [128, 64], F32, "dagsub")
            k.load("sp", gcol[:], da_g_d[l], [b_g])
            k.load("sp", lam[:], da_lam_d[l], [b_lam])
            k.load("sp", gsub[:], da_sub_d[l], [b_gsub])
            k.ts("dve", gsub[:], gsub[:], 1.0 - lam_init, None, ALU.mult, None, [b_gsub], [b_gsub])
            k.tt("dve", lam[:, 0:32], lam[:, 0:32], lam[:, 32:64], ALU.mult, [b_lam], [b_lam])
            k.tt("dve", lam[:, 64:96], lam[:, 64:96], lam[:, 96:128], ALU.mult, [b_lam], [b_lam])
            P.op("dve", lambda e: e.tensor_reduce(out=lt[:, 0:1], in_=lam[:, 0:32], axis=AX.X, op=ALU.add), reads=[b_lam], writes=[b_lt])
            P.op("dve", lambda e: e.tensor_reduce(out=lt[:, 1:2], in_=lam[:, 64:96], axis=AX.X, op=ALU.add), reads=[b_lam], writes=[b_lt])
            k.act(lt[:, 2:4], lt[:, 0:2], AF.Exp, [b_lt], [b_lt])
            k.tt("dve", lt[:, 4:5], lt[:, 3:4], lt[:, 2:3], ALU.subtract, [b_lt], [b_lt])
            k.ts("dve", lt[:, 4:5], lt[:, 4:5], -lam_init, None, ALU.add, None, [b_lt], [b_lt])
            k.memset("pool", V[:, :, :, 64:65], 1.0, [b_V])
            with ExitStack() as s2:
                wqk, b_wqk = load_w("pool", s2, w_da_qk[l], 768, "wqk")
                wv, b_wv = load_w("pool", s2, w_da_v[l], 256, "wv")
                hgs = [k.sb(s2, [128, 8, 512], BF16, "hg") for _ in range(2)]
                tmp, tmpb = k.sb(s2, [128, 512], F32, "htmp")
                scr = norm_scratch(s2)
                for g, (c0, n) in enumerate(GROUPS):
                    hg, hb = hgs[g % 2]
                    h_group(l, 0, g, hg, hb, tmp, tmpb)
                    for ti in range(6):
                        bk = k.bank()
                        proj(bk, wqk, b_wqk, ti * 128, hg, hb, n)
                        dst = (qT if ti < 3 else kT)
                        dstb = (b_q if ti < 3 else b_k)
                        headnorm_rope(s2, bk, n, c0, bo32, 1.0 / 32, gcol[:, (ti // 3):(ti // 3) + 1], b_g, Rda, da_cos_d, da_sin_d,
                                      dst[:, ti % 3, c0:c0 + n], dstb, scr)
                    for qi in range(n // 128):
                        tt_ = (c0 + qi * 128) // 128
                        bk = k.bank()
                        for kt in range(8):
                            k.mm(bk, k.ps[bk][:, 0:256], hg[:, kt, qi * 128:(qi + 1) * 128], wv[:, kt, :], kt == 0, kt == 7, [hb, b_wv])
                        k.copy("act", V[:, tt_, :, 0:64], k.ps[bk][:, 0:256].rearrange("p (h d) -> p h d", h=4), [k.pb[bk]], [b_V])
                P.barrier()
            dump("da_q", qT[:], b_q, [128, 3, NT], BF16)
            dump("da_k", kT[:], b_k, [128, 3, NT], BF16)
            dump("da_v", V[:], b_V, [128, NTT, 4, 65], BF16)
            with ExitStack() as s3:
                ytok, b_ytok = k.sb(s3, [128, 4, 256], BF16, "ytok")
                o0, b_o0 = k.sb(s3, [128, 4, 64], F32, "o0")
                dd, b_dd = k.sb(s3, [128, 4, 64], F32, "dd")
                junk, b_junk = k.sb(s3, [128, 64], F32, "junk")
                rc, b_rc = k.sb(s3, [128, 4, 4], F32, "rc")
                lnt, b_lnt = k.sb(s3, [128, 4], F32, "lnt2")
                state = {}

                def epi(g, c0, nq, hidx, ob):
                    h, m = hidx // 2, hidx % 2
                    if m == 0:
                        state["ob0"] = ob
                        return
                    ob0 = state["ob0"]
                    O0 = k.ps[ob0][:, 0:nq * 65].rearrange("p (q c) -> p q c", c=65)
                    O1 = k.ps[ob][:, 0:nq * 65].rearrange("p (q c) -> p q c", c=65)
                    P.op("dve", lambda e: e.reciprocal(out=rc[:, 0, 0:nq], in_=O0[:, :, 64]), reads=[k.pb[ob0]], writes=[b_rc])
                    P.op("dve", lambda e: e.reciprocal(out=rc[:, 1, 0:nq], in_=O1[:, :, 64]), reads=[k.pb[ob]], writes=[b_rc])
                    k.ts("dve", rc[:, 1, 0:nq], rc[:, 1, 0:nq], lt[:, 4:5], None, ALU.mult, None, [b_rc, b_lt], [b_rc])
                    for qi in range(nq):
                        k.ts("dve", o0[:, qi, :], O0[:, qi, 0:64], rc[:, 0, qi:qi + 1], None, ALU.mult, None, [k.pb[ob0], b_rc], [b_o0])
                        k.stt(dd[:, qi, :], O1[:, qi, 0:64], rc[:, 1, qi:qi + 1], o0[:, qi, :], ALU.mult, ALU.add,
                              [k.pb[ob], b_rc, b_o0], [b_dd])
                        P.op("act", lambda e: e.activation(out=junk[:], in_=dd[:, qi, :], func=AF.Square, accum_out=rc[:, 2, qi:qi + 1]),
                             reads=[b_dd], writes=[b_junk, b_rc])
                    P.op("act", lambda e: e.activation(out=lnt[:, 0:nq], in_=rc[:, 2, 0:nq], func=AF.Ln, scale=1.0 / 64, bias=k.eps_col[:]),
                         reads=[b_rc, k.b_const], writes=[b_lnt])
                    k.act(rc[:, 3, 0:nq], lnt[:, 0:nq], AF.Exp, [b_lnt], [b_rc], scale=-0.5)
                    for qi in range(nq):
                        k.stt(ytok[:, qi, h * 64:(h + 1) * 64], dd[:, qi, :], rc[:, 3, qi:qi + 1], gsub[:], ALU.mult, ALU.mult,
                              [b_dd, b_rc, b_gsub], [b_ytok])
                    if h == 3:
                        k.rrset = [0, 1, 2, 3]
                        transpose_out(ytok, b_ytok, nq, c0, ydst, b_ydst)
                        k.rrset = list(range(8))

                heads = [(j // 3, 32 * (j % 3), 32, j // 2) for j in range(8)]
                attention(s3, qT, b_q, kT, b_k, V, b_V, heads, 32 ** -0.5, epi, skip_ctx=(l == DEPTH - 1))
                P.barrier()
        dump("ya", ydst[:], b_ydst, [128, 2, NT], BF16)

    def mla_mixer(l):
        ydst, b_ydst = yT[2]
        with ExitStack() as st:
            qT, b_q = k.sb(st, [128, 2, NT], BF16, "mlq")
            kT, b_k = k.sb(st, [128, 2, NT], BF16, "mlk")
            V, b_V = k.sb(st, [128, NTT, 4, 65], BF16, "mlv")
            gcol, b_g = k.sb(st, [128, 2], F32, "mlg")
            gc, b_gc = k.sb(st, [128, 3], F32, "mlgc")
            k.load("sp", gcol[:], mla_g_d[l], [b_g])
            k.load("sp", gc[:], mla_gc_d[l], [b_gc])
            k.memset("pool", V[:, :, :, 64:65], 1.0, [b_V])
            with ExitStack() as s2:
                wc, b_wc = load_w("pool", s2, w_mla_c[l], 384, "wc")
                wkr, b_wkr = load_w("pool", s2, w_mla_kr[l], 128, "wkr")
                wf, b_wf = k.sb(s2, [128, 4, 256], F32, "wf")
                wuq, b_wuq = k.sb(s2, [128, 2, 256], BF16, "wuq")
                wkk, b_wkk = k.sb(s2, [128, 256], BF16, "wkk")
                wvv, b_wvv = k.sb(s2, [128, 256], BF16, "wvv")
                k.load("sp", wf[:, 0:2, :], w_uq_d[l], [b_wf])
                k.load("sp", wf[:, 2, :], w_ukvk_d[l], [b_wf])
                k.load("sp", wf[:, 3, :], w_ukvv_d[l], [b_wf])
                for kt in range(2):
                    k.ts("dve", wuq[:, kt, :], wf[:, kt, :], gc[:, kt:kt + 1], None, ALU.mult, None, [b_wf, b_gc], [b_wuq])
                k.ts("dve", wkk[:], wf[:, 2, :], gc[:, 2:3], None, ALU.mult, None, [b_wf, b_gc], [b_wkk])
                k.ts("dve", wvv[:], wf[:, 3, :], gc[:, 2:3], None, ALU.mult, None, [b_wf, b_gc], [b_wvv])
                hgs = [k.sb(s2, [128, 8, 512], BF16, "hg") for _ in range(2)]
                tmp, tmpb = [k.sb(s2, [128, 512], F32, "htmp") for _ in range(2)], None
                scr = norm_scratch(s2)
                sq2, b_sq2 = k.sb(s2, [128, 2, 512], BF16, "sq2")
                rsq, b_rsq = k.sb(s2, [128, 512], F32, "rsq")
                lnq, b_lnq = k.sb(s2, [128, 512], F32, "lnq")
                cqn, b_cqn = k.sb(s2, [128, 2, 512], BF16, "cqn")
                ckvn, b_ckvn = k.sb(s2, [128, 512], BF16, "ckvn")
                for g, (c0, n) in enumerate(GROUPS):
                    hg, hb = hgs[g % 2]
                    h_group(l, 0, g, hg, hb, tmp, tmpb)
                    bA = k.bank(); proj(bA, wc, b_wc, 0, hg, hb, n)
                    bB = k.bank(); proj(bB, wc, b_wc, 128, hg, hb, n)
                    k.act(sq2[:, 0, :n], k.ps[bA][:, :n], AF.Square, [k.pb[bA]], [b_sq2])
                    k.act(sq2[:, 1, :n], k.ps[bB][:, :n], AF.Square, [k.pb[bB]], [b_sq2])
                    bS = k.bank()
                    k.mm(bS, k.ps[bS][:, :n], onesb[:], sq2[:, 0, :n], True, False, [b_sq2, b_ones])
                    k.mm(bS, k.ps[bS][:, :n], onesb[:], sq2[:, 1, :n], False, True, [b_sq2, b_ones])
                    k.rstd_from_ss(rsq[:, :n], k.ps[bS][:, :n], bS, 1.0 / 192, lnq[:, :n], b_lnq, [b_rsq])
                    k.tt("dve", cqn[:, 0, :n], k.ps[bA][:, :n], rsq[:, :n], ALU.mult, [k.pb[bA], b_rsq], [b_cqn])
                    k.tt("dve", cqn[:, 1, :n], k.ps[bB][:, :n], rsq[:, :n], ALU.mult, [k.pb[bB], b_rsq], [b_cqn])
                    bC = k.bank(); proj(bC, wc, b_wc, 256, hg, hb, n)
                    k.act(sq2[:, 0, :n], k.ps[bC][:, :n], AF.Square, [k.pb[bC]], [b_sq2])
                    bS = k.bank()
                    k.mm(bS, k.ps[bS][:, :n], onesb[:], sq2[:, 0, :n], True, True, [b_sq2, b_ones])
                    k.rstd_from_ss(rsq[:, :n], k.ps[bS][:, :n], bS, 1.0 / 128, lnq[:, :n], b_lnq, [b_rsq])
                    k.tt("dve", ckvn[:, :n], k.ps[bC][:, :n], rsq[:, :n], ALU.mult, [k.pb[bC], b_rsq], [b_ckvn])
                    for tile in range(2):
                        bk = k.bank()
                        k.mm(bk, k.ps[bk][:, :n], wuq[:, 0, tile * 128:(tile + 1) * 128], cqn[:, 0, :n], True, False, [b_wuq, b_cqn])
                        k.mm(bk, k.ps[bk][:, :n], wuq[0:64, 1, tile * 128:(tile + 1) * 128], cqn[0:64, 1, :n], False, True, [b_wuq, b_cqn])
                        headnorm_rope(s2, bk, n, c0, bo64, 1.0 / 48, gcol[:, 0:1], b_g, Rmla, ml_cos_d, ml_sin_d,
                                      qT[:, tile, c0:c0 + n], b_q, scr)
                    for tile in range(2):
                        bk = k.bank()
                        k.mm(bk, k.ps[bk][:, :n], wkk[:, tile * 128:(tile + 1) * 128], ckvn[:, :n], True, False, [b_wkk, b_ckvn])
                        for kt in range(8):
                            k.mm(bk, k.ps[bk][:, :n], wkr[:, kt, :], hg[:, kt, :n], False, kt == 7, [b_wkr, hb])
                        headnorm_rope(s2, bk, n, c0, bo64, 1.0 / 48, gcol[:, 1:2], b_g, Rmla, ml_cos_d, ml_sin_d,
                                      kT[:, tile, c0:c0 + n], b_k, scr)
                    for qi in range(n // 128):
                        tt_ = (c0 + qi * 128) // 128
                        bk = k.bank()
                        k.mm(bk, k.ps[bk][:, 0:256], ckvn[:, qi * 128:(qi + 1) * 128], wvv[:], True, True, [b_ckvn, b_wvv])
                        k.copy("act", V[:, tt_, :, 0:64], k.ps[bk][:, 0:256].rearrange("p (h d) -> p h d", h=4), [k.pb[bk]], [b_V])
                P.barrier()
            dump("ml_q", qT[:], b_q, [128, 2, NT], BF16)
            dump("ml_k", kT[:], b_k, [128, 2, NT], BF16)
            with ExitStack() as s3:
                ytok, b_ytok = k.sb(s3, [128, 4, 256], BF16, "ytok")
                rc, b_rc = k.sb(s3, [128, 4], F32, "rc")

                def epi(g, c0, nq, h, ob):
                    O = k.ps[ob][:, 0:nq * 65].rearrange("p (q c) -> p q c", c=65)
                    P.op("dve", lambda e: e.reciprocal(out=rc[:, 0:nq], in_=O[:, :, 64]), reads=[k.pb[ob]], writes=[b_rc])
                    for qi in range(nq):
                        k.ts("dve", ytok[:, qi, h * 64:(h + 1) * 64], O[:, qi, 0:64], rc[:, qi:qi + 1], None, ALU.mult, None,
                             [k.pb[ob], b_rc], [b_ytok])
                    if h == 3:
                        k.rrset = [0, 1, 2, 3]
                        transpose_out(ytok, b_ytok, nq, c0, ydst, b_ydst)
                        k.rrset = list(range(8))

                heads = [(h // 2, 64 * (h % 2), 48, h) for h in range(4)]
                attention(s3, qT, b_q, kT, b_k, V, b_V, heads, 48 ** -0.5, epi, skip_ctx=(l == DEPTH - 1))
                P.barrier()
        dump("yc", ydst[:], b_ydst, [128, 2, NT], BF16)

    def merge(l):
        with ExitStack() as st:
            wbr, b_wbr = k.sb(st, [128, 2, 4, D], BF16, "wbr")
            wout, b_wout = k.sb(st, [128, 8, D], BF16, "wout")
            wgs = [k.sb(st, [128, 8, 4, 128], BF16, "wg") for _ in range(2)]
            hgs = [k.sb(st, [128, 8, 512], BF16, "hg") for _ in range(2)]
            tmp, tmpb = k.sb(st, [128, 512], F32, "htmp")
            sig = [k.sb(st, [128, 512], F32, "sig") for _ in range(2)]
            prod = [k.sb(st, [128, 512], F32, "prod") for _ in range(2)]
            macc, b_macc = k.sb(st, [128, 512], F32, "macc")
            mbf, b_mbf = k.sb(st, [128, 8, 512], BF16, "mbf")
            wi = 0; si = 0
            for g, (c0, n) in enumerate(GROUPS):
                if l == DEPTH - 1 and g == 0:
                    continue
                j = jof(g)
                hg, hb = hgs[g % 2]
                h_group(l, 0, g, hg, hb, tmp, tmpb)
                for ct in range(8):
                    wg, b_wg = wgs[wi % 2]; wi += 1
                    k.load("pool", wg[:].rearrange("p a b c -> p (a b c)"), w_gates_d[l, ct], [b_wg])
                    if wi == 1:
                        k.load("pool", wbr[:].rearrange("p a b c -> p (a b c)"), w_branch_d[l], [b_wbr])
                    if wi == 3:
                        k.load("pool", wout[:], w_out_d[l].rearrange("(kt p) n -> p kt n", p=128), [b_wout])
                    for i in range(4):
                        bg = k.bank()
                        for kt in range(8):
                            k.mm(bg, k.ps[bg][:, :n], wg[:, kt, i, :], hg[:, kt, :n], kt == 0, kt == 7, [b_wg, hb])
                        sg, b_sg = sig[si % 2]; pr, b_pr = prod[si % 2]; si += 1
                        k.act(sg[:, :n], k.ps[bg][:, :n], AF.Sigmoid, [k.pb[bg]], [b_sg])
                        bp = k.bank()
                        for kt2 in range(2):
                            k.mm(bp, k.ps[bp][:, :n], wbr[:, kt2, i, ct * 128:(ct + 1) * 128], yT[i][0][:, kt2, c0:c0 + n],
                                 kt2 == 0, kt2 == 1, [b_wbr, yT[i][1]])
                        if i == 0:
                            k.tt("dve", macc[:, :n], k.ps[bp][:, :n], sg[:, :n], ALU.mult, [k.pb[bp], b_sg], [b_macc])
                        else:
                            k.tt("dve", pr[:, :n], k.ps[bp][:, :n], sg[:, :n], ALU.mult, [k.pb[bp], b_sg], [b_pr])
                            if i < 3:
                                k.tt("dve", macc[:, :n], macc[:, :n], pr[:, :n], ALU.add, [b_macc, b_pr], [b_macc])
                            else:
                                k.tt("dve", mbf[:, ct, :n], macc[:, :n], pr[:, :n], ALU.add, [b_macc, b_pr], [b_mbf])
                for co in range(8):
                    bo = k.bank()
                    for ct in range(8):
                        k.mm(bo, k.ps[bo][:, :n], wout[:, ct, co * 128:(co + 1) * 128], mbf[:, ct, :n], ct == 0, ct == 7, [b_wout, b_mbf])
                    k.stt(xT[:, co, c0:c0 + n], k.ps[bo][:, :n], MOD[:, l, 2, co, j:j + 1], xT[:, co, c0:c0 + n], ALU.mult, ALU.add,
                          [k.pb[bo], b_MOD, b_xT], [b_xT])
            P.barrier()
        dump("xmix", xT[:], b_xT, [128, 8, NT])

    def moe(l):
        fT = yall[:].rearrange("p i k t -> p (i k) t")
        b_fT = Buf("fT")
        with ExitStack() as st:
            rms_stats(st)
            P.barrier()
        with ExitStack() as st:
            combT, b_comb = k.sb(st, [16, NT], BF16, "combT")
            selb, b_sel = k.sb(st, [16, 16, 128], BF16, "selb")
            k.load("pool", selb[:], sel_d, [b_sel])
            wgs = [k.sb(st, [128, 8, 512], BF16, "ewg") for _ in range(2)]
            wus = [k.sb(st, [128, 8, 512], BF16, "ewu") for _ in range(2)]
            wds = [k.sb(st, [128, 4, D], BF16, "ewd") for _ in range(2)]

            def load_expert(e_):
                wg, b_wg = wgs[e_ % 2]; wu, b_wu = wus[e_ % 2]; wd, b_wd = wds[e_ % 2]
                k.load("pool", wg[:], exp_g_d[l, e_].rearrange("(kt p) n -> p kt n", p=128), [b_wg])
                k.load("pool", wu[:], exp_u_d[l, e_].rearrange("(kt p) n -> p kt n", p=128), [b_wu])
                k.load("pool", wd[:], exp_d_d[l, e_].rearrange("(kt p) n -> p kt n", p=128), [b_wd])
            load_expert(0)
            with ExitStack() as s2:
                tmp, tmpb = k.sb(s2, [128, 512], F32, "htmp")
                f32s = [k.sb(s2, [128, 512], F32, "f32") for _ in range(2)]
                rw, b_rw = k.sb(s2, [128, 8, 16], F32, "rw")
                k.load("sp", rw[:], router_w_d.rearrange("(kt p) n -> p kt n", p=128), [b_rw])
                rb, b_rb = k.sb(s2, [128, 16], F32, "rb")
                k.load("sp", rb[:], router_b_d, [b_rb])
                lg, b_lg = k.sb(s2, [128, NTT, 16], F32, "lg")
                fi_ = 0
                skip0 = (l == DEPTH - 1)
                if skip0:
                    k.memset("dve", lg[:, 0:2, :], 0.0, [b_lg])
                for g in range(5):
                    if skip0 and g == 0:
                        continue
                    c0, n = GROUPS[g]
                    j = jof(g)
                    nq = n // 128
                    bl = k.bank()
                    for kt in range(8):
                        k.tt("dve", tmp[:, :n], xT[:, kt, c0:c0 + n], rstd_b[:, c0:c0 + n], ALU.mult, [b_xT, b_rstd], [tmpb])
                        P.op("act", lambda e: e.activation(out=fT[:, kt, c0:c0 + n], in_=tmp[:, :n], func=AF.Identity,
                                                           scale=AB[:, l, 1, 0, kt, j:j + 1], bias=AB[:, l, 1, 1, kt, j:j + 1]),
                             reads=[tmpb, b_AB], writes=[b_fT])
                        f32, b_f32 = f32s[fi_ % 2]; fi_ += 1
                        k.ts("pool", f32[:, :n], tmp[:, :n], AB[:, l, 1, 0, kt, j:j + 1], AB[:, l, 1, 1, kt, j:j + 1], ALU.mult, ALU.add,
                             [tmpb, b_AB], [b_f32])
                        for qi in range(nq):
                            P.op("pe", lambda e: e.matmul(k.ps[bl][:, qi * 16:(qi + 1) * 16], lhsT=f32[:, qi * 128:(qi + 1) * 128], rhs=rw[:, kt, :],
                                                          start=(kt == 0 and qi == 0), stop=(kt == 7), skip_group_check=True),
                                 reads=[b_f32, b_rw], writes=[k.pb[bl]], inc=(qi == nq - 1))
                    tt0 = c0 // 128
                    k.copy("dve", lg[:, tt0:tt0 + nq, :], k.ps[bl][:, 0:nq * 16].rearrange("p (q e) -> p q e", e=16), [k.pb[bl]], [b_lg])
                r = {}
                NR = NTT * 16
                for nm, wd_ in (("sc", NR), ("bi", NR), ("m1", NR // 4), ("eq", NR), ("bi2", NR), ("m2", NR // 4),
                                ("gs", NR // 4), ("gm", NTT), ("gsel", NR // 4), ("sel", NR), ("w", NR), ("ws", NTT), ("cmb", NR)):
                    r[nm] = k.sb(s2, [128, wd_], F32, "r_" + nm)
                v4 = lambda ap: ap.rearrange("p (g e) -> p g e", e=4)
                b4 = lambda ap: ap.unsqueeze(2).to_broadcast([128, NR // 4, 4])
                sc, b_sc = r["sc"]; bi, b_bi = r["bi"]; m1, b_m1 = r["m1"]; eq, b_eq = r["eq"]; bi2, b_bi2 = r["bi2"]
                m2, b_m2 = r["m2"]; gs, b_gs = r["gs"]; gm_, b_gm_ = r["gm"]; gsel, b_gsel = r["gsel"]; sel, b_sl = r["sel"]
                w_, b_w = r["w"]; ws, b_ws = r["ws"]; cmb, b_cmb = r["cmb"]
                k.act(sc[:], lg[:].rearrange("p t e -> p (t e)"), AF.Sigmoid, [b_lg], [b_sc])
                k.tt("dve", sc[:].rearrange("p (t e) -> p t e", e=16) if False else bi[:].rearrange("p (t e) -> p t e", e=16),
                     sc[:].rearrange("p (t e) -> p t e", e=16), rb[:].unsqueeze(1).to_broadcast([128, NTT, 16]), ALU.add, [b_sc, b_rb], [b_bi])
                P.op("dve", lambda e: e.tensor_reduce(out=m1[:], in_=v4(bi[:]), axis=AX.X, op=ALU.max), reads=[b_bi], writes=[b_m1])
                k.tt("dve", v4(eq[:]), v4(bi[:]), b4(m1[:]), ALU.is_equal, [b_bi, b_m1], [b_eq])
                k.stt(bi2[:], eq[:], -1e9, bi[:], ALU.mult, ALU.add, [b_eq, b_bi], [b_bi2])
                P.op("dve", lambda e: e.tensor_reduce(out=m2[:], in_=v4(bi2[:]), axis=AX.X, op=ALU.max), reads=[b_bi2], writes=[b_m2])
                k.tt("dve", gs[:], m1[:], m2[:], ALU.add, [b_m1, b_m2], [b_gs])
                P.op("dve", lambda e: e.tensor_reduce(out=gm_[:], in_=v4(gs[:]), axis=AX.X, op=ALU.max), reads=[b_gs], writes=[b_gm_])
                k.tt("dve", v4(gsel[:]), v4(gs[:]), gm_[:].unsqueeze(2).to_broadcast([128, NTT, 4]), ALU.is_equal, [b_gs, b_gm_], [b_gsel])
                k.tt("dve", v4(sel[:]), v4(bi[:]), b4(m2[:]), ALU.is_ge, [b_bi, b_m2], [b_sl])
                k.tt("dve", v4(sel[:]), v4(sel[:]), b4(gsel[:]), ALU.mult, [b_sl, b_gsel], [b_sl])
                k.tt("dve", w_[:], sc[:], sel[:], ALU.mult, [b_sc, b_sl], [b_w])
                P.op("dve", lambda e: e.tensor_reduce(out=ws[:], in_=w_[:].rearrange("p (t e) -> p t e", e=16), axis=AX.X, op=ALU.add),
                     reads=[b_w], writes=[b_ws])
                P.op("dve", lambda e: e.reciprocal(out=ws[:], in_=ws[:]), reads=[b_ws], writes=[b_ws])
                k.tt("dve", cmb[:].rearrange("p (t e) -> p t e", e=16), w_[:].rearrange("p (t e) -> p t e", e=16),
                     ws[:].unsqueeze(2).to_broadcast([128, NTT, 16]), ALU.mult, [b_w, b_ws], [b_cmb])
                for tq in range(0, NTT, 4):
                    nn = min(4, NTT - tq)
                    bt = k.bank()
                    for i_ in range(nn):
                        P.op("pe", lambda e: e.transpose(k.ps[bt][0:16, i_ * 128:(i_ + 1) * 128], cmb[:, (tq + i_) * 16:(tq + i_ + 1) * 16], identf),
                             reads=[b_cmb, b_cstf], writes=[k.pb[bt]], inc=(i_ == nn - 1))
                    k.copy("act", combT[:, tq * 128:(tq + nn) * 128], k.ps[bt][0:16, 0:nn * 128], [k.pb[bt]], [b_comb])
                P.barrier()
            dump("combT", combT[:], b_comb, [16, NT], BF16)
            cbs = [k.sb(st, [128, 512], BF16, "cbs") for _ in range(2)]
            sl = [k.sb(st, [128, 512], F32, "esl") for _ in range(2)]
            tl = [k.sb(st, [128, 512], F32, "etl") for _ in range(2)]
            aa = [k.sb(st, [128, 4, 512], BF16, "eaa") for _ in range(2)]
            ci = 0; fi = 0
            for e_ in range(16):
                wg, b_wg = wgs[e_ % 2]; wu, b_wu = wus[e_ % 2]; wd, b_wd = wds[e_ % 2]
                if e_ > 0:
                    load_expert(e_)
                for g, (c0, n) in enumerate(GROUPS):
                    if l == DEPTH - 1 and g == 0:
                        continue
                    j = jof(g)
                    cb, b_cb = cbs[ci % 2]; a_, b_a = aa[ci % 2]; ci += 1
                    bc = k.bank()
                    k.mm(bc, k.ps[bc][:, :n], selb[:, e_, :], combT[:, c0:c0 + n], True, True, [b_sel, b_comb])
                    k.copy("act", cb[:, :n], k.ps[bc][:, :n], [k.pb[bc]], [b_cb])
                    for fj in range(4):
                        bg = k.bank()
                        for kt in range(8):
                            k.mm(bg, k.ps[bg][:, :n], wg[:, kt, fj * 128:(fj + 1) * 128], fT[:, kt, c0:c0 + n], kt == 0, kt == 7, [b_wg, b_fT])
                        bu = k.bank()
                        for kt in range(8):
                            k.mm(bu, k.ps[bu][:, :n], wu[:, kt, fj * 128:(fj + 1) * 128], fT[:, kt, c0:c0 + n], kt == 0, kt == 7, [b_wu, b_fT])
                        s_, b_s = sl[fi % 2]; t_, b_t = tl[fi % 2]; fi += 1
                        k.act(s_[:, :n], k.ps[bg][:, :n], AF.Silu, [k.pb[bg]], [b_s])
                        k.tt("dve", t_[:, :n], k.ps[bu][:, :n], s_[:, :n], ALU.mult, [k.pb[bu], b_s], [b_t])
                        k.tt("dve", a_[:, fj, :n], t_[:, :n], cb[:, :n], ALU.mult, [b_t, b_cb], [b_a])
                    for co in range(8):
                        bo = k.bank()
                        for fj in range(4):
                            k.mm(bo, k.ps[bo][:, :n], wd[:, fj, co * 128:(co + 1) * 128], a_[:, fj, :n], fj == 0, fj == 3, [b_wd, b_a])
                        k.stt(xT[:, co, c0:c0 + n], k.ps[bo][:, :n], MOD[:, l, 5, co, j:j + 1], xT[:, co, c0:c0 + n], ALU.mult, ALU.add,
                              [k.pb[bo], b_MOD, b_xT], [b_xT])
            P.barrier()
        dump("xout", xT[:], b_xT, [128, 8, NT])

    TWO_PI = 2.0 * math.pi

    def sincos(ang, b_ang, N, sc4, sin_out, cos_out, b_out):
        (kf, b_kf), (r_, b_r), (mk, b_mk), (sh, b_sh) = sc4
        ki = kf[:, :N].bitcast(I32)
        k.ts("dve", r_[:, :N], ang, 1.0 / TWO_PI, None, ALU.mult, None, [b_ang], [b_r])
        k.copy("dve", ki, r_[:, :N], [b_r], [b_kf])
        k.copy("dve", mk[:, :N], ki, [b_kf], [b_mk])
        k.stt(r_[:, :N], mk[:, :N], -TWO_PI, ang, ALU.mult, ALU.add, [b_mk, b_ang], [b_r])
        k.ts("dve", mk[:, :N], r_[:, :N], math.pi, -TWO_PI, ALU.is_gt, ALU.mult, [b_r], [b_mk])
        k.tt("dve", r_[:, :N], r_[:, :N], mk[:, :N], ALU.add, [b_r, b_mk], [b_r])
        k.ts("dve", mk[:, :N], r_[:, :N], -math.pi, TWO_PI, ALU.is_lt, ALU.mult, [b_r], [b_mk])
        k.tt("dve", r_[:, :N], r_[:, :N], mk[:, :N], ALU.add, [b_r, b_mk], [b_r])
        k.ts("dve", r_[:, :N], r_[:, :N], math.pi, -math.pi, ALU.min, ALU.max, [b_r], [b_r])
        k.act(sin_out, r_[:, :N], AF.Sin, [b_r], [b_out])
        k.act(sh[:, :N], r_[:, :N], AF.Sin, [b_r], [b_sh], scale=0.5)
        k.tt("dve", sh[:, :N], sh[:, :N], sh[:, :N], ALU.mult, [b_sh], [b_sh])
        k.ts("dve", cos_out, sh[:, :N], -2.0, 1.0, ALU.mult, ALU.add, [b_sh], [b_out])

    def s5_mixer(l):
        ydst, b_ydst = yT[1]
        with ExitStack() as st:
            uT, b_u = k.sb(st, [128, 2, NT], BF16, "s5u")
            yacc, b_yacc = k.sb(st, [128, 2, NT], F32, "s5y")
            dcol, b_dcol = k.sb(st, [128, 2], F32, "s5d")
            k.load("sp", dcol[:], s5d_d[l], [b_dcol])
            with ExitStack() as s2:
                ws, b_ws = load_w("pool", s2, w_s5_d[l], 256, "ws5")
                hgs = [k.sb(s2, [128, 8, 512], BF16, "hg") for _ in range(2)]
                tmp, tmpb = [k.sb(s2, [128, 512], F32, "htmp") for _ in range(2)], None
                for g, (c0, n) in enumerate(GROUPS):
                    hg, hb = hgs[g % 2]
                    h_group(l, 0, g, hg, hb, tmp, tmpb)
                    for ti in range(2):
                        bk = k.bank()
                        proj(bk, ws, b_ws, ti * 128, hg, hb, n)
                        k.copy("act", uT[:, ti, c0:c0 + n], k.ps[bk][:, :n], [k.pb[bk]], [b_u])
                P.barrier()
            for dr in range(2):
                with ExitStack() as sd:
                    pr, b_pr = k.sb(sd, [128, 1024], F32, "pr"); pi_, b_pi = k.sb(sd, [128, 1024], F32, "pi")
                    qr, b_qr = k.sb(sd, [128, 1024], F32, "qr"); qi, b_qi = k.sb(sd, [128, 1024], F32, "qi")
                    Bb, b_Bb = k.sb(sd, [128, 2, 1024], BF16, "Bb")
                    Cb, b_Cb = k.sb(sd, [128, 3, 8, 128], BF16, "Cb")
                    triT, b_tri = k.sb(sd, [128, 2, 128], BF16, "triT")
                    trif, b_trif = k.sb(sd, [128, 128], F32, "trif")
                    posc, b_posc = k.sb(sd, [128, 2], F32, "posc")
                    posr, b_posr = k.sb(sd, [128, 128], F32, "posr")
                    fm, b_fm = k.sb(sd, [128, 3, 8], F32, "fm")
                    cc, b_cc = k.sb(sd, [128, 16, 8], F32, "cc")
                    A128, b_A128 = k.sb(sd, [128, 2, 8], F32, "A128")
                    k.load("pool", Bb[:], s5B_d[l, dr].rearrange("kt p n -> p kt n"), [b_Bb])
                    with ExitStack() as sc_:
                        Cf, b_Cf = k.sb(sc_, [128, 2, 8, 128], F32, "Cf")
                        k.load("sp", Cf[:, 0], s5C_d[l, dr, 0], [b_Cf]); k.load("sp", Cf[:, 1], s5C_d[l, dr, 1], [b_Cf])
                        k.copy("dve", Cb[:, 0], Cf[:, 0], [b_Cf], [b_Cb])
                        k.ts("dve", Cb[:, 1], Cf[:, 0], -1.0, None, ALU.mult, None, [b_Cf], [b_Cb])
                        k.ts("dve", Cb[:, 2], Cf[:, 1], -1.0, None, ALU.mult, None, [b_Cf], [b_Cb])
                        P.barrier()
                    k.load("sp", trif[:], tri_d[dr], [b_trif])
                    k.copy("dve", triT[:, 0, :], trif[:], [b_trif], [b_tri])
                    k.ts("dve", triT[:, 1, :], trif[:], -1.0, None, ALU.mult, None, [b_trif], [b_tri])
                    k.load("sp", posc[:], posc_d[dr], [b_posc]); k.load("sp", posr[:], posr_d[dr], [b_posr])
                    k.load("sp", fm[:], s5fm_d[l, dr], [b_fm])
                    with ExitStack() as sx:
                        sc4 = [k.sb(sx, [128, 1024], F32, "sc4") for _ in range(4)]
                        rho, b_rho = k.sb(sx, [128, 1024], F32, "rho"); th, b_th = k.sb(sx, [128, 1024], F32, "th")
                        ang, b_angb = k.sb(sx, [128, 1024], F32, "ang")
                        k.load("sp", rho[:], s5tm_d[l, dr, 0], [b_rho]); k.load("sp", th[:], s5tm_d[l, dr, 1], [b_th])
                        k.load("sp", ang[:], s5tm_d[l, dr, 2], [b_angb])
                        k.act(ang[:], ang[:], AF.Exp, [b_angb], [b_angb])
                        k.tt("dve", rho[:], rho[:], ang[:], ALU.mult, [b_rho, b_angb], [b_rho])
                        k.tt("dve", th[:], th[:], ang[:], ALU.mult, [b_th, b_angb], [b_th])
                        k.ts("dve", ang[:], th[:], posc[:, 0:1], None, ALU.mult, None, [b_th, b_posc], [b_angb])
                        sincos(ang[:], b_angb, 1024, sc4, pi_[:], pr[:], b_pr)
                        b_pi.w = b_pr.w
                        P.op("act", lambda e: e.activation(out=ang[:], in_=rho[:], func=AF.Exp, scale=posc[:, 0:1]), reads=[b_rho, b_posc], writes=[b_angb])
                        k.tt("dve", pr[:], pr[:], ang[:], ALU.mult, [b_pr, b_angb], [b_pr])
                        k.tt("dve", pi_[:], pi_[:], ang[:], ALU.mult, [b_pr, b_angb], [b_pr])
                        dtc = cc[:, 0, :]; rc_ = cc[:, 1, :]; tc_ = cc[:, 2, :]
                        k.act(dtc, fm[:, 2, :], AF.Exp, [b_fm], [b_cc])
                        k.tt("dve", rc_, fm[:, 0, :], dtc, ALU.mult, [b_fm, b_cc], [b_cc])
                        k.tt("dve", tc_, fm[:, 1, :], dtc, ALU.mult, [b_fm, b_cc], [b_cc])
                        k.copy("dve", ang[:, 0:8], tc_, [b_cc], [b_angb])
                        k.ts("dve", ang[:, 8:16], tc_, 128.0, None, ALU.mult, None, [b_cc], [b_angb])
                        sincos(ang[:, 0:16], b_angb, 16, sc4, th[:, 0:16], th[:, 16:32], b_th)
                        er = cc[:, 3, :]; e128 = cc[:, 4, :]
                        k.act(er, rc_, AF.Exp, [b_cc], [b_cc])
                        k.act(e128, rc_, AF.Exp, [b_cc], [b_cc], scale=128.0)
                        k.tt("dve", A128[:, 0, :], e128, th[:, 24:32], ALU.mult, [b_cc, b_th], [b_A128])
                        k.tt("dve", A128[:, 1, :], e128, th[:, 8:16], ALU.mult, [b_cc, b_th], [b_A128])
                        ar1 = cc[:, 5, :]; ai = cc[:, 6, :]; den = cc[:, 7, :]; cr = cc[:, 8, :]; ci = cc[:, 9, :]; t1 = cc[:, 10, :]
                        k.tt("dve", ar1, er, th[:, 16:24], ALU.mult, [b_cc, b_th], [b_cc])
                        k.ts("dve", ar1, ar1, -1.0, None, ALU.add, None, [b_cc], [b_cc])
                        k.tt("dve", ai, er, th[:, 0:8], ALU.mult, [b_cc, b_th], [b_cc])
                        k.tt("dve", den, fm[:, 0, :], fm[:, 0, :], ALU.mult, [b_fm], [b_cc])
                        k.tt("dve", t1, fm[:, 1, :], fm[:, 1, :], ALU.mult, [b_fm], [b_cc])
                        k.tt("dve", den, den, t1, ALU.add, [b_cc], [b_cc])
                        P.op("dve", lambda e: e.reciprocal(out=den, in_=den), reads=[b_cc], writes=[b_cc])
                        k.tt("dve", cr, ar1, fm[:, 0, :], ALU.mult, [b_cc, b_fm], [b_cc])
                        k.tt("dve", t1, ai, fm[:, 1, :], ALU.mult, [b_cc, b_fm], [b_cc])
                        k.tt("dve", cr, cr, t1, ALU.add, [b_cc], [b_cc])
                        k.tt("dve", cr, cr, den, ALU.mult, [b_cc], [b_cc])
                        k.tt("dve", ci, ai, fm[:, 0, :], ALU.mult, [b_cc, b_fm], [b_cc])
                        k.tt("dve", t1, ar1, fm[:, 1, :], ALU.mult, [b_cc, b_fm], [b_cc])
                        k.tt("dve", ci, ci, t1, ALU.subtract, [b_cc], [b_cc])
                        k.tt("dve", ci, ci, den, ALU.mult, [b_cc], [b_cc])
                        for i in range(8):
                            k.ts("dve", ang[:, i * 128:(i + 1) * 128], posr[:], cc[:, 2, i:i + 1], None, ALU.mult, None, [b_posr, b_cc], [b_angb])
                            P.op("act", lambda e: e.activation(out=rho[:, i * 128:(i + 1) * 128], in_=posr[:], func=AF.Exp, scale=cc[:, 1, i:i + 1]),
                                 reads=[b_posr, b_cc], writes=[b_rho])
                        sincos(ang[:], b_angb, 1024, sc4, qi[:], qr[:], b_qr)
                        k.tt("dve", qr[:], qr[:], rho[:], ALU.mult, [b_qr, b_rho], [b_qr])
                        k.tt("dve", qi[:], qi[:], rho[:], ALU.mult, [b_qr, b_rho], [b_qr])
                        for i in range(8):
                            sl_ = slice(i * 128, (i + 1) * 128)
                            k.ts("dve", th[:, sl_], qi[:, sl_], cc[:, 9, i:i + 1], None, ALU.mult, None, [b_qr, b_cc], [b_th])
                            k.ts("dve", ang[:, sl_], qr[:, sl_], cc[:, 9, i:i + 1], None, ALU.mult, None, [b_qr, b_cc], [b_angb])
                            k.stt(qr[:, sl_], qr[:, sl_], cc[:, 8, i:i + 1], th[:, sl_], ALU.mult, ALU.subtract, [b_qr, b_cc, b_th], [b_qr])
                            k.stt(qi[:, sl_], qi[:, sl_], cc[:, 8, i:i + 1], ang[:, sl_], ALU.mult, ALU.add, [b_qr, b_cc, b_angb], [b_qr])
                        P.barrier()
                    if l == 0 and dr == 0:
                        dump("s5pr", pr[:], b_pr, [128, 1024]); dump("s5pi", pi_[:], b_pr, [128, 1024])
                        dump("s5qr", qr[:], b_qr, [128, 1024]); dump("s5qi", qi[:], b_qr, [128, 1024])
                        dump("s5A128", A128[:], b_A128, [128, 2, 8])
                    with ExitStack() as sp_:
                        zp, _ = k.sb(sp_, [128, 4, 1024], BF16, "zp")
                        hp, _ = k.sb(sp_, [128, 4, 1024], BF16, "hp")
                        Dg, _ = k.sb(sp_, [128, 16, 128], BF16, "Dg")
                        zl, _ = k.sb(sp_, [128, 16], F32, "zl")
                        car, _ = k.sb(sp_, [128, 16], F32, "car")
                        ct_, _ = k.sb(sp_, [128, 4, 8], F32, "ctmp")
                        hb2 = lambda nm: [Buf(nm + "0"), Buf(nm + "1")]
                        b_zp, b_hp, b_DgT, b_zl, b_car, b_ct = hb2("zp"), hb2("hp"), hb2("Dg"), hb2("zl"), hb2("car"), hb2("ct")
                        k.memset("pool", Dg[:], 0.0, b_DgT)
                        order = [0, 1] + list(range(2, NTT)) if dr == 0 else [1, 0] + list(range(NTT - 1, 1, -1))
                        tl = 127 if dr == 0 else 0

                        def half_gen(kt, banks):
                            bre, bim, zre, zim = banks
                            cs = slice(kt * 512, (kt + 1) * 512)
                            i4 = slice(kt * 4, kt * 4 + 4); i4m = slice(8 + kt * 4, 8 + kt * 4 + 4)
                            for tt in order:
                                cols = slice(tt * 128, (tt + 1) * 128)
                                k.mm(bre, k.ps[bre][:, :], uT[:, kt, cols], Bb[:, kt, 0:512], True, True, [b_u, b_Bb])
                                k.mm(bim, k.ps[bim][:, :], uT[:, kt, cols], Bb[:, kt, 512:1024], True, True, [b_u, b_Bb])
                                yield
                                k.tt("dve", zp[:, 0, cs], k.ps[bre][:, :], pr[:, cs], ALU.mult, [k.pb[bre], b_pr], [b_zp[kt]])
                                k.tt("dve", zp[:, 1, cs], k.ps[bim][:, :], pi_[:, cs], ALU.mult, [k.pb[bim], b_pr], [b_zp[kt]])
                                k.tt("dve", zp[:, 2, cs], k.ps[bim][:, :], pr[:, cs], ALU.mult, [k.pb[bim], b_pr], [b_zp[kt]])
                                k.tt("dve", zp[:, 3, cs], k.ps[bre][:, :], pi_[:, cs], ALU.mult, [k.pb[bre], b_pr], [b_zp[kt]])
                                yield
                                for ii in range(4):
                                    i = kt * 4 + ii
                                    sl_ = slice(i * 128, (i + 1) * 128)
                                    oc = slice(ii * 128, (ii + 1) * 128)
                                    k.mm(zre, k.ps[zre][:, oc], zp[:, 0, sl_], triT[:, 0, :], True, False, [b_zp[kt], b_tri])
                                    k.mm(zre, k.ps[zre][:, oc], zp[:, 1, sl_], triT[:, 1, :], False, False, [b_zp[kt], b_tri])
                                    k.mm(zre, k.ps[zre][:, oc], Dg[:, i, :], onesb[:], False, True, [b_DgT[kt], b_ones])
                                    k.mm(zim, k.ps[zim][:, oc], zp[:, 2, sl_], triT[:, 0, :], True, False, [b_zp[kt], b_tri])
                                    k.mm(zim, k.ps[zim][:, oc], zp[:, 3, sl_], triT[:, 0, :], False, False, [b_zp[kt], b_tri])
                                    k.mm(zim, k.ps[zim][:, oc], Dg[:, 8 + i, :], onesb[:], False, True, [b_DgT[kt], b_ones])
                                yield
                                k.copy("act", zl[:, i4], k.ps[zre][:, :].rearrange("p (i t) -> p i t", t=128)[:, :, tl], [k.pb[zre]], [b_zl[kt]])
                                k.copy("act", zl[:, i4m], k.ps[zim][:, :].rearrange("p (i t) -> p i t", t=128)[:, :, tl], [k.pb[zim]], [b_zl[kt]])
                                zr_, zi_ = zl[:, i4], zl[:, i4m]
                                k.tt("dve", ct_[:, 0, i4], A128[:, 0, i4], zr_, ALU.mult, [b_A128, b_zl[kt]], [b_ct[kt]])
                                k.tt("dve", ct_[:, 1, i4], A128[:, 1, i4], zi_, ALU.mult, [b_A128, b_zl[kt]], [b_ct[kt]])
                                k.tt("dve", ct_[:, 2, i4], A128[:, 0, i4], zi_, ALU.mult, [b_A128, b_zl[kt]], [b_ct[kt]])
                                k.tt("dve", ct_[:, 3, i4], A128[:, 1, i4], zr_, ALU.mult, [b_A128, b_zl[kt]], [b_ct[kt]])
                                k.tt("dve", car[:, i4], ct_[:, 0, i4], ct_[:, 1, i4], ALU.subtract, [b_ct[kt]], [b_car[kt]])
                                k.tt("dve", car[:, i4m], ct_[:, 2, i4], ct_[:, 3, i4], ALU.add, [b_ct[kt]], [b_car[kt]])
                                for isl in (i4, i4m):
                                    k.tt("pool", Dg[:, isl, :], identf.unsqueeze(1).to_broadcast([128, 4, 128]),
                                         car[:, isl].unsqueeze(2).to_broadcast([128, 4, 128]), ALU.mult, [b_cstf, b_car[kt]], [b_DgT[kt]])
                                yield
                                k.tt("dve", hp[:, 0, cs], k.ps[zre][:, :], qr[:, cs], ALU.mult, [k.pb[zre], b_qr], [b_hp[kt]])
                                k.tt("dve", hp[:, 1, cs], k.ps[zim][:, :], qi[:, cs], ALU.mult, [k.pb[zim], b_qr], [b_hp[kt]])
                                k.tt("dve", hp[:, 2, cs], k.ps[zim][:, :], qr[:, cs], ALU.mult, [k.pb[zim], b_qr], [b_hp[kt]])
                                k.tt("dve", hp[:, 3, cs], k.ps[zre][:, :], qi[:, cs], ALU.mult, [k.pb[zre], b_qr], [b_hp[kt]])
                                yield
                                bk = bre
                                first = True
                                for ii in range(4):
                                    i = kt * 4 + ii
                                    sl_ = slice(i * 128, (i + 1) * 128)
                                    for (pi_x, ci_x) in ((0, 0), (1, 1), (2, 2), (3, 2)):
                                        last = (ii == 3 and pi_x == 3)
                                        k.mm(bk, k.ps[bk][:, 0:128], Cb[:, ci_x, i, :], hp[:, pi_x, sl_], first, last, [b_Cb, b_hp[kt]])
                                        first = False
                                if dr == 0:
                                    k.stt(yacc[:, kt, cols], uT[:, kt, cols], dcol[:, kt:kt + 1], k.ps[bk][:, 0:128], ALU.mult, ALU.add,
                                          [b_u, b_dcol, k.pb[bk]], [b_yacc])
                                else:
                                    k.tt("dve", yacc[:, kt, cols], k.ps[bk][:, 0:128], yacc[:, kt, cols], ALU.add, [k.pb[bk], b_yacc], [b_yacc])
                                yield

                        live = [half_gen(0, [0, 1, 2, 3]), half_gen(1, [4, 5, 6, 7])]
                        for _ in range(S5SKEW):
                            next(live[0])
                        while live:
                            for g_ in list(live):
                                try:
                                    next(g_)
                                except StopIteration:
                                    live.remove(g_)
                        P.barrier()
            dump("s5yacc", yacc[:], b_yacc, [128, 2, NT])
            with ExitStack() as s4:
                wgf, b_wgf = k.sb(s4, [128, 2, 256], F32, "wgf"); wgb, b_wgb = k.sb(s4, [128, 2, 256], BF16, "wgb")
                bg, b_bg = k.sb(s4, [128, 2], F32, "bglu")
                k.load("sp", wgf[:], w_glu_d[l].rearrange("(kt p) n -> p kt n", p=128), [b_wgf])
                k.copy("dve", wgb[:], wgf[:], [b_wgf], [b_wgb])
                k.load("sp", bg[:], s5bg_d[l], [b_bg])
                zT, b_zT = k.sb(s4, [128, 2, 512], BF16, "zT")
                x2, b_x2 = k.sb(s4, [128, 512], F32, "x2"); thh, b_thh = k.sb(s4, [128, 512], F32, "thh")
                sg, b_sg = k.sb(s4, [128, 512], F32, "sg")
                for g, (c0, n) in enumerate(GROUPS):
                    for kt in range(2):
                        x_ = yacc[:, kt, c0:c0 + n]
                        k.tt("dve", x2[:, :n], x_, x_, ALU.mult, [b_yacc], [b_x2])
                        k.ts("dve", x2[:, :n], x2[:, :n], 0.044715, 1.0, ALU.mult, ALU.add, [b_x2], [b_x2])
                        k.tt("dve", x2[:, :n], x2[:, :n], x_, ALU.mult, [b_x2, b_yacc], [b_x2])
                        k.act(thh[:, :n], x2[:, :n], AF.Tanh, [b_x2], [b_thh], scale=math.sqrt(2.0 / math.pi))
                        k.stt(thh[:, :n], thh[:, :n], 1.0, x_, ALU.add, ALU.mult, [b_thh, b_yacc], [b_thh])
                        k.ts("dve", zT[:, kt, :n], thh[:, :n], 0.5, None, ALU.mult, None, [b_thh], [b_zT])
                    for ct in range(2):
                        bk = k.bank()
                        for kt in range(2):
                            k.mm(bk, k.ps[bk][:, :n], wgb[:, kt, ct * 128:(ct + 1) * 128], zT[:, kt, :n], kt == 0, kt == 1, [b_wgb, b_zT])
                        P.op("act", lambda e: e.activation(out=sg[:, :n], in_=k.ps[bk][:, :n], func=AF.Sigmoid, bias=bg[:, ct:ct + 1]),
                             reads=[k.pb[bk], b_bg], writes=[b_sg])
                        k.tt("dve", ydst[:, ct, c0:c0 + n], zT[:, ct, :n], sg[:, :n], ALU.mult, [b_zT, b_sg], [b_ydst])
                P.barrier()
        dump("yb", ydst[:], b_ydst, [128, 2, NT], BF16)

    def rwkv_mixer(l):
        ydst, b_ydst = yT[3]
        xd, b_xd = ydst, Buf("xd")
        with ExitStack() as st:
            rS, b_r = yall[:, 0], Buf("rw_r")
            kS, b_k = yall[:, 1], Buf("rw_k")
            vS, b_v = yall[:, 2], Buf("rw_v")
            kkS, b_kk = k.sb(st, [128, 2, NT], BF16, "rw_kk")
            cols, b_cols = k.sb(st, [128, 2, 9], F32, "rw_cols")
            k.load("sp", cols[:], rw_cols_d[l], [b_cols])
            C_KK, C_KA, C_LNG, C_LNB, C_RK, C_W0, C_A0 = 0, 1, 2, 3, 4, 5, 7
            for half in range(2):
              with ExitStack() as s2:
                WA, b_WA = load_w("pool", s2, w_rw_d[l, :, half * 512:(half + 1) * 512], 512, "rwWA")
                WB, b_WB = k.sb(s2, [128, 8, 512], BF16, "rwWB")
                with ExitStack() as s3:
                    mu, b_mu = k.sb(s3, [128, 512], F32, "rwmu")
                    k.load("sp", mu[:], rw_mu_d[l, :, half * 512:(half + 1) * 512], [b_mu])
                    for kt in range(8):
                        k.stt(WB[:, kt, :], WA[:, kt, :], 0.5, mu[:], ALU.mult, ALU.mult, [b_WA, b_mu], [b_WB])
                    k.ts("dve", mu[:], mu[:], -1.0, 1.0, ALU.mult, ALU.add, [b_mu], [b_mu])
                    for kt in range(8):
                        k.tt("pool", WA[:, kt, :], WA[:, kt, :], mu[:], ALU.mult, [b_WA, b_mu], [b_WA])
                    P.barrier()
                hgxs = [k.sb(s2, [128, 8, 516], BF16, "hgx") for _ in range(2)]
                hsxs = [k.sb(s2, [128, 8, 512], BF16, "hsx") for _ in range(2)]
                tmps = [k.sb(s2, [128, 514], F32, "htmp") for _ in range(2)]
                sq, b_sq = k.sb(s2, [128, 512], BF16, "rwsq"); kq, b_kq = k.sb(s2, [128, 512], F32, "rwkq")
                rs_, b_rs = k.sb(s2, [128, 512], F32, "rwrs"); lnt, b_lnt = k.sb(s2, [128, 512], F32, "rwlnt")
                for g, (c0, n) in enumerate(GROUPS if RW_SUB >= 2 else []):
                    j = jof(g)
                    hgx, b_hgx = hgxs[g % 2]; hsx, b_hsx = hsxs[g % 2]
                    s_lo, s_hi = (0, NCTX) if g == 0 else (NCTX, NT)
                    lo, hi = max(s_lo, c0 - 1), min(s_hi, c0 + n + 1)
                    o0 = lo - (c0 - 1)
                    if lo > c0 - 1:
                        k.memset("dve", hgx[:, :, 0:3], 0.0, [b_hgx])
                    if hi < c0 + n + 1:
                        k.memset("dve", hgx[:, :, n + 1:n + 3], 0.0, [b_hgx])
                    w_ = hi - lo
                    for kt in range(8):
                        tmp, tmpb = tmps[kt % 2]
                        k.tt("dve", tmp[:, :w_], xT[:, kt, lo:hi], rstd_b[:, lo:hi], ALU.mult, [b_xT, b_rstd], [tmpb])
                        P.op("act", lambda e: e.activation(out=hgx[:, kt, 1 + o0:1 + o0 + w_], in_=tmp[:, :w_], func=AF.Identity,
                                                           scale=AB[:, l, 0, 0, kt, j:j + 1], bias=AB[:, l, 0, 1, kt, j:j + 1]),
                             reads=[tmpb, b_AB], writes=[b_hgx])
                    for kt in range(8):
                        k.tt("pool", hsx[:, kt, :n], hgx[:, kt, 1:n + 1], hgx[:, kt, 3:n + 3], ALU.add, [b_hgx], [b_hsx])
                    for tl_ in range(4 if RW_SUB >= 3 else 0):
                        ti = half * 4 + tl_
                        bk = k.bank()
                        for kt in range(8):
                            k.mm(bk, k.ps[bk][:, :n], WA[:, kt, tl_ * 128:(tl_ + 1) * 128], hgx[:, kt, 2:n + 2], kt == 0, False, [b_WA, b_hgx])
                        for kt in range(8):
                            k.mm(bk, k.ps[bk][:, :n], WB[:, kt, tl_ * 128:(tl_ + 1) * 128], hsx[:, kt, :n], False, kt == 7, [b_WB, b_hsx])
                        dst, dstb = [(rS, b_r), (kS, b_k), (vS, b_v), (xd, b_xd)][ti // 2]
                        k.copy("act", dst[:, ti % 2, c0:c0 + n], k.ps[bk][:, :n], [k.pb[bk]], [dstb])
                        if ti // 2 == 1 and RW_SUB >= 4:
                            ci = ti % 2
                            k.ts("dve", kq[:, :n], k.ps[bk][:, :n], cols[:, ci, C_KK:C_KK + 1], None, ALU.mult, None, [k.pb[bk], b_cols], [b_kq])
                            k.act(sq[:, :n], kq[:, :n], AF.Square, [b_kq], [b_sq])
                            b2 = k.bank()
                            k.mm(b2, k.ps[b2][:, :n], bo64, sq[:, :n], True, True, [b_sq, b_cstb])
                            k.rstd_from_ss(rs_[:, :n], k.ps[b2][:, :n], b2, 1.0, lnt[:, :n], b_lnt, [b_rs])
                            k.tt("dve", kkS[:, ci, c0:c0 + n], kq[:, :n], rs_[:, :n], ALU.mult, [b_kq, b_rs], [b_kk])
                P.barrier()
            dump("rw_r", rS[:], b_r, [128, 2, NT], BF16); dump("rw_kk", kkS[:], b_kk, [128, 2, NT], BF16)
            dump("rw_xd", xd[:], b_xd, [128, 2, NT], BF16)
            yacc, _ = k.sb(st, [128, 2, NT], BF16, "rw_yacc")
            sw = {}
            def small(name, shape, src, dt=BF16, q="pool"):
                t, b = k.sb(st, shape, dt, name)
                k.load(q if dt == BF16 else "sp", t[:], src, [b])
                sw[name] = (t, b)
            for dr in range(2 if RW_LEVEL >= 1 else 0):
                small(f"w1_{dr}", [128, 2, 32], rw_w1_d[l, dr].rearrange("(kt p) n -> p kt n", p=128))
                small(f"w2_{dr}", [32, 256], rw_w2_d[l, dr])
                small(f"a1_{dr}", [128, 2, 32], rw_a1_d[l, dr].rearrange("(kt p) n -> p kt n", p=128))
                small(f"a2_{dr}", [32, 256], rw_a2_d[l, dr])
                small(f"w0r_{dr}", [1, 256], rw_w0row_d[l, dr], F32)
                small(f"tri_{dr}", [128, 2, 128], rw_tri_d[dr], F32)
                small(f"m1_{dr}", [128, 256], rw_m1_d[dr])
                small(f"mnt_{dr}", [128, 128], rw_mnt_d[dr])
            if RW_LEVEL >= 0:
                small("g1", [128, 2, 64], rw_g1_d[l].rearrange("(kt p) n -> p kt n", p=128))
                small("g2", [64, 256], rw_g2_d[l])
                small("ind", [128, 2], rw_ind_d, F32)
            ones1, b_ones1 = k.sb(st, [1, 128], F32, "ones1")
            k.memset("dve", ones1[:], 1.0, [b_ones1])

            def a_of(dr, xsrc, n, a_out, b_aout, x1t, b_x1t):
                a1, b_a1 = sw[f"a1_{dr}"]; a2, b_a2 = sw[f"a2_{dr}"]
                bk = k.bank()
                for kt in range(2):
                    k.mm(bk, k.ps[bk][0:32, :n], a1[:, kt, :], xsrc[:, kt, :], kt == 0, kt == 1, [b_a1, b_xd])
                k.copy("act", x1t[0:32, :n], k.ps[bk][0:32, :n], [k.pb[bk]], [b_x1t])
                for ci in range(2):
                    b2 = k.bank()
                    k.mm(b2, k.ps[b2][:, :n], a2[:, ci * 128:(ci + 1) * 128], x1t[0:32, :n], True, True, [b_a2, b_x1t])
                    P.op("act", lambda e: e.activation(out=a_out[:, ci, :n], in_=k.ps[b2][:, :n], func=AF.Sigmoid,
                                                       bias=cols[:, ci, C_A0 + dr:C_A0 + dr + 1]),
                         reads=[k.pb[b2], b_cols], writes=[b_aout])

            def kmod_of(a_in, b_ain, ksrc, n, out, b_out_, tmpf, b_tmpf):
                for ci in range(2):
                    k.ts("dve", tmpf[:, ci, :n], a_in[:, ci, :n], -1.0, cols[:, ci, C_KA:C_KA + 1], ALU.add, ALU.mult, [b_ain, b_cols], [b_tmpf])
                    k.stt(out[:, ci, :n], tmpf[:, ci, :n], 1.0, ksrc[:, ci, :], ALU.add, ALU.mult, [b_tmpf, b_k], [b_out_])

            b_yt = [Buf(f"yacc{t}") for t in range(NTT)]
            for (c0_, n_) in GROUPS:
                k.memset("dve", yacc[:, :, c0_:c0_ + n_], 0.0, b_yt[c0_ // 128:(c0_ + n_) // 128])

            def scan_gen(dr, sd, banks):
                    w1, b_w1 = sw[f"w1_{dr}"]; w2, b_w2 = sw[f"w2_{dr}"]; w0r, b_w0r = sw[f"w0r_{dr}"]
                    tri, b_tri = sw[f"tri_{dr}"]; m1, b_m1 = sw[f"m1_{dr}"]; mnt, b_mnt = sw[f"mnt_{dr}"]; ind, b_ind = sw["ind"]
                    M32, b_M32 = k.sb(sd, [128, 2, 64], F32, "M32"); Mbf, b_Mbf = k.sb(sd, [128, 2, 128], BF16, "Mblk")
                    k.memset("dve", M32[:], 0.0, [b_M32]); k.memset("dve", Mbf[:], 0.0, [b_Mbf])
                    x1t, b_x1t = k.sb(sd, [32, 128], BF16, "x1t")
                    tnh, b_tnh = k.sb(sd, [32, 128], BF16, "tnh")
                    lwT, b_lwT = k.sb(sd, [128, 256], F32, "lwT")
                    lam, b_lam = k.sb(sd, [128, 2, 3, 128], F32, "lam")
                    gam, b_gam = k.sb(sd, [128, 2, 2], F32, "gam")
                    aF, b_aF = k.sb(sd, [128, 2, 128], F32, "aF")
                    tF, b_tF = k.sb(sd, [128, 2, 128], F32, "tF")
                    kmod, b_kmod = k.sb(sd, [128, 2, 128], F32, "kmod")
                    AR, b_AR = k.sb(sd, [128, 2, 2, 128], BF16, "AR")
                    BK, b_BK = k.sb(sd, [128, 2, 2, 128], BF16, "BK")
                    BKt2, b_BKt = k.sb(sd, [128, 2, 2, 2, 128], BF16, "BKt")
                    k.memset("pool", BKt2[:], 0.0, [b_BKt])
                    Vt, b_Vt = k.sb(sd, [128, 2, 128], BF16, "Vt")
                    SCb, b_SCb = k.sb(sd, [128, 4, 256], BF16, "SCb"); SCk, b_SCk = k.sb(sd, [128, 4, 256], BF16, "SCk")
                    Nb = [k.sb(sd, [128, 4, 128], BF16, f"Nb{i}") for i in range(2)]
                    Tb = [k.sb(sd, [128, 4, 128], BF16, f"Tb{i}") for i in range(2)]
                    R, b_R = k.sb(sd, [128, 4, 128], BF16, "Rinv")
                    Wsb, b_Wsb = k.sb(sd, [128, 256], BF16, "Wsb"); Usb, b_Usb = k.sb(sd, [128, 256], BF16, "Usb")
                    mt, b_mt = k.sb(sd, [128, 2, 64], F32, "mt")
                    v4h = lambda ap: ap.rearrange("p (h t) -> p h t", h=4)
                    order = list(range(NTT)) if dr == 0 else [1, 0] + list(range(NTT - 1, 1, -1))
                    yield
                    for tt in order:
                        tc_ = slice(tt * 128, (tt + 1) * 128)
                        bk = k.bank()
                        for kt in range(2):
                            k.mm(bk, k.ps[bk][0:32, 0:128], w1[:, kt, :], xd[:, kt, tc_], kt == 0, kt == 1, [b_w1, b_xd])
                        k.act(tnh[:], k.ps[bk][0:32, 0:128], AF.Tanh, [k.pb[bk]], [b_tnh])
                        a1, b_a1 = sw[f"a1_{dr}"]; a2, b_a2 = sw[f"a2_{dr}"]
                        bk = k.bank()
                        for kt in range(2):
                            k.mm(bk, k.ps[bk][0:32, 0:128], a1[:, kt, :], xd[:, kt, tc_], kt == 0, kt == 1, [b_a1, b_xd])
                        k.copy("dve", x1t[0:32, 0:128], k.ps[bk][0:32, 0:128], [k.pb[bk]], [b_x1t])
                        yield
                        bk = k.bank()
                        k.mm(bk, k.ps[bk][:, 0:256], tnh[:], w2[:], True, False, [b_tnh, b_w2])
                        k.mm(bk, k.ps[bk][:, 0:256], ones1[:], w0r[:], False, True, [b_ones1, b_w0r])
                        k.act(lwT[:], k.ps[bk][:, 0:256], AF.Sigmoid, [k.pb[bk]], [b_lwT])
                        k.ts("dve", lwT[:], lwT[:], -math.exp(-0.5), None, ALU.mult, None, [b_lwT], [b_lwT])
                        b2 = k.bank()
                        for ci in range(2):
                            k.mm(b2, k.ps[b2][:, ci * 128:(ci + 1) * 128], a2[:, ci * 128:(ci + 1) * 128], x1t[0:32, 0:128], True, True, [b_a2, b_x1t],
                                 inc=(ci == 1))
                        for ci in range(2):
                            P.op("act", lambda e: e.activation(out=aF[:, ci, :], in_=k.ps[b2][:, ci * 128:(ci + 1) * 128], func=AF.Sigmoid,
                                                               bias=cols[:, ci, C_A0 + dr:C_A0 + dr + 1]),
                                 reads=[k.pb[b2], b_cols], writes=[b_aF])
                        yield
                        kmod_of(aF, b_aF, kS[:, :, tc_], 128, kmod, b_kmod, tF, b_tF)
                        for ci in range(2):
                            k.tt("pool", tF[:, ci, :], kkS[:, ci, tc_], aF[:, ci, :], ALU.mult, [b_kk, b_aF], [b_tF])
                        lbk = []
                        for ci in range(2):
                            bk = k.bank(); lbk.append(bk)
                            k.mm(bk, k.ps[bk][:, 0:128], lwT[:, ci * 128:(ci + 1) * 128], tri[:, 0, :], True, True, [b_lwT, b_tri], inc=False)
                            k.mm(bk, k.ps[bk][:, 128:256], lwT[:, ci * 128:(ci + 1) * 128], tri[:, 1, :], True, True, [b_lwT, b_tri], inc=False)
                            k.mm(bk, k.ps[bk][:, 256:258], lwT[:, ci * 128:(ci + 1) * 128], ind[:], True, True, [b_lwT, b_ind])
                        yield
                        for ci in range(2):
                            bk = lbk[ci]
                            k.act(lam[:, ci, 0, :], k.ps[bk][:, 0:128], AF.Exp, [k.pb[bk]], [b_lam])
                            k.act(lam[:, ci, 1, :], k.ps[bk][:, 0:128], AF.Exp, [k.pb[bk]], [b_lam], scale=-1.0)
                            k.act(lam[:, ci, 2, :], k.ps[bk][:, 128:256], AF.Exp, [k.pb[bk]], [b_lam])
                            k.act(gam[:, ci, :], k.ps[bk][:, 256:258], AF.Exp, [k.pb[bk]], [b_gam])
                        yield
                        if RWSTOP <= 1:
                            continue
                        for ci in range(2):
                            k.tt("dve", AR[:, ci, 1, :], rS[:, ci, tc_], lam[:, ci, 0, :], ALU.mult, [b_r, b_lam], [b_AR])
                            k.stt(AR[:, ci, 0, :], kkS[:, ci, tc_], -1.0, lam[:, ci, 2, :], ALU.mult, ALU.mult, [b_kk, b_lam], [b_AR])
                            k.tt("dve", BK[:, ci, 1, :], kmod[:, ci, :], lam[:, ci, 1, :], ALU.mult, [b_kmod, b_lam], [b_BK])
                            k.tt("pool", BK[:, ci, 0, :], tF[:, ci, :], lam[:, ci, 1, :], ALU.mult, [b_tF, b_lam], [b_BK])
                        yield
                        if RWSTOP <= 2:
                            continue
                        bk = k.bank()
                        pv = k.ps[bk][:].bitcast(BF16)
                        for ci in range(2):
                            for x_ in range(2):
                                P.op("pe", lambda e: e.transpose(pv[:, (ci * 2 + x_) * 128:(ci * 2 + x_ + 1) * 128], BK[:, ci, x_, :], identb),
                                     reads=[b_BK, b_cstb], writes=[k.pb[bk]], inc=False)
                            P.op("pe", lambda e: e.transpose(pv[:, (4 + ci) * 128:(5 + ci) * 128], vS[:, ci, tc_], identb),
                                 reads=[b_v, b_cstb], writes=[k.pb[bk]], inc=(ci == 1))
                        k.copy("act", BKt2[0:64, 0].rearrange("p a b c -> p (a b c)"), pv[0:64, 0:512], [k.pb[bk]], [b_BKt])
                        k.copy("act", BKt2[64:128, 1].rearrange("p a b c -> p (a b c)"), pv[64:128, 0:512], [k.pb[bk]], [b_BKt])
                        k.copy("dve", Vt[:].rearrange("p a c -> p (a c)"), pv[:, 512:768], [k.pb[bk]], [b_Vt])
                        if RWSTOP <= 3:
                            continue
                        m1b = m1[:].unsqueeze(1).to_broadcast([128, 2, 256])
                        for hh in range(2):
                            pb = 64 * hh
                            bA = k.bank(); bB = k.bank()
                            for x_, bx in ((0, bA), (1, bB)):
                                for ci in range(2):
                                    arv = AR[pb:pb + 64, ci, :, :].rearrange("p a t -> p (a t)")
                                    k.mm(bx, k.ps[bx][:, ci * 256:(ci + 1) * 256], BK[pb:pb + 64, ci, x_, :], arv, True, True, [b_BK, b_AR], inc=(ci == 1))
                            k.tt("dve", SCb[:, hh * 2:hh * 2 + 2, :], k.ps[bA][:, 0:512].rearrange("p (h t) -> p h t", h=2), m1b, ALU.mult,
                                 [k.pb[bA], b_m1], [b_SCb])
                            k.tt("dve", SCk[:, hh * 2:hh * 2 + 2, :], k.ps[bB][:, 0:512].rearrange("p (h t) -> p h t", h=2), m1b, ALU.mult,
                                 [k.pb[bB], b_m1], [b_SCk])
                            yield
                        Tc, b_Tc = Tb[0]
                        for hh in range(2):
                            pb = 64 * hh
                            bC = k.bank()
                            for ci in range(2):
                                k.mm(bC, k.ps[bC][:, ci * 128:(ci + 1) * 128], AR[pb:pb + 64, ci, 0, :], BK[pb:pb + 64, ci, 0, :], True, True, [b_AR, b_BK],
                                     inc=(ci == 1))
                            k.tt("dve", Tc[:, hh * 2:hh * 2 + 2, :], k.ps[bC][:, 0:256].rearrange("p (h t) -> p h t", h=2),
                                 mnt[:].unsqueeze(1).to_broadcast([128, 2, 128]), ALU.mult, [k.pb[bC], b_mnt], [b_Tc])
                        k.tt("pool", R[:], SCb[:, :, 0:128], identb.unsqueeze(1).to_broadcast([128, 4, 128]), ALU.add, [b_SCb, b_cstb], [b_R])
                        yield
                        if RWSTOP <= 4:
                            continue
                        Nc, b_Nc = SCb[:, :, 0:128], b_SCb
                        for i in range(1, 6):
                            Nn, b_Nn = Nb[i % 2]; Tn, b_Tn = Tb[i % 2]
                            bt_ = k.bank()
                            for h in range(4):
                                k.mm(bt_, k.ps[bt_][:, h * 128:(h + 1) * 128], Nc[:, h, :], Tc[:, h, :], True, True, [b_Nc, b_Tc], inc=(h == 3))
                            k.copy("dve", Tn[:], v4h(k.ps[bt_][:, 0:512]), [k.pb[bt_]], [b_Tn])
                            if i < 5:
                                bn = k.bank()
                                for h in range(4):
                                    k.mm(bn, k.ps[bn][:, h * 128:(h + 1) * 128], Tc[:, h, :], Nc[:, h, :], True, True, [b_Tc, b_Nc], inc=(h == 3))
                                k.copy("act", Nn[:], v4h(k.ps[bn][:, 0:512]), [k.pb[bn]], [b_Nn])
                            yield
                            br_ = k.bank()
                            for h in range(4):
                                k.mm(br_, k.ps[br_][:, h * 128:(h + 1) * 128], Tn[:, h, :], R[:, h, :], True, True, [b_Tn, b_R], inc=(h == 3))
                            k.tt("dve", R[:], v4h(k.ps[br_][:, 0:512]), R[:], ALU.add, [k.pb[br_], b_R], [b_R])
                            yield
                            Nc, b_Nc = Nn[:], b_Nn
                            Tc, b_Tc = Tn, b_Tn
                        if RWSTOP <= 5:
                            continue
                        bY, bW, bU, bM = banks
                        for jj in ([0, 1] if dr == 0 else [1, 0]):
                            pj = 64 * jj
                            for ci in range(2):
                                k.mm(bW, k.ps[bW][:, ci * 128:(ci + 1) * 128], AR[:, ci, 0, :], Mbf[:, ci, :], True, False, [b_AR, b_Mbf], inc=False)
                                for hh in range(2):
                                    h = ci * 2 + hh
                                    pb = 64 * hh
                                    k.mm(bW, k.ps[bW][:, h * 64:(h + 1) * 64], SCk[:, hh * 2 + ci, 0:128], Vt[:, ci, pb:pb + 64], False, True, [b_SCk, b_Vt],
                                         inc=(h == 3))
                            k.copy("act", Wsb[:], k.ps[bW][:, 0:256], [k.pb[bW]], [b_Wsb])
                            yield
                            for h in range(4):
                                oc = slice(h * 64, (h + 1) * 64)
                                k.mm(bU, k.ps[bU][:, oc], R[:, (h % 2) * 2 + h // 2, :], Wsb[:, oc], True, True, [b_R, b_Wsb], inc=(h == 3))
                            k.copy("dve", Usb[:], k.ps[bU][:, 0:256], [k.pb[bU]], [b_Usb])
                            yield
                            for ci in range(2):
                                for hh in range(2):
                                    h = ci * 2 + hh
                                    pb = 64 * hh
                                    oc = slice(h * 64, (h + 1) * 64)
                                    mo = k.ps[bM][pb:pb + 64, ci * 64:(ci + 1) * 64]
                                    k.mm(bM, mo, BKt2[:, jj, ci, 0, pb:pb + 64], Usb[:, oc], True, False, [b_BKt, b_Usb], inc=False)
                                    k.mm(bM, mo, BKt2[:, jj, ci, 1, pb:pb + 64], Vt[:, ci, pb:pb + 64], False, True, [b_BKt, b_Vt], inc=(h == 3))
                            for ci in range(2):
                                ycol = slice(ci * 128 + pj, ci * 128 + pj + 64)
                                k.mm(bY, k.ps[bY][:, ycol], Mbf[:, ci, :], AR[:, ci, 1, pj:pj + 64], True, False, [b_Mbf, b_AR], inc=False)
                                for hh in range(2):
                                    h = ci * 2 + hh
                                    pb = 64 * hh
                                    oc = slice(h * 64, (h + 1) * 64)
                                    k.mm(bY, k.ps[bY][pb:pb + 64, ycol], Usb[:, oc], SCb[:, hh * 2 + ci, 128 + pj:128 + pj + 64], False, False, [b_Usb, b_SCb], inc=False)
                                    k.mm(bY, k.ps[bY][pb:pb + 64, ycol], Vt[:, ci, pb:pb + 64], SCk[:, hh * 2 + ci, 128 + pj:128 + pj + 64], False, True, [b_Vt, b_SCk],
                                         inc=(h == 3))
                            k.tt("dve", mt[:], k.ps[bM][:, 0:128].rearrange("p (c n) -> p c n", c=2), M32[:], ALU.add, [k.pb[bM], b_M32], [b_mt])
                            k.tt("dve", Mbf[0:64, :, 0:64], mt[0:64, :, :], gam[0:64, :, jj:jj + 1].to_broadcast([64, 2, 64]), ALU.mult,
                                 [b_mt, b_gam], [b_Mbf])
                            k.tt("dve", Mbf[64:128, :, 64:128], mt[64:128, :, :], gam[64:128, :, jj:jj + 1].to_broadcast([64, 2, 64]), ALU.mult,
                                 [b_mt, b_gam], [b_Mbf])
                            k.tt("dve", M32[:], mt[:], gam[:, :, jj:jj + 1].to_broadcast([128, 2, 64]), ALU.mult, [b_mt, b_gam], [b_M32])
                            yield
                        k.tt("dve", yacc[:, :, tc_], k.ps[bY][:, 0:256].rearrange("p (c t) -> p c t", c=2), yacc[:, :, tc_], ALU.add,
                             [k.pb[bY], b_yt[tt]], [b_yt[tt]])
                        yield

            with ExitStack() as sd:
                threads = [[scan_gen(dr, sd, [4 * dr + i for i in range(4)]), [4 * dr + i for i in range(4)], 0] for dr in range(2)]
                live = list(threads)
                k.rrset, k.rr = threads[0][1], threads[0][2]
                for _ in range(RWSKEW):
                    next(threads[0][0])
                threads[0][2] = k.rr
                if os.environ.get("RWSEQ"):
                    for th in threads:
                        k.rrset, k.rr = th[1], th[2]
                        for _ in th[0]:
                            pass
                    live = []
                while live:
                    for th in list(live):
                        k.rrset, k.rr = th[1], th[2]
                        try:
                            next(th[0])
                        except StopIteration:
                            live.remove(th)
                        th[2] = k.rr
                k.rrset = list(range(8)); k.rr = 0
                P.barrier()
            b_yacc = Buf("rw_yacc_all")
            dump("rw_yacc", yacc[:], b_yacc, [128, 2, NT], BF16)
            with ExitStack() as s5_:
                    g1, b_g1 = sw["g1"]; g2, b_g2 = sw["g2"]
                    lneps, b_lneps = k.sb(s5_, [128, 1], F32, "lneps")
                    k.memset("dve", lneps[:], 64e-5, [b_lneps])

                    def epi_bufs():
                        d = {}
                        for nm, shp, dt in (("x1t", [32, 512], BF16), ("aF", [128, 2, 512], F32), ("tF", [128, 2, 512], F32),
                                            ("kmod", [128, 2, 512], F32), ("bon", [128, 2, 512], F32), ("ybf", [128, 512], BF16),
                                            ("yc", [128, 512], F32), ("lnt", [128, 512], F32),
                                            ("gh", [64, 512], BF16), ("gg", [128, 2, 512], F32), ("rkb", [128, 512], BF16)):
                            d[nm] = k.sb(s5_, shp, dt, "e" + nm)
                        return d

                    def epi_gen(groups, d):
                        x1t, b_x1t = d["x1t"]; aF, b_aF = d["aF"]; tF, b_tF = d["tF"]; kmod, b_kmod = d["kmod"]
                        bon, b_bon = d["bon"]; ybf, b_ybf = d["ybf"]; yc, b_yc = d["yc"]
                        lnt, b_lnt = d["lnt"]; rs_, b_rs = lnt, b_lnt; gh, b_gh = d["gh"]; gg, b_gg = d["gg"]; rkb, b_rkb = d["rkb"]
                        for g in groups:
                            c0, n = GROUPS[g]
                            gc_ = slice(c0, c0 + n)
                            for dr in range(2):
                                a_of(dr, xd[:, :, gc_], n, aF, b_aF, x1t, b_x1t)
                                yield
                                kmod_of(aF, b_aF, kS[:, :, gc_], n, kmod, b_kmod, tF, b_tF)
                                for ci in range(2):
                                    k.stt(rkb[:, :n], kmod[:, ci, :n], cols[:, ci, C_RK:C_RK + 1], rS[:, ci, gc_], ALU.mult, ALU.mult,
                                          [b_kmod, b_cols, b_r], [b_rkb])
                                    bk = k.bank()
                                    k.mm(bk, k.ps[bk][:, :n], bo64, rkb[:, :n], True, True, [b_rkb, b_cstb])
                                    yield
                                    if dr == 0:
                                        k.tt("dve", bon[:, ci, :n], k.ps[bk][:, :n], vS[:, ci, gc_], ALU.mult, [k.pb[bk], b_v], [b_bon])
                                    else:
                                        k.tt("dve", tF[:, ci, :n], k.ps[bk][:, :n], vS[:, ci, gc_], ALU.mult, [k.pb[bk], b_v], [b_tF])
                                        k.tt("pool", bon[:, ci, :n], bon[:, ci, :n], tF[:, ci, :n], ALU.add, [b_bon, b_tF], [b_bon])
                            bk = k.bank()
                            for kt in range(2):
                                k.mm(bk, k.ps[bk][0:64, :n], g1[:, kt, :], xd[:, kt, gc_], kt == 0, kt == 1, [b_g1, b_xd])
                            k.act(gh[:, :n], k.ps[bk][0:64, :n], AF.Sigmoid, [k.pb[bk]], [b_gh])
                            yield
                            for ci in range(2):
                                b2 = k.bank()
                                k.mm(b2, k.ps[b2][:, :n], g2[:, ci * 128:(ci + 1) * 128], gh[:, :n], True, True, [b_g2, b_gh])
                                k.copy("act", gg[:, ci, :n], k.ps[b2][:, :n], [k.pb[b2]], [b_gg])
                            yield
                            for ci in range(2):
                                bk = k.bank()
                                k.mm(bk, k.ps[bk][:, :n], bo64, yacc[:, ci, gc_], True, True, [b_yacc, b_cstb])
                                k.stt(yc[:, :n], k.ps[bk][:, :n], -1.0 / 64, yacc[:, ci, gc_], ALU.mult, ALU.add, [k.pb[bk], b_yacc], [b_yc])
                                k.act(ybf[:, :n], yc[:, :n], AF.Square, [b_yc], [b_ybf])
                                yield
                                b2 = k.bank()
                                k.mm(b2, k.ps[b2][:, :n], bo64, ybf[:, :n], True, True, [b_ybf, b_cstb])
                                P.op("act", lambda e: e.activation(out=lnt[:, :n], in_=k.ps[b2][:, :n], func=AF.Ln, scale=1.0 / 64, bias=lneps[:]),
                                     reads=[k.pb[b2], b_lneps], writes=[b_lnt])
                                k.act(rs_[:, :n], lnt[:, :n], AF.Exp, [b_lnt], [b_rs], scale=-0.5)
                                yield
                                k.stt(yc[:, :n], yc[:, :n], cols[:, ci, C_LNG:C_LNG + 1], rs_[:, :n], ALU.mult, ALU.mult, [b_yc, b_cols, b_rs], [b_yc])
                                k.stt(yc[:, :n], yc[:, :n], cols[:, ci, C_LNB:C_LNB + 1], bon[:, ci, :n], ALU.add, ALU.add, [b_yc, b_cols, b_bon], [b_yc])
                                k.tt("dve", ydst[:, ci, gc_], yc[:, :n], gg[:, ci, :n], ALU.mult, [b_yc, b_gg, b_xd], [b_ydst])
                                yield

                    glist = [g for g in range(len(GROUPS)) if not (l == DEPTH - 1 and g == 0)]
                    eths = [[epi_gen(glist[0::2], epi_bufs()), [0, 1, 2, 3], 0], [epi_gen(glist[1::2], epi_bufs()), [4, 5, 6, 7], 0]]
                    live = list(eths)
                    while live:
                        for th in list(live):
                            k.rrset, k.rr = th[1], th[2]
                            try:
                                next(th[0])
                            except StopIteration:
                                live.remove(th)
                            th[2] = k.rr
                    k.rrset = list(range(8)); k.rr = 0
                    P.barrier()
        dump("yd", ydst[:], b_ydst, [128, 2, NT], BF16)

    def final_out():
        with ExitStack() as st:
            ot = [k.sb(st, [128, D], F32, "ostage") for _ in range(2)]
            for tt in range(NLAT // 128):
                o, ob = ot[tt % 2]
                c0 = NCTX + tt * 128
                for half in range(2):
                    bk = k.bank()
                    for j in range(4):
                        kt = half * 4 + j
                        P.op("pe", lambda e: e.transpose(k.ps[bk][:, j * 128:(j + 1) * 128], xT[:, kt, c0:c0 + 128], identf),
                             reads=[b_xT, b_cstf], writes=[k.pb[bk]], inc=(j == 3))
                    k.copy("dve" if half == 0 else "act", o[:, half * 512:(half + 1) * 512], k.ps[bk][:], [k.pb[bk]], [ob])
                P.dma("sp", out_d[tt * 128:(tt + 1) * 128, :], o[:], reads=[ob])
            P.finish([b for _, b in ot])
            P.barrier()

    for l in range(DEPTH):
        if ("L%d" % l) not in stages:
            continue
        with ExitStack() as st:
            rms_stats(st)
            P.barrier()
        if "rw" in stages:
            rwkv_mixer(l)
        if "da" in stages:
            da_mixer(l)
        if "mla" in stages:
            mla_mixer(l)
        if "s5" in stages:
            s5_mixer(l)
        if "merge" in stages:
            merge(l)
        if "moe" in stages:
            moe(l)
    final_out()
    for e in ("sp",):
        toks = [(kk, v) for kk, v in P.cnt.items() if v > 0 and isinstance(kk, tuple)]
        for tok in toks:
            P._wait(e, tok)
    top.close()
    return nc, k, dbg_out


_CONSTS = None


def prep_shared(inp):
    g = {}
    g.update(host_consts())
    f32 = np.float32
    w_in = np.asarray(inp["w_in"], f32)
    offs = np.cumsum([0, 256, 256, 256, 256, 192, 128, 16, 256, 256, 256, 256, 4096])
    seg = lambda i: w_in[:, :, offs[i]:offs[i + 1]]
    g["w_ada"] = np.ascontiguousarray(inp["w_ada"], f32)
    ba = np.asarray(inp["b_ada"], f32)
    g["bada"] = np.ascontiguousarray(ba.reshape(DEPTH, 48, 128).transpose(0, 2, 1))
    g["gmix"] = np.stack([fm_cols(inp["norm_mix_g"][l], 8) for l in range(DEPTH)])
    g["gffn"] = np.stack([fm_cols(inp["norm_ffn_g"][l], 8) for l in range(DEPTH)])
    def pad3(w):
        o = np.zeros((DEPTH, D, 384), f32)
        for j in range(8):
            o[:, :, (j // 3) * 128 + (j % 3) * 32:(j // 3) * 128 + (j % 3) * 32 + 32] = w[:, :, j * 32:(j + 1) * 32]
        return o
    g["w_da_qk"] = np.ascontiguousarray(np.concatenate([pad3(seg(0)), pad3(seg(1))], axis=2))
    g["w_da_v"] = np.ascontiguousarray(seg(2))
    dg = np.asarray(inp["da_qk_norm_g"], f32)
    g["da_g"] = np.ascontiguousarray(np.stack([np.tile(dg[:, 0, :], (1, 4)), np.tile(dg[:, 1, :], (1, 4))], axis=2))
    g["da_lam"] = np.ascontiguousarray(np.broadcast_to(np.asarray(inp["da_lambda"], f32).reshape(DEPTH, 1, 128), (DEPTH, 128, 128)))
    g["da_sub"] = np.ascontiguousarray(np.broadcast_to(np.asarray(inp["da_subln_g"], f32).reshape(DEPTH, 1, 64), (DEPTH, 128, 64)))
    g["w_mla_c"] = np.zeros((DEPTH, D, 384), f32)
    g["w_mla_c"][:, :, 0:192] = seg(4)
    g["w_mla_c"][:, :, 256:384] = seg(5)
    g["w_mla_kr"] = np.zeros((DEPTH, D, 128), f32)
    g["w_mla_kr"][:, :, 32:48] = seg(6)
    g["w_mla_kr"][:, :, 96:112] = seg(6)
    wuq = np.asarray(inp["mla_w_uq"], f32)
    wuqp = np.zeros((DEPTH, 256, 256), f32)
    for h in range(4):
        wuqp[:, 0:192, 64 * h:64 * h + 48] = wuq[:, :, 48 * h:48 * h + 48]
    g["w_uq"] = np.ascontiguousarray(wuqp.reshape(DEPTH, 2, 128, 256).transpose(0, 2, 1, 3))
    wukv = np.asarray(inp["mla_w_ukv"], f32)
    g["w_ukvk"] = np.zeros((DEPTH, 128, 256), f32)
    g["w_ukvv"] = np.zeros((DEPTH, 128, 256), f32)
    for h in range(4):
        g["w_ukvk"][:, :, 64 * h:64 * h + 32] = wukv[:, :, 96 * h:96 * h + 32]
        g["w_ukvv"][:, :, 64 * h:64 * h + 64] = wukv[:, :, 96 * h + 32:96 * h + 96]
    gcq = np.zeros((DEPTH, 256), f32); gcq[:, 0:192] = np.asarray(inp["mla_cq_norm_g"], f32)
    gkv = np.asarray(inp["mla_ckv_norm_g"], f32)
    g["mla_gc"] = np.ascontiguousarray(np.stack([gcq[:, 0:128], gcq[:, 128:256], gkv], axis=2))
    mg = np.asarray(inp["mla_qk_norm_g"], f32)
    mgp = np.zeros((DEPTH, 2, 128), f32)
    for h in range(2):
        mgp[:, :, 64 * h:64 * h + 48] = mg
    g["mla_g"] = np.ascontiguousarray(mgp.transpose(0, 2, 1))
    g["w_s5"] = np.ascontiguousarray(seg(3))
    Bre = np.asarray(inp["s5_b_re"], f32); Bim = np.asarray(inp["s5_b_im"], f32)
    Cre = np.asarray(inp["s5_c_re"], f32); Cim = np.asarray(inp["s5_c_im"], f32)
    s5B = np.zeros((DEPTH, 2, 2, 128, 1024), f32)
    s5C = np.zeros((DEPTH, 2, 2, 128, 8, 128), f32)
    for gi in range(16):
        kt, gl = gi // 8, gi % 8
        s5B[:, :, kt, gl * 16:(gl + 1) * 16, gl * 64:(gl + 1) * 64] = Bre[:, :, gi].transpose(0, 1, 3, 2)
        s5B[:, :, kt, gl * 16:(gl + 1) * 16, 512 + gl * 64:512 + (gl + 1) * 64] = Bim[:, :, gi].transpose(0, 1, 3, 2)
        i, po = gi // 2, (gi % 2) * 64
        s5C[:, :, 0, po:po + 64, i, gl * 16:(gl + 1) * 16] = Cre[:, :, gi].transpose(0, 1, 3, 2)
        s5C[:, :, 1, po:po + 64, i, gl * 16:(gl + 1) * 16] = Cim[:, :, gi].transpose(0, 1, 3, 2)
    g["s5B"] = s5B; g["s5C"] = s5C
    lre = np.asarray(inp["s5_lam_re"], f32).reshape(DEPTH, 2, 1024)
    lim = np.asarray(inp["s5_lam_im"], f32).reshape(DEPTH, 2, 1024)
    ldt = np.repeat(np.asarray(inp["s5_log_dt"], f32), 64, axis=2)
    tm = np.stack([lre, lim, ldt], axis=2)
    g["s5tm"] = np.ascontiguousarray(np.broadcast_to(tm[:, :, :, None, :], (DEPTH, 2, 3, 128, 1024)))
    g["s5fm"] = np.ascontiguousarray(tm.reshape(DEPTH, 2, 3, 8, 128).transpose(0, 1, 4, 2, 3))
    g["s5d"] = np.stack([fm_cols(np.asarray(inp["s5_d"], f32)[l].reshape(256), 2) for l in range(DEPTH)])
    g["s5bg"] = np.stack([fm_cols(np.asarray(inp["s5_b_glu"], f32)[l], 2) for l in range(DEPTH)])
    g["w_glu"] = np.ascontiguousarray(inp["s5_w_glu"], f32)
    pidx = np.arange(128, dtype=f32)
    posc = np.zeros((2, 128, 2), f32); posc[0, :, 0] = -pidx; posc[1, :, 0] = -(127 - pidx)
    posr = np.zeros((2, 128, 128), f32); posr[0] = pidx[None, :]; posr[1] = (127 - pidx)[None, :]
    tri = np.zeros((2, 128, 128), f32)
    tri[0] = (pidx[:, None] <= pidx[None, :]).astype(f32)
    tri[1] = (pidx[:, None] >= pidx[None, :]).astype(f32)
    g["posc"] = posc; g["posr"] = posr; g["tri"] = tri
    g["w_rw"] = np.ascontiguousarray(np.concatenate([seg(7), seg(8), seg(9), seg(10)], axis=2))
    mu = np.asarray(inp["rw_mu"], f32).reshape(DEPTH, 1, 1024)
    g["rw_mu_row"] = np.ascontiguousarray(np.broadcast_to(mu, (DEPTH, 128, 1024)))
    colsl = []
    for l in range(DEPTH):
        cs = [inp["rw_k_k"][l], inp["rw_k_a"][l], inp["rw_ln_g"][l], inp["rw_ln_b"][l], np.asarray(inp["rw_r_k"][l]).reshape(256),
              inp["rw_w0"][l, 0], inp["rw_w0"][l, 1], inp["rw_a0"][l, 0], inp["rw_a0"][l, 1]]
        colsl.append(np.stack([fm_cols(c, 2) for c in cs], axis=2))
    g["rw_cols"] = np.ascontiguousarray(np.stack(colsl))
    g["rw_w0row"] = np.ascontiguousarray(np.asarray(inp["rw_w0"], f32).reshape(DEPTH, 2, 1, 256))
    for nm in ("rw_w1", "rw_w2", "rw_a1", "rw_a2", "rw_g1", "rw_g2"):
        g[nm] = np.ascontiguousarray(inp[nm], f32)
    t = np.arange(128)
    same = (t[:, None] // 64) == (t[None, :] // 64)
    tri = np.zeros((2, 128, 2, 128), f32)
    tri[0, :, 0, :] = (same & (t[:, None] <= t[None, :])); tri[0, :, 1, :] = (same & (t[:, None] < t[None, :]))
    tri[1, :, 0, :] = (same & (t[:, None] >= t[None, :])); tri[1, :, 1, :] = (same & (t[:, None] > t[None, :]))
    g["rw_tri"] = tri
    ind = np.zeros((128, 2), f32); ind[:64, 0] = 1; ind[64:, 1] = 1
    g["rw_ind"] = ind
    m1 = np.zeros((2, 128, 256), f32)
    m1[0, :, :128] = (same & (t[:, None] < t[None, :])); m1[0, :, 128:] = (same & (t[:, None] <= t[None, :]))
    m1[1, :, :128] = (same & (t[:, None] > t[None, :])); m1[1, :, 128:] = (same & (t[:, None] >= t[None, :]))
    g["rw_m1"] = m1
    mnt = np.zeros((2, 128, 128), f32)
    mnt[0] = (same & (t[None, :] < t[:, None])); mnt[1] = (same & (t[None, :] > t[:, None]))
    g["rw_mnt"] = mnt
    g["w_gates"] = np.ascontiguousarray(seg(11).reshape(DEPTH, 8, 128, 4, 8, 128).transpose(0, 4, 2, 1, 3, 5)).reshape(DEPTH, 8, 128, 4096)
    g["w_branch"] = np.ascontiguousarray(np.asarray(inp["w_branch"], f32).reshape(DEPTH, 4, 2, 128, D).transpose(0, 3, 2, 1, 4)).reshape(DEPTH, 128, 8192)
    g["w_out"] = np.ascontiguousarray(inp["w_out"], f32)
    g["router_w"] = np.ascontiguousarray(inp["router_w"], f32)
    g["router_b"] = np.ascontiguousarray(np.broadcast_to(np.asarray(inp["router_bias"], f32).reshape(1, 16), (128, 16)))
    sel = np.zeros((16, 16, 128), f32)
    for e in range(16):
        sel[e, e, :] = 1.0
    g["sel"] = sel
    g["exp_w_gate"] = np.ascontiguousarray(inp["exp_w_gate"], f32)
    g["exp_w_up"] = np.ascontiguousarray(inp["exp_w_up"], f32)
    g["exp_w_down"] = np.ascontiguousarray(inp["exp_w_down"], f32)
    return g


def prep_core(inp, b):
    x = np.asarray(inp["x"], np.float32)[b]
    ctx = np.asarray(inp["ctx"], np.float32)[b]
    xin = np.ascontiguousarray(np.concatenate([ctx, x], axis=0))
    c2 = np.stack([np.asarray(inp["c"], np.float32)[b], np.asarray(inp["c_ctx"], np.float32)], axis=0)
    c2T = np.ascontiguousarray(c2.reshape(2, 8, 128).transpose(2, 1, 0))
    return {"xin": xin, "c2T": c2T}


RW_LEVEL = int(os.environ.get('RW_LEVEL', '9'))
RWSTOP = int(os.environ.get('RWSTOP', '9'))
S5SKEW = int(os.environ.get('S5SKEW', '1'))
RWSKEW = int(os.environ.get('RWSKEW', '0'))
RW_SUB = int(os.environ.get('RW_SUB', '9'))
STAGES_ALL = ("L0", "L1", "da", "mla", "s5", "rw", "merge", "moe")


def kernel(**inputs):
    nc, k, _ = build(STAGES_ALL)
    shared = prep_shared(inputs)
    in_maps = []
    for b in range(8):
        m = dict(shared)
        m.update(prep_core(inputs, b))
        in_maps.append({kk: m[kk] for kk in k.ins})
    res = run_bass_kernel_spmd(nc, in_maps, core_ids=list(range(8)))
    return np.stack([np.asarray(r["out"]) for r in res.results], axis=0).astype(np.float32)
```

```python
import math
import os
import numpy as np
import concourse.bass as bass
import concourse.mybir as mybir
from concourse.bass_utils import run_bass_kernel_spmd

F32 = mybir.dt.float32
BF16 = mybir.dt.bfloat16
I32 = mybir.dt.int32
AF = mybir.ActivationFunctionType
ALU = mybir.AluOpType
AX = mybir.AxisListType

D = 1024
NT = 2304
NCTX = 256
NLAT = 2048
DEPTH = 2
EPS = 1e-6
GROUPS = [(0, 256)] + [(256 + 512 * i, 512) for i in range(4)]
NTT = 18


class Buf:
    __slots__ = ("name", "w", "r", "excl")

    def __init__(self, name="", excl=False):
        self.name = name
        self.w = None
        self.r = {}
        self.excl = excl


class Prog:
    NDMA = 8

    def __init__(self, nc):
        self.nc = nc
        self.eng = {"pe": nc.tensor, "act": nc.scalar, "dve": nc.vector, "pool": nc.gpsimd, "sp": nc.sync}
        self.sems = {}
        self.cnt = {}
        self.seen = {e: {} for e in self.eng}
        self.pend = {e: ([], []) for e in self.eng}
        for e in self.eng:
            self.sems[e] = nc.alloc_semaphore("s_" + e)
            self.cnt[e] = 0
        self.dq = {}
        for q in ("sp", "pool"):
            lst = []
            for i in range(self.NDMA):
                key = ("d", q, i)
                self.sems[key] = nc.alloc_semaphore(f"d_{q}{i}")
                self.cnt[key] = 0
                lst.append(key)
            self.dq[q] = [lst, 0, [None] * self.NDMA]
        self.ninst = 0

    def _wait(self, e, tok):
        key, val = tok
        if key == e:
            if e == "pe":
                return
            if val < self.cnt[e] - 1:
                return
        if self.seen[e].get(key, 0) >= val:
            return
        self.eng[e].wait_ge(self.sems[key], val)
        self.seen[e][key] = val
        self.ninst += 1

    def _deps(self, e, reads, writes):
        for b in reads:
            if b.w is not None:
                self._wait(e, b.w)
            if b.excl:
                for tok in b.r.values():
                    if tok[0] != e:
                        self._wait(e, tok)
        for b in writes:
            if b.w is not None:
                self._wait(e, b.w)
            for tok in b.r.values():
                if tok[0] != e:
                    self._wait(e, tok)

    def op(self, e, fn, reads=(), writes=(), inc=True):
        self._deps(e, reads, writes)
        inst = fn(self.eng[e])
        self.ninst += 1
        pr, pw = self.pend[e]
        pr.extend(reads)
        pw.extend(writes)
        if inc:
            self.cnt[e] += 1
            tok = (e, self.cnt[e])
            inst.then_inc(self.sems[e], 1)
            for b in pr:
                b.r[e] = tok
            for b in pw:
                b.w = tok
                b.r = {}
            self.pend[e] = ([], [])
        return inst

    def dma(self, q, out, in_, reads=(), writes=(), **kw):
        lst, rr, last = self.dq[q]
        k = rr
        self.dq[q][1] = (rr + 1) % self.NDMA
        if last[k] is not None:
            self._wait(q, last[k])
        self._deps(q, reads, writes)
        inst = self.eng[q].dma_start(out=out, in_=in_, **kw)
        self.ninst += 1
        key = lst[k]
        self.cnt[key] += 16
        tok = (key, self.cnt[key])
        inst.then_inc(self.sems[key], 16)
        last[k] = tok
        for b in reads:
            b.r[key] = tok
        for b in writes:
            b.w = tok
            b.r = {}
        return tok

    def finish(self, bufs, e="sp"):
        for b in bufs:
            if b.w is not None:
                self._wait(e, b.w)
            for tok in b.r.values():
                self._wait(e, tok)

    def barrier(self):
        toks = [(k, v) for k, v in self.cnt.items() if v > 0]
        for e in self.eng:
            for tok in toks:
                if tok[0] != e:
                    self._wait(e, tok)


from contextlib import ExitStack


class K:
    def __init__(self, nc, dbg=None):
        self.nc = nc
        self.P = Prog(nc)
        self.dbg = dbg or {}
        self.ins = {}
        self.uid = 0
        self.ps = []
        self.pb = []
        for i in range(8):
            self.ps.append(nc.alloc_psum_tensor(f"ps{i}", [128, 512], F32))
            self.pb.append(Buf(f"ps{i}", excl=True))
        self.rr = 0
        self.rrset = list(range(8))

    def din(self, name, shape, dtype=F32):
        t = self.nc.dram_tensor(name, list(shape), dtype, kind="ExternalInput").ap()
        self.ins[name] = t
        return t

    def sb(self, stack, shape, dtype, name=None):
        self.uid += 1
        nm = f"{name or 't'}_{self.uid}"
        t = stack.enter_context(self.nc.sbuf_tensor(nm, list(shape), dtype))
        nb = int(np.prod(shape[1:])) * (2 if dtype == BF16 else 4)
        self.live = getattr(self, "live", 0) + nb
        if self.live > getattr(self, "peak", 0):
            self.peak = self.live; self.peak_at = nm
        def _dec(nb=nb):
            self.live -= nb
        stack.callback(_dec)
        return t, Buf(nm)

    def bank(self):
        i = self.rrset[self.rr % len(self.rrset)]
        self.rr += 1
        return i

    def mm(self, bank, out, lhsT, rhs, start, stop, reads, inc=None):
        self.P.op("pe", lambda e: e.matmul(out, lhsT=lhsT, rhs=rhs, start=start, stop=stop, skip_group_check=True),
                  reads=reads, writes=[self.pb[bank]], inc=(stop if inc is None else inc))

    def act(self, out, in_, func, reads, writes, scale=None, bias=None, eng="act"):
        kw = {}
        if scale is not None:
            kw["scale"] = scale
        if bias is not None:
            kw["bias"] = bias
        self.P.op("act", lambda e: e.activation(out=out, in_=in_, func=func, **kw), reads=reads, writes=writes)

    def tt(self, eng, out, in0, in1, op, reads, writes):
        self.P.op(eng, lambda e: e.tensor_tensor(out=out, in0=in0, in1=in1, op=op), reads=reads, writes=writes)

    def ts(self, eng, out, in0, s1, s2, op0, op1, reads, writes):
        if op1 is None:
            self.P.op(eng, lambda e: e.tensor_scalar(out=out, in0=in0, scalar1=s1, scalar2=None, op0=op0),
                      reads=reads, writes=writes)
        else:
            self.P.op(eng, lambda e: e.tensor_scalar(out=out, in0=in0, scalar1=s1, scalar2=s2, op0=op0, op1=op1),
                      reads=reads, writes=writes)

    def stt(self, out, in0, scalar, in1, op0, op1, reads, writes):
        self.P.op("dve", lambda e: e.scalar_tensor_tensor(out=out, in0=in0, scalar=scalar, in1=in1, op0=op0, op1=op1),
                  reads=reads, writes=writes)

    def copy(self, eng, out, in_, reads, writes):
        if eng == "act":
            self.P.op("act", lambda e: e.copy(out=out, in_=in_), reads=reads, writes=writes)
        else:
            self.P.op(eng, lambda e: e.tensor_copy(out=out, in_=in_), reads=reads, writes=writes)

    def memset(self, eng, ap, val, writes):
        self.P.op(eng, lambda e: e.memset(ap, val), writes=writes)

    def load(self, q, out, in_, writes, reads=()):
        self.P.dma(q, out, in_, reads=reads, writes=writes)

    def rstd_from_ss(self, out, ss_ps, bank, inv_n, tmp, tmpb, writes):
        self.P.op("act", lambda e: e.activation(out=tmp, in_=ss_ps, func=AF.Ln, scale=inv_n, bias=self.eps_col[:]),
                  reads=[self.pb[bank], self.b_const], writes=[tmpb])
        self.P.op("act", lambda e: e.activation(out=out, in_=tmp, func=AF.Exp, scale=-0.5), reads=[tmpb], writes=writes)


def host_consts():
    ident = np.eye(128, dtype=np.float32)
    bo32 = np.kron(np.eye(4, dtype=np.float32), np.ones((32, 32), np.float32))
    bo64 = np.kron(np.eye(2, dtype=np.float32), np.ones((64, 64), np.float32))
    Rda = np.zeros((128, 128), np.float32)
    for h in range(4):
        for d in range(16):
            Rda[32 * h + d, 32 * h + d + 16] = -1.0
            Rda[32 * h + d + 16, 32 * h + d] = 1.0
    Rmla = np.zeros((128, 128), np.float32)
    for h in range(2):
        for d in range(8):
            Rmla[64 * h + 32 + d, 64 * h + 40 + d] = -1.0
            Rmla[64 * h + 40 + d, 64 * h + 32 + d] = 1.0
    cst = np.concatenate([ident, bo32, bo64, Rda.T.copy(), Rmla.T.copy()], axis=1)
    def tables(rot_dim):
        rows = NLAT // 64
        row = np.repeat(np.arange(rows, dtype=np.float32), 64)
        col = np.tile(np.arange(64, dtype=np.float32), rows)
        n_freq = rot_dim // 4
        inv = (10000.0 ** (-np.arange(n_freq, dtype=np.float32) / n_freq)).astype(np.float32)
        ang = np.concatenate([row[:, None] * inv, col[:, None] * inv], axis=-1)
        return np.cos(ang).astype(np.float32), np.sin(ang).astype(np.float32)
    c, s = tables(32)
    da_cos = np.ones((128, NT), np.float32)
    da_sin = np.zeros((128, NT), np.float32)
    for h in range(4):
        for d in range(32):
            da_cos[32 * h + d, NCTX:] = c[:, d % 16]
            da_sin[32 * h + d, NCTX:] = s[:, d % 16]
    c, s = tables(16)
    ml_cos = np.ones((128, NT), np.float32)
    ml_sin = np.zeros((128, NT), np.float32)
    for h in range(2):
        for d in range(16):
            ml_cos[64 * h + 32 + d, NCTX:] = c[:, d % 8]
            ml_sin[64 * h + 32 + d, NCTX:] = s[:, d % 8]
    return dict(cst=cst, da_cos=da_cos, da_sin=da_sin, ml_cos=ml_cos, ml_sin=ml_sin)


def fm_cols(v, ntile):
    return np.ascontiguousarray(np.asarray(v, np.float32).reshape(ntile, 128).T)


def build(stages, dbg_names=()):
    nc = bass.Bass("TRN2", target_bir_lowering=False)
    k = K(nc)
    P = k.P
    top = ExitStack()
    xin = k.din("xin", [NT, D])
    c2T = k.din("c2T", [128, 8, 2])
    cst_d = k.din("cst", [128, 640])
    da_cos_d = k.din("da_cos", [128, NT]); da_sin_d = k.din("da_sin", [128, NT])
    ml_cos_d = k.din("ml_cos", [128, NT]); ml_sin_d = k.din("ml_sin", [128, NT])
    w_ada = k.din("w_ada", [DEPTH, D, 6 * D])
    bada_d = k.din("bada", [DEPTH, 128, 48])
    gmix_d = k.din("gmix", [DEPTH, 128, 8]); gffn_d = k.din("gffn", [DEPTH, 128, 8])
    w_da_qk = k.din("w_da_qk", [DEPTH, D, 768]); w_da_v = k.din("w_da_v", [DEPTH, D, 256])
    da_g_d = k.din("da_g", [DEPTH, 128, 2])
    da_lam_d = k.din("da_lam", [DEPTH, 128, 128])
    da_sub_d = k.din("da_sub", [DEPTH, 128, 64])
    w_mla_c = k.din("w_mla_c", [DEPTH, D, 384]); w_mla_kr = k.din("w_mla_kr", [DEPTH, D, 128])
    w_uq_d = k.din("w_uq", [DEPTH, 128, 2, 256]); w_ukvk_d = k.din("w_ukvk", [DEPTH, 128, 256]); w_ukvv_d = k.din("w_ukvv", [DEPTH, 128, 256])
    mla_gc_d = k.din("mla_gc", [DEPTH, 128, 3])
    mla_g_d = k.din("mla_g", [DEPTH, 128, 2])
    w_gates_d = k.din("w_gates", [DEPTH, 8, 128, 4096]); w_branch_d = k.din("w_branch", [DEPTH, 128, 8192]); w_out_d = k.din("w_out", [DEPTH, D, D])
    router_w_d = k.din("router_w", [D, 16]); router_b_d = k.din("router_b", [128, 16]); sel_d = k.din("sel", [16, 16, 128])
    exp_g_d = k.din("exp_w_gate", [DEPTH, 16, D, 512]); exp_u_d = k.din("exp_w_up", [DEPTH, 16, D, 512]); exp_d_d = k.din("exp_w_down", [DEPTH, 16, 512, D])
    w_s5_d = k.din("w_s5", [DEPTH, D, 256])
    s5B_d = k.din("s5B", [DEPTH, 2, 2, 128, 1024])
    s5C_d = k.din("s5C", [DEPTH, 2, 2, 128, 8, 128])
    s5tm_d = k.din("s5tm", [DEPTH, 2, 3, 128, 1024])
    s5fm_d = k.din("s5fm", [DEPTH, 2, 128, 3, 8])
    s5d_d = k.din("s5d", [DEPTH, 128, 2]); s5bg_d = k.din("s5bg", [DEPTH, 128, 2])
    w_glu_d = k.din("w_glu", [DEPTH, 256, 256])
    posc_d = k.din("posc", [2, 128, 2]); posr_d = k.din("posr", [2, 128, 128]); tri_d = k.din("tri", [2, 128, 128])
    w_rw_d = k.din("w_rw", [DEPTH, D, 1024]); rw_mu_d = k.din("rw_mu_row", [DEPTH, 128, 1024])
    rw_cols_d = k.din("rw_cols", [DEPTH, 128, 2, 9])
    rw_w0row_d = k.din("rw_w0row", [DEPTH, 2, 1, 256])
    rw_w1_d = k.din("rw_w1", [DEPTH, 2, 256, 32]); rw_w2_d = k.din("rw_w2", [DEPTH, 2, 32, 256])
    rw_a1_d = k.din("rw_a1", [DEPTH, 2, 256, 32]); rw_a2_d = k.din("rw_a2", [DEPTH, 2, 32, 256])
    rw_g1_d = k.din("rw_g1", [DEPTH, 256, 64]); rw_g2_d = k.din("rw_g2", [DEPTH, 64, 256])
    rw_tri_d = k.din("rw_tri", [2, 128, 2, 128]); rw_ind_d = k.din("rw_ind", [128, 2])
    rw_m1_d = k.din("rw_m1", [2, 128, 256]); rw_mnt_d = k.din("rw_mnt", [2, 128, 128])
    out_d = nc.dram_tensor("out", [NLAT, D], F32, kind="ExternalOutput").ap()
    dbg_out = {}

    xT, b_xT = k.sb(top, [128, 8, NT], F32, "xT")
    cstf, b_cstf = k.sb(top, [128, 640], F32, "cstf")
    cstb, b_cstb = k.sb(top, [128, 640], BF16, "cstb")
    onesb, b_ones = k.sb(top, [128, 128], BF16, "ones")
    k.eps_col, k.b_const = k.sb(top, [128, 1], F32, "eps")
    MOD, b_MOD = k.sb(top, [128, DEPTH, 6, 8, 2], F32, "MOD")
    AB, b_AB = k.sb(top, [128, DEPTH, 2, 2, 8, 2], F32, "AB")
    rstd_b, b_rstd = k.sb(top, [128, NT], F32, "rstd")
    yall, b_yall = k.sb(top, [128, 4, 2, NT], BF16, "yall")
    yT = []
    for i in range(4):
        yT.append((yall[:, i], Buf(f"y{i}")))
    k.memset("dve", k.eps_col[:], EPS, [k.b_const])
    k.memset("dve", onesb[:], 1.0, [b_ones])
    k.load("sp", cstf[:], cst_d, [b_cstf])
    k.copy("dve", cstb[:], cstf[:], [b_cstf], [b_cstb])
    identf = cstf[:, 0:128]
    identb = cstb[:, 0:128]
    bo32 = cstb[:, 128:256]; bo64 = cstb[:, 256:384]; Rda = cstb[:, 384:512]; Rmla = cstb[:, 512:640]

    def dump(name, ap_sb, buf, shape, dtype=F32):
        if name in dbg_names:
            d = nc.dram_tensor("dbg_" + name, list(shape), dtype, kind="ExternalOutput").ap()
            dbg_out[name] = d
            P.dma("sp", d, ap_sb, reads=[buf])
            P.barrier()

    with ExitStack() as st:
        xs = [k.sb(st, [128, D], F32, "xstage") for _ in range(2)]
        for tt in range(NTT):
            xt, xb = xs[tt % 2]
            k.load("sp", xt[:], xin[tt * 128:(tt + 1) * 128, :], [xb])
            for half in range(2):
                bk = k.bank()
                for j in range(4):
                    kt = half * 4 + j
                    P.op("pe", lambda e: e.transpose(k.ps[bk][:, j * 128:(j + 1) * 128], xt[:, kt * 128:(kt + 1) * 128], identf),
                         reads=[xb, b_cstf], writes=[k.pb[bk]], inc=(j == 3))
                k.copy("dve" if half == 0 else "act", xT[:, half * 4:half * 4 + 4, tt * 128:(tt + 1) * 128],
                       k.ps[bk][:].rearrange("p (j t) -> p j t", j=4), [k.pb[bk]], [b_xT])
        scT, b_scT = k.sb(st, [128, 8, 2], F32, "scT")
        scb, b_scb = k.sb(st, [128, 8, 2], BF16, "scb")
        bada, b_bada = k.sb(st, [128, DEPTH, 48], F32, "bada")
        gm, b_gm = k.sb(st, [128, DEPTH, 2, 8], F32, "gm")
        k.load("sp", scT[:], c2T, [b_scT])
        k.act(scb[:], scT[:], AF.Silu, [b_scT], [b_scb])
        for l in range(DEPTH):
            k.load("sp", bada[:, l, :], bada_d[l], [b_bada])
            k.load("sp", gm[:, l, 0, :], gmix_d[l], [b_gm])
            k.load("sp", gm[:, l, 1, :], gffn_d[l], [b_gm])
        wa = [k.sb(st, [128, 8, 1024], BF16, "wada") for _ in range(2)]
        ci = 0
        for l in range(DEPTH):
            for ch in range(6):
                wt, wb = wa[ci % 2]; ci += 1
                k.load("pool", wt[:], w_ada[l, :, ch * 1024:(ch + 1) * 1024].rearrange("(kt p) n -> p kt n", p=128), [wb])
                bk = k.bank()
                for ft in range(8):
                    for kt in range(8):
                        k.mm(bk, k.ps[bk][:, ft * 2:ft * 2 + 2], wt[:, kt, ft * 128:(ft + 1) * 128], scb[:, kt, :],
                             kt == 0, kt == 7, [wb, b_scb])
                k.tt("dve", MOD[:, l, ch, :, :], k.ps[bk][:, 0:16].rearrange("p (f j) -> p f j", j=2),
                     bada[:, l, ch * 8:(ch + 1) * 8].unsqueeze(2).to_broadcast([128, 8, 2]), ALU.add,
                     [k.pb[bk], b_bada], [b_MOD])
        for l in range(DEPTH):
            for m in range(2):
                sh, sc = (0, 1) if m == 0 else (3, 4)
                P.op("dve", lambda e: e.scalar_tensor_tensor(out=AB[:, l, m, 0, :, :], in0=MOD[:, l, sc, :, :], scalar=1.0,
                                                             in1=gm[:, l, m, :].unsqueeze(2).to_broadcast([128, 8, 2]),
                                                             op0=ALU.add, op1=ALU.mult),
                     reads=[b_MOD, b_gm], writes=[b_AB])
                k.copy("dve", AB[:, l, m, 1, :, :], MOD[:, l, sh, :, :], [b_MOD], [b_AB])
        P.barrier()
    dump("xT", xT[:], b_xT, [128, 8, NT])
    dump("MOD", MOD[:], b_MOD, [128, DEPTH, 6, 8, 2])

    def jof(g):
        return 1 if g == 0 else 0

    def rms_stats(st):
        sq = [k.sb(st, [128, 512], BF16, "sq") for _ in range(2)]
        lnt, b_lnt = k.sb(st, [128, 512], F32, "lnt")
        i = 0
        for (c0, n) in GROUPS:
            bk = k.bank()
            for kt in range(8):
                s, sbf = sq[i % 2]; i += 1
                k.act(s[:, :n], xT[:, kt, c0:c0 + n], AF.Square, [b_xT], [sbf])
                k.mm(bk, k.ps[bk][:, :n], onesb[:], s[:, :n], kt == 0, kt == 7, [sbf, b_ones])
            k.rstd_from_ss(rstd_b[:, c0:c0 + n], k.ps[bk][:, :n], bk, 1.0 / D, lnt[:, :n], b_lnt, [b_rstd])

    def h_group(l, m, g, hg, hb, tmp_, tmpb_):
        c0, n = GROUPS[g]
        j = jof(g)
        for kt in range(8):
            if isinstance(tmp_, list):
                tmp, tmpb = tmp_[kt % 2]
            else:
                tmp, tmpb = tmp_, tmpb_
            k.tt("dve", tmp[:, :n], xT[:, kt, c0:c0 + n], rstd_b[:, c0:c0 + n], ALU.mult, [b_xT, b_rstd], [tmpb])
            P.op("act", lambda e: e.activation(out=hg[:, kt, :n], in_=tmp[:, :n], func=AF.Identity,
                                               scale=AB[:, l, m, 0, kt, j:j + 1], bias=AB[:, l, m, 1, kt, j:j + 1]),
                 reads=[tmpb, b_AB], writes=[hb])

    def load_w(q, st, src, ncols, name):
        wt, wb = k.sb(st, [128, 8, ncols], BF16, name)
        k.load(q, wt[:], src.rearrange("(kt p) n -> p kt n", p=128), [wb])
        return wt, wb

    def proj(bk, wt, wb, col0, hg, hb, n, start=True, stop=True):
        for kt in range(8):
            k.mm(bk, k.ps[bk][:, :n], wt[:, kt, col0:col0 + 128], hg[:, kt, :n], start and kt == 0, stop and kt == 7, [wb, hb])

    def headnorm_rope(st, src_bk, n, c0, blockones, inv_dim, gcol, gbuf, Rm, cos_d, sin_d, dst, dstb, scr):
        sq, b_sq, rs, b_rs, lnt, b_lnt, qn, b_qn, ct, b_ct, sn, b_sn, t1, b_t1, t2, b_t2 = scr
        src = k.ps[src_bk][:, :n]
        k.act(sq[:, :n], src, AF.Square, [k.pb[src_bk]], [b_sq])
        b2 = k.bank()
        k.mm(b2, k.ps[b2][:, :n], blockones, sq[:, :n], True, True, [b_sq, b_cstb])
        k.rstd_from_ss(rs[:, :n], k.ps[b2][:, :n], b2, inv_dim, lnt[:, :n], b_lnt, [b_rs])
        k.stt(qn[:, :n], src, gcol, rs[:, :n], ALU.mult, ALU.mult, [k.pb[src_bk], gbuf, b_rs], [b_qn])
        k.load("sp", ct[:, :n], cos_d[:, c0:c0 + n], [b_ct])
        k.load("sp", sn[:, :n], sin_d[:, c0:c0 + n], [b_sn])
        b3 = k.bank()
        k.mm(b3, k.ps[b3][:, :n], Rm, qn[:, :n], True, True, [b_qn, b_cstb])
        k.tt("pool", t1[:, :n], qn[:, :n], ct[:, :n], ALU.mult, [b_qn, b_ct], [b_t1])
        k.tt("dve", t2[:, :n], k.ps[b3][:, :n], sn[:, :n], ALU.mult, [k.pb[b3], b_sn], [b_t2])
        k.tt("pool", dst, t1[:, :n], t2[:, :n], ALU.add, [b_t1, b_t2], [dstb])

    def norm_scratch(st):
        out = []
        for nm, dt in (("sq", BF16), ("rs", F32), ("lnt", F32), ("qn", BF16), ("ct", F32), ("sn", F32), ("t1", F32), ("t2", F32)):
            t, b = k.sb(st, [128, 512], dt, nm)
            out += [t, b]
        return out

    def attention(st, qT, b_q, kT, b_k, V, b_V, heads, scale, epilogue, skip_ctx=False):
        Et = [k.sb(st, [128, 512], BF16, "E") for _ in range(4)]
        sbanks = [0, 1, 2, 3]
        obanks = [4, 5, 6, 7]
        assert len(heads) % 2 == 0
        steps = []
        for g, (c0, n) in enumerate(GROUPS):
            if skip_ctx and g == 0:
                continue
            ktiles = [0, 1] if g == 0 else list(range(NTT))
            for hp in range(len(heads) // 2):
                for ki, kt in enumerate(ktiles):
                    steps.append((g, c0, n, hp, ki, kt, len(ktiles)))

        def qk_exp(si):
            g, c0, n, hp, ki, kt, nk = steps[si]
            outs = []
            for m in range(2):
                tile, pb, Kd, vh = heads[2 * hp + m]
                sbk = sbanks[(2 * si + m) % 4]
                k.mm(sbk, k.ps[sbk][:, :n], kT[pb:pb + Kd, tile, kt * 128:(kt + 1) * 128], qT[pb:pb + Kd, tile, c0:c0 + n],
                     True, True, [b_k, b_q])
                outs.append(sbk)
            res = []
            for m in range(2):
                sbk = outs[m]
                E, Eb = Et[(2 * si + m) % 4]
                k.act(E[:, :n], k.ps[sbk][:, :n], AF.Exp, [k.pb[sbk]], [Eb], scale=scale)
                res.append((E, Eb))
            return res

        oi = 0
        obs = None
        nxt = qk_exp(0)
        for si, (g, c0, n, hp, ki, kt, nk) in enumerate(steps):
            cur = nxt
            if si + 1 < len(steps):
                nxt = qk_exp(si + 1)
            nq = n // 128
            if ki == 0:
                obs = [obanks[(2 * oi) % 4], obanks[(2 * oi + 1) % 4]]; oi += 1
            for m in range(2):
                E, Eb = cur[m]
                vh = heads[2 * hp + m][3]
                ob = obs[m]
                for qi in range(nq):
                    P.op("pe", lambda e: e.matmul(k.ps[ob][:, qi * 65:(qi + 1) * 65], lhsT=E[:, qi * 128:(qi + 1) * 128],
                                                  rhs=V[:, kt, vh, :], start=(ki == 0 and qi == 0), stop=(ki == nk - 1),
                                                  skip_group_check=True),
                         reads=[Eb, b_V], writes=[k.pb[ob]], inc=(qi == nq - 1))
            if ki == nk - 1:
                for m in range(2):
                    epilogue(g, c0, nq, 2 * hp + m, obs[m])

    def transpose_out(ytok, b_ytok, nq, c0, ydst, b_ydst):
        for qi in range(nq):
            bk = k.bank()
            pv = k.ps[bk][:].bitcast(BF16)
            for tile in range(2):
                P.op("pe", lambda e: e.transpose(pv[:, tile * 128:(tile + 1) * 128], ytok[:, qi, tile * 128:(tile + 1) * 128], identb),
                     reads=[b_ytok, b_cstb], writes=[k.pb[bk]], inc=(tile == 1))
            k.copy("dve", ydst[:, :, c0 + qi * 128:c0 + (qi + 1) * 128], pv[:, 0:256].rearrange("p (j t) -> p j t", j=2),
                   [k.pb[bk]], [b_ydst])

    def da_mixer(l):
        lam_init = 0.8 - 0.6 * math.exp(-0.3 * l)
        ydst, b_ydst = yT[0]
        with ExitStack() as st:
            qT, b_q = k.sb(st, [128, 3, NT], BF16, "daq")
            kT, b_k = k.sb(st, [128, 3, NT], BF16, "dak")
            V, b_V = k.sb(st, [128, NTT, 4, 65], BF16, "dav")
            gcol, b_g = k.sb(st, [128, 2], F32, "dag")
            lam, b_lam = k.sb(st, [128, 128], F32, "dalam")
            lt, b_lt = k.sb(st, [128, 8], F32, "dalt")
            gsub, b_gsub = k.sb(st, [128, 64], F32, "dagsub")
            k.load("sp", gcol[:], da_g_d[l], [b_g])
            k.load("sp", lam[:], da_lam_d[l], [b_lam])
            k.load("sp", gsub[:], da_sub_d[l], [b_gsub])
            k.ts("dve", gsub[:], gsub[:], 1.0 - lam_init, None, ALU.mult, None, [b_gsub], [b_gsub])
            k.tt("dve", lam[:, 0:32], lam[:, 0:32], lam[:, 32:64], ALU.mult, [b_lam], [b_lam])
            k.tt("dve", lam[:, 64:96], lam[:, 64:96], lam[:, 96:128], ALU.mult, [b_lam], [b_lam])
            P.op("dve", lambda e: e.tensor_reduce(out=lt[:, 0:1], in_=lam[:, 0:32], axis=AX.X, op=ALU.add), reads=[b_lam], writes=[b_lt])
            P.op("dve", lambda e: e.tensor_reduce(out=lt[:, 1:2], in_=lam[:, 64:96], axis=AX.X, op=ALU.add), reads=[b_lam], writes=[b_lt])
            k.act(lt[:, 2:4], lt[:, 0:2], AF.Exp, [b_lt], [b_lt])
            k.tt("dve", lt[:, 4:5], lt[:, 3:4], lt[:, 2:3], ALU.subtract, [b_lt], [b_lt])
            k.ts("dve", lt[:, 4:5], lt[:, 4:5], -lam_init, None, ALU.add, None, [b_lt], [b_lt])
            k.memset("pool", V[:, :, :, 64:65], 1.0, [b_V])
            with ExitStack() as s2:
                wqk, b_wqk = load_w("pool", s2, w_da_qk[l], 768, "wqk")
                wv, b_wv = load_w("pool", s2, w_da_v[l], 256, "wv")
                hgs = [k.sb(s2, [128, 8, 512], BF16, "hg") for _ in range(2)]
                tmp, tmpb = k.sb(s2, [128, 512], F32, "htmp")
                scr = norm_scratch(s2)
                for g, (c0, n) in enumerate(GROUPS):
                    hg, hb = hgs[g % 2]
                    h_group(l, 0, g, hg, hb, tmp, tmpb)
                    for ti in range(6):
                        bk = k.bank()
                        proj(bk, wqk, b_wqk, ti * 128, hg, hb, n)
                        dst = (qT if ti < 3 else kT)
                        dstb = (b_q if ti < 3 else b_k)
                        headnorm_rope(s2, bk, n, c0, bo32, 1.0 / 32, gcol[:, (ti // 3):(ti // 3) + 1], b_g, Rda, da_cos_d, da_sin_d,
                                      dst[:, ti % 3, c0:c0 + n], dstb, scr)
                    for qi in range(n // 128):
                        tt_ = (c0 + qi * 128) // 128
                        bk = k.bank()
                        for kt in range(8):
                            k.mm(bk, k.ps[bk][:, 0:256], hg[:, kt, qi * 128:(qi + 1) * 128], wv[:, kt, :], kt == 0, kt == 7, [hb, b_wv])
                        k.copy("act", V[:, tt_, :, 0:64], k.ps[bk][:, 0:256].rearrange("p (h d) -> p h d", h=4), [k.pb[bk]], [b_V])
                P.barrier()
            dump("da_q", qT[:], b_q, [128, 3, NT], BF16)
            dump("da_k", kT[:], b_k, [128, 3, NT], BF16)
            dump("da_v", V[:], b_V, [128, NTT, 4, 65], BF16)
            with ExitStack() as s3:
                ytok, b_ytok = k.sb(s3, [128, 4, 256], BF16, "ytok")
                o0, b_o0 = k.sb(s3, [128, 4, 64], F32, "o0")
                dd, b_dd = k.sb(s3, [128, 4, 64], F32, "dd")
                junk, b_junk = k.sb(s3, [128, 64], F32, "junk")
                rc, b_rc = k.sb(s3, [128, 4, 4], F32, "rc")
                lnt, b_lnt = k.sb(s3, [128, 4], F32, "lnt2")
                state = {}

                def epi(g, c0, nq, hidx, ob):
                    h, m = hidx // 2, hidx % 2
                    if m == 0:
                        state["ob0"] = ob
                        return
                    ob0 = state["ob0"]
                    O0 = k.ps[ob0][:, 0:nq * 65].rearrange("p (q c) -> p q c", c=65)
                    O1 = k.ps[ob][:, 0:nq * 65].rearrange("p (q c) -> p q c", c=65)
                    P.op("dve", lambda e: e.reciprocal(out=rc[:, 0, 0:nq], in_=O0[:, :, 64]), reads=[k.pb[ob0]], writes=[b_rc])
                    P.op("dve", lambda e: e.reciprocal(out=rc[:, 1, 0:nq], in_=O1[:, :, 64]), reads=[k.pb[ob]], writes=[b_rc])
                    k.ts("dve", rc[:, 1, 0:nq], rc[:, 1, 0:nq], lt[:, 4:5], None, ALU.mult, None, [b_rc, b_lt], [b_rc])
                    for qi in range(nq):
                        k.ts("dve", o0[:, qi, :], O0[:, qi, 0:64], rc[:, 0, qi:qi + 1], None, ALU.mult, None, [k.pb[ob0], b_rc], [b_o0])
                        k.stt(dd[:, qi, :], O1[:, qi, 0:64], rc[:, 1, qi:qi + 1], o0[:, qi, :], ALU.mult, ALU.add,
                              [k.pb[ob], b_rc, b_o0], [b_dd])
                        P.op("act", lambda e: e.activation(out=junk[:], in_=dd[:, qi, :], func=AF.Square, accum_out=rc[:, 2, qi:qi + 1]),
                             reads=[b_dd], writes=[b_junk, b_rc])
                    P.op("act", lambda e: e.activation(out=lnt[:, 0:nq], in_=rc[:, 2, 0:nq], func=AF.Ln, scale=1.0 / 64, bias=k.eps_col[:]),
                         reads=[b_rc, k.b_const], writes=[b_lnt])
                    k.act(rc[:, 3, 0:nq], lnt[:, 0:nq], AF.Exp, [b_lnt], [b_rc], scale=-0.5)
                    for qi in range(nq):
                        k.stt(ytok[:, qi, h * 64:(h + 1) * 64], dd[:, qi, :], rc[:, 3, qi:qi + 1], gsub[:], ALU.mult, ALU.mult,
                              [b_dd, b_rc, b_gsub], [b_ytok])
                    if h == 3:
                        k.rrset = [0, 1, 2, 3]
                        transpose_out(ytok, b_ytok, nq, c0, ydst, b_ydst)
                        k.rrset = list(range(8))

                heads = [(j // 3, 32 * (j % 3), 32, j // 2) for j in range(8)]
                attention(s3, qT, b_q, kT, b_k, V, b_V, heads, 32 ** -0.5, epi, skip_ctx=(l == DEPTH - 1))
                P.barrier()
        dump("ya", ydst[:], b_ydst, [128, 2, NT], BF16)

    def mla_mixer(l):
        ydst, b_ydst = yT[2]
        with ExitStack() as st:
            qT, b_q = k.sb(st, [128, 2, NT], BF16, "mlq")
            kT, b_k = k.sb(st, [128, 2, NT], BF16, "mlk")
            V, b_V = k.sb(st, [128, NTT, 4, 65], BF16, "mlv")
            gcol, b_g = k.sb(st, [128, 2], F32, "mlg")
            gc, b_gc = k.sb(st, [128, 3], F32, "mlgc")
            k.load("sp", gcol[:], mla_g_d[l], [b_g])
            k.load("sp", gc[:], mla_gc_d[l], [b_gc])
            k.memset("pool", V[:, :, :, 64:65], 1.0, [b_V])
            with ExitStack() as s2:
                wc, b_wc = load_w("pool", s2, w_mla_c[l], 384, "wc")
                wkr, b_wkr = load_w("pool", s2, w_mla_kr[l], 128, "wkr")
                wf, b_wf = k.sb(s2, [128, 4, 256], F32, "wf")
                wuq, b_wuq = k.sb(s2, [128, 2, 256], BF16, "wuq")
                wkk, b_wkk = k.sb(s2, [128, 256], BF16, "wkk")
                wvv, b_wvv = k.sb(s2, [128, 256], BF16, "wvv")
                k.load("sp", wf[:, 0:2, :], w_uq_d[l], [b_wf])
                k.load("sp", wf[:, 2, :], w_ukvk_d[l], [b_wf])
                k.load("sp", wf[:, 3, :], w_ukvv_d[l], [b_wf])
                for kt in range(2):
                    k.ts("dve", wuq[:, kt, :], wf[:, kt, :], gc[:, kt:kt + 1], None, ALU.mult, None, [b_wf, b_gc], [b_wuq])
                k.ts("dve", wkk[:], wf[:, 2, :], gc[:, 2:3], None, ALU.mult, None, [b_wf, b_gc], [b_wkk])
                k.ts("dve", wvv[:], wf[:, 3, :], gc[:, 2:3], None, ALU.mult, None, [b_wf, b_gc], [b_wvv])
                hgs = [k.sb(s2, [128, 8, 512], BF16, "hg") for _ in range(2)]
                tmp, tmpb = [k.sb(s2, [128, 512], F32, "htmp") for _ in range(2)], None
                scr = norm_scratch(s2)
                sq2, b_sq2 = k.sb(s2, [128, 2, 512], BF16, "sq2")
                rsq, b_rsq = k.sb(s2, [128, 512], F32, "rsq")
                lnq, b_lnq = k.sb(s2, [128, 512], F32, "lnq")
                cqn, b_cqn = k.sb(s2, [128, 2, 512], BF16, "cqn")
                ckvn, b_ckvn = k.sb(s2, [128, 512], BF16, "ckvn")
                for g, (c0, n) in enumerate(GROUPS):
                    hg, hb = hgs[g % 2]
                    h_group(l, 0, g, hg, hb, tmp, tmpb)
                    bA = k.bank(); proj(bA, wc, b_wc, 0, hg, hb, n)
                    bB = k.bank(); proj(bB, wc, b_wc, 128, hg, hb, n)
                    k.act(sq2[:, 0, :n], k.ps[bA][:, :n], AF.Square, [k.pb[bA]], [b_sq2])
                    k.act(sq2[:, 1, :n], k.ps[bB][:, :n], AF.Square, [k.pb[bB]], [b_sq2])
                    bS = k.bank()
                    k.mm(bS, k.ps[bS][:, :n], onesb[:], sq2[:, 0, :n], True, False, [b_sq2, b_ones])
                    k.mm(bS, k.ps[bS][:, :n], onesb[:], sq2[:, 1, :n], False, True, [b_sq2, b_ones])
                    k.rstd_from_ss(rsq[:, :n], k.ps[bS][:, :n], bS, 1.0 / 192, lnq[:, :n], b_lnq, [b_rsq])
                    k.tt("dve", cqn[:, 0, :n], k.ps[bA][:, :n], rsq[:, :n], ALU.mult, [k.pb[bA], b_rsq], [b_cqn])
                    k.tt("dve", cqn[:, 1, :n], k.ps[bB][:, :n], rsq[:, :n], ALU.mult, [k.pb[bB], b_rsq], [b_cqn])
                    bC = k.bank(); proj(bC, wc, b_wc, 256, hg, hb, n)
                    k.act(sq2[:, 0, :n], k.ps[bC][:, :n], AF.Square, [k.pb[bC]], [b_sq2])
                    bS = k.bank()
                    k.mm(bS, k.ps[bS][:, :n], onesb[:], sq2[:, 0, :n], True, True, [b_sq2, b_ones])
                    k.rstd_from_ss(rsq[:, :n], k.ps[bS][:, :n], bS, 1.0 / 128, lnq[:, :n], b_lnq, [b_rsq])
                    k.tt("dve", ckvn[:, :n], k.ps[bC][:, :n], rsq[:, :n], ALU.mult, [k.pb[bC], b_rsq], [b_ckvn])
                    for tile in range(2):
                        bk = k.bank()
                        k.mm(bk, k.ps[bk][:, :n], wuq[:, 0, tile * 128:(tile + 1) * 128], cqn[:, 0, :n], True, False, [b_wuq, b_cqn])
                        k.mm(bk, k.ps[bk][:, :n], wuq[0:64, 1, tile * 128:(tile + 1) * 128], cqn[0:64, 1, :n], False, True, [b_wuq, b_cqn])
                        headnorm_rope(s2, bk, n, c0, bo64, 1.0 / 48, gcol[:, 0:1], b_g, Rmla, ml_cos_d, ml_sin_d,
                                      qT[:, tile, c0:c0 + n], b_q, scr)
                    for tile in range(2):
                        bk = k.bank()
                        k.mm(bk, k.ps[bk][:, :n], wkk[:, tile * 128:(tile + 1) * 128], ckvn[:, :n], True, False, [b_wkk, b_ckvn])
                        for kt in range(8):
                            k.mm(bk, k.ps[bk][:, :n], wkr[:, kt, :], hg[:, kt, :n], False, kt == 7, [b_wkr, hb])
                        headnorm_rope(s2, bk, n, c0, bo64, 1.0 / 48, gcol[:, 1:2], b_g, Rmla, ml_cos_d, ml_sin_d,
                                      kT[:, tile, c0:c0 + n], b_k, scr)
                    for qi in range(n // 128):
                        tt_ = (c0 + qi * 128) // 128
                        bk = k.bank()
                        k.mm(bk, k.ps[bk][:, 0:256], ckvn[:, qi * 128:(qi + 1) * 128], wvv[:], True, True, [b_ckvn, b_wvv])
                        k.copy("act", V[:, tt_, :, 0:64], k.ps[bk][:, 0:256].rearrange("p (h d) -> p h d", h=4), [k.pb[bk]], [b_V])
                P.barrier()
            dump("ml_q", qT[:], b_q, [128, 2, NT], BF16)
            dump("ml_k", kT[:], b_k, [128, 2, NT], BF16)
            with ExitStack() as s3:
                ytok, b_ytok = k.sb(s3, [128, 4, 256], BF16, "ytok")
                rc, b_rc = k.sb(s3, [128, 4], F32, "rc")

                def epi(g, c0, nq, h, ob):
                    O = k.ps[ob][:, 0:nq * 65].rearrange("p (q c) -> p q c", c=65)
                    P.op("dve", lambda e: e.reciprocal(out=rc[:, 0:nq], in_=O[:, :, 64]), reads=[k.pb[ob]], writes=[b_rc])
                    for qi in range(nq):
                        k.ts("dve", ytok[:, qi, h * 64:(h + 1) * 64], O[:, qi, 0:64], rc[:, qi:qi + 1], None, ALU.mult, None,
                             [k.pb[ob], b_rc], [b_ytok])
                    if h == 3:
                        k.rrset = [0, 1, 2, 3]
                        transpose_out(ytok, b_ytok, nq, c0, ydst, b_ydst)
                        k.rrset = list(range(8))

                heads = [(h // 2, 64 * (h % 2), 48, h) for h in range(4)]
                attention(s3, qT, b_q, kT, b_k, V, b_V, heads, 48 ** -0.5, epi, skip_ctx=(l == DEPTH - 1))
                P.barrier()
        dump("yc", ydst[:], b_ydst, [128, 2, NT], BF16)

    def merge(l):
        with ExitStack() as st:
            wbr, b_wbr = k.sb(st, [128, 2, 4, D], BF16, "wbr")
            wout, b_wout = k.sb(st, [128, 8, D], BF16, "wout")
            wgs = [k.sb(st, [128, 8, 4, 128], BF16, "wg") for _ in range(2)]
            hgs = [k.sb(st, [128, 8, 512], BF16, "hg") for _ in range(2)]
            tmp, tmpb = k.sb(st, [128, 512], F32, "htmp")
            sig = [k.sb(st, [128, 512], F32, "sig") for _ in range(2)]
            prod = [k.sb(st, [128, 512], F32, "prod") for _ in range(2)]
            macc, b_macc = k.sb(st, [128, 512], F32, "macc")
            mbf, b_mbf = k.sb(st, [128, 8, 512], BF16, "mbf")
            wi = 0; si = 0
            for g, (c0, n) in enumerate(GROUPS):
                if l == DEPTH - 1 and g == 0:
                    continue
                j = jof(g)
                hg, hb = hgs[g % 2]
                h_group(l, 0, g, hg, hb, tmp, tmpb)
                for ct in range(8):
                    wg, b_wg = wgs[wi % 2]; wi += 1
                    k.load("pool", wg[:].rearrange("p a b c -> p (a b c)"), w_gates_d[l, ct], [b_wg])
                    if wi == 1:
                        k.load("pool", wbr[:].rearrange("p a b c -> p (a b c)"), w_branch_d[l], [b_wbr])
                    if wi == 3:
                        k.load("pool", wout[:], w_out_d[l].rearrange("(kt p) n -> p kt n", p=128), [b_wout])
                    for i in range(4):
                        bg = k.bank()
                        for kt in range(8):
                            k.mm(bg, k.ps[bg][:, :n], wg[:, kt, i, :], hg[:, kt, :n], kt == 0, kt == 7, [b_wg, hb])
                        sg, b_sg = sig[si % 2]; pr, b_pr = prod[si % 2]; si += 1
                        k.act(sg[:, :n], k.ps[bg][:, :n], AF.Sigmoid, [k.pb[bg]], [b_sg])
                        bp = k.bank()
                        for kt2 in range(2):
                            k.mm(bp, k.ps[bp][:, :n], wbr[:, kt2, i, ct * 128:(ct + 1) * 128], yT[i][0][:, kt2, c0:c0 + n],
                                 kt2 == 0, kt2 == 1, [b_wbr, yT[i][1]])
                        if i == 0:
                            k.tt("dve", macc[:, :n], k.ps[bp][:, :n], sg[:, :n], ALU.mult, [k.pb[bp], b_sg], [b_macc])
                        else:
                            k.tt("dve", pr[:, :n], k.ps[bp][:, :n], sg[:, :n], ALU.mult, [k.pb[bp], b_sg], [b_pr])
                            if i < 3:
                                k.tt("dve", macc[:, :n], macc[:, :n], pr[:, :n], ALU.add, [b_macc, b_pr], [b_macc])
                            else:
                                k.tt("dve", mbf[:, ct, :n], macc[:, :n], pr[:, :n], ALU.add, [b_macc, b_pr], [b_mbf])
                for co in range(8):
                    bo = k.bank()
                    for ct in range(8):
                        k.mm(bo, k.ps[bo][:, :n], wout[:, ct, co * 128:(co + 1) * 128], mbf[:, ct, :n], ct == 0, ct == 7, [b_wout, b_mbf])
                    k.stt(xT[:, co, c0:c0 + n], k.ps[bo][:, :n], MOD[:, l, 2, co, j:j + 1], xT[:, co, c0:c0 + n], ALU.mult, ALU.add,
                          [k.pb[bo], b_MOD, b_xT], [b_xT])
            P.barrier()
        dump("xmix", xT[:], b_xT, [128, 8, NT])

    def moe(l):
        fT = yall[:].rearrange("p i k t -> p (i k) t")
        b_fT = Buf("fT")
        with ExitStack() as st:
            rms_stats(st)
            P.barrier()
        with ExitStack() as st:
            combT, b_comb = k.sb(st, [16, NT], BF16, "combT")
            selb, b_sel = k.sb(st, [16, 16, 128], BF16, "selb")
            k.load("pool", selb[:], sel_d, [b_sel])
            wgs = [k.sb(st, [128, 8, 512], BF16, "ewg") for _ in range(2)]
            wus = [k.sb(st, [128, 8, 512], BF16, "ewu") for _ in range(2)]
            wds = [k.sb(st, [128, 4, D], BF16, "ewd") for _ in range(2)]

            def load_expert(e_):
                wg, b_wg = wgs[e_ % 2]; wu, b_wu = wus[e_ % 2]; wd, b_wd = wds[e_ % 2]
                k.load("pool", wg[:], exp_g_d[l, e_].rearrange("(kt p) n -> p kt n", p=128), [b_wg])
                k.load("pool", wu[:], exp_u_d[l, e_].rearrange("(kt p) n -> p kt n", p=128), [b_wu])
                k.load("pool", wd[:], exp_d_d[l, e_].rearrange("(kt p) n -> p kt n", p=128), [b_wd])
            load_expert(0)
            with ExitStack() as s2:
                tmps = [k.sb(s2, [128, 512], F32, "htmp") for _ in range(2)]
                f32s = [k.sb(s2, [128, 512], F32, "f32") for _ in range(2)]
                rw, b_rw = k.sb(s2, [128, 8, 16], F32, "rw")
                k.load("sp", rw[:], router_w_d.rearrange("(kt p) n -> p kt n", p=128), [b_rw])
                rb, b_rb = k.sb(s2, [128, 16], F32, "rb")
                k.load("sp", rb[:], router_b_d, [b_rb])
                lg, b_lg = k.sb(s2, [128, NTT, 16], F32, "lg")
                fi_ = 0
                skip0 = (l == DEPTH - 1)
                if skip0:
                    k.memset("dve", lg[:, 0:2, :], 0.0, [b_lg])
                for g in range(5):
                    if skip0 and g == 0:
                        continue
                    c0, n = GROUPS[g]
                    j = jof(g)
                    nq = n // 128
                    bl = k.bank()
                    for kt in range(8):
                        tmp, tmpb = tmps[kt % 2]
                        k.tt("dve", tmp[:, :n], xT[:, kt, c0:c0 + n], rstd_b[:, c0:c0 + n], ALU.mult, [b_xT, b_rstd], [tmpb])
                        P.op("act", lambda e: e.activation(out=fT[:, kt, c0:c0 + n], in_=tmp[:, :n], func=AF.Identity,
                                                           scale=AB[:, l, 1, 0, kt, j:j + 1], bias=AB[:, l, 1, 1, kt, j:j + 1]),
                             reads=[tmpb, b_AB], writes=[b_fT])
                        f32, b_f32 = f32s[fi_ % 2]; fi_ += 1
                        k.ts("pool", f32[:, :n], tmp[:, :n], AB[:, l, 1, 0, kt, j:j + 1], AB[:, l, 1, 1, kt, j:j + 1], ALU.mult, ALU.add,
                             [tmpb, b_AB], [b_f32])
                        for qi in range(nq):
                            P.op("pe", lambda e: e.matmul(k.ps[bl][:, qi * 16:(qi + 1) * 16], lhsT=f32[:, qi * 128:(qi + 1) * 128], rhs=rw[:, kt, :],
                                                          start=(kt == 0 and qi == 0), stop=(kt == 7), skip_group_check=True),
                                 reads=[b_f32, b_rw], writes=[k.pb[bl]], inc=(qi == nq - 1))
                    tt0 = c0 // 128
                    k.copy("dve", lg[:, tt0:tt0 + nq, :], k.ps[bl][:, 0:nq * 16].rearrange("p (q e) -> p q e", e=16), [k.pb[bl]], [b_lg])
                r = {}
                NR = NTT * 16
                for nm, wd_ in (("sc", NR), ("bi", NR), ("m1", NR // 4), ("eq", NR), ("bi2", NR), ("m2", NR // 4),
                                ("gs", NR // 4), ("gm", NTT), ("gsel", NR // 4), ("sel", NR), ("w", NR), ("ws", NTT), ("cmb", NR)):
                    r[nm] = k.sb(s2, [128, wd_], F32, "r_" + nm)
                v4 = lambda ap: ap.rearrange("p (g e) -> p g e", e=4)
                b4 = lambda ap: ap.unsqueeze(2).to_broadcast([128, NR // 4, 4])
                sc, b_sc = r["sc"]; bi, b_bi = r["bi"]; m1, b_m1 = r["m1"]; eq, b_eq = r["eq"]; bi2, b_bi2 = r["bi2"]
                m2, b_m2 = r["m2"]; gs, b_gs = r["gs"]; gm_, b_gm_ = r["gm"]; gsel, b_gsel = r["gsel"]; sel, b_sl = r["sel"]
                w_, b_w = r["w"]; ws, b_ws = r["ws"]; cmb, b_cmb = r["cmb"]
                k.act(sc[:], lg[:].rearrange("p t e -> p (t e)"), AF.Sigmoid, [b_lg], [b_sc])
                k.tt("dve", sc[:].rearrange("p (t e) -> p t e", e=16) if False else bi[:].rearrange("p (t e) -> p t e", e=16),
                     sc[:].rearrange("p (t e) -> p t e", e=16), rb[:].unsqueeze(1).to_broadcast([128, NTT, 16]), ALU.add, [b_sc, b_rb], [b_bi])
                P.op("dve", lambda e: e.tensor_reduce(out=m1[:], in_=v4(bi[:]), axis=AX.X, op=ALU.max), reads=[b_bi], writes=[b_m1])
                k.tt("dve", v4(eq[:]), v4(bi[:]), b4(m1[:]), ALU.is_equal, [b_bi, b_m1], [b_eq])
                k.stt(bi2[:], eq[:], -1e9, bi[:], ALU.mult, ALU.add, [b_eq, b_bi], [b_bi2])
                P.op("dve", lambda e: e.tensor_reduce(out=m2[:], in_=v4(bi2[:]), axis=AX.X, op=ALU.max), reads=[b_bi2], writes=[b_m2])
                k.tt("dve", gs[:], m1[:], m2[:], ALU.add, [b_m1, b_m2], [b_gs])
                P.op("dve", lambda e: e.tensor_reduce(out=gm_[:], in_=v4(gs[:]), axis=AX.X, op=ALU.max), reads=[b_gs], writes=[b_gm_])
                k.tt("dve", v4(gsel[:]), v4(gs[:]), gm_[:].unsqueeze(2).to_broadcast([128, NTT, 4]), ALU.is_equal, [b_gs, b_gm_], [b_gsel])
                k.tt("dve", v4(sel[:]), v4(bi[:]), b4(m2[:]), ALU.is_ge, [b_bi, b_m2], [b_sl])
                k.tt("dve", v4(sel[:]), v4(sel[:]), b4(gsel[:]), ALU.mult, [b_sl, b_gsel], [b_sl])
                k.tt("dve", w_[:], sc[:], sel[:], ALU.mult, [b_sc, b_sl], [b_w])
                P.op("dve", lambda e: e.tensor_reduce(out=ws[:], in_=w_[:].rearrange("p (t e) -> p t e", e=16), axis=AX.X, op=ALU.add),
                     reads=[b_w], writes=[b_ws])
                P.op("dve", lambda e: e.reciprocal(out=ws[:], in_=ws[:]), reads=[b_ws], writes=[b_ws])
                k.tt("dve", cmb[:].rearrange("p (t e) -> p t e", e=16), w_[:].rearrange("p (t e) -> p t e", e=16),
                     ws[:].unsqueeze(2).to_broadcast([128, NTT, 16]), ALU.mult, [b_w, b_ws], [b_cmb])
                for tq in range(0, NTT, 4):
                    nn = min(4, NTT - tq)
                    bt = k.bank()
                    for i_ in range(nn):
                        P.op("pe", lambda e: e.transpose(k.ps[bt][0:16, i_ * 128:(i_ + 1) * 128], cmb[:, (tq + i_) * 16:(tq + i_ + 1) * 16], identf),
                             reads=[b_cmb, b_cstf], writes=[k.pb[bt]], inc=(i_ == nn - 1))
                    k.copy("act", combT[:, tq * 128:(tq + nn) * 128], k.ps[bt][0:16, 0:nn * 128], [k.pb[bt]], [b_comb])
                P.barrier()
            dump("combT", combT[:], b_comb, [16, NT], BF16)
            cbs = [k.sb(st, [128, 512], BF16, "cbs") for _ in range(2)]
            sl = [k.sb(st, [128, 512], F32, "esl") for _ in range(2)]
            tl = [k.sb(st, [128, 512], F32, "etl") for _ in range(2)]
            aa = [k.sb(st, [128, 4, 512], BF16, "eaa") for _ in range(2)]
            ci = 0; fi = 0
            for e_ in range(16):
                wg, b_wg = wgs[e_ % 2]; wu, b_wu = wus[e_ % 2]; wd, b_wd = wds[e_ % 2]
                if e_ > 0:
                    load_expert(e_)
                for g, (c0, n) in enumerate(GROUPS):
                    if l == DEPTH - 1 and g == 0:
                        continue
                    j = jof(g)
                    cb, b_cb = cbs[ci % 2]; a_, b_a = aa[ci % 2]; ci += 1
                    bc = k.bank()
                    k.mm(bc, k.ps[bc][:, :n], selb[:, e_, :], combT[:, c0:c0 + n], True, True, [b_sel, b_comb])
                    k.copy("act", cb[:, :n], k.ps[bc][:, :n], [k.pb[bc]], [b_cb])
                    for fj in range(4):
                        bg = k.bank()
                        for kt in range(8):
                            k.mm(bg, k.ps[bg][:, :n], wg[:, kt, fj * 128:(fj + 1) * 128], fT[:, kt, c0:c0 + n], kt == 0, kt == 7, [b_wg, b_fT])
                        bu = k.bank()
                        for kt in range(8):
                            k.mm(bu, k.ps[bu][:, :n], wu[:, kt, fj * 128:(fj + 1) * 128], fT[:, kt, c0:c0 + n], kt == 0, kt == 7, [b_wu, b_fT])
                        s_, b_s = sl[fi % 2]; t_, b_t = tl[fi % 2]; fi += 1
                        k.act(s_[:, :n], k.ps[bg][:, :n], AF.Silu, [k.pb[bg]], [b_s])
                        k.tt("dve", t_[:, :n], k.ps[bu][:, :n], s_[:, :n], ALU.mult, [k.pb[bu], b_s], [b_t])
                        k.tt("dve", a_[:, fj, :n], t_[:, :n], cb[:, :n], ALU.mult, [b_t, b_cb], [b_a])
                    for co in range(8):
                        bo = k.bank()
                        for fj in range(4):
                            k.mm(bo, k.ps[bo][:, :n], wd[:, fj, co * 128:(co + 1) * 128], a_[:, fj, :n], fj == 0, fj == 3, [b_wd, b_a])
                        k.stt(xT[:, co, c0:c0 + n], k.ps[bo][:, :n], MOD[:, l, 5, co, j:j + 1], xT[:, co, c0:c0 + n], ALU.mult, ALU.add,
                              [k.pb[bo], b_MOD, b_xT], [b_xT])
            P.barrier()
        dump("xout", xT[:], b_xT, [128, 8, NT])

    TWO_PI = 2.0 * math.pi

    def sincos(ang, b_ang, N, sc4, sin_out, cos_out, b_out):
        (kf, b_kf), (r_, b_r), (mk, b_mk), (sh, b_sh) = sc4
        ki = kf[:, :N].bitcast(I32)
        k.ts("dve", r_[:, :N], ang, 1.0 / TWO_PI, None, ALU.mult, None, [b_ang], [b_r])
        k.copy("dve", ki, r_[:, :N], [b_r], [b_kf])
        k.copy("dve", mk[:, :N], ki, [b_kf], [b_mk])
        k.stt(r_[:, :N], mk[:, :N], -TWO_PI, ang, ALU.mult, ALU.add, [b_mk, b_ang], [b_r])
        k.ts("dve", mk[:, :N], r_[:, :N], math.pi, -TWO_PI, ALU.is_gt, ALU.mult, [b_r], [b_mk])
        k.tt("dve", r_[:, :N], r_[:, :N], mk[:, :N], ALU.add, [b_r, b_mk], [b_r])
        k.ts("dve", mk[:, :N], r_[:, :N], -math.pi, TWO_PI, ALU.is_lt, ALU.mult, [b_r], [b_mk])
        k.tt("dve", r_[:, :N], r_[:, :N], mk[:, :N], ALU.add, [b_r, b_mk], [b_r])
        k.ts("dve", r_[:, :N], r_[:, :N], math.pi, -math.pi, ALU.min, ALU.max, [b_r], [b_r])
        k.act(sin_out, r_[:, :N], AF.Sin, [b_r], [b_out])
        k.act(sh[:, :N], r_[:, :N], AF.Sin, [b_r], [b_sh], scale=0.5)
        k.tt("dve", sh[:, :N], sh[:, :N], sh[:, :N], ALU.mult, [b_sh], [b_sh])
        k.ts("dve", cos_out, sh[:, :N], -2.0, 1.0, ALU.mult, ALU.add, [b_sh], [b_out])

    def s5_mixer(l):
        ydst, b_ydst = yT[1]
        with ExitStack() as st:
            uT, b_u = k.sb(st, [128, 2, NT], BF16, "s5u")
            yacc, b_yacc = k.sb(st, [128, 2, NT], F32, "s5y")
            dcol, b_dcol = k.sb(st, [128, 2], F32, "s5d")
            k.load("sp", dcol[:], s5d_d[l], [b_dcol])
            with ExitStack() as s2:
                ws, b_ws = load_w("pool", s2, w_s5_d[l], 256, "ws5")
                hgs = [k.sb(s2, [128, 8, 512], BF16, "hg") for _ in range(2)]
                tmp, tmpb = [k.sb(s2, [128, 512], F32, "htmp") for _ in range(2)], None
                for g, (c0, n) in enumerate(GROUPS):
                    hg, hb = hgs[g % 2]
                    h_group(l, 0, g, hg, hb, tmp, tmpb)
                    for ti in range(2):
                        bk = k.bank()
                        proj(bk, ws, b_ws, ti * 128, hg, hb, n)
                        k.copy("act", uT[:, ti, c0:c0 + n], k.ps[bk][:, :n], [k.pb[bk]], [b_u])
                P.barrier()
            for dr in range(2):
                with ExitStack() as sd:
                    pr, b_pr = k.sb(sd, [128, 1024], F32, "pr"); pi_, b_pi = k.sb(sd, [128, 1024], F32, "pi")
                    qr, b_qr = k.sb(sd, [128, 1024], F32, "qr"); qi, b_qi = k.sb(sd, [128, 1024], F32, "qi")
                    Bb, b_Bb = k.sb(sd, [128, 2, 1024], BF16, "Bb")
                    Cb, b_Cb = k.sb(sd, [128, 3, 8, 128], BF16, "Cb")
                    triT, b_tri = k.sb(sd, [128, 2, 128], BF16, "triT")
                    trif, b_trif = k.sb(sd, [128, 128], F32, "trif")
                    posc, b_posc = k.sb(sd, [128, 2], F32, "posc")
                    posr, b_posr = k.sb(sd, [128, 128], F32, "posr")
                    fm, b_fm = k.sb(sd, [128, 3, 8], F32, "fm")
                    cc, b_cc = k.sb(sd, [128, 16, 8], F32, "cc")
                    A128, b_A128 = k.sb(sd, [128, 2, 8], F32, "A128")
                    k.load("pool", Bb[:], s5B_d[l, dr].rearrange("kt p n -> p kt n"), [b_Bb])
                    with ExitStack() as sc_:
                        Cf, b_Cf = k.sb(sc_, [128, 2, 8, 128], F32, "Cf")
                        k.load("sp", Cf[:, 0], s5C_d[l, dr, 0], [b_Cf]); k.load("sp", Cf[:, 1], s5C_d[l, dr, 1], [b_Cf])
                        k.copy("dve", Cb[:, 0], Cf[:, 0], [b_Cf], [b_Cb])
                        k.ts("dve", Cb[:, 1], Cf[:, 0], -1.0, None, ALU.mult, None, [b_Cf], [b_Cb])
                        k.ts("dve", Cb[:, 2], Cf[:, 1], -1.0, None, ALU.mult, None, [b_Cf], [b_Cb])
                        P.barrier()
                    k.load("sp", trif[:], tri_d[dr], [b_trif])
                    k.copy("dve", triT[:, 0, :], trif[:], [b_trif], [b_tri])
                    k.ts("dve", triT[:, 1, :], trif[:], -1.0, None, ALU.mult, None, [b_trif], [b_tri])
                    k.load("sp", posc[:], posc_d[dr], [b_posc]); k.load("sp", posr[:], posr_d[dr], [b_posr])
                    k.load("sp", fm[:], s5fm_d[l, dr], [b_fm])
                    with ExitStack() as sx:
                        sc4 = [k.sb(sx, [128, 1024], F32, "sc4") for _ in range(4)]
                        rho, b_rho = k.sb(sx, [128, 1024], F32, "rho"); th, b_th = k.sb(sx, [128, 1024], F32, "th")
                        ang, b_angb = k.sb(sx, [128, 1024], F32, "ang")
                        k.load("sp", rho[:], s5tm_d[l, dr, 0], [b_rho]); k.load("sp", th[:], s5tm_d[l, dr, 1], [b_th])
                        k.load("sp", ang[:], s5tm_d[l, dr, 2], [b_angb])
                        k.act(ang[:], ang[:], AF.Exp, [b_angb], [b_angb])
                        k.tt("dve", rho[:], rho[:], ang[:], ALU.mult, [b_rho, b_angb], [b_rho])
                        k.tt("dve", th[:], th[:], ang[:], ALU.mult, [b_th, b_angb], [b_th])
                        k.ts("dve", ang[:], th[:], posc[:, 0:1], None, ALU.mult, None, [b_th, b_posc], [b_angb])
                        sincos(ang[:], b_angb, 1024, sc4, pi_[:], pr[:], b_pr)
                        b_pi.w = b_pr.w
                        P.op("act", lambda e: e.activation(out=ang[:], in_=rho[:], func=AF.Exp, scale=posc[:, 0:1]), reads=[b_rho, b_posc], writes=[b_angb])
                        k.tt("dve", pr[:], pr[:], ang[:], ALU.mult, [b_pr, b_angb], [b_pr])
                        k.tt("dve", pi_[:], pi_[:], ang[:], ALU.mult, [b_pr, b_angb], [b_pr])
                        dtc = cc[:, 0, :]; rc_ = cc[:, 1, :]; tc_ = cc[:, 2, :]
                        k.act(dtc, fm[:, 2, :], AF.Exp, [b_fm], [b_cc])
                        k.tt("dve", rc_, fm[:, 0, :], dtc, ALU.mult, [b_fm, b_cc], [b_cc])
                        k.tt("dve", tc_, fm[:, 1, :], dtc, ALU.mult, [b_fm, b_cc], [b_cc])
                        k.copy("dve", ang[:, 0:8], tc_, [b_cc], [b_angb])
                        k.ts("dve", ang[:, 8:16], tc_, 128.0, None, ALU.mult, None, [b_cc], [b_angb])
                        sincos(ang[:, 0:16], b_angb, 16, sc4, th[:, 0:16], th[:, 16:32], b_th)
                        er = cc[:, 3, :]; e128 = cc[:, 4, :]
                        k.act(er, rc_, AF.Exp, [b_cc], [b_cc])
                        k.act(e128, rc_, AF.Exp, [b_cc], [b_cc], scale=128.0)
                        k.tt("dve", A128[:, 0, :], e128, th[:, 24:32], ALU.mult, [b_cc, b_th], [b_A128])
                        k.tt("dve", A128[:, 1, :], e128, th[:, 8:16], ALU.mult, [b_cc, b_th], [b_A128])
                        ar1 = cc[:, 5, :]; ai = cc[:, 6, :]; den = cc[:, 7, :]; cr = cc[:, 8, :]; ci = cc[:, 9, :]; t1 = cc[:, 10, :]
                        k.tt("dve", ar1, er, th[:, 16:24], ALU.mult, [b_cc, b_th], [b_cc])
                        k.ts("dve", ar1, ar1, -1.0, None, ALU.add, None, [b_cc], [b_cc])
                        k.tt("dve", ai, er, th[:, 0:8], ALU.mult, [b_cc, b_th], [b_cc])
                        k.tt("dve", den, fm[:, 0, :], fm[:, 0, :], ALU.mult, [b_fm], [b_cc])
                        k.tt("dve", t1, fm[:, 1, :], fm[:, 1, :], ALU.mult, [b_fm], [b_cc])
                        k.tt("dve", den, den, t1, ALU.add, [b_cc], [b_cc])
                        P.op("dve", lambda e: e.reciprocal(out=den, in_=den), reads=[b_cc], writes=[b_cc])
                        k.tt("dve", cr, ar1, fm[:, 0, :], ALU.mult, [b_cc, b_fm], [b_cc])
                        k.tt("dve", t1, ai, fm[:, 1, :], ALU.mult, [b_cc, b_fm], [b_cc])
                        k.tt("dve", cr, cr, t1, ALU.add, [b_cc], [b_cc])
                        k.tt("dve", cr, cr, den, ALU.mult, [b_cc], [b_cc])
                        k.tt("dve", ci, ai, fm[:, 0, :], ALU.mult, [b_cc, b_fm], [b_cc])
                        k.tt("dve", t1, ar1, fm[:, 1, :], ALU.mult, [b_cc, b_fm], [b_cc])
                        k.tt("dve", ci, ci, t1, ALU.subtract, [b_cc], [b_cc])
                        k.tt("dve", ci, ci, den, ALU.mult, [b_cc], [b_cc])
                        for i in range(8):
                            k.ts("dve", ang[:, i * 128:(i + 1) * 128], posr[:], cc[:, 2, i:i + 1], None, ALU.mult, None, [b_posr, b_cc], [b_angb])
                            P.op("act", lambda e: e.activation(out=rho[:, i * 128:(i + 1) * 128], in_=posr[:], func=AF.Exp, scale=cc[:, 1, i:i + 1]),
                                 reads=[b_posr, b_cc], writes=[b_rho])
                        sincos(ang[:], b_angb, 1024, sc4, qi[:], qr[:], b_qr)
                        k.tt("dve", qr[:], qr[:], rho[:], ALU.mult, [b_qr, b_rho], [b_qr])
                        k.tt("dve", qi[:], qi[:], rho[:], ALU.mult, [b_qr, b_rho], [b_qr])
                        for i in range(8):
                            sl_ = slice(i * 128, (i + 1) * 128)
                            k.ts("dve", th[:, sl_], qi[:, sl_], cc[:, 9, i:i + 1], None, ALU.mult, None, [b_qr, b_cc], [b_th])
                            k.ts("dve", ang[:, sl_], qr[:, sl_], cc[:, 9, i:i + 1], None, ALU.mult, None, [b_qr, b_cc], [b_angb])
                            k.stt(qr[:, sl_], qr[:, sl_], cc[:, 8, i:i + 1], th[:, sl_], ALU.mult, ALU.subtract, [b_qr, b_cc, b_th], [b_qr])
                            k.stt(qi[:, sl_], qi[:, sl_], cc[:, 8, i:i + 1], ang[:, sl_], ALU.mult, ALU.add, [b_qr, b_cc, b_angb], [b_qr])
                        P.barrier()
                    if l == 0 and dr == 0:
                        dump("s5pr", pr[:], b_pr, [128, 1024]); dump("s5pi", pi_[:], b_pr, [128, 1024])
                        dump("s5qr", qr[:], b_qr, [128, 1024]); dump("s5qi", qi[:], b_qr, [128, 1024])
                        dump("s5A128", A128[:], b_A128, [128, 2, 8])
                    with ExitStack() as sp_:
                        zp, _ = k.sb(sp_, [128, 4, 1024], BF16, "zp")
                        hp, _ = k.sb(sp_, [128, 4, 1024], BF16, "hp")
                        Dg, _ = k.sb(sp_, [128, 16, 128], BF16, "Dg")
                        zl, _ = k.sb(sp_, [128, 16], F32, "zl")
                        car, _ = k.sb(sp_, [128, 16], F32, "car")
                        ct_, _ = k.sb(sp_, [128, 4, 8], F32, "ctmp")
                        hb2 = lambda nm: [Buf(nm + "0"), Buf(nm + "1")]
                        b_zp, b_hp, b_DgT, b_zl, b_car, b_ct = hb2("zp"), hb2("hp"), hb2("Dg"), hb2("zl"), hb2("car"), hb2("ct")
                        k.memset("pool", Dg[:], 0.0, b_DgT)
                        order = [0, 1] + list(range(2, NTT)) if dr == 0 else [1, 0] + list(range(NTT - 1, 1, -1))
                        tl = 127 if dr == 0 else 0

                        def half_gen(kt, banks):
                            bre, bim, zre, zim = banks
                            cs = slice(kt * 512, (kt + 1) * 512)
                            i4 = slice(kt * 4, kt * 4 + 4); i4m = slice(8 + kt * 4, 8 + kt * 4 + 4)
                            for tt in order:
                                cols = slice(tt * 128, (tt + 1) * 128)
                                k.mm(bre, k.ps[bre][:, :], uT[:, kt, cols], Bb[:, kt, 0:512], True, True, [b_u, b_Bb])
                                k.mm(bim, k.ps[bim][:, :], uT[:, kt, cols], Bb[:, kt, 512:1024], True, True, [b_u, b_Bb])
                                yield
                                k.tt("dve", zp[:, 0, cs], k.ps[bre][:, :], pr[:, cs], ALU.mult, [k.pb[bre], b_pr], [b_zp[kt]])
                                k.tt("dve", zp[:, 1, cs], k.ps[bim][:, :], pi_[:, cs], ALU.mult, [k.pb[bim], b_pr], [b_zp[kt]])
                                k.tt("dve", zp[:, 2, cs], k.ps[bim][:, :], pr[:, cs], ALU.mult, [k.pb[bim], b_pr], [b_zp[kt]])
                                k.tt("dve", zp[:, 3, cs], k.ps[bre][:, :], pi_[:, cs], ALU.mult, [k.pb[bre], b_pr], [b_zp[kt]])
                                yield
                                for ii in range(4):
                                    i = kt * 4 + ii
                                    sl_ = slice(i * 128, (i + 1) * 128)
                                    oc = slice(ii * 128, (ii + 1) * 128)
                                    k.mm(zre, k.ps[zre][:, oc], zp[:, 0, sl_], triT[:, 0, :], True, False, [b_zp[kt], b_tri])
                                    k.mm(zre, k.ps[zre][:, oc], zp[:, 1, sl_], triT[:, 1, :], False, False, [b_zp[kt], b_tri])
                                    k.mm(zre, k.ps[zre][:, oc], Dg[:, i, :], onesb[:], False, True, [b_DgT[kt], b_ones])
                                    k.mm(zim, k.ps[zim][:, oc], zp[:, 2, sl_], triT[:, 0, :], True, False, [b_zp[kt], b_tri])
                                    k.mm(zim, k.ps[zim][:, oc], zp[:, 3, sl_], triT[:, 0, :], False, False, [b_zp[kt], b_tri])
                                    k.mm(zim, k.ps[zim][:, oc], Dg[:, 8 + i, :], onesb[:], False, True, [b_DgT[kt], b_ones])
                                yield
                                k.copy("act", zl[:, i4], k.ps[zre][:, :].rearrange("p (i t) -> p i t", t=128)[:, :, tl], [k.pb[zre]], [b_zl[kt]])
                                k.copy("act", zl[:, i4m], k.ps[zim][:, :].rearrange("p (i t) -> p i t", t=128)[:, :, tl], [k.pb[zim]], [b_zl[kt]])
                                zr_, zi_ = zl[:, i4], zl[:, i4m]
                                k.tt("dve", ct_[:, 0, i4], A128[:, 0, i4], zr_, ALU.mult, [b_A128, b_zl[kt]], [b_ct[kt]])
                                k.tt("dve", ct_[:, 1, i4], A128[:, 1, i4], zi_, ALU.mult, [b_A128, b_zl[kt]], [b_ct[kt]])
                                k.tt("dve", ct_[:, 2, i4], A128[:, 0, i4], zi_, ALU.mult, [b_A128, b_zl[kt]], [b_ct[kt]])
                                k.tt("dve", ct_[:, 3, i4], A128[:, 1, i4], zr_, ALU.mult, [b_A128, b_zl[kt]], [b_ct[kt]])
                                k.tt("dve", car[:, i4], ct_[:, 0, i4], ct_[:, 1, i4], ALU.subtract, [b_ct[kt]], [b_car[kt]])
                                k.tt("dve", car[:, i4m], ct_[:, 2, i4], ct_[:, 3, i4], ALU.add, [b_ct[kt]], [b_car[kt]])
                                for isl in (i4, i4m):
                                    k.tt("pool", Dg[:, isl, :], identf.unsqueeze(1).to_broadcast([128, 4, 128]),
                                         car[:, isl].unsqueeze(2).to_broadcast([128, 4, 128]), ALU.mult, [b_cstf, b_car[kt]], [b_DgT[kt]])
                                yield
                                k.tt("dve", hp[:, 0, cs], k.ps[zre][:, :], qr[:, cs], ALU.mult, [k.pb[zre], b_qr], [b_hp[kt]])
                                k.tt("dve", hp[:, 1, cs], k.ps[zim][:, :], qi[:, cs], ALU.mult, [k.pb[zim], b_qr], [b_hp[kt]])
                                k.tt("dve", hp[:, 2, cs], k.ps[zim][:, :], qr[:, cs], ALU.mult, [k.pb[zim], b_qr], [b_hp[kt]])
                                k.tt("dve", hp[:, 3, cs], k.ps[zre][:, :], qi[:, cs], ALU.mult, [k.pb[zre], b_qr], [b_hp[kt]])
                                yield
                                bk = bre
                                first = True
                                for ii in range(4):
                                    i = kt * 4 + ii
                                    sl_ = slice(i * 128, (i + 1) * 128)
                                    for (pi_x, ci_x) in ((0, 0), (1, 1), (2, 2), (3, 2)):
                                        last = (ii == 3 and pi_x == 3)
                                        k.mm(bk, k.ps[bk][:, 0:128], Cb[:, ci_x, i, :], hp[:, pi_x, sl_], first, last, [b_Cb, b_hp[kt]])
                                        first = False
                                if dr == 0:
                                    k.stt(yacc[:, kt, cols], uT[:, kt, cols], dcol[:, kt:kt + 1], k.ps[bk][:, 0:128], ALU.mult, ALU.add,
                                          [b_u, b_dcol, k.pb[bk]], [b_yacc])
                                else:
                                    k.tt("dve", yacc[:, kt, cols], k.ps[bk][:, 0:128], yacc[:, kt, cols], ALU.add, [k.pb[bk], b_yacc], [b_yacc])
                                yield

                        live = [half_gen(0, [0, 1, 2, 3]), half_gen(1, [4, 5, 6, 7])]
                        for _ in range(S5SKEW):
                            next(live[0])
                        while live:
                            for g_ in list(live):
                                try:
                                    next(g_)
                                except StopIteration:
                                    live.remove(g_)
                        P.barrier()
            dump("s5yacc", yacc[:], b_yacc, [128, 2, NT])
            with ExitStack() as s4:
                wgf, b_wgf = k.sb(s4, [128, 2, 256], F32, "wgf"); wgb, b_wgb = k.sb(s4, [128, 2, 256], BF16, "wgb")
                bg, b_bg = k.sb(s4, [128, 2], F32, "bglu")
                k.load("sp", wgf[:], w_glu_d[l].rearrange("(kt p) n -> p kt n", p=128), [b_wgf])
                k.copy("dve", wgb[:], wgf[:], [b_wgf], [b_wgb])
                k.load("sp", bg[:], s5bg_d[l], [b_bg])
                zT, b_zT = k.sb(s4, [128, 2, 512], BF16, "zT")
                x2, b_x2 = k.sb(s4, [128, 512], F32, "x2"); thh, b_thh = k.sb(s4, [128, 512], F32, "thh")
                sg, b_sg = k.sb(s4, [128, 512], F32, "sg")
                for g, (c0, n) in enumerate(GROUPS):
                    for kt in range(2):
                        x_ = yacc[:, kt, c0:c0 + n]
                        k.tt("dve", x2[:, :n], x_, x_, ALU.mult, [b_yacc], [b_x2])
                        k.ts("dve", x2[:, :n], x2[:, :n], 0.044715, 1.0, ALU.mult, ALU.add, [b_x2], [b_x2])
                        k.tt("dve", x2[:, :n], x2[:, :n], x_, ALU.mult, [b_x2, b_yacc], [b_x2])
                        k.act(thh[:, :n], x2[:, :n], AF.Tanh, [b_x2], [b_thh], scale=math.sqrt(2.0 / math.pi))
                        k.stt(thh[:, :n], thh[:, :n], 1.0, x_, ALU.add, ALU.mult, [b_thh, b_yacc], [b_thh])
                        k.ts("dve", zT[:, kt, :n], thh[:, :n], 0.5, None, ALU.mult, None, [b_thh], [b_zT])
                    for ct in range(2):
                        bk = k.bank()
                        for kt in range(2):
                            k.mm(bk, k.ps[bk][:, :n], wgb[:, kt, ct * 128:(ct + 1) * 128], zT[:, kt, :n], kt == 0, kt == 1, [b_wgb, b_zT])
                        P.op("act", lambda e: e.activation(out=sg[:, :n], in_=k.ps[bk][:, :n], func=AF.Sigmoid, bias=bg[:, ct:ct + 1]),
                             reads=[k.pb[bk], b_bg], writes=[b_sg])
                        k.tt("dve", ydst[:, ct, c0:c0 + n], zT[:, ct, :n], sg[:, :n], ALU.mult, [b_zT, b_sg], [b_ydst])
                P.barrier()
        dump("yb", ydst[:], b_ydst, [128, 2, NT], BF16)

    def rwkv_mixer(l):
        ydst, b_ydst = yT[3]
        xd, b_xd = ydst, Buf("xd")
        with ExitStack() as st:
            rS, b_r = yall[:, 0], Buf("rw_r")
            kS, b_k = yall[:, 1], Buf("rw_k")
            vS, b_v = yall[:, 2], Buf("rw_v")
            kkS, b_kk = k.sb(st, [128, 2, NT], BF16, "rw_kk")
            cols, b_cols = k.sb(st, [128, 2, 9], F32, "rw_cols")
            k.load("sp", cols[:], rw_cols_d[l], [b_cols])
            C_KK, C_KA, C_LNG, C_LNB, C_RK, C_W0, C_A0 = 0, 1, 2, 3, 4, 5, 7
            for half in range(2):
              with ExitStack() as s2:
                WA, b_WA = load_w("pool", s2, w_rw_d[l, :, half * 512:(half + 1) * 512], 512, "rwWA")
                WB, b_WB = k.sb(s2, [128, 8, 512], BF16, "rwWB")
                with ExitStack() as s3:
                    mu, b_mu = k.sb(s3, [128, 512], F32, "rwmu")
                    k.load("sp", mu[:], rw_mu_d[l, :, half * 512:(half + 1) * 512], [b_mu])
                    for kt in range(8):
                        k.stt(WB[:, kt, :], WA[:, kt, :], 0.5, mu[:], ALU.mult, ALU.mult, [b_WA, b_mu], [b_WB])
                    k.ts("dve", mu[:], mu[:], -1.0, 1.0, ALU.mult, ALU.add, [b_mu], [b_mu])
                    for kt in range(8):
                        k.tt("pool", WA[:, kt, :], WA[:, kt, :], mu[:], ALU.mult, [b_WA, b_mu], [b_WA])
                    P.barrier()
                hgxs = [k.sb(s2, [128, 8, 516], BF16, "hgx") for _ in range(2)]
                hsxs = [k.sb(s2, [128, 8, 512], BF16, "hsx") for _ in range(2)]
                tmps = [k.sb(s2, [128, 514], F32, "htmp") for _ in range(2)]
                sq, b_sq = k.sb(s2, [128, 512], BF16, "rwsq"); kq, b_kq = k.sb(s2, [128, 512], F32, "rwkq")
                rs_, b_rs = k.sb(s2, [128, 512], F32, "rwrs"); lnt, b_lnt = k.sb(s2, [128, 512], F32, "rwlnt")
                for g, (c0, n) in enumerate(GROUPS if RW_SUB >= 2 else []):
                    j = jof(g)
                    hgx, b_hgx = hgxs[g % 2]; hsx, b_hsx = hsxs[g % 2]
                    s_lo, s_hi = (0, NCTX) if g == 0 else (NCTX, NT)
                    lo, hi = max(s_lo, c0 - 1), min(s_hi, c0 + n + 1)
                    o0 = lo - (c0 - 1)
                    if lo > c0 - 1:
                        k.memset("dve", hgx[:, :, 0:3], 0.0, [b_hgx])
                    if hi < c0 + n + 1:
                        k.memset("dve", hgx[:, :, n + 1:n + 3], 0.0, [b_hgx])
                    w_ = hi - lo
                    for kt in range(8):
                        tmp, tmpb = tmps[kt % 2]
                        k.tt("dve", tmp[:, :w_], xT[:, kt, lo:hi], rstd_b[:, lo:hi], ALU.mult, [b_xT, b_rstd], [tmpb])
                        P.op("act", lambda e: e.activation(out=hgx[:, kt, 1 + o0:1 + o0 + w_], in_=tmp[:, :w_], func=AF.Identity,
                                                           scale=AB[:, l, 0, 0, kt, j:j + 1], bias=AB[:, l, 0, 1, kt, j:j + 1]),
                             reads=[tmpb, b_AB], writes=[b_hgx])
                    for kt in range(8):
                        k.tt("pool", hsx[:, kt, :n], hgx[:, kt, 1:n + 1], hgx[:, kt, 3:n + 3], ALU.add, [b_hgx], [b_hsx])
                    for tl_ in range(4 if RW_SUB >= 3 else 0):
                        ti = half * 4 + tl_
                        bk = k.bank()
                        for kt in range(8):
                            k.mm(bk, k.ps[bk][:, :n], WA[:, kt, tl_ * 128:(tl_ + 1) * 128], hgx[:, kt, 2:n + 2], kt == 0, False, [b_WA, b_hgx])
                        for kt in range(8):
                            k.mm(bk, k.ps[bk][:, :n], WB[:, kt, tl_ * 128:(tl_ + 1) * 128], hsx[:, kt, :n], False, kt == 7, [b_WB, b_hsx])
                        dst, dstb = [(rS, b_r), (kS, b_k), (vS, b_v), (xd, b_xd)][ti // 2]
                        k.copy("act", dst[:, ti % 2, c0:c0 + n], k.ps[bk][:, :n], [k.pb[bk]], [dstb])
                        if ti // 2 == 1 and RW_SUB >= 4:
                            ci = ti % 2
                            k.ts("dve", kq[:, :n], k.ps[bk][:, :n], cols[:, ci, C_KK:C_KK + 1], None, ALU.mult, None, [k.pb[bk], b_cols], [b_kq])
                            k.act(sq[:, :n], kq[:, :n], AF.Square, [b_kq], [b_sq])
                            b2 = k.bank()
                            k.mm(b2, k.ps[b2][:, :n], bo64, sq[:, :n], True, True, [b_sq, b_cstb])
                            k.rstd_from_ss(rs_[:, :n], k.ps[b2][:, :n], b2, 1.0, lnt[:, :n], b_lnt, [b_rs])
                            k.tt("dve", kkS[:, ci, c0:c0 + n], kq[:, :n], rs_[:, :n], ALU.mult, [b_kq, b_rs], [b_kk])
                P.barrier()
            dump("rw_r", rS[:], b_r, [128, 2, NT], BF16); dump("rw_kk", kkS[:], b_kk, [128, 2, NT], BF16)
            dump("rw_xd", xd[:], b_xd, [128, 2, NT], BF16)
            yacc, _ = k.sb(st, [128, 2, NT], BF16, "rw_yacc")
            sw = {}
            def small(name, shape, src, dt=BF16, q="pool"):
                t, b = k.sb(st, shape, dt, name)
                k.load(q if dt == BF16 else "sp", t[:], src, [b])
                sw[name] = (t, b)
            for dr in range(2 if RW_LEVEL >= 1 else 0):
                small(f"w1_{dr}", [128, 2, 32], rw_w1_d[l, dr].rearrange("(kt p) n -> p kt n", p=128))
                small(f"w2_{dr}", [32, 256], rw_w2_d[l, dr])
                small(f"a1_{dr}", [128, 2, 32], rw_a1_d[l, dr].rearrange("(kt p) n -> p kt n", p=128))
                small(f"a2_{dr}", [32, 256], rw_a2_d[l, dr])
                small(f"w0r_{dr}", [1, 256], rw_w0row_d[l, dr], F32)
                small(f"tri_{dr}", [128, 2, 128], rw_tri_d[dr], F32)
                small(f"m1_{dr}", [128, 256], rw_m1_d[dr])
                small(f"mnt_{dr}", [128, 128], rw_mnt_d[dr])
            if RW_LEVEL >= 0:
                small("g1", [128, 2, 64], rw_g1_d[l].rearrange("(kt p) n -> p kt n", p=128))
                small("g2", [64, 256], rw_g2_d[l])
                small("ind", [128, 2], rw_ind_d, F32)
            ones1, b_ones1 = k.sb(st, [1, 128], F32, "ones1")
            k.memset("dve", ones1[:], 1.0, [b_ones1])

            def a_of(dr, xsrc, n, a_out, b_aout, x1t, b_x1t):
                a1, b_a1 = sw[f"a1_{dr}"]; a2, b_a2 = sw[f"a2_{dr}"]
                bk = k.bank()
                for kt in range(2):
                    k.mm(bk, k.ps[bk][0:32, :n], a1[:, kt, :], xsrc[:, kt, :], kt == 0, kt == 1, [b_a1, b_xd])
                k.copy("act", x1t[0:32, :n], k.ps[bk][0:32, :n], [k.pb[bk]], [b_x1t])
                for ci in range(2):
                    b2 = k.bank()
                    k.mm(b2, k.ps[b2][:, :n], a2[:, ci * 128:(ci + 1) * 128], x1t[0:32, :n], True, True, [b_a2, b_x1t])
                    P.op("act", lambda e: e.activation(out=a_out[:, ci, :n], in_=k.ps[b2][:, :n], func=AF.Sigmoid,
                                                       bias=cols[:, ci, C_A0 + dr:C_A0 + dr + 1]),
                         reads=[k.pb[b2], b_cols], writes=[b_aout])

            def kmod_of(a_in, b_ain, ksrc, n, out, b_out_, tmpf, b_tmpf):
                for ci in range(2):
                    k.ts("dve", tmpf[:, ci, :n], a_in[:, ci, :n], -1.0, cols[:, ci, C_KA:C_KA + 1], ALU.add, ALU.mult, [b_ain, b_cols], [b_tmpf])
                    k.stt(out[:, ci, :n], tmpf[:, ci, :n], 1.0, ksrc[:, ci, :], ALU.add, ALU.mult, [b_tmpf, b_k], [b_out_])

            b_yt = [Buf(f"yacc{t}") for t in range(NTT)]
            for (c0_, n_) in GROUPS:
                k.memset("dve", yacc[:, :, c0_:c0_ + n_], 0.0, b_yt[c0_ // 128:(c0_ + n_) // 128])

            def scan_gen(dr, sd, banks):
                    w1, b_w1 = sw[f"w1_{dr}"]; w2, b_w2 = sw[f"w2_{dr}"]; w0r, b_w0r = sw[f"w0r_{dr}"]
                    tri, b_tri = sw[f"tri_{dr}"]; m1, b_m1 = sw[f"m1_{dr}"]; mnt, b_mnt = sw[f"mnt_{dr}"]; ind, b_ind = sw["ind"]
                    M32, b_M32 = k.sb(sd, [128, 2, 64], F32, "M32"); Mbf, b_Mbf = k.sb(sd, [128, 2, 128], BF16, "Mblk")
                    k.memset("dve", M32[:], 0.0, [b_M32]); k.memset("dve", Mbf[:], 0.0, [b_Mbf])
                    x1t, b_x1t = k.sb(sd, [32, 128], BF16, "x1t")
                    tnh, b_tnh = k.sb(sd, [32, 128], BF16, "tnh")
                    lwT, b_lwT = k.sb(sd, [128, 256], F32, "lwT")
                    lam, b_lam = k.sb(sd, [128, 2, 3, 128], F32, "lam")
                    gam, b_gam = k.sb(sd, [128, 2, 2], F32, "gam")
                    aF, b_aF = k.sb(sd, [128, 2, 128], F32, "aF")
                    tF, b_tF = k.sb(sd, [128, 2, 128], F32, "tF")
                    kmod, b_kmod = k.sb(sd, [128, 2, 128], F32, "kmod")
                    AR, b_AR = k.sb(sd, [128, 2, 2, 128], BF16, "AR")
                    BK, b_BK = k.sb(sd, [128, 2, 2, 128], BF16, "BK")
                    BKt2, b_BKt = k.sb(sd, [128, 2, 2, 2, 128], BF16, "BKt")
                    k.memset("pool", BKt2[:], 0.0, [b_BKt])
                    Vt, b_Vt = k.sb(sd, [128, 2, 128], BF16, "Vt")
                    SCb, b_SCb = k.sb(sd, [128, 4, 256], BF16, "SCb"); SCk, b_SCk = k.sb(sd, [128, 4, 256], BF16, "SCk")
                    Nb = [k.sb(sd, [128, 4, 128], BF16, f"Nb{i}") for i in range(2)]
                    Tb = [k.sb(sd, [128, 4, 128], BF16, f"Tb{i}") for i in range(2)]
                    R, b_R = k.sb(sd, [128, 4, 128], BF16, "Rinv")
                    Wsb, b_Wsb = k.sb(sd, [128, 256], BF16, "Wsb"); Usb, b_Usb = k.sb(sd, [128, 256], BF16, "Usb")
                    mt, b_mt = k.sb(sd, [128, 2, 64], F32, "mt")
                    v4h = lambda ap: ap.rearrange("p (h t) -> p h t", h=4)
                    order = list(range(NTT)) if dr == 0 else [1, 0] + list(range(NTT - 1, 1, -1))
                    yield
                    for tt in order:
                        tc_ = slice(tt * 128, (tt + 1) * 128)
                        bk = k.bank()
                        for kt in range(2):
                            k.mm(bk, k.ps[bk][0:32, 0:128], w1[:, kt, :], xd[:, kt, tc_], kt == 0, kt == 1, [b_w1, b_xd])
                        k.act(tnh[:], k.ps[bk][0:32, 0:128], AF.Tanh, [k.pb[bk]], [b_tnh])
                        a1, b_a1 = sw[f"a1_{dr}"]; a2, b_a2 = sw[f"a2_{dr}"]
                        bk = k.bank()
                        for kt in range(2):
                            k.mm(bk, k.ps[bk][0:32, 0:128], a1[:, kt, :], xd[:, kt, tc_], kt == 0, kt == 1, [b_a1, b_xd])
                        k.copy("dve", x1t[0:32, 0:128], k.ps[bk][0:32, 0:128], [k.pb[bk]], [b_x1t])
                        yield
                        bk = k.bank()
                        k.mm(bk, k.ps[bk][:, 0:256], tnh[:], w2[:], True, False, [b_tnh, b_w2])
                        k.mm(bk, k.ps[bk][:, 0:256], ones1[:], w0r[:], False, True, [b_ones1, b_w0r])
                        k.act(lwT[:], k.ps[bk][:, 0:256], AF.Sigmoid, [k.pb[bk]], [b_lwT])
                        k.ts("dve", lwT[:], lwT[:], -math.exp(-0.5), None, ALU.mult, None, [b_lwT], [b_lwT])
                        b2 = k.bank()
                        for ci in range(2):
                            k.mm(b2, k.ps[b2][:, ci * 128:(ci + 1) * 128], a2[:, ci * 128:(ci + 1) * 128], x1t[0:32, 0:128], True, True, [b_a2, b_x1t],
                                 inc=(ci == 1))
                        for ci in range(2):
                            P.op("act", lambda e: e.activation(out=aF[:, ci, :], in_=k.ps[b2][:, ci * 128:(ci + 1) * 128], func=AF.Sigmoid,
                                                               bias=cols[:, ci, C_A0 + dr:C_A0 + dr + 1]),
                                 reads=[k.pb[b2], b_cols], writes=[b_aF])
                        yield
                        kmod_of(aF, b_aF, kS[:, :, tc_], 128, kmod, b_kmod, tF, b_tF)
                        for ci in range(2):
                            k.tt("pool", tF[:, ci, :], kkS[:, ci, tc_], aF[:, ci, :], ALU.mult, [b_kk, b_aF], [b_tF])
                        lbk = []
                        for ci in range(2):
                            bk = k.bank(); lbk.append(bk)
                            k.mm(bk, k.ps[bk][:, 0:128], lwT[:, ci * 128:(ci + 1) * 128], tri[:, 0, :], True, True, [b_lwT, b_tri], inc=False)
                            k.mm(bk, k.ps[bk][:, 128:256], lwT[:, ci * 128:(ci + 1) * 128], tri[:, 1, :], True, True, [b_lwT, b_tri], inc=False)
                            k.mm(bk, k.ps[bk][:, 256:258], lwT[:, ci * 128:(ci + 1) * 128], ind[:], True, True, [b_lwT, b_ind])
                        yield
                        for ci in range(2):
                            bk = lbk[ci]
                            k.act(lam[:, ci, 0, :], k.ps[bk][:, 0:128], AF.Exp, [k.pb[bk]], [b_lam])
                            k.act(lam[:, ci, 1, :], k.ps[bk][:, 0:128], AF.Exp, [k.pb[bk]], [b_lam], scale=-1.0)
                            k.act(lam[:, ci, 2, :], k.ps[bk][:, 128:256], AF.Exp, [k.pb[bk]], [b_lam])
                            k.act(gam[:, ci, :], k.ps[bk][:, 256:258], AF.Exp, [k.pb[bk]], [b_gam])
                        yield
                        if RWSTOP <= 1:
                            continue
                        for ci in range(2):
                            k.tt("dve", AR[:, ci, 1, :], rS[:, ci, tc_], lam[:, ci, 0, :], ALU.mult, [b_r, b_lam], [b_AR])
                            k.stt(AR[:, ci, 0, :], kkS[:, ci, tc_], -1.0, lam[:, ci, 2, :], ALU.mult, ALU.mult, [b_kk, b_lam], [b_AR])
                            k.tt("dve", BK[:, ci, 1, :], kmod[:, ci, :], lam[:, ci, 1, :], ALU.mult, [b_kmod, b_lam], [b_BK])
                            k.tt("pool", BK[:, ci, 0, :], tF[:, ci, :], lam[:, ci, 1, :], ALU.mult, [b_tF, b_lam], [b_BK])
                        yield
                        if RWSTOP <= 2:
                            continue
                        bk = k.bank()
                        pv = k.ps[bk][:].bitcast(BF16)
                        for ci in range(2):
                            for x_ in range(2):
                                P.op("pe", lambda e: e.transpose(pv[:, (ci * 2 + x_) * 128:(ci * 2 + x_ + 1) * 128], BK[:, ci, x_, :], identb),
                                     reads=[b_BK, b_cstb], writes=[k.pb[bk]], inc=False)
                            P.op("pe", lambda e: e.transpose(pv[:, (4 + ci) * 128:(5 + ci) * 128], vS[:, ci, tc_], identb),
                                 reads=[b_v, b_cstb], writes=[k.pb[bk]], inc=(ci == 1))
                        k.copy("act", BKt2[0:64, 0].rearrange("p a b c -> p (a b c)"), pv[0:64, 0:512], [k.pb[bk]], [b_BKt])
                        k.copy("act", BKt2[64:128, 1].rearrange("p a b c -> p (a b c)"), pv[64:128, 0:512], [k.pb[bk]], [b_BKt])
                        k.copy("dve", Vt[:].rearrange("p a c -> p (a c)"), pv[:, 512:768], [k.pb[bk]], [b_Vt])
                        if RWSTOP <= 3:
                            continue
                        m1b = m1[:].unsqueeze(1).to_broadcast([128, 2, 256])
                        for hh in range(2):
                            pb = 64 * hh
                            bA = k.bank(); bB = k.bank()
                            for x_, bx in ((0, bA), (1, bB)):
                                for ci in range(2):
                                    arv = AR[pb:pb + 64, ci, :, :].rearrange("p a t -> p (a t)")
                                    k.mm(bx, k.ps[bx][:, ci * 256:(ci + 1) * 256], BK[pb:pb + 64, ci, x_, :], arv, True, True, [b_BK, b_AR], inc=(ci == 1))
                            k.tt("dve", SCb[:, hh * 2:hh * 2 + 2, :], k.ps[bA][:, 0:512].rearrange("p (h t) -> p h t", h=2), m1b, ALU.mult,
                                 [k.pb[bA], b_m1], [b_SCb])
                            k.tt("dve", SCk[:, hh * 2:hh * 2 + 2, :], k.ps[bB][:, 0:512].rearrange("p (h t) -> p h t", h=2), m1b, ALU.mult,
                                 [k.pb[bB], b_m1], [b_SCk])
                            yield
                        Tc, b_Tc = Tb[0]
                        for hh in range(2):
                            pb = 64 * hh
                            bC = k.bank()
                            for ci in range(2):
                                k.mm(bC, k.ps[bC][:, ci * 128:(ci + 1) * 128], AR[pb:pb + 64, ci, 0, :], BK[pb:pb + 64, ci, 0, :], True, True, [b_AR, b_BK],
                                     inc=(ci == 1))
                            k.tt("dve", Tc[:, hh * 2:hh * 2 + 2, :], k.ps[bC][:, 0:256].rearrange("p (h t) -> p h t", h=2),
                                 mnt[:].unsqueeze(1).to_broadcast([128, 2, 128]), ALU.mult, [k.pb[bC], b_mnt], [b_Tc])
                        k.tt("pool", R[:], SCb[:, :, 0:128], identb.unsqueeze(1).to_broadcast([128, 4, 128]), ALU.add, [b_SCb, b_cstb], [b_R])
                        yield
                        if RWSTOP <= 4:
                            continue
                        Nc, b_Nc = SCb[:, :, 0:128], b_SCb
                        for i in range(1, 6):
                            Nn, b_Nn = Nb[i % 2]; Tn, b_Tn = Tb[i % 2]
                            bt_ = k.bank()
                            for h in range(4):
                                k.mm(bt_, k.ps[bt_][:, h * 128:(h + 1) * 128], Nc[:, h, :], Tc[:, h, :], True, True, [b_Nc, b_Tc], inc=(h == 3))
                            k.copy("dve", Tn[:], v4h(k.ps[bt_][:, 0:512]), [k.pb[bt_]], [b_Tn])
                            if i < 5:
                                bn = k.bank()
                                for h in range(4):
                                    k.mm(bn, k.ps[bn][:, h * 128:(h + 1) * 128], Tc[:, h, :], Nc[:, h, :], True, True, [b_Tc, b_Nc], inc=(h == 3))
                                k.copy("act", Nn[:], v4h(k.ps[bn][:, 0:512]), [k.pb[bn]], [b_Nn])
                            yield
                            br_ = k.bank()
                            for h in range(4):
                                k.mm(br_, k.ps[br_][:, h * 128:(h + 1) * 128], Tn[:, h, :], R[:, h, :], True, True, [b_Tn, b_R], inc=(h == 3))
                            k.tt("dve", R[:], v4h(k.ps[br_][:, 0:512]), R[:], ALU.add, [k.pb[br_], b_R], [b_R])
                            yield
                            Nc, b_Nc = Nn[:], b_Nn
                            Tc, b_Tc = Tn, b_Tn
                        if RWSTOP <= 5:
                            continue
                        bY, bW, bU, bM = banks
                        for jj in ([0, 1] if dr == 0 else [1, 0]):
                            pj = 64 * jj
                            for ci in range(2):
                                k.mm(bW, k.ps[bW][:, ci * 128:(ci + 1) * 128], AR[:, ci, 0, :], Mbf[:, ci, :], True, False, [b_AR, b_Mbf], inc=False)
                                for hh in range(2):
                                    h = ci * 2 + hh
                                    pb = 64 * hh
                                    k.mm(bW, k.ps[bW][:, h * 64:(h + 1) * 64], SCk[:, hh * 2 + ci, 0:128], Vt[:, ci, pb:pb + 64], False, True, [b_SCk, b_Vt],
                                         inc=(h == 3))
                            k.copy("act", Wsb[:], k.ps[bW][:, 0:256], [k.pb[bW]], [b_Wsb])
                            yield
                            for h in range(4):
                                oc = slice(h * 64, (h + 1) * 64)
                                k.mm(bU, k.ps[bU][:, oc], R[:, (h % 2) * 2 + h // 2, :], Wsb[:, oc], True, True, [b_R, b_Wsb], inc=(h == 3))
                            k.copy("dve", Usb[:], k.ps[bU][:, 0:256], [k.pb[bU]], [b_Usb])
                            yield
                            for ci in range(2):
                                for hh in range(2):
                                    h = ci * 2 + hh
                                    pb = 64 * hh
                                    oc = slice(h * 64, (h + 1) * 64)
                                    mo = k.ps[bM][pb:pb + 64, ci * 64:(ci + 1) * 64]
                                    k.mm(bM, mo, BKt2[:, jj, ci, 0, pb:pb + 64], Usb[:, oc], True, False, [b_BKt, b_Usb], inc=False)
                                    k.mm(bM, mo, BKt2[:, jj, ci, 1, pb:pb + 64], Vt[:, ci, pb:pb + 64], False, True, [b_BKt, b_Vt], inc=(h == 3))
                            for ci in range(2):
                                ycol = slice(ci * 128 + pj, ci * 128 + pj + 64)
                                k.mm(bY, k.ps[bY][:, ycol], Mbf[:, ci, :], AR[:, ci, 1, pj:pj + 64], True, False, [b_Mbf, b_AR], inc=False)
                                for hh in range(2):
                                    h = ci * 2 + hh
                                    pb = 64 * hh
                                    oc = slice(h * 64, (h + 1) * 64)
                                    k.mm(bY, k.ps[bY][pb:pb + 64, ycol], Usb[:, oc], SCb[:, hh * 2 + ci, 128 + pj:128 + pj + 64], False, False, [b_Usb, b_SCb], inc=False)
                                    k.mm(bY, k.ps[bY][pb:pb + 64, ycol], Vt[:, ci, pb:pb + 64], SCk[:, hh * 2 + ci, 128 + pj:128 + pj + 64], False, True, [b_Vt, b_SCk],
                                         inc=(h == 3))
                            k.tt("dve", mt[:], k.ps[bM][:, 0:128].rearrange("p (c n) -> p c n", c=2), M32[:], ALU.add, [k.pb[bM], b_M32], [b_mt])
                            k.tt("dve", Mbf[0:64, :, 0:64], mt[0:64, :, :], gam[0:64, :, jj:jj + 1].to_broadcast([64, 2, 64]), ALU.mult,
                                 [b_mt, b_gam], [b_Mbf])
                            k.tt("dve", Mbf[64:128, :, 64:128], mt[64:128, :, :], gam[64:128, :, jj:jj + 1].to_broadcast([64, 2, 64]), ALU.mult,
                                 [b_mt, b_gam], [b_Mbf])
                            k.tt("dve", M32[:], mt[:], gam[:, :, jj:jj + 1].to_broadcast([128, 2, 64]), ALU.mult, [b_mt, b_gam], [b_M32])
                            yield
                        k.tt("dve", yacc[:, :, tc_], k.ps[bY][:, 0:256].rearrange("p (c t) -> p c t", c=2), yacc[:, :, tc_], ALU.add,
                             [k.pb[bY], b_yt[tt]], [b_yt[tt]])
                        yield

            with ExitStack() as sd:
                threads = [[scan_gen(dr, sd, [4 * dr + i for i in range(4)]), [4 * dr + i for i in range(4)], 0] for dr in range(2)]
                live = list(threads)
                k.rrset, k.rr = threads[0][1], threads[0][2]
                for _ in range(RWSKEW):
                    next(threads[0][0])
                threads[0][2] = k.rr
                if os.environ.get("RWSEQ"):
                    for th in threads:
                        k.rrset, k.rr = th[1], th[2]
                        for _ in th[0]:
                            pass
                    live = []
                while live:
                    for th in list(live):
                        k.rrset, k.rr = th[1], th[2]
                        try:
                            next(th[0])
                        except StopIteration:
                            live.remove(th)
                        th[2] = k.rr
                k.rrset = list(range(8)); k.rr = 0
                P.barrier()
            b_yacc = Buf("rw_yacc_all")
            dump("rw_yacc", yacc[:], b_yacc, [128, 2, NT], BF16)
            with ExitStack() as s5_:
                    g1, b_g1 = sw["g1"]; g2, b_g2 = sw["g2"]
                    lneps, b_lneps = k.sb(s5_, [128, 1], F32, "lneps")
                    k.memset("dve", lneps[:], 64e-5, [b_lneps])

                    def epi_bufs():
                        d = {}
                        for nm, shp, dt in (("x1t", [32, 512], BF16), ("aF", [128, 2, 512], F32), ("tF", [128, 2, 512], F32),
                                            ("kmod", [128, 2, 512], F32), ("bon", [128, 2, 512], F32), ("ybf", [128, 512], BF16),
                                            ("yc", [128, 512], F32), ("lnt", [128, 512], F32),
                                            ("gh", [64, 512], BF16), ("gg", [128, 2, 512], F32), ("rkb", [128, 512], BF16)):
                            d[nm] = k.sb(s5_, shp, dt, "e" + nm)
                        return d

                    def epi_gen(groups, d):
                        x1t, b_x1t = d["x1t"]; aF, b_aF = d["aF"]; tF, b_tF = d["tF"]; kmod, b_kmod = d["kmod"]
                        bon, b_bon = d["bon"]; ybf, b_ybf = d["ybf"]; yc, b_yc = d["yc"]
                        lnt, b_lnt = d["lnt"]; rs_, b_rs = lnt, b_lnt; gh, b_gh = d["gh"]; gg, b_gg = d["gg"]; rkb, b_rkb = d["rkb"]
                        for g in groups:
                            c0, n = GROUPS[g]
                            gc_ = slice(c0, c0 + n)
                            for dr in range(2):
                                a_of(dr, xd[:, :, gc_], n, aF, b_aF, x1t, b_x1t)
                                yield
                                kmod_of(aF, b_aF, kS[:, :, gc_], n, kmod, b_kmod, tF, b_tF)
                                for ci in range(2):
                                    k.stt(rkb[:, :n], kmod[:, ci, :n], cols[:, ci, C_RK:C_RK + 1], rS[:, ci, gc_], ALU.mult, ALU.mult,
                                          [b_kmod, b_cols, b_r], [b_rkb])
                                    bk = k.bank()
                                    k.mm(bk, k.ps[bk][:, :n], bo64, rkb[:, :n], True, True, [b_rkb, b_cstb])
                                    yield
                                    if dr == 0:
                                        k.tt("dve", bon[:, ci, :n], k.ps[bk][:, :n], vS[:, ci, gc_], ALU.mult, [k.pb[bk], b_v], [b_bon])
                                    else:
                                        k.tt("dve", tF[:, ci, :n], k.ps[bk][:, :n], vS[:, ci, gc_], ALU.mult, [k.pb[bk], b_v], [b_tF])
                                        k.tt("pool", bon[:, ci, :n], bon[:, ci, :n], tF[:, ci, :n], ALU.add, [b_bon, b_tF], [b_bon])
                            bk = k.bank()
                            for kt in range(2):
                                k.mm(bk, k.ps[bk][0:64, :n], g1[:, kt, :], xd[:, kt, gc_], kt == 0, kt == 1, [b_g1, b_xd])
                            k.act(gh[:, :n], k.ps[bk][0:64, :n], AF.Sigmoid, [k.pb[bk]], [b_gh])
                            yield
                            for ci in range(2):
                                b2 = k.bank()
                                k.mm(b2, k.ps[b2][:, :n], g2[:, ci * 128:(ci + 1) * 128], gh[:, :n], True, True, [b_g2, b_gh])
                                k.copy("act", gg[:, ci, :n], k.ps[b2][:, :n], [k.pb[b2]], [b_gg])
                            yield
                            for ci in range(2):
                                bk = k.bank()
                                k.mm(bk, k.ps[bk][:, :n], bo64, yacc[:, ci, gc_], True, True, [b_yacc, b_cstb])
                                k.stt(yc[:, :n], k.ps[bk][:, :n], -1.0 / 64, yacc[:, ci, gc_], ALU.mult, ALU.add, [k.pb[bk], b_yacc], [b_yc])
                                k.act(ybf[:, :n], yc[:, :n], AF.Square, [b_yc], [b_ybf])
                                yield
                                b2 = k.bank()
                                k.mm(b2, k.ps[b2][:, :n], bo64, ybf[:, :n], True, True, [b_ybf, b_cstb])
                                P.op("act", lambda e: e.activation(out=lnt[:, :n], in_=k.ps[b2][:, :n], func=AF.Ln, scale=1.0 / 64, bias=lneps[:]),
                                     reads=[k.pb[b2], b_lneps], writes=[b_lnt])
                                k.act(rs_[:, :n], lnt[:, :n], AF.Exp, [b_lnt], [b_rs], scale=-0.5)
                                yield
                                k.stt(yc[:, :n], yc[:, :n], cols[:, ci, C_LNG:C_LNG + 1], rs_[:, :n], ALU.mult, ALU.mult, [b_yc, b_cols, b_rs], [b_yc])
                                k.stt(yc[:, :n], yc[:, :n], cols[:, ci, C_LNB:C_LNB + 1], bon[:, ci, :n], ALU.add, ALU.add, [b_yc, b_cols, b_bon], [b_yc])
                                k.tt("dve", ydst[:, ci, gc_], yc[:, :n], gg[:, ci, :n], ALU.mult, [b_yc, b_gg, b_xd], [b_ydst])
                                yield

                    glist = [g for g in range(len(GROUPS)) if not (l == DEPTH - 1 and g == 0)]
                    eths = [[epi_gen(glist[0::2], epi_bufs()), [0, 1, 2, 3], 0], [epi_gen(glist[1::2], epi_bufs()), [4, 5, 6, 7], 0]]
                    live = list(eths)
                    while live:
                        for th in list(live):
                            k.rrset, k.rr = th[1], th[2]
                            try:
                                next(th[0])
                            except StopIteration:
                                live.remove(th)
                            th[2] = k.rr
                    k.rrset = list(range(8)); k.rr = 0
                    P.barrier()
        dump("yd", ydst[:], b_ydst, [128, 2, NT], BF16)

    def final_out():
        with ExitStack() as st:
            ot = [k.sb(st, [128, D], F32, "ostage") for _ in range(2)]
            for tt in range(NLAT // 128):
                o, ob = ot[tt % 2]
                c0 = NCTX + tt * 128
                for half in range(2):
                    bk = k.bank()
                    for j in range(4):
                        kt = half * 4 + j
                        P.op("pe", lambda e: e.transpose(k.ps[bk][:, j * 128:(j + 1) * 128], xT[:, kt, c0:c0 + 128], identf),
                             reads=[b_xT, b_cstf], writes=[k.pb[bk]], inc=(j == 3))
                    k.copy("dve" if half == 0 else "act", o[:, half * 512:(half + 1) * 512], k.ps[bk][:], [k.pb[bk]], [ob])
                P.dma("sp", out_d[tt * 128:(tt + 1) * 128, :], o[:], reads=[ob])
            P.finish([b for _, b in ot])
            P.barrier()

    for l in range(DEPTH):
        if ("L%d" % l) not in stages:
            continue
        with ExitStack() as st:
            rms_stats(st)
            P.barrier()
        if "rw" in stages:
            rwkv_mixer(l)
        if "da" in stages:
            da_mixer(l)
        if "mla" in stages:
            mla_mixer(l)
        if "s5" in stages:
            s5_mixer(l)
        if "merge" in stages:
            merge(l)
        if "moe" in stages:
            moe(l)
    final_out()
    for e in ("sp",):
        toks = [(kk, v) for kk, v in P.cnt.items() if v > 0 and isinstance(kk, tuple)]
        for tok in toks:
            P._wait(e, tok)
    top.close()
    return nc, k, dbg_out


_CONSTS = None


def prep_shared(inp):
    g = {}
    g.update(host_consts())
    f32 = np.float32
    w_in = np.asarray(inp["w_in"], f32)
    offs = np.cumsum([0, 256, 256, 256, 256, 192, 128, 16, 256, 256, 256, 256, 4096])
    seg = lambda i: w_in[:, :, offs[i]:offs[i + 1]]
    g["w_ada"] = np.ascontiguousarray(inp["w_ada"], f32)
    ba = np.asarray(inp["b_ada"], f32)
    g["bada"] = np.ascontiguousarray(ba.reshape(DEPTH, 48, 128).transpose(0, 2, 1))
    g["gmix"] = np.stack([fm_cols(inp["norm_mix_g"][l], 8) for l in range(DEPTH)])
    g["gffn"] = np.stack([fm_cols(inp["norm_ffn_g"][l], 8) for l in range(DEPTH)])
    def pad3(w):
        o = np.zeros((DEPTH, D, 384), f32)
        for j in range(8):
            o[:, :, (j // 3) * 128 + (j % 3) * 32:(j // 3) * 128 + (j % 3) * 32 + 32] = w[:, :, j * 32:(j + 1) * 32]
        return o
    g["w_da_qk"] = np.ascontiguousarray(np.concatenate([pad3(seg(0)), pad3(seg(1))], axis=2))
    g["w_da_v"] = np.ascontiguousarray(seg(2))
    dg = np.asarray(inp["da_qk_norm_g"], f32)
    g["da_g"] = np.ascontiguousarray(np.stack([np.tile(dg[:, 0, :], (1, 4)), np.tile(dg[:, 1, :], (1, 4))], axis=2))
    g["da_lam"] = np.ascontiguousarray(np.broadcast_to(np.asarray(inp["da_lambda"], f32).reshape(DEPTH, 1, 128), (DEPTH, 128, 128)))
    g["da_sub"] = np.ascontiguousarray(np.broadcast_to(np.asarray(inp["da_subln_g"], f32).reshape(DEPTH, 1, 64), (DEPTH, 128, 64)))
    g["w_mla_c"] = np.zeros((DEPTH, D, 384), f32)
    g["w_mla_c"][:, :, 0:192] = seg(4)
    g["w_mla_c"][:, :, 256:384] = seg(5)
    g["w_mla_kr"] = np.zeros((DEPTH, D, 128), f32)
    g["w_mla_kr"][:, :, 32:48] = seg(6)
    g["w_mla_kr"][:, :, 96:112] = seg(6)
    wuq = np.asarray(inp["mla_w_uq"], f32)
    wuqp = np.zeros((DEPTH, 256, 256), f32)
    for h in range(4):
        wuqp[:, 0:192, 64 * h:64 * h + 48] = wuq[:, :, 48 * h:48 * h + 48]
    g["w_uq"] = np.ascontiguousarray(wuqp.reshape(DEPTH, 2, 128, 256).transpose(0, 2, 1, 3))
    wukv = np.asarray(inp["mla_w_ukv"], f32)
    g["w_ukvk"] = np.zeros((DEPTH, 128, 256), f32)
    g["w_ukvv"] = np.zeros((DEPTH, 128, 256), f32)
    for h in range(4):
        g["w_ukvk"][:, :, 64 * h:64 * h + 32] = wukv[:, :, 96 * h:96 * h + 32]
        g["w_ukvv"][:, :, 64 * h:64 * h + 64] = wukv[:, :, 96 * h + 32:96 * h + 96]
    gcq = np.zeros((DEPTH, 256), f32); gcq[:, 0:192] = np.asarray(inp["mla_cq_norm_g"], f32)
    gkv = np.asarray(inp["mla_ckv_norm_g"], f32)
    g["mla_gc"] = np.ascontiguousarray(np.stack([gcq[:, 0:128], gcq[:, 128:256], gkv], axis=2))
    mg = np.asarray(inp["mla_qk_norm_g"], f32)
    mgp = np.zeros((DEPTH, 2, 128), f32)
    for h in range(2):
        mgp[:, :, 64 * h:64 * h + 48] = mg
    g["mla_g"] = np.ascontiguousarray(mgp.transpose(0, 2, 1))
    g["w_s5"] = np.ascontiguousarray(seg(3))
    Bre = np.asarray(inp["s5_b_re"], f32); Bim = np.asarray(inp["s5_b_im"], f32)
    Cre = np.asarray(inp["s5_c_re"], f32); Cim = np.asarray(inp["s5_c_im"], f32)
    s5B = np.zeros((DEPTH, 2, 2, 128, 1024), f32)
    s5C = np.zeros((DEPTH, 2, 2, 128, 8, 128), f32)
    for gi in range(16):
        kt, gl = gi // 8, gi % 8
        s5B[:, :, kt, gl * 16:(gl + 1) * 16, gl * 64:(gl + 1) * 64] = Bre[:, :, gi].transpose(0, 1, 3, 2)
        s5B[:, :, kt, gl * 16:(gl + 1) * 16, 512 + gl * 64:512 + (gl + 1) * 64] = Bim[:, :, gi].transpose(0, 1, 3, 2)
        i, po = gi // 2, (gi % 2) * 64
        s5C[:, :, 0, po:po + 64, i, gl * 16:(gl + 1) * 16] = Cre[:, :, gi].transpose(0, 1, 3, 2)
        s5C[:, :, 1, po:po + 64, i, gl * 16:(gl + 1) * 16] = Cim[:, :, gi].transpose(0, 1, 3, 2)
    g["s5B"] = s5B; g["s5C"] = s5C
    lre = np.asarray(inp["s5_lam_re"], f32).reshape(DEPTH, 2, 1024)
    lim = np.asarray(inp["s5_lam_im"], f32).reshape(DEPTH, 2, 1024)
    ldt = np.repeat(np.asarray(inp["s5_log_dt"], f32), 64, axis=2)
    tm = np.stack([lre, lim, ldt], axis=2)
    g["s5tm"] = np.ascontiguousarray(np.broadcast_to(tm[:, :, :, None, :], (DEPTH, 2, 3, 128, 1024)))
    g["s5fm"] = np.ascontiguousarray(tm.reshape(DEPTH, 2, 3, 8, 128).transpose(0, 1, 4, 2, 3))
    g["s5d"] = np.stack([fm_cols(np.asarray(inp["s5_d"], f32)[l].reshape(256), 2) for l in range(DEPTH)])
    g["s5bg"] = np.stack([fm_cols(np.asarray(inp["s5_b_glu"], f32)[l], 2) for l in range(DEPTH)])
    g["w_glu"] = np.ascontiguousarray(inp["s5_w_glu"], f32)
    pidx = np.arange(128, dtype=f32)
    posc = np.zeros((2, 128, 2), f32); posc[0, :, 0] = -pidx; posc[1, :, 0] = -(127 - pidx)
    posr = np.zeros((2, 128, 128), f32); posr[0] = pidx[None, :]; posr[1] = (127 - pidx)[None, :]
    tri = np.zeros((2, 128, 128), f32)
    tri[0] = (pidx[:, None] <= pidx[None, :]).astype(f32)
    tri[1] = (pidx[:, None] >= pidx[None, :]).astype(f32)
    g["posc"] = posc; g["posr"] = posr; g["tri"] = tri
    g["w_rw"] = np.ascontiguousarray(np.concatenate([seg(7), seg(8), seg(9), seg(10)], axis=2))
    mu = np.asarray(inp["rw_mu"], f32).reshape(DEPTH, 1, 1024)
    g["rw_mu_row"] = np.ascontiguousarray(np.broadcast_to(mu, (DEPTH, 128, 1024)))
    colsl = []
    for l in range(DEPTH):
        cs = [inp["rw_k_k"][l], inp["rw_k_a"][l], inp["rw_ln_g"][l], inp["rw_ln_b"][l], np.asarray(inp["rw_r_k"][l]).reshape(256),
              inp["rw_w0"][l, 0], inp["rw_w0"][l, 1], inp["rw_a0"][l, 0], inp["rw_a0"][l, 1]]
        colsl.append(np.stack([fm_cols(c, 2) for c in cs], axis=2))
    g["rw_cols"] = np.ascontiguousarray(np.stack(colsl))
    g["rw_w0row"] = np.ascontiguousarray(np.asarray(inp["rw_w0"], f32).reshape(DEPTH, 2, 1, 256))
    for nm in ("rw_w1", "rw_w2", "rw_a1", "rw_a2", "rw_g1", "rw_g2"):
        g[nm] = np.ascontiguousarray(inp[nm], f32)
    t = np.arange(128)
    same = (t[:, None] // 64) == (t[None, :] // 64)
    tri = np.zeros((2, 128, 2, 128), f32)
    tri[0, :, 0, :] = (same & (t[:, None] <= t[None, :])); tri[0, :, 1, :] = (same & (t[:, None] < t[None, :]))
    tri[1, :, 0, :] = (same & (t[:, None] >= t[None, :])); tri[1, :, 1, :] = (same & (t[:, None] > t[None, :]))
    g["rw_tri"] = tri
    ind = np.zeros((128, 2), f32); ind[:64, 0] = 1; ind[64:, 1] = 1
    g["rw_ind"] = ind
    m1 = np.zeros((2, 128, 256), f32)
    m1[0, :, :128] = (same & (t[:, None] < t[None, :])); m1[0, :, 128:] = (same & (t[:, None] <= t[None, :]))
    m1[1, :, :128] = (same & (t[:, None] > t[None, :])); m1[1, :, 128:] = (same & (t[:, None] >= t[None, :]))
    g["rw_m1"] = m1
    mnt = np.zeros((2, 128, 128), f32)
    mnt[0] = (same & (t[None, :] < t[:, None])); mnt[1] = (same & (t[None, :] > t[:, None]))
    g["rw_mnt"] = mnt
    g["w_gates"] = np.ascontiguousarray(seg(11).reshape(DEPTH, 8, 128, 4, 8, 128).transpose(0, 4, 2, 1, 3, 5)).reshape(DEPTH, 8, 128, 4096)
    g["w_branch"] = np.ascontiguousarray(np.asarray(inp["w_branch"], f32).reshape(DEPTH, 4, 2, 128, D).transpose(0, 3, 2, 1, 4)).reshape(DEPTH, 128, 8192)
    g["w_out"] = np.ascontiguousarray(inp["w_out"], f32)
    g["router_w"] = np.ascontiguousarray(inp["router_w"], f32)
    g["router_b"] = np.ascontiguousarray(np.broadcast_to(np.asarray(inp["router_bias"], f32).reshape(1, 16), (128, 16)))
    sel = np.zeros((16, 16, 128), f32)
    for e in range(16):
        sel[e, e, :] = 1.0
    g["sel"] = sel
    g["exp_w_gate"] = np.ascontiguousarray(inp["exp_w_gate"], f32)
    g["exp_w_up"] = np.ascontiguousarray(inp["exp_w_up"], f32)
    g["exp_w_down"] = np.ascontiguousarray(inp["exp_w_down"], f32)
    return g


def prep_core(inp, b):
    x = np.asarray(inp["x"], np.float32)[b]
    ctx = np.asarray(inp["ctx"], np.float32)[b]
    xin = np.ascontiguousarray(np.concatenate([ctx, x], axis=0))
    c2 = np.stack([np.asarray(inp["c"], np.float32)[b], np.asarray(inp["c_ctx"], np.float32)], axis=0)
    c2T = np.ascontiguousarray(c2.reshape(2, 8, 128).transpose(2, 1, 0))
    return {"xin": xin, "c2T": c2T}


RW_LEVEL = int(os.environ.get('RW_LEVEL', '9'))
RWSTOP = int(os.environ.get('RWSTOP', '9'))
S5SKEW = int(os.environ.get('S5SKEW', '1'))
RWSKEW = int(os.environ.get('RWSKEW', '0'))
RW_SUB = int(os.environ.get('RW_SUB', '9'))
STAGES_ALL = ("L0", "L1", "da", "mla", "s5", "rw", "merge", "moe")


def kernel(**inputs):
    nc, k, _ = build(STAGES_ALL)
    shared = prep_shared(inputs)
    in_maps = []
    for b in range(8):
        m = dict(shared)
        m.update(prep_core(inputs, b))
        in_maps.append({kk: m[kk] for kk in k.ins})
    res = run_bass_kernel_spmd(nc, in_maps, core_ids=list(range(8)))
    return np.stack([np.asarray(r["out"]) for r in res.results], axis=0).astype(np.float32)
```

```python
import math
import os
import numpy as np
import concourse.bass as bass
import concourse.mybir as mybir
from concourse.bass_utils import run_bass_kernel_spmd

F32 = mybir.dt.float32
BF16 = mybir.dt.bfloat16
I32 = mybir.dt.int32
AF = mybir.ActivationFunctionType
ALU = mybir.AluOpType
AX = mybir.AxisListType

D = 1024
NT = 2304
NCTX = 256
NLAT = 2048
DEPTH = 2
EPS = 1e-6
GROUPS = [(0, 256)] + [(256 + 512 * i, 512) for i in range(4)]
NTT = 18


class Buf:
    __slots__ = ("name", "w", "r", "excl")

    def __init__(self, name="", excl=False):
        self.name = name
        self.w = None
        self.r = {}
        self.excl = excl


class Prog:
    NDMA = 8

    def __init__(self, nc):
        self.nc = nc
        self.eng = {"pe": nc.tensor, "act": nc.scalar, "dve": nc.vector, "pool": nc.gpsimd, "sp": nc.sync}
        self.sems = {}
        self.cnt = {}
        self.seen = {e: {} for e in self.eng}
        self.pend = {e: ([], []) for e in self.eng}
        for e in self.eng:
            self.sems[e] = nc.alloc_semaphore("s_" + e)
            self.cnt[e] = 0
        self.dq = {}
        for q in ("sp", "pool"):
            lst = []
            for i in range(self.NDMA):
                key = ("d", q, i)
                self.sems[key] = nc.alloc_semaphore(f"d_{q}{i}")
                self.cnt[key] = 0
                lst.append(key)
            self.dq[q] = [lst, 0, [None] * self.NDMA]
        self.ninst = 0

    def _wait(self, e, tok):
        key, val = tok
        if key == e:
            if e == "pe":
                return
            if val < self.cnt[e] - 1:
                return
        if self.seen[e].get(key, 0) >= val:
            return
        self.eng[e].wait_ge(self.sems[key], val)
        self.seen[e][key] = val
        self.ninst += 1

    def _deps(self, e, reads, writes):
        for b in reads:
            if b.w is not None:
                self._wait(e, b.w)
            if b.excl:
                for tok in b.r.values():
                    if tok[0] != e:
                        self._wait(e, tok)
        for b in writes:
            if b.w is not None:
                self._wait(e, b.w)
            for tok in b.r.values():
                if tok[0] != e:
                    self._wait(e, tok)

    def op(self, e, fn, reads=(), writes=(), inc=True):
        self._deps(e, reads, writes)
        inst = fn(self.eng[e])
        self.ninst += 1
        pr, pw = self.pend[e]
        pr.extend(reads)
        pw.extend(writes)
        if inc:
            self.cnt[e] += 1
            tok = (e, self.cnt[e])
            inst.then_inc(self.sems[e], 1)
            for b in pr:
                b.r[e] = tok
            for b in pw:
                b.w = tok
                b.r = {}
            self.pend[e] = ([], [])
        return inst

    def dma(self, q, out, in_, reads=(), writes=(), **kw):
        lst, rr, last = self.dq[q]
        k = rr
        self.dq[q][1] = (rr + 1) % self.NDMA
        if last[k] is not None:
            self._wait(q, last[k])
        self._deps(q, reads, writes)
        inst = self.eng[q].dma_start(out=out, in_=in_, **kw)
        self.ninst += 1
        key = lst[k]
        self.cnt[key] += 16
        tok = (key, self.cnt[key])
        inst.then_inc(self.sems[key], 16)
        last[k] = tok
        for b in reads:
            b.r[key] = tok
        for b in writes:
            b.w = tok
            b.r = {}
        return tok

    def finish(self, bufs, e="sp"):
        for b in bufs:
            if b.w is not None:
                self._wait(e, b.w)
            for tok in b.r.values():
                self._wait(e, tok)

    def barrier(self):
        toks = [(k, v) for k, v in self.cnt.items() if v > 0]
        for e in self.eng:
            for tok in toks:
                if tok[0] != e:
                    self._wait(e, tok)


from contextlib import ExitStack


class K:
    def __init__(self, nc, dbg=None):
        self.nc = nc
        self.P = Prog(nc)
        self.dbg = dbg or {}
        self.ins = {}
        self.uid = 0
        self.ps = []
        self.pb = []
        for i in range(8):
            self.ps.append(nc.alloc_psum_tensor(f"ps{i}", [128, 512], F32))
            self.pb.append(Buf(f"ps{i}", excl=True))
        self.rr = 0
        self.rrset = list(range(8))

    def din(self, name, shape, dtype=F32):
        t = self.nc.dram_tensor(name, list(shape), dtype, kind="ExternalInput").ap()
        self.ins[name] = t
        return t

    def sb(self, stack, shape, dtype, name=None):
        self.uid += 1
        nm = f"{name or 't'}_{self.uid}"
        t = stack.enter_context(self.nc.sbuf_tensor(nm, list(shape), dtype))
        nb = int(np.prod(shape[1:])) * (2 if dtype == BF16 else 4)
        self.live = getattr(self, "live", 0) + nb
        if self.live > getattr(self, "peak", 0):
            self.peak = self.live; self.peak_at = nm
        def _dec(nb=nb):
            self.live -= nb
        stack.callback(_dec)
        return t, Buf(nm)

    def bank(self):
        i = self.rrset[self.rr % len(self.rrset)]
        self.rr += 1
        return i

    def mm(self, bank, out, lhsT, rhs, start, stop, reads, inc=None):
        self.P.op("pe", lambda e: e.matmul(out, lhsT=lhsT, rhs=rhs, start=start, stop=stop, skip_group_check=True),
                  reads=reads, writes=[self.pb[bank]], inc=(stop if inc is None else inc))

    def act(self, out, in_, func, reads, writes, scale=None, bias=None, eng="act"):
        kw = {}
        if scale is not None:
            kw["scale"] = scale
        if bias is not None:
            kw["bias"] = bias
        self.P.op("act", lambda e: e.activation(out=out, in_=in_, func=func, **kw), reads=reads, writes=writes)

    def tt(self, eng, out, in0, in1, op, reads, writes):
        self.P.op(eng, lambda e: e.tensor_tensor(out=out, in0=in0, in1=in1, op=op), reads=reads, writes=writes)

    def ts(self, eng, out, in0, s1, s2, op0, op1, reads, writes):
        if op1 is None:
            self.P.op(eng, lambda e: e.tensor_scalar(out=out, in0=in0, scalar1=s1, scalar2=None, op0=op0),
                      reads=reads, writes=writes)
        else:
            self.P.op(eng, lambda e: e.tensor_scalar(out=out, in0=in0, scalar1=s1, scalar2=s2, op0=op0, op1=op1),
                      reads=reads, writes=writes)

    def stt(self, out, in0, scalar, in1, op0, op1, reads, writes):
        self.P.op("dve", lambda e: e.scalar_tensor_tensor(out=out, in0=in0, scalar=scalar, in1=in1, op0=op0, op1=op1),
                  reads=reads, writes=writes)

    def copy(self, eng, out, in_, reads, writes):
        if eng == "act":
            self.P.op("act", lambda e: e.copy(out=out, in_=in_), reads=reads, writes=writes)
        else:
            self.P.op(eng, lambda e: e.tensor_copy(out=out, in_=in_), reads=reads, writes=writes)

    def memset(self, eng, ap, val, writes):
        self.P.op(eng, lambda e: e.memset(ap, val), writes=writes)

    def load(self, q, out, in_, writes, reads=()):
        self.P.dma(q, out, in_, reads=reads, writes=writes)

    def rstd_from_ss(self, out, ss_ps, bank, inv_n, tmp, tmpb, writes):
        self.P.op("act", lambda e: e.activation(out=tmp, in_=ss_ps, func=AF.Ln, scale=inv_n, bias=self.eps_col[:]),
                  reads=[self.pb[bank], self.b_const], writes=[tmpb])
        self.P.op("act", lambda e: e.activation(out=out, in_=tmp, func=AF.Exp, scale=-0.5), reads=[tmpb], writes=writes)


def host_consts():
    ident = np.eye(128, dtype=np.float32)
    bo32 = np.kron(np.eye(4, dtype=np.float32), np.ones((32, 32), np.float32))
    bo64 = np.kron(np.eye(2, dtype=np.float32), np.ones((64, 64), np.float32))
    Rda = np.zeros((128, 128), np.float32)
    for h in range(4):
        for d in range(16):
            Rda[32 * h + d, 32 * h + d + 16] = -1.0
            Rda[32 * h + d + 16, 32 * h + d] = 1.0
    Rmla = np.zeros((128, 128), np.float32)
    for h in range(2):
        for d in range(8):
            Rmla[64 * h + 32 + d, 64 * h + 40 + d] = -1.0
            Rmla[64 * h + 40 + d, 64 * h + 32 + d] = 1.0
    cst = np.concatenate([ident, bo32, bo64, Rda.T.copy(), Rmla.T.copy()], axis=1)
    def tables(rot_dim):
        rows = NLAT // 64
        row = np.repeat(np.arange(rows, dtype=np.float32), 64)
        col = np.tile(np.arange(64, dtype=np.float32), rows)
        n_freq = rot_dim // 4
        inv = (10000.0 ** (-np.arange(n_freq, dtype=np.float32) / n_freq)).astype(np.float32)
        ang = np.concatenate([row[:, None] * inv, col[:, None] * inv], axis=-1)
        return np.cos(ang).astype(np.float32), np.sin(ang).astype(np.float32)
    c, s = tables(32)
    da_cos = np.ones((128, NT), np.float32)
    da_sin = np.zeros((128, NT), np.float32)
    for h in range(4):
        for d in range(32):
            da_cos[32 * h + d, NCTX:] = c[:, d % 16]
            da_sin[32 * h + d, NCTX:] = s[:, d % 16]
    c, s = tables(16)
    ml_cos = np.ones((128, NT), np.float32)
    ml_sin = np.zeros((128, NT), np.float32)
    for h in range(2):
        for d in range(16):
            ml_cos[64 * h + 32 + d, NCTX:] = c[:, d % 8]
            ml_sin[64 * h + 32 + d, NCTX:] = s[:, d % 8]
    return dict(cst=cst, da_cos=da_cos, da_sin=da_sin, ml_cos=ml_cos, ml_sin=ml_sin)


def fm_cols(v, ntile):
    return np.ascontiguousarray(np.asarray(v, np.float32).reshape(ntile, 128).T)


def build(stages, dbg_names=()):
    nc = bass.Bass("TRN2", target_bir_lowering=False)
    k = K(nc)
    P = k.P
    top = ExitStack()
    xin = k.din("xin", [NT, D])
    c2T = k.din("c2T", [128, 8, 2])
    cst_d = k.din("cst", [128, 640])
    da_cos_d = k.din("da_cos", [128, NT]); da_sin_d = k.din("da_sin", [128, NT])
    ml_cos_d = k.din("ml_cos", [128, NT]); ml_sin_d = k.din("ml_sin", [128, NT])
    w_ada = k.din("w_ada", [DEPTH, D, 6 * D])
    bada_d = k.din("bada", [DEPTH, 128, 48])
    gmix_d = k.din("gmix", [DEPTH, 128, 8]); gffn_d = k.din("gffn", [DEPTH, 128, 8])
    w_da_qk = k.din("w_da_qk", [DEPTH, D, 768]); w_da_v = k.din("w_da_v", [DEPTH, D, 256])
    da_g_d = k.din("da_g", [DEPTH, 128, 2])
    da_lam_d = k.din("da_lam", [DEPTH, 128, 128])
    da_sub_d = k.din("da_sub", [DEPTH, 128, 64])
    w_mla_c = k.din("w_mla_c", [DEPTH, D, 384]); w_mla_kr = k.din("w_mla_kr", [DEPTH, D, 128])
    w_uq_d = k.din("w_uq", [DEPTH, 128, 2, 256]); w_ukvk_d = k.din("w_ukvk", [DEPTH, 128, 256]); w_ukvv_d = k.din("w_ukvv", [DEPTH, 128, 256])
    mla_gc_d = k.din("mla_gc", [DEPTH, 128, 3])
    mla_g_d = k.din("mla_g", [DEPTH, 128, 2])
    w_gates_d = k.din("w_gates", [DEPTH, 8, 128, 4096]); w_branch_d = k.din("w_branch", [DEPTH, 128, 8192]); w_out_d = k.din("w_out", [DEPTH, D, D])
    router_w_d = k.din("router_w", [D, 16]); router_b_d = k.din("router_b", [128, 16]); sel_d = k.din("sel", [16, 16, 128])
    exp_g_d = k.din("exp_w_gate", [DEPTH, 16, D, 512]); exp_u_d = k.din("exp_w_up", [DEPTH, 16, D, 512]); exp_d_d = k.din("exp_w_down", [DEPTH, 16, 512, D])
    w_s5_d = k.din("w_s5", [DEPTH, D, 256])
    s5B_d = k.din("s5B", [DEPTH, 2, 2, 128, 1024])
    s5C_d = k.din("s5C", [DEPTH, 2, 2, 128, 8, 128])
    s5tm_d = k.din("s5tm", [DEPTH, 2, 3, 128, 1024])
    s5fm_d = k.din("s5fm", [DEPTH, 2, 128, 3, 8])
    s5d_d = k.din("s5d", [DEPTH, 128, 2]); s5bg_d = k.din("s5bg", [DEPTH, 128, 2])
    w_glu_d = k.din("w_glu", [DEPTH, 256, 256])
    posc_d = k.din("posc", [2, 128, 2]); posr_d = k.din("posr", [2, 128, 128]); tri_d = k.din("tri", [2, 128, 128])
    w_rw_d = k.din("w_rw", [DEPTH, D, 1024]); rw_mu_d = k.din("rw_mu_row", [DEPTH, 128, 1024])
    rw_cols_d = k.din("rw_cols", [DEPTH, 128, 2, 9])
    rw_w0row_d = k.din("rw_w0row", [DEPTH, 2, 1, 256])
    rw_w1_d = k.din("rw_w1", [DEPTH, 2, 256, 32]); rw_w2_d = k.din("rw_w2", [DEPTH, 2, 32, 256])
    rw_a1_d = k.din("rw_a1", [DEPTH, 2, 256, 32]); rw_a2_d = k.din("rw_a2", [DEPTH, 2, 32, 256])
    rw_g1_d = k.din("rw_g1", [DEPTH, 256, 64]); rw_g2_d = k.din("rw_g2", [DEPTH, 64, 256])
    rw_tri_d = k.din("rw_tri", [2, 128, 2, 128]); rw_ind_d = k.din("rw_ind", [128, 2])
    rw_m1_d = k.din("rw_m1", [2, 128, 256]); rw_mnt_d = k.din("rw_mnt", [2, 128, 128])
    out_d = nc.dram_tensor("out", [NLAT, D], F32, kind="ExternalOutput").ap()
    dbg_out = {}

    xT, b_xT = k.sb(top, [128, 8, NT], F32, "xT")
    cstf, b_cstf = k.sb(top, [128, 640], F32, "cstf")
    cstb, b_cstb = k.sb(top, [128, 640], BF16, "cstb")
    onesb, b_ones = k.sb(top, [128, 128], BF16, "ones")
    k.eps_col, k.b_const = k.sb(top, [128, 1], F32, "eps")
    MOD, b_MOD = k.sb(top, [128, DEPTH, 6, 8, 2], F32, "MOD")
    AB, b_AB = k.sb(top, [128, DEPTH, 2, 2, 8, 2], F32, "AB")
    rstd_b, b_rstd = k.sb(top, [128, NT], F32, "rstd")
    yall, b_yall = k.sb(top, [128, 4, 2, NT], BF16, "yall")
    yT = []
    for i in range(4):
        yT.append((yall[:, i], Buf(f"y{i}")))
    k.memset("dve", k.eps_col[:], EPS, [k.b_const])
    k.memset("dve", onesb[:], 1.0, [b_ones])
    k.load("sp", cstf[:], cst_d, [b_cstf])
    k.copy("dve", cstb[:], cstf[:], [b_cstf], [b_cstb])
    identf = cstf[:, 0:128]
    identb = cstb[:, 0:128]
    bo32 = cstb[:, 128:256]; bo64 = cstb[:, 256:384]; Rda = cstb[:, 384:512]; Rmla = cstb[:, 512:640]

    def dump(name, ap_sb, buf, shape, dtype=F32):
        if name in dbg_names:
            d = nc.dram_tensor("dbg_" + name, list(shape), dtype, kind="ExternalOutput").ap()
            dbg_out[name] = d
            P.dma("sp", d, ap_sb, reads=[buf])
            P.barrier()

    with ExitStack() as st:
        xs = [k.sb(st, [128, D], F32, "xstage") for _ in range(2)]
        for tt in range(NTT):
            xt, xb = xs[tt % 2]
            k.load("sp", xt[:], xin[tt * 128:(tt + 1) * 128, :], [xb])
            for half in range(2):
                bk = k.bank()
                for j in range(4):
                    kt = half * 4 + j
                    P.op("pe", lambda e: e.transpose(k.ps[bk][:, j * 128:(j + 1) * 128], xt[:, kt * 128:(kt + 1) * 128], identf),
                         reads=[xb, b_cstf], writes=[k.pb[bk]], inc=(j == 3))
                k.copy("dve" if half == 0 else "act", xT[:, half * 4:half * 4 + 4, tt * 128:(tt + 1) * 128],
                       k.ps[bk][:].rearrange("p (j t) -> p j t", j=4), [k.pb[bk]], [b_xT])
        scT, b_scT = k.sb(st, [128, 8, 2], F32, "scT")
        scb, b_scb = k.sb(st, [128, 8, 2], BF16, "scb")
        bada, b_bada = k.sb(st, [128, DEPTH, 48], F32, "bada")
        gm, b_gm = k.sb(st, [128, DEPTH, 2, 8], F32, "gm")
        k.load("sp", scT[:], c2T, [b_scT])
        k.act(scb[:], scT[:], AF.Silu, [b_scT], [b_scb])
        for l in range(DEPTH):
            k.load("sp", bada[:, l, :], bada_d[l], [b_bada])
            k.load("sp", gm[:, l, 0, :], gmix_d[l], [b_gm])
            k.load("sp", gm[:, l, 1, :], gffn_d[l], [b_gm])
        wa = [k.sb(st, [128, 8, 1024], BF16, "wada") for _ in range(2)]
        ci = 0
        for l in range(DEPTH):
            for ch in range(6):
                wt, wb = wa[ci % 2]; ci += 1
                k.load("pool", wt[:], w_ada[l, :, ch * 1024:(ch + 1) * 1024].rearrange("(kt p) n -> p kt n", p=128), [wb])
                bk = k.bank()
                for ft in range(8):
                    for kt in range(8):
                        k.mm(bk, k.ps[bk][:, ft * 2:ft * 2 + 2], wt[:, kt, ft * 128:(ft + 1) * 128], scb[:, kt, :],
                             kt == 0, kt == 7, [wb, b_scb])
                k.tt("dve", MOD[:, l, ch, :, :], k.ps[bk][:, 0:16].rearrange("p (f j) -> p f j", j=2),
                     bada[:, l, ch * 8:(ch + 1) * 8].unsqueeze(2).to_broadcast([128, 8, 2]), ALU.add,
                     [k.pb[bk], b_bada], [b_MOD])
        for l in range(DEPTH):
            for m in range(2):
                sh, sc = (0, 1) if m == 0 else (3, 4)
                P.op("dve", lambda e: e.scalar_tensor_tensor(out=AB[:, l, m, 0, :, :], in0=MOD[:, l, sc, :, :], scalar=1.0,
                                                             in1=gm[:, l, m, :].unsqueeze(2).to_broadcast([128, 8, 2]),
                                                             op0=ALU.add, op1=ALU.mult),
                     reads=[b_MOD, b_gm], writes=[b_AB])
                k.copy("dve", AB[:, l, m, 1, :, :], MOD[:, l, sh, :, :], [b_MOD], [b_AB])
        P.barrier()
    dump("xT", xT[:], b_xT, [128, 8, NT])
    dump("MOD", MOD[:], b_MOD, [128, DEPTH, 6, 8, 2])

    def jof(g):
        return 1 if g == 0 else 0

    def rms_stats(st):
        sq = [k.sb(st, [128, 512], BF16, "sq") for _ in range(2)]
        lnt, b_lnt = k.sb(st, [128, 512], F32, "lnt")
        i = 0
        for (c0, n) in GROUPS:
            bk = k.bank()
            for kt in range(8):
                s, sbf = sq[i % 2]; i += 1
                k.act(s[:, :n], xT[:, kt, c0:c0 + n], AF.Square, [b_xT], [sbf])
                k.mm(bk, k.ps[bk][:, :n], onesb[:], s[:, :n], kt == 0, kt == 7, [sbf, b_ones])
            k.rstd_from_ss(rstd_b[:, c0:c0 + n], k.ps[bk][:, :n], bk, 1.0 / D, lnt[:, :n], b_lnt, [b_rstd])

    def h_group(l, m, g, hg, hb, tmp_, tmpb_):
        c0, n = GROUPS[g]
        j = jof(g)
        for kt in range(8):
            if isinstance(tmp_, list):
                tmp, tmpb = tmp_[kt % 2]
            else:
                tmp, tmpb = tmp_, tmpb_
            k.tt("dve", tmp[:, :n], xT[:, kt, c0:c0 + n], rstd_b[:, c0:c0 + n], ALU.mult, [b_xT, b_rstd], [tmpb])
            P.op("act", lambda e: e.activation(out=hg[:, kt, :n], in_=tmp[:, :n], func=AF.Identity,
                                               scale=AB[:, l, m, 0, kt, j:j + 1], bias=AB[:, l, m, 1, kt, j:j + 1]),
                 reads=[tmpb, b_AB], writes=[hb])

    def load_w(q, st, src, ncols, name):
        wt, wb = k.sb(st, [128, 8, ncols], BF16, name)
        k.load(q, wt[:], src.rearrange("(kt p) n -> p kt n", p=128), [wb])
        return wt, wb

    def proj(bk, wt, wb, col0, hg, hb, n, start=True, stop=True):
        for kt in range(8):
            k.mm(bk, k.ps[bk][:, :n], wt[:, kt, col0:col0 + 128], hg[:, kt, :n], start and kt == 0, stop and kt == 7, [wb, hb])

    def headnorm_rope(st, src_bk, n, c0, blockones, inv_dim, gcol, gbuf, Rm, cos_d, sin_d, dst, dstb, scr):
        sq, b_sq, rs, b_rs, lnt, b_lnt, qn, b_qn, ct, b_ct, sn, b_sn, t1, b_t1, t2, b_t2 = scr
        src = k.ps[src_bk][:, :n]
        k.act(sq[:, :n], src, AF.Square, [k.pb[src_bk]], [b_sq])
        b2 = k.bank()
        k.mm(b2, k.ps[b2][:, :n], blockones, sq[:, :n], True, True, [b_sq, b_cstb])
        k.rstd_from_ss(rs[:, :n], k.ps[b2][:, :n], b2, inv_dim, lnt[:, :n], b_lnt, [b_rs])
        k.stt(qn[:, :n], src, gcol, rs[:, :n], ALU.mult, ALU.mult, [k.pb[src_bk], gbuf, b_rs], [b_qn])
        k.load("sp", ct[:, :n], cos_d[:, c0:c0 + n], [b_ct])
        k.load("sp", sn[:, :n], sin_d[:, c0:c0 + n], [b_sn])
        b3 = k.bank()
        k.mm(b3, k.ps[b3][:, :n], Rm, qn[:, :n], True, True, [b_qn, b_cstb])
        k.tt("pool", t1[:, :n], qn[:, :n], ct[:, :n], ALU.mult, [b_qn, b_ct], [b_t1])
        k.tt("dve", t2[:, :n], k.ps[b3][:, :n], sn[:, :n], ALU.mult, [k.pb[b3], b_sn], [b_t2])
        k.tt("pool", dst, t1[:, :n], t2[:, :n], ALU.add, [b_t1, b_t2], [dstb])

    def norm_scratch(st):
        out = []
        for nm, dt in (("sq", BF16), ("rs", F32), ("lnt", F32), ("qn", BF16), ("ct", F32), ("sn", F32), ("t1", F32), ("t2", F32)):
            t, b = k.sb(st, [128, 512], dt, nm)
            out += [t, b]
        return out

    def attention(st, qT, b_q, kT, b_k, V, b_V, heads, scale, epilogue, skip_ctx=False):
        Et = [k.sb(st, [128, 512], BF16, "E") for _ in range(4)]
        sbanks = [0, 1, 2, 3]
        obanks = [4, 5, 6, 7]
        assert len(heads) % 2 == 0
        steps = []
        for g, (c0, n) in enumerate(GROUPS):
            if skip_ctx and g == 0:
                continue
            ktiles = [0, 1] if g == 0 else list(range(NTT))
            for hp in range(len(heads) // 2):
                for ki, kt in enumerate(ktiles):
                    steps.append((g, c0, n, hp, ki, kt, len(ktiles)))

        def qk_exp(si):
            g, c0, n, hp, ki, kt, nk = steps[si]
            outs = []
            for m in range(2):
                tile, pb, Kd, vh = heads[2 * hp + m]
                sbk = sbanks[(2 * si + m) % 4]
                k.mm(sbk, k.ps[sbk][:, :n], kT[pb:pb + Kd, tile, kt * 128:(kt + 1) * 128], qT[pb:pb + Kd, tile, c0:c0 + n],
                     True, True, [b_k, b_q])
                outs.append(sbk)
            res = []
            for m in range(2):
                sbk = outs[m]
                E, Eb = Et[(2 * si + m) % 4]
                k.act(E[:, :n], k.ps[sbk][:, :n], AF.Exp, [k.pb[sbk]], [Eb], scale=scale)
                res.append((E, Eb))
            return res

        oi = 0
        obs = None
        nxt = qk_exp(0)
        for si, (g, c0, n, hp, ki, kt, nk) in enumerate(steps):
            cur = nxt
            if si + 1 < len(steps):
                nxt = qk_exp(si + 1)
            nq = n // 128
            if ki == 0:
                obs = [obanks[(2 * oi) % 4], obanks[(2 * oi + 1) % 4]]; oi += 1
            for m in range(2):
                E, Eb = cur[m]
                vh = heads[2 * hp + m][3]
                ob = obs[m]
                for qi in range(nq):
                    P.op("pe", lambda e: e.matmul(k.ps[ob][:, qi * 65:(qi + 1) * 65], lhsT=E[:, qi * 128:(qi + 1) * 128],
                                                  rhs=V[:, kt, vh, :], start=(ki == 0 and qi == 0), stop=(ki == nk - 1),
                                                  skip_group_check=True),
                         reads=[Eb, b_V], writes=[k.pb[ob]], inc=(qi == nq - 1))
            if ki == nk - 1:
                for m in range(2):
                    epilogue(g, c0, nq, 2 * hp + m, obs[m])

    def transpose_out(ytok, b_ytok, nq, c0, ydst, b_ydst):
        for qi in range(nq):
            bk = k.bank()
            pv = k.ps[bk][:].bitcast(BF16)
            for tile in range(2):
                P.op("pe", lambda e: e.transpose(pv[:, tile * 128:(tile + 1) * 128], ytok[:, qi, tile * 128:(tile + 1) * 128], identb),
                     reads=[b_ytok, b_cstb], writes=[k.pb[bk]], inc=(tile == 1))
            k.copy("dve", ydst[:, :, c0 + qi * 128:c0 + (qi + 1) * 128], pv[:, 0:256].rearrange("p (j t) -> p j t", j=2),
                   [k.pb[bk]], [b_ydst])

    def da_mixer(l):
        lam_init = 0.8 - 0.6 * math.exp(-0.3 * l)
        ydst, b_ydst = yT[0]
        with ExitStack() as st:
            qT, b_q = k.sb(st, [128, 3, NT], BF16, "daq")
            kT, b_k = k.sb(st, [128, 3, NT], BF16, "dak")
            V, b_V = k.sb(st, [128, NTT, 4, 65], BF16, "dav")
            gcol, b_g = k.sb(st, [128, 2], F32, "dag")
            lam, b_lam = k.sb(st, [128, 128], F32, "dalam")
            lt, b_lt = k.sb(st, [128, 8], F32, "dalt")
            gsub, b_gsub = k.sb(st, [128, 64], F32, "dagsub")
            k.load("sp", gcol[:], da_g_d[l], [b_g])
            k.load("sp", lam[:], da_lam_d[l], [b_lam])
            k.load("sp", gsub[:], da_sub_d[l], [b_gsub])
            k.ts("dve", gsub[:], gsub[:], 1.0 - lam_init, None, ALU.mult, None, [b_gsub], [b_gsub])
            k.tt("dve", lam[:, 0:32], lam[:, 0:32], lam[:, 32:64], ALU.mult, [b_lam], [b_lam])
            k.tt("dve", lam[:, 64:96], lam[:, 64:96], lam[:, 96:128], ALU.mult, [b_lam], [b_lam])
            P.op("dve", lambda e: e.tensor_reduce(out=lt[:, 0:1], in_=lam[:, 0:32], axis=AX.X, op=ALU.add), reads=[b_lam], writes=[b_lt])
            P.op("dve", lambda e: e.tensor_reduce(out=lt[:, 1:2], in_=lam[:, 64:96], axis=AX.X, op=ALU.add), reads=[b_lam], writes=[b_lt])
            k.act(lt[:, 2:4], lt[:, 0:2], AF.Exp, [b_lt], [b_lt])
            k.tt("dve", lt[:, 4:5], lt[:, 3:4], lt[:, 2:3], ALU.subtract, [b_lt], [b_lt])
            k.ts("dve", lt[:, 4:5], lt[:, 4:5], -lam_init, None, ALU.add, None, [b_lt], [b_lt])
            k.memset("pool", V[:, :, :, 64:65], 1.0, [b_V])
            with ExitStack() as s2:
                wqk, b_wqk = load_w("pool", s2, w_da_qk[l], 768, "wqk")
                wv, b_wv = load_w("pool", s2, w_da_v[l], 256, "wv")
                hgs = [k.sb(s2, [128, 8, 512], BF16, "hg") for _ in range(2)]
                tmp, tmpb = k.sb(s2, [128, 512], F32, "htmp")
                scr = norm_scratch(s2)
                for g, (c0, n) in enumerate(GROUPS):
                    hg, hb = hgs[g % 2]
                    h_group(l, 0, g, hg, hb, [(tmp, tmpb), (scr[12], scr[13])], None)
                    for ti in range(6):
                        bk = k.bank()
                        proj(bk, wqk, b_wqk, ti * 128, hg, hb, n)
                        dst = (qT if ti < 3 else kT)
                        dstb = (b_q if ti < 3 else b_k)
                        headnorm_rope(s2, bk, n, c0, bo32, 1.0 / 32, gcol[:, (ti // 3):(ti // 3) + 1], b_g, Rda, da_cos_d, da_sin_d,
                                      dst[:, ti % 3, c0:c0 + n], dstb, scr)
                    for qi in range(n // 128):
                        tt_ = (c0 + qi * 128) // 128
                        bk = k.bank()
                        for kt in range(8):
                            k.mm(bk, k.ps[bk][:, 0:256], hg[:, kt, qi * 128:(qi + 1) * 128], wv[:, kt, :], kt == 0, kt == 7, [hb, b_wv])
                        k.copy("act", V[:, tt_, :, 0:64], k.ps[bk][:, 0:256].rearrange("p (h d) -> p h d", h=4), [k.pb[bk]], [b_V])
                P.barrier()
            dump("da_q", qT[:], b_q, [128, 3, NT], BF16)
            dump("da_k", kT[:], b_k, [128, 3, NT], BF16)
            dump("da_v", V[:], b_V, [128, NTT, 4, 65], BF16)
            with ExitStack() as s3:
                ytok, b_ytok = k.sb(s3, [128, 4, 256], BF16, "ytok")
                o0, b_o0 = k.sb(s3, [128, 4, 64], F32, "o0")
                dd, b_dd = k.sb(s3, [128, 4, 64], F32, "dd")
                junk, b_junk = k.sb(s3, [128, 64], F32, "junk")
                rc, b_rc = k.sb(s3, [128, 4, 4], F32, "rc")
                lnt, b_lnt = k.sb(s3, [128, 4], F32, "lnt2")
                state = {}

                def epi(g, c0, nq, hidx, ob):
                    h, m = hidx // 2, hidx % 2
                    if m == 0:
                        state["ob0"] = ob
                        return
                    ob0 = state["ob0"]
                    O0 = k.ps[ob0][:, 0:nq * 65].rearrange("p (q c) -> p q c", c=65)
                    O1 = k.ps[ob][:, 0:nq * 65].rearrange("p (q c) -> p q c", c=65)
                    P.op("dve", lambda e: e.reciprocal(out=rc[:, 0, 0:nq], in_=O0[:, :, 64]), reads=[k.pb[ob0]], writes=[b_rc])
                    P.op("dve", lambda e: e.reciprocal(out=rc[:, 1, 0:nq], in_=O1[:, :, 64]), reads=[k.pb[ob]], writes=[b_rc])
                    k.ts("dve", rc[:, 1, 0:nq], rc[:, 1, 0:nq], lt[:, 4:5], None, ALU.mult, None, [b_rc, b_lt], [b_rc])
                    for qi in range(nq):
                        k.ts("dve", o0[:, qi, :], O0[:, qi, 0:64], rc[:, 0, qi:qi + 1], None, ALU.mult, None, [k.pb[ob0], b_rc], [b_o0])
                        k.stt(dd[:, qi, :], O1[:, qi, 0:64], rc[:, 1, qi:qi + 1], o0[:, qi, :], ALU.mult, ALU.add,
                              [k.pb[ob], b_rc, b_o0], [b_dd])
                        P.op("act", lambda e: e.activation(out=junk[:], in_=dd[:, qi, :], func=AF.Square, accum_out=rc[:, 2, qi:qi + 1]),
                             reads=[b_dd], writes=[b_junk, b_rc])
                    P.op("act", lambda e: e.activation(out=lnt[:, 0:nq], in_=rc[:, 2, 0:nq], func=AF.Ln, scale=1.0 / 64, bias=k.eps_col[:]),
                         reads=[b_rc, k.b_const], writes=[b_lnt])
                    k.act(rc[:, 3, 0:nq], lnt[:, 0:nq], AF.Exp, [b_lnt], [b_rc], scale=-0.5)
                    for qi in range(nq):
                        k.stt(ytok[:, qi, h * 64:(h + 1) * 64], dd[:, qi, :], rc[:, 3, qi:qi + 1], gsub[:], ALU.mult, ALU.mult,
                              [b_dd, b_rc, b_gsub], [b_ytok])
                    if h == 3:
                        k.rrset = [0, 1, 2, 3]
                        transpose_out(ytok, b_ytok, nq, c0, ydst, b_ydst)
                        k.rrset = list(range(8))

                heads = [(j // 3, 32 * (j % 3), 32, j // 2) for j in range(8)]
                attention(s3, qT, b_q, kT, b_k, V, b_V, heads, 32 ** -0.5, epi, skip_ctx=(l == DEPTH - 1))
                P.barrier()
        dump("ya", ydst[:], b_ydst, [128, 2, NT], BF16)

    def mla_mixer(l):
        ydst, b_ydst = yT[2]
        with ExitStack() as st:
            qT, b_q = k.sb(st, [128, 2, NT], BF16, "mlq")
            kT, b_k = k.sb(st, [128, 2, NT], BF16, "mlk")
            V, b_V = k.sb(st, [128, NTT, 4, 65], BF16, "mlv")
            gcol, b_g = k.sb(st, [128, 2], F32, "mlg")
            gc, b_gc = k.sb(st, [128, 3], F32, "mlgc")
            k.load("sp", gcol[:], mla_g_d[l], [b_g])
            k.load("sp", gc[:], mla_gc_d[l], [b_gc])
            k.memset("pool", V[:, :, :, 64:65], 1.0, [b_V])
            with ExitStack() as s2:
                wc, b_wc = load_w("pool", s2, w_mla_c[l], 384, "wc")
                wkr, b_wkr = load_w("pool", s2, w_mla_kr[l], 128, "wkr")
                wf, b_wf = k.sb(s2, [128, 4, 256], F32, "wf")
                wuq, b_wuq = k.sb(s2, [128, 2, 256], BF16, "wuq")
                wkk, b_wkk = k.sb(s2, [128, 256], BF16, "wkk")
                wvv, b_wvv = k.sb(s2, [128, 256], BF16, "wvv")
                k.load("sp", wf[:, 0:2, :], w_uq_d[l], [b_wf])
                k.load("sp", wf[:, 2, :], w_ukvk_d[l], [b_wf])
                k.load("sp", wf[:, 3, :], w_ukvv_d[l], [b_wf])
                for kt in range(2):
                    k.ts("dve", wuq[:, kt, :], wf[:, kt, :], gc[:, kt:kt + 1], None, ALU.mult, None, [b_wf, b_gc], [b_wuq])
                k.ts("dve", wkk[:], wf[:, 2, :], gc[:, 2:3], None, ALU.mult, None, [b_wf, b_gc], [b_wkk])
                k.ts("dve", wvv[:], wf[:, 3, :], gc[:, 2:3], None, ALU.mult, None, [b_wf, b_gc], [b_wvv])
                hgs = [k.sb(s2, [128, 8, 512], BF16, "hg") for _ in range(2)]
                tmp, tmpb = [k.sb(s2, [128, 512], F32, "htmp") for _ in range(2)], None
                scr = norm_scratch(s2)
                sq2, b_sq2 = k.sb(s2, [128, 2, 512], BF16, "sq2")
                rsq, b_rsq = k.sb(s2, [128, 512], F32, "rsq")
                lnq, b_lnq = k.sb(s2, [128, 512], F32, "lnq")
                cqn, b_cqn = k.sb(s2, [128, 2, 512], BF16, "cqn")
                ckvn, b_ckvn = k.sb(s2, [128, 512], BF16, "ckvn")
                for g, (c0, n) in enumerate(GROUPS):
                    hg, hb = hgs[g % 2]
                    h_group(l, 0, g, hg, hb, tmp, tmpb)
                    bA = k.bank(); proj(bA, wc, b_wc, 0, hg, hb, n)
                    bB = k.bank(); proj(bB, wc, b_wc, 128, hg, hb, n)
                    k.act(sq2[:, 0, :n], k.ps[bA][:, :n], AF.Square, [k.pb[bA]], [b_sq2])
                    k.act(sq2[:, 1, :n], k.ps[bB][:, :n], AF.Square, [k.pb[bB]], [b_sq2])
                    bS = k.bank()
                    k.mm(bS, k.ps[bS][:, :n], onesb[:], sq2[:, 0, :n], True, False, [b_sq2, b_ones])
                    k.mm(bS, k.ps[bS][:, :n], onesb[:], sq2[:, 1, :n], False, True, [b_sq2, b_ones])
                    k.rstd_from_ss(rsq[:, :n], k.ps[bS][:, :n], bS, 1.0 / 192, lnq[:, :n], b_lnq, [b_rsq])
                    k.tt("dve", cqn[:, 0, :n], k.ps[bA][:, :n], rsq[:, :n], ALU.mult, [k.pb[bA], b_rsq], [b_cqn])
                    k.tt("dve", cqn[:, 1, :n], k.ps[bB][:, :n], rsq[:, :n], ALU.mult, [k.pb[bB], b_rsq], [b_cqn])
                    bC = k.bank(); proj(bC, wc, b_wc, 256, hg, hb, n)
                    k.act(sq2[:, 0, :n], k.ps[bC][:, :n], AF.Square, [k.pb[bC]], [b_sq2])
                    bS = k.bank()
                    k.mm(bS, k.ps[bS][:, :n], onesb[:], sq2[:, 0, :n], True, True, [b_sq2, b_ones])
                    k.rstd_from_ss(rsq[:, :n], k.ps[bS][:, :n], bS, 1.0 / 128, lnq[:, :n], b_lnq, [b_rsq])
                    k.tt("dve", ckvn[:, :n], k.ps[bC][:, :n], rsq[:, :n], ALU.mult, [k.pb[bC], b_rsq], [b_ckvn])
                    for tile in range(2):
                        bk = k.bank()
                        k.mm(bk, k.ps[bk][:, :n], wuq[:, 0, tile * 128:(tile + 1) * 128], cqn[:, 0, :n], True, False, [b_wuq, b_cqn])
                        k.mm(bk, k.ps[bk][:, :n], wuq[0:64, 1, tile * 128:(tile + 1) * 128], cqn[0:64, 1, :n], False, True, [b_wuq, b_cqn])
                        headnorm_rope(s2, bk, n, c0, bo64, 1.0 / 48, gcol[:, 0:1], b_g, Rmla, ml_cos_d, ml_sin_d,
                                      qT[:, tile, c0:c0 + n], b_q, scr)
                    for tile in range(2):
                        bk = k.bank()
                        k.mm(bk, k.ps[bk][:, :n], wkk[:, tile * 128:(tile + 1) * 128], ckvn[:, :n], True, False, [b_wkk, b_ckvn])
                        for kt in range(8):
                            k.mm(bk, k.ps[bk][:, :n], wkr[:, kt, :], hg[:, kt, :n], False, kt == 7, [b_wkr, hb])
                        headnorm_rope(s2, bk, n, c0, bo64, 1.0 / 48, gcol[:, 1:2], b_g, Rmla, ml_cos_d, ml_sin_d,
                                      kT[:, tile, c0:c0 + n], b_k, scr)
                    for qi in range(n // 128):
                        tt_ = (c0 + qi * 128) // 128
                        bk = k.bank()
                        k.mm(bk, k.ps[bk][:, 0:256], ckvn[:, qi * 128:(qi + 1) * 128], wvv[:], True, True, [b_ckvn, b_wvv])
                        k.copy("act", V[:, tt_, :, 0:64], k.ps[bk][:, 0:256].rearrange("p (h d) -> p h d", h=4), [k.pb[bk]], [b_V])
                P.barrier()
            dump("ml_q", qT[:], b_q, [128, 2, NT], BF16)
            dump("ml_k", kT[:], b_k, [128, 2, NT], BF16)
            with ExitStack() as s3:
                ytok, b_ytok = k.sb(s3, [128, 4, 256], BF16, "ytok")
                rc, b_rc = k.sb(s3, [128, 4], F32, "rc")

                def epi(g, c0, nq, h, ob):
                    O = k.ps[ob][:, 0:nq * 65].rearrange("p (q c) -> p q c", c=65)
                    P.op("dve", lambda e: e.reciprocal(out=rc[:, 0:nq], in_=O[:, :, 64]), reads=[k.pb[ob]], writes=[b_rc])
                    for qi in range(nq):
                        k.ts("dve", ytok[:, qi, h * 64:(h + 1) * 64], O[:, qi, 0:64], rc[:, qi:qi + 1], None, ALU.mult, None,
                             [k.pb[ob], b_rc], [b_ytok])
                    if h == 3:
                        k.rrset = [0, 1, 2, 3]
                        transpose_out(ytok, b_ytok, nq, c0, ydst, b_ydst)
                        k.rrset = list(range(8))

                heads = [(h // 2, 64 * (h % 2), 48, h) for h in range(4)]
                attention(s3, qT, b_q, kT, b_k, V, b_V, heads, 48 ** -0.5, epi, skip_ctx=(l == DEPTH - 1))
                P.barrier()
        dump("yc", ydst[:], b_ydst, [128, 2, NT], BF16)

    def merge(l):
        with ExitStack() as st:
            wbr, b_wbr = k.sb(st, [128, 2, 4, D], BF16, "wbr")
            wout, b_wout = k.sb(st, [128, 8, D], BF16, "wout")
            wgs = [k.sb(st, [128, 8, 4, 128], BF16, "wg") for _ in range(2)]
            hgs = [k.sb(st, [128, 8, 512], BF16, "hg") for _ in range(2)]
            tmp, tmpb = k.sb(st, [128, 512], F32, "htmp")
            sig = [k.sb(st, [128, 512], F32, "sig") for _ in range(2)]
            prod = [k.sb(st, [128, 512], F32, "prod") for _ in range(2)]
            macc, b_macc = k.sb(st, [128, 512], F32, "macc")
            mbf, b_mbf = k.sb(st, [128, 8, 512], BF16, "mbf")
            wi = 0; si = 0
            for g, (c0, n) in enumerate(GROUPS):
                if l == DEPTH - 1 and g == 0:
                    continue
                j = jof(g)
                hg, hb = hgs[g % 2]
                h_group(l, 0, g, hg, hb, [(tmp, tmpb), prod[1]], None)
                for ct in range(8):
                    wg, b_wg = wgs[wi % 2]; wi += 1
                    k.load("pool", wg[:].rearrange("p a b c -> p (a b c)"), w_gates_d[l, ct], [b_wg])
                    if wi == 1:
                        k.load("pool", wbr[:].rearrange("p a b c -> p (a b c)"), w_branch_d[l], [b_wbr])
                    if wi == 3:
                        k.load("pool", wout[:], w_out_d[l].rearrange("(kt p) n -> p kt n", p=128), [b_wout])
                    for i in range(4):
                        bg = k.bank()
                        for kt in range(8):
                            k.mm(bg, k.ps[bg][:, :n], wg[:, kt, i, :], hg[:, kt, :n], kt == 0, kt == 7, [b_wg, hb])
                        sg, b_sg = sig[si % 2]; pr, b_pr = prod[si % 2]; si += 1
                        k.act(sg[:, :n], k.ps[bg][:, :n], AF.Sigmoid, [k.pb[bg]], [b_sg])
                        bp = k.bank()
                        for kt2 in range(2):
                            k.mm(bp, k.ps[bp][:, :n], wbr[:, kt2, i, ct * 128:(ct + 1) * 128], yT[i][0][:, kt2, c0:c0 + n],
                                 kt2 == 0, kt2 == 1, [b_wbr, yT[i][1]])
                        if i == 0:
                            k.tt("dve", macc[:, :n], k.ps[bp][:, :n], sg[:, :n], ALU.mult, [k.pb[bp], b_sg], [b_macc])
                        else:
                            k.tt("dve", pr[:, :n], k.ps[bp][:, :n], sg[:, :n], ALU.mult, [k.pb[bp], b_sg], [b_pr])
                            if i < 3:
                                k.tt("dve", macc[:, :n], macc[:, :n], pr[:, :n], ALU.add, [b_macc, b_pr], [b_macc])
                            else:
                                k.tt("dve", mbf[:, ct, :n], macc[:, :n], pr[:, :n], ALU.add, [b_macc, b_pr], [b_mbf])
                for co in range(8):
                    bo = k.bank()
                    for ct in range(8):
                        k.mm(bo, k.ps[bo][:, :n], wout[:, ct, co * 128:(co + 1) * 128], mbf[:, ct, :n], ct == 0, ct == 7, [b_wout, b_mbf])
                    k.stt(xT[:, co, c0:c0 + n], k.ps[bo][:, :n], MOD[:, l, 2, co, j:j + 1], xT[:, co, c0:c0 + n], ALU.mult, ALU.add,
                          [k.pb[bo], b_MOD, b_xT], [b_xT])
            P.barrier()
        dump("xmix", xT[:], b_xT, [128, 8, NT])

    def moe(l):
        fT = yall[:].rearrange("p i k t -> p (i k) t")
        b_fT = Buf("fT")
        with ExitStack() as st:
            rms_stats(st)
            P.barrier()
        with ExitStack() as st:
            combT, b_comb = k.sb(st, [16, NT], BF16, "combT")
            selb, b_sel = k.sb(st, [16, 16, 128], BF16, "selb")
            k.load("pool", selb[:], sel_d, [b_sel])
            wgs = [k.sb(st, [128, 8, 512], BF16, "ewg") for _ in range(2)]
            wus = [k.sb(st, [128, 8, 512], BF16, "ewu") for _ in range(2)]
            wds = [k.sb(st, [128, 4, D], BF16, "ewd") for _ in range(2)]

            def load_expert(e_):
                wg, b_wg = wgs[e_ % 2]; wu, b_wu = wus[e_ % 2]; wd, b_wd = wds[e_ % 2]
                k.load("pool", wg[:], exp_g_d[l, e_].rearrange("(kt p) n -> p kt n", p=128), [b_wg])
                k.load("pool", wu[:], exp_u_d[l, e_].rearrange("(kt p) n -> p kt n", p=128), [b_wu])
                k.load("pool", wd[:], exp_d_d[l, e_].rearrange("(kt p) n -> p kt n", p=128), [b_wd])
            load_expert(0)
            with ExitStack() as s2:
                tmps = [k.sb(s2, [128, 512], F32, "htmp") for _ in range(2)]
                f32s = [k.sb(s2, [128, 512], F32, "f32") for _ in range(2)]
                rw, b_rw = k.sb(s2, [128, 8, 16], F32, "rw")
                k.load("sp", rw[:], router_w_d.rearrange("(kt p) n -> p kt n", p=128), [b_rw])
                rb, b_rb = k.sb(s2, [128, 16], F32, "rb")
                k.load("sp", rb[:], router_b_d, [b_rb])
                lg, b_lg = k.sb(s2, [128, NTT, 16], F32, "lg")
                fi_ = 0
                skip0 = (l == DEPTH - 1)
                if skip0:
                    k.memset("dve", lg[:, 0:2, :], 0.0, [b_lg])
                for g in range(5):
                    if skip0 and g == 0:
                        continue
                    c0, n = GROUPS[g]
                    j = jof(g)
                    nq = n // 128
                    bl = k.bank()
                    for kt in range(8):
                        tmp, tmpb = tmps[kt % 2]
                        k.tt("dve", tmp[:, :n], xT[:, kt, c0:c0 + n], rstd_b[:, c0:c0 + n], ALU.mult, [b_xT, b_rstd], [tmpb])
                        P.op("act", lambda e: e.activation(out=fT[:, kt, c0:c0 + n], in_=tmp[:, :n], func=AF.Identity,
                                                           scale=AB[:, l, 1, 0, kt, j:j + 1], bias=AB[:, l, 1, 1, kt, j:j + 1]),
                             reads=[tmpb, b_AB], writes=[b_fT])
                        f32, b_f32 = f32s[fi_ % 2]; fi_ += 1
                        k.ts("pool", f32[:, :n], tmp[:, :n], AB[:, l, 1, 0, kt, j:j + 1], AB[:, l, 1, 1, kt, j:j + 1], ALU.mult, ALU.add,
                             [tmpb, b_AB], [b_f32])
                        for qi in range(nq):
                            P.op("pe", lambda e: e.matmul(k.ps[bl][:, qi * 16:(qi + 1) * 16], lhsT=f32[:, qi * 128:(qi + 1) * 128], rhs=rw[:, kt, :],
                                                          start=(kt == 0 and qi == 0), stop=(kt == 7), skip_group_check=True),
                                 reads=[b_f32, b_rw], writes=[k.pb[bl]], inc=(qi == nq - 1))
                    tt0 = c0 // 128
                    k.copy("dve", lg[:, tt0:tt0 + nq, :], k.ps[bl][:, 0:nq * 16].rearrange("p (q e) -> p q e", e=16), [k.pb[bl]], [b_lg])
                r = {}
                NR = NTT * 16
                for nm, wd_ in (("sc", NR), ("bi", NR), ("m1", NR // 4), ("eq", NR), ("bi2", NR), ("m2", NR // 4),
                                ("gs", NR // 4), ("gm", NTT), ("gsel", NR // 4), ("sel", NR), ("w", NR), ("ws", NTT), ("cmb", NR)):
                    r[nm] = k.sb(s2, [128, wd_], F32, "r_" + nm)
                v4 = lambda ap: ap.rearrange("p (g e) -> p g e", e=4)
                b4 = lambda ap: ap.unsqueeze(2).to_broadcast([128, NR // 4, 4])
                sc, b_sc = r["sc"]; bi, b_bi = r["bi"]; m1, b_m1 = r["m1"]; eq, b_eq = r["eq"]; bi2, b_bi2 = r["bi2"]
                m2, b_m2 = r["m2"]; gs, b_gs = r["gs"]; gm_, b_gm_ = r["gm"]; gsel, b_gsel = r["gsel"]; sel, b_sl = r["sel"]
                w_, b_w = r["w"]; ws, b_ws = r["ws"]; cmb, b_cmb = r["cmb"]
                k.act(sc[:], lg[:].rearrange("p t e -> p (t e)"), AF.Sigmoid, [b_lg], [b_sc])
                k.tt("dve", sc[:].rearrange("p (t e) -> p t e", e=16) if False else bi[:].rearrange("p (t e) -> p t e", e=16),
                     sc[:].rearrange("p (t e) -> p t e", e=16), rb[:].unsqueeze(1).to_broadcast([128, NTT, 16]), ALU.add, [b_sc, b_rb], [b_bi])
                P.op("dve", lambda e: e.tensor_reduce(out=m1[:], in_=v4(bi[:]), axis=AX.X, op=ALU.max), reads=[b_bi], writes=[b_m1])
                k.tt("dve", v4(eq[:]), v4(bi[:]), b4(m1[:]), ALU.is_equal, [b_bi, b_m1], [b_eq])
                k.stt(bi2[:], eq[:], -1e9, bi[:], ALU.mult, ALU.add, [b_eq, b_bi], [b_bi2])
                P.op("dve", lambda e: e.tensor_reduce(out=m2[:], in_=v4(bi2[:]), axis=AX.X, op=ALU.max), reads=[b_bi2], writes=[b_m2])
                k.tt("dve", gs[:], m1[:], m2[:], ALU.add, [b_m1, b_m2], [b_gs])
                P.op("dve", lambda e: e.tensor_reduce(out=gm_[:], in_=v4(gs[:]), axis=AX.X, op=ALU.max), reads=[b_gs], writes=[b_gm_])
                k.tt("dve", v4(gsel[:]), v4(gs[:]), gm_[:].unsqueeze(2).to_broadcast([128, NTT, 4]), ALU.is_equal, [b_gs, b_gm_], [b_gsel])
                k.tt("dve", v4(sel[:]), v4(bi[:]), b4(m2[:]), ALU.is_ge, [b_bi, b_m2], [b_sl])
                k.tt("dve", v4(sel[:]), v4(sel[:]), b4(gsel[:]), ALU.mult, [b_sl, b_gsel], [b_sl])
                k.tt("dve", w_[:], sc[:], sel[:], ALU.mult, [b_sc, b_sl], [b_w])
                P.op("dve", lambda e: e.tensor_reduce(out=ws[:], in_=w_[:].rearrange("p (t e) -> p t e", e=16), axis=AX.X, op=ALU.add),
                     reads=[b_w], writes=[b_ws])
                P.op("dve", lambda e: e.reciprocal(out=ws[:], in_=ws[:]), reads=[b_ws], writes=[b_ws])
                k.tt("dve", cmb[:].rearrange("p (t e) -> p t e", e=16), w_[:].rearrange("p (t e) -> p t e", e=16),
                     ws[:].unsqueeze(2).to_broadcast([128, NTT, 16]), ALU.mult, [b_w, b_ws], [b_cmb])
                for tq in range(0, NTT, 4):
                    nn = min(4, NTT - tq)
                    bt = k.bank()
                    for i_ in range(nn):
                        P.op("pe", lambda e: e.transpose(k.ps[bt][0:16, i_ * 128:(i_ + 1) * 128], cmb[:, (tq + i_) * 16:(tq + i_ + 1) * 16], identf),
                             reads=[b_cmb, b_cstf], writes=[k.pb[bt]], inc=(i_ == nn - 1))
                    k.copy("act", combT[:, tq * 128:(tq + nn) * 128], k.ps[bt][0:16, 0:nn * 128], [k.pb[bt]], [b_comb])
                P.barrier()
            dump("combT", combT[:], b_comb, [16, NT], BF16)
            cbs = [k.sb(st, [128, 512], BF16, "cbs") for _ in range(2)]
            sl = [k.sb(st, [128, 512], F32, "esl") for _ in range(2)]
            tl = [k.sb(st, [128, 512], F32, "etl") for _ in range(2)]
            aa = [k.sb(st, [128, 4, 512], BF16, "eaa") for _ in range(2)]
            ci = 0; fi = 0
            for e_ in range(16):
                wg, b_wg = wgs[e_ % 2]; wu, b_wu = wus[e_ % 2]; wd, b_wd = wds[e_ % 2]
                if e_ > 0:
                    load_expert(e_)
                for g, (c0, n) in enumerate(GROUPS):
                    if l == DEPTH - 1 and g == 0:
                        continue
                    j = jof(g)
                    cb, b_cb = cbs[ci % 2]; a_, b_a = aa[ci % 2]; ci += 1
                    bc = k.bank()
                    k.mm(bc, k.ps[bc][:, :n], selb[:, e_, :], combT[:, c0:c0 + n], True, True, [b_sel, b_comb])
                    k.copy("act", cb[:, :n], k.ps[bc][:, :n], [k.pb[bc]], [b_cb])
                    for fj in range(4):
                        bg = k.bank()
                        for kt in range(8):
                            k.mm(bg, k.ps[bg][:, :n], wg[:, kt, fj * 128:(fj + 1) * 128], fT[:, kt, c0:c0 + n], kt == 0, kt == 7, [b_wg, b_fT])
                        bu = k.bank()
                        for kt in range(8):
                            k.mm(bu, k.ps[bu][:, :n], wu[:, kt, fj * 128:(fj + 1) * 128], fT[:, kt, c0:c0 + n], kt == 0, kt == 7, [b_wu, b_fT])
                        s_, b_s = sl[fi % 2]; t_, b_t = tl[fi % 2]; fi += 1
                        k.act(s_[:, :n], k.ps[bg][:, :n], AF.Silu, [k.pb[bg]], [b_s])
                        k.tt("dve", t_[:, :n], k.ps[bu][:, :n], s_[:, :n], ALU.mult, [k.pb[bu], b_s], [b_t])
                        k.tt("dve", a_[:, fj, :n], t_[:, :n], cb[:, :n], ALU.mult, [b_t, b_cb], [b_a])
                    for co in range(8):
                        bo = k.bank()
                        for fj in range(4):
                            k.mm(bo, k.ps[bo][:, :n], wd[:, fj, co * 128:(co + 1) * 128], a_[:, fj, :n], fj == 0, fj == 3, [b_wd, b_a])
                        k.stt(xT[:, co, c0:c0 + n], k.ps[bo][:, :n], MOD[:, l, 5, co, j:j + 1], xT[:, co, c0:c0 + n], ALU.mult, ALU.add,
                              [k.pb[bo], b_MOD, b_xT], [b_xT])
            P.barrier()
        dump("xout", xT[:], b_xT, [128, 8, NT])

    TWO_PI = 2.0 * math.pi

    def sincos(ang, b_ang, N, sc4, sin_out, cos_out, b_out):
        (kf, b_kf), (r_, b_r), (mk, b_mk), (sh, b_sh) = sc4
        ki = kf[:, :N].bitcast(I32)
        k.ts("dve", r_[:, :N], ang, 1.0 / TWO_PI, None, ALU.mult, None, [b_ang], [b_r])
        k.copy("dve", ki, r_[:, :N], [b_r], [b_kf])
        k.copy("dve", mk[:, :N], ki, [b_kf], [b_mk])
        k.stt(r_[:, :N], mk[:, :N], -TWO_PI, ang, ALU.mult, ALU.add, [b_mk, b_ang], [b_r])
        k.ts("dve", mk[:, :N], r_[:, :N], math.pi, -TWO_PI, ALU.is_gt, ALU.mult, [b_r], [b_mk])
        k.tt("dve", r_[:, :N], r_[:, :N], mk[:, :N], ALU.add, [b_r, b_mk], [b_r])
        k.ts("dve", mk[:, :N], r_[:, :N], -math.pi, TWO_PI, ALU.is_lt, ALU.mult, [b_r], [b_mk])
        k.tt("dve", r_[:, :N], r_[:, :N], mk[:, :N], ALU.add, [b_r, b_mk], [b_r])
        k.ts("dve", r_[:, :N], r_[:, :N], math.pi, -math.pi, ALU.min, ALU.max, [b_r], [b_r])
        k.act(sin_out, r_[:, :N], AF.Sin, [b_r], [b_out])
        k.act(sh[:, :N], r_[:, :N], AF.Sin, [b_r], [b_sh], scale=0.5)
        k.tt("dve", sh[:, :N], sh[:, :N], sh[:, :N], ALU.mult, [b_sh], [b_sh])
        k.ts("dve", cos_out, sh[:, :N], -2.0, 1.0, ALU.mult, ALU.add, [b_sh], [b_out])

    def s5_mixer(l):
        ydst, b_ydst = yT[1]
        with ExitStack() as st:
            uT, b_u = k.sb(st, [128, 2, NT], BF16, "s5u")
            yacc, b_yacc = k.sb(st, [128, 2, NT], F32, "s5y")
            dcol, b_dcol = k.sb(st, [128, 2], F32, "s5d")
            k.load("sp", dcol[:], s5d_d[l], [b_dcol])
            with ExitStack() as s2:
                ws, b_ws = load_w("pool", s2, w_s5_d[l], 256, "ws5")
                hgs = [k.sb(s2, [128, 8, 512], BF16, "hg") for _ in range(2)]
                tmp, tmpb = [k.sb(s2, [128, 512], F32, "htmp") for _ in range(2)], None
                for g, (c0, n) in enumerate(GROUPS):
                    hg, hb = hgs[g % 2]
                    h_group(l, 0, g, hg, hb, tmp, tmpb)
                    for ti in range(2):
                        bk = k.bank()
                        proj(bk, ws, b_ws, ti * 128, hg, hb, n)
                        k.copy("act", uT[:, ti, c0:c0 + n], k.ps[bk][:, :n], [k.pb[bk]], [b_u])
                P.barrier()
            for dr in range(2):
                with ExitStack() as sd:
                    pr, b_pr = k.sb(sd, [128, 1024], F32, "pr"); pi_, b_pi = k.sb(sd, [128, 1024], F32, "pi")
                    qr, b_qr = k.sb(sd, [128, 1024], F32, "qr"); qi, b_qi = k.sb(sd, [128, 1024], F32, "qi")
                    Bb, b_Bb = k.sb(sd, [128, 2, 1024], BF16, "Bb")
                    Cb, b_Cb = k.sb(sd, [128, 3, 8, 128], BF16, "Cb")
                    triT, b_tri = k.sb(sd, [128, 2, 128], BF16, "triT")
                    trif, b_trif = k.sb(sd, [128, 128], F32, "trif")
                    posc, b_posc = k.sb(sd, [128, 2], F32, "posc")
                    posr, b_posr = k.sb(sd, [128, 128], F32, "posr")
                    fm, b_fm = k.sb(sd, [128, 3, 8], F32, "fm")
                    cc, b_cc = k.sb(sd, [128, 16, 8], F32, "cc")
                    A128, b_A128 = k.sb(sd, [128, 2, 8], F32, "A128")
                    k.load("pool", Bb[:], s5B_d[l, dr].rearrange("kt p n -> p kt n"), [b_Bb])
                    with ExitStack() as sc_:
                        Cf, b_Cf = k.sb(sc_, [128, 2, 8, 128], F32, "Cf")
                        k.load("sp", Cf[:, 0], s5C_d[l, dr, 0], [b_Cf]); k.load("sp", Cf[:, 1], s5C_d[l, dr, 1], [b_Cf])
                        k.copy("dve", Cb[:, 0], Cf[:, 0], [b_Cf], [b_Cb])
                        k.ts("dve", Cb[:, 1], Cf[:, 0], -1.0, None, ALU.mult, None, [b_Cf], [b_Cb])
                        k.ts("dve", Cb[:, 2], Cf[:, 1], -1.0, None, ALU.mult, None, [b_Cf], [b_Cb])
                        P.barrier()
                    k.load("sp", trif[:], tri_d[dr], [b_trif])
                    k.copy("dve", triT[:, 0, :], trif[:], [b_trif], [b_tri])
                    k.ts("dve", triT[:, 1, :], trif[:], -1.0, None, ALU.mult, None, [b_trif], [b_tri])
                    k.load("sp", posc[:], posc_d[dr], [b_posc]); k.load("sp", posr[:], posr_d[dr], [b_posr])
                    k.load("sp", fm[:], s5fm_d[l, dr], [b_fm])
                    with ExitStack() as sx:
                        sc4 = [k.sb(sx, [128, 1024], F32, "sc4") for _ in range(4)]
                        rho, b_rho = k.sb(sx, [128, 1024], F32, "rho"); th, b_th = k.sb(sx, [128, 1024], F32, "th")
                        ang, b_angb = k.sb(sx, [128, 1024], F32, "ang")
                        k.load("sp", rho[:], s5tm_d[l, dr, 0], [b_rho]); k.load("sp", th[:], s5tm_d[l, dr, 1], [b_th])
                        k.load("sp", ang[:], s5tm_d[l, dr, 2], [b_angb])
                        k.act(ang[:], ang[:], AF.Exp, [b_angb], [b_angb])
                        k.tt("dve", rho[:], rho[:], ang[:], ALU.mult, [b_rho, b_angb], [b_rho])
                        k.tt("dve", th[:], th[:], ang[:], ALU.mult, [b_th, b_angb], [b_th])
                        k.ts("dve", ang[:], th[:], posc[:, 0:1], None, ALU.mult, None, [b_th, b_posc], [b_angb])
                        sincos(ang[:], b_angb, 1024, sc4, pi_[:], pr[:], b_pr)
                        b_pi.w = b_pr.w
                        P.op("act", lambda e: e.activation(out=ang[:], in_=rho[:], func=AF.Exp, scale=posc[:, 0:1]), reads=[b_rho, b_posc], writes=[b_angb])
                        k.tt("dve", pr[:], pr[:], ang[:], ALU.mult, [b_pr, b_angb], [b_pr])
                        k.tt("dve", pi_[:], pi_[:], ang[:], ALU.mult, [b_pr, b_angb], [b_pr])
                        dtc = cc[:, 0, :]; rc_ = cc[:, 1, :]; tc_ = cc[:, 2, :]
                        k.act(dtc, fm[:, 2, :], AF.Exp, [b_fm], [b_cc])
                        k.tt("dve", rc_, fm[:, 0, :], dtc, ALU.mult, [b_fm, b_cc], [b_cc])
                        k.tt("dve", tc_, fm[:, 1, :], dtc, ALU.mult, [b_fm, b_cc], [b_cc])
                        k.copy("dve", ang[:, 0:8], tc_, [b_cc], [b_angb])
                        k.ts("dve", ang[:, 8:16], tc_, 128.0, None, ALU.mult, None, [b_cc], [b_angb])
                        sincos(ang[:, 0:16], b_angb, 16, sc4, th[:, 0:16], th[:, 16:32], b_th)
                        er = cc[:, 3, :]; e128 = cc[:, 4, :]
                        k.act(er, rc_, AF.Exp, [b_cc], [b_cc])
                        k.act(e128, rc_, AF.Exp, [b_cc], [b_cc], scale=128.0)
                        k.tt("dve", A128[:, 0, :], e128, th[:, 24:32], ALU.mult, [b_cc, b_th], [b_A128])
                        k.tt("dve", A128[:, 1, :], e128, th[:, 8:16], ALU.mult, [b_cc, b_th], [b_A128])
                        ar1 = cc[:, 5, :]; ai = cc[:, 6, :]; den = cc[:, 7, :]; cr = cc[:, 8, :]; ci = cc[:, 9, :]; t1 = cc[:, 10, :]
                        k.tt("dve", ar1, er, th[:, 16:24], ALU.mult, [b_cc, b_th], [b_cc])
                        k.ts("dve", ar1, ar1, -1.0, None, ALU.add, None, [b_cc], [b_cc])
                        k.tt("dve", ai, er, th[:, 0:8], ALU.mult, [b_cc, b_th], [b_cc])
                        k.tt("dve", den, fm[:, 0, :], fm[:, 0, :], ALU.mult, [b_fm], [b_cc])
                        k.tt("dve", t1, fm[:, 1, :], fm[:, 1, :], ALU.mult, [b_fm], [b_cc])
                        k.tt("dve", den, den, t1, ALU.add, [b_cc], [b_cc])
                        P.op("dve", lambda e: e.reciprocal(out=den, in_=den), reads=[b_cc], writes=[b_cc])
                        k.tt("dve", cr, ar1, fm[:, 0, :], ALU.mult, [b_cc, b_fm], [b_cc])
                        k.tt("dve", t1, ai, fm[:, 1, :], ALU.mult, [b_cc, b_fm], [b_cc])
                        k.tt("dve", cr, cr, t1, ALU.add, [b_cc], [b_cc])
                        k.tt("dve", cr, cr, den, ALU.mult, [b_cc], [b_cc])
                        k.tt("dve", ci, ai, fm[:, 0, :], ALU.mult, [b_cc, b_fm], [b_cc])
                        k.tt("dve", t1, ar1, fm[:, 1, :], ALU.mult, [b_cc, b_fm], [b_cc])
                        k.tt("dve", ci, ci, t1, ALU.subtract, [b_cc], [b_cc])
                        k.tt("dve", ci, ci, den, ALU.mult, [b_cc], [b_cc])
                        for i in range(8):
                            k.ts("dve", ang[:, i * 128:(i + 1) * 128], posr[:], cc[:, 2, i:i + 1], None, ALU.mult, None, [b_posr, b_cc], [b_angb])
                            P.op("act", lambda e: e.activation(out=rho[:, i * 128:(i + 1) * 128], in_=posr[:], func=AF.Exp, scale=cc[:, 1, i:i + 1]),
                                 reads=[b_posr, b_cc], writes=[b_rho])
                        sincos(ang[:], b_angb, 1024, sc4, qi[:], qr[:], b_qr)
                        k.tt("dve", qr[:], qr[:], rho[:], ALU.mult, [b_qr, b_rho], [b_qr])
                        k.tt("dve", qi[:], qi[:], rho[:], ALU.mult, [b_qr, b_rho], [b_qr])
                        for i in range(8):
                            sl_ = slice(i * 128, (i + 1) * 128)
                            k.ts("dve", th[:, sl_], qi[:, sl_], cc[:, 9, i:i + 1], None, ALU.mult, None, [b_qr, b_cc], [b_th])
                            k.ts("dve", ang[:, sl_], qr[:, sl_], cc[:, 9, i:i + 1], None, ALU.mult, None, [b_qr, b_cc], [b_angb])
                            k.stt(qr[:, sl_], qr[:, sl_], cc[:, 8, i:i + 1], th[:, sl_], ALU.mult, ALU.subtract, [b_qr, b_cc, b_th], [b_qr])
                            k.stt(qi[:, sl_], qi[:, sl_], cc[:, 8, i:i + 1], ang[:, sl_], ALU.mult, ALU.add, [b_qr, b_cc, b_angb], [b_qr])
                        P.barrier()
                    if l == 0 and dr == 0:
                        dump("s5pr", pr[:], b_pr, [128, 1024]); dump("s5pi", pi_[:], b_pr, [128, 1024])
                        dump("s5qr", qr[:], b_qr, [128, 1024]); dump("s5qi", qi[:], b_qr, [128, 1024])
                        dump("s5A128", A128[:], b_A128, [128, 2, 8])
                    with ExitStack() as sp_:
                        zp, _ = k.sb(sp_, [128, 4, 1024], BF16, "zp")
                        hp, _ = k.sb(sp_, [128, 4, 1024], BF16, "hp")
                        Dg, _ = k.sb(sp_, [128, 16, 128], BF16, "Dg")
                        zl, _ = k.sb(sp_, [128, 16], F32, "zl")
                        car, _ = k.sb(sp_, [128, 16], F32, "car")
                        ct_, _ = k.sb(sp_, [128, 4, 8], F32, "ctmp")
                        hb2 = lambda nm: [Buf(nm + "0"), Buf(nm + "1")]
                        b_zp, b_hp, b_DgT, b_zl, b_car, b_ct = hb2("zp"), hb2("hp"), hb2("Dg"), hb2("zl"), hb2("car"), hb2("ct")
                        k.memset("pool", Dg[:], 0.0, b_DgT)
                        order = [0, 1] + list(range(2, NTT)) if dr == 0 else [1, 0] + list(range(NTT - 1, 1, -1))
                        tl = 127 if dr == 0 else 0

                        def half_gen(kt, banks):
                            bre, bim, zre, zim = banks
                            cs = slice(kt * 512, (kt + 1) * 512)
                            i4 = slice(kt * 4, kt * 4 + 4); i4m = slice(8 + kt * 4, 8 + kt * 4 + 4)
                            for tt in order:
                                cols = slice(tt * 128, (tt + 1) * 128)
                                k.mm(bre, k.ps[bre][:, :], uT[:, kt, cols], Bb[:, kt, 0:512], True, True, [b_u, b_Bb])
                                k.mm(bim, k.ps[bim][:, :], uT[:, kt, cols], Bb[:, kt, 512:1024], True, True, [b_u, b_Bb])
                                yield
                                k.tt("dve", zp[:, 0, cs], k.ps[bre][:, :], pr[:, cs], ALU.mult, [k.pb[bre], b_pr], [b_zp[kt]])
                                k.tt("dve", zp[:, 1, cs], k.ps[bim][:, :], pi_[:, cs], ALU.mult, [k.pb[bim], b_pr], [b_zp[kt]])
                                k.tt("dve", zp[:, 2, cs], k.ps[bim][:, :], pr[:, cs], ALU.mult, [k.pb[bim], b_pr], [b_zp[kt]])
                                k.tt("dve", zp[:, 3, cs], k.ps[bre][:, :], pi_[:, cs], ALU.mult, [k.pb[bre], b_pr], [b_zp[kt]])
                                yield
                                for ii in range(4):
                                    i = kt * 4 + ii
                                    sl_ = slice(i * 128, (i + 1) * 128)
                                    oc = slice(ii * 128, (ii + 1) * 128)
                                    k.mm(zre, k.ps[zre][:, oc], zp[:, 0, sl_], triT[:, 0, :], True, False, [b_zp[kt], b_tri])
                                    k.mm(zre, k.ps[zre][:, oc], zp[:, 1, sl_], triT[:, 1, :], False, False, [b_zp[kt], b_tri])
                                    k.mm(zre, k.ps[zre][:, oc], Dg[:, i, :], onesb[:], False, True, [b_DgT[kt], b_ones])
                                    k.mm(zim, k.ps[zim][:, oc], zp[:, 2, sl_], triT[:, 0, :], True, False, [b_zp[kt], b_tri])
                                    k.mm(zim, k.ps[zim][:, oc], zp[:, 3, sl_], triT[:, 0, :], False, False, [b_zp[kt], b_tri])
                                    k.mm(zim, k.ps[zim][:, oc], Dg[:, 8 + i, :], onesb[:], False, True, [b_DgT[kt], b_ones])
                                yield
                                k.copy("act", zl[:, i4], k.ps[zre][:, :].rearrange("p (i t) -> p i t", t=128)[:, :, tl], [k.pb[zre]], [b_zl[kt]])
                                k.copy("act", zl[:, i4m], k.ps[zim][:, :].rearrange("p (i t) -> p i t", t=128)[:, :, tl], [k.pb[zim]], [b_zl[kt]])
                                zr_, zi_ = zl[:, i4], zl[:, i4m]
                                k.tt("dve", ct_[:, 0, i4], A128[:, 0, i4], zr_, ALU.mult, [b_A128, b_zl[kt]], [b_ct[kt]])
                                k.tt("dve", ct_[:, 1, i4], A128[:, 1, i4], zi_, ALU.mult, [b_A128, b_zl[kt]], [b_ct[kt]])
                                k.tt("dve", ct_[:, 2, i4], A128[:, 0, i4], zi_, ALU.mult, [b_A128, b_zl[kt]], [b_ct[kt]])
                                k.tt("dve", ct_[:, 3, i4], A128[:, 1, i4], zr_, ALU.mult, [b_A128, b_zl[kt]], [b_ct[kt]])
                                k.tt("dve", car[:, i4], ct_[:, 0, i4], ct_[:, 1, i4], ALU.subtract, [b_ct[kt]], [b_car[kt]])
                                k.tt("dve", car[:, i4m], ct_[:, 2, i4], ct_[:, 3, i4], ALU.add, [b_ct[kt]], [b_car[kt]])
                                for isl in (i4, i4m):
                                    k.tt("pool", Dg[:, isl, :], identf.unsqueeze(1).to_broadcast([128, 4, 128]),
                                         car[:, isl].unsqueeze(2).to_broadcast([128, 4, 128]), ALU.mult, [b_cstf, b_car[kt]], [b_DgT[kt]])
                                yield
                                k.tt("dve", hp[:, 0, cs], k.ps[zre][:, :], qr[:, cs], ALU.mult, [k.pb[zre], b_qr], [b_hp[kt]])
                                k.tt("dve", hp[:, 1, cs], k.ps[zim][:, :], qi[:, cs], ALU.mult, [k.pb[zim], b_qr], [b_hp[kt]])
                                k.tt("dve", hp[:, 2, cs], k.ps[zim][:, :], qr[:, cs], ALU.mult, [k.pb[zim], b_qr], [b_hp[kt]])
                                k.tt("dve", hp[:, 3, cs], k.ps[zre][:, :], qi[:, cs], ALU.mult, [k.pb[zre], b_qr], [b_hp[kt]])
                                yield
                                bk = bre
                                first = True
                                for ii in range(4):
                                    i = kt * 4 + ii
                                    sl_ = slice(i * 128, (i + 1) * 128)
                                    for (pi_x, ci_x) in ((0, 0), (1, 1), (2, 2), (3, 2)):
                                        last = (ii == 3 and pi_x == 3)
                                        k.mm(bk, k.ps[bk][:, 0:128], Cb[:, ci_x, i, :], hp[:, pi_x, sl_], first, last, [b_Cb, b_hp[kt]])
                                        first = False
                                if dr == 0:
                                    k.stt(yacc[:, kt, cols], uT[:, kt, cols], dcol[:, kt:kt + 1], k.ps[bk][:, 0:128], ALU.mult, ALU.add,
                                          [b_u, b_dcol, k.pb[bk]], [b_yacc])
                                else:
                                    k.tt("dve", yacc[:, kt, cols], k.ps[bk][:, 0:128], yacc[:, kt, cols], ALU.add, [k.pb[bk], b_yacc], [b_yacc])
                                yield

                        live = [half_gen(0, [0, 1, 2, 3]), half_gen(1, [4, 5, 6, 7])]
                        for _ in range(S5SKEW):
                            next(live[0])
                        while live:
                            for g_ in list(live):
                                try:
                                    next(g_)
                                except StopIteration:
                                    live.remove(g_)
                        P.barrier()
            dump("s5yacc", yacc[:], b_yacc, [128, 2, NT])
            with ExitStack() as s4:
                wgf, b_wgf = k.sb(s4, [128, 2, 256], F32, "wgf"); wgb, b_wgb = k.sb(s4, [128, 2, 256], BF16, "wgb")
                bg, b_bg = k.sb(s4, [128, 2], F32, "bglu")
                k.load("sp", wgf[:], w_glu_d[l].rearrange("(kt p) n -> p kt n", p=128), [b_wgf])
                k.copy("dve", wgb[:], wgf[:], [b_wgf], [b_wgb])
                k.load("sp", bg[:], s5bg_d[l], [b_bg])
                zT, b_zT = k.sb(s4, [128, 2, 512], BF16, "zT")
                x2, b_x2 = k.sb(s4, [128, 512], F32, "x2"); thh, b_thh = k.sb(s4, [128, 512], F32, "thh")
                sg, b_sg = k.sb(s4, [128, 512], F32, "sg")
                for g, (c0, n) in enumerate(GROUPS):
                    for kt in range(2):
                        x_ = yacc[:, kt, c0:c0 + n]
                        k.tt("dve", x2[:, :n], x_, x_, ALU.mult, [b_yacc], [b_x2])
                        k.ts("dve", x2[:, :n], x2[:, :n], 0.044715, 1.0, ALU.mult, ALU.add, [b_x2], [b_x2])
                        k.tt("dve", x2[:, :n], x2[:, :n], x_, ALU.mult, [b_x2, b_yacc], [b_x2])
                        k.act(thh[:, :n], x2[:, :n], AF.Tanh, [b_x2], [b_thh], scale=math.sqrt(2.0 / math.pi))
                        k.stt(thh[:, :n], thh[:, :n], 1.0, x_, ALU.add, ALU.mult, [b_thh, b_yacc], [b_thh])
                        k.ts("dve", zT[:, kt, :n], thh[:, :n], 0.5, None, ALU.mult, None, [b_thh], [b_zT])
                    for ct in range(2):
                        bk = k.bank()
                        for kt in range(2):
                            k.mm(bk, k.ps[bk][:, :n], wgb[:, kt, ct * 128:(ct + 1) * 128], zT[:, kt, :n], kt == 0, kt == 1, [b_wgb, b_zT])
                        P.op("act", lambda e: e.activation(out=sg[:, :n], in_=k.ps[bk][:, :n], func=AF.Sigmoid, bias=bg[:, ct:ct + 1]),
                             reads=[k.pb[bk], b_bg], writes=[b_sg])
                        k.tt("dve", ydst[:, ct, c0:c0 + n], zT[:, ct, :n], sg[:, :n], ALU.mult, [b_zT, b_sg], [b_ydst])
                P.barrier()
        dump("yb", ydst[:], b_ydst, [128, 2, NT], BF16)

    def rwkv_mixer(l):
        ydst, b_ydst = yT[3]
        xd, b_xd = ydst, Buf("xd")
        with ExitStack() as st:
            rS, b_r = yall[:, 0], Buf("rw_r")
            kS, b_k = yall[:, 1], Buf("rw_k")
            vS, b_v = yall[:, 2], Buf("rw_v")
            kkS, b_kk = k.sb(st, [128, 2, NT], BF16, "rw_kk")
            cols, b_cols = k.sb(st, [128, 2, 9], F32, "rw_cols")
            k.load("sp", cols[:], rw_cols_d[l], [b_cols])
            C_KK, C_KA, C_LNG, C_LNB, C_RK, C_W0, C_A0 = 0, 1, 2, 3, 4, 5, 7
            for half in range(2):
              with ExitStack() as s2:
                WA, b_WA = load_w("pool", s2, w_rw_d[l, :, half * 512:(half + 1) * 512], 512, "rwWA")
                WB, b_WB = k.sb(s2, [128, 8, 512], BF16, "rwWB")
                with ExitStack() as s3:
                    mu, b_mu = k.sb(s3, [128, 512], F32, "rwmu")
                    k.load("sp", mu[:], rw_mu_d[l, :, half * 512:(half + 1) * 512], [b_mu])
                    for kt in range(8):
                        k.stt(WB[:, kt, :], WA[:, kt, :], 0.5, mu[:], ALU.mult, ALU.mult, [b_WA, b_mu], [b_WB])
                    k.ts("dve", mu[:], mu[:], -1.0, 1.0, ALU.mult, ALU.add, [b_mu], [b_mu])
                    for kt in range(8):
                        k.tt("pool", WA[:, kt, :], WA[:, kt, :], mu[:], ALU.mult, [b_WA, b_mu], [b_WA])
                    P.barrier()
                hgxs = [k.sb(s2, [128, 8, 516], BF16, "hgx") for _ in range(2)]
                hsxs = [k.sb(s2, [128, 8, 512], BF16, "hsx") for _ in range(2)]
                tmps = [k.sb(s2, [128, 514], F32, "htmp") for _ in range(2)]
                sq, b_sq = k.sb(s2, [128, 512], BF16, "rwsq"); kq, b_kq = k.sb(s2, [128, 512], F32, "rwkq")
                rs_, b_rs = k.sb(s2, [128, 512], F32, "rwrs"); lnt, b_lnt = k.sb(s2, [128, 512], F32, "rwlnt")
                for g, (c0, n) in enumerate(GROUPS if RW_SUB >= 2 else []):
                    j = jof(g)
                    hgx, b_hgx = hgxs[g % 2]; hsx, b_hsx = hsxs[g % 2]
                    s_lo, s_hi = (0, NCTX) if g == 0 else (NCTX, NT)
                    lo, hi = max(s_lo, c0 - 1), min(s_hi, c0 + n + 1)
                    o0 = lo - (c0 - 1)
                    if lo > c0 - 1:
                        k.memset("dve", hgx[:, :, 0:3], 0.0, [b_hgx])
                    if hi < c0 + n + 1:
                        k.memset("dve", hgx[:, :, n + 1:n + 3], 0.0, [b_hgx])
                    w_ = hi - lo
                    for kt in range(8):
                        tmp, tmpb = tmps[kt % 2]
                        k.tt("dve", tmp[:, :w_], xT[:, kt, lo:hi], rstd_b[:, lo:hi], ALU.mult, [b_xT, b_rstd], [tmpb])
                        P.op("act", lambda e: e.activation(out=hgx[:, kt, 1 + o0:1 + o0 + w_], in_=tmp[:, :w_], func=AF.Identity,
                                                           scale=AB[:, l, 0, 0, kt, j:j + 1], bias=AB[:, l, 0, 1, kt, j:j + 1]),
                             reads=[tmpb, b_AB], writes=[b_hgx])
                    for kt in range(8):
                        k.tt("pool", hsx[:, kt, :n], hgx[:, kt, 1:n + 1], hgx[:, kt, 3:n + 3], ALU.add, [b_hgx], [b_hsx])
                    for tl_ in range(4 if RW_SUB >= 3 else 0):
                        ti = half * 4 + tl_
                        bk = k.bank()
                        for kt in range(8):
                            k.mm(bk, k.ps[bk][:, :n], WA[:, kt, tl_ * 128:(tl_ + 1) * 128], hgx[:, kt, 2:n + 2], kt == 0, False, [b_WA, b_hgx])
                        for kt in range(8):
                            k.mm(bk, k.ps[bk][:, :n], WB[:, kt, tl_ * 128:(tl_ + 1) * 128], hsx[:, kt, :n], False, kt == 7, [b_WB, b_hsx])
                        dst, dstb = [(rS, b_r), (kS, b_k), (vS, b_v), (xd, b_xd)][ti // 2]
                        k.copy("act", dst[:, ti % 2, c0:c0 + n], k.ps[bk][:, :n], [k.pb[bk]], [dstb])
                        if ti // 2 == 1 and RW_SUB >= 4:
                            ci = ti % 2
                            k.ts("dve", kq[:, :n], k.ps[bk][:, :n], cols[:, ci, C_KK:C_KK + 1], None, ALU.mult, None, [k.pb[bk], b_cols], [b_kq])
                            k.act(sq[:, :n], kq[:, :n], AF.Square, [b_kq], [b_sq])
                            b2 = k.bank()
                            k.mm(b2, k.ps[b2][:, :n], bo64, sq[:, :n], True, True, [b_sq, b_cstb])
                            k.rstd_from_ss(rs_[:, :n], k.ps[b2][:, :n], b2, 1.0, lnt[:, :n], b_lnt, [b_rs])
                            k.tt("dve", kkS[:, ci, c0:c0 + n], kq[:, :n], rs_[:, :n], ALU.mult, [b_kq, b_rs], [b_kk])
                P.barrier()
            dump("rw_r", rS[:], b_r, [128, 2, NT], BF16); dump("rw_kk", kkS[:], b_kk, [128, 2, NT], BF16)
            dump("rw_xd", xd[:], b_xd, [128, 2, NT], BF16)
            yacc, _ = k.sb(st, [128, 2, NT], BF16, "rw_yacc")
            sw = {}
            def small(name, shape, src, dt=BF16, q="pool"):
                t, b = k.sb(st, shape, dt, name)
                k.load(q if dt == BF16 else "sp", t[:], src, [b])
                sw[name] = (t, b)
            for dr in range(2 if RW_LEVEL >= 1 else 0):
                small(f"w1_{dr}", [128, 2, 32], rw_w1_d[l, dr].rearrange("(kt p) n -> p kt n", p=128))
                small(f"w2_{dr}", [32, 256], rw_w2_d[l, dr])
                small(f"a1_{dr}", [128, 2, 32], rw_a1_d[l, dr].rearrange("(kt p) n -> p kt n", p=128))
                small(f"a2_{dr}", [32, 256], rw_a2_d[l, dr])
                small(f"w0r_{dr}", [1, 256], rw_w0row_d[l, dr], F32)
                small(f"tri_{dr}", [128, 2, 128], rw_tri_d[dr], F32)
                small(f"m1_{dr}", [128, 256], rw_m1_d[dr])
                small(f"mnt_{dr}", [128, 128], rw_mnt_d[dr])
            if RW_LEVEL >= 0:
                small("g1", [128, 2, 64], rw_g1_d[l].rearrange("(kt p) n -> p kt n", p=128))
                small("g2", [64, 256], rw_g2_d[l])
                small("ind", [128, 2], rw_ind_d, F32)
            ones1, b_ones1 = k.sb(st, [1, 128], F32, "ones1")
            k.memset("dve", ones1[:], 1.0, [b_ones1])

            def a_of(dr, xsrc, n, a_out, b_aout, x1t, b_x1t):
                a1, b_a1 = sw[f"a1_{dr}"]; a2, b_a2 = sw[f"a2_{dr}"]
                bk = k.bank()
                for kt in range(2):
                    k.mm(bk, k.ps[bk][0:32, :n], a1[:, kt, :], xsrc[:, kt, :], kt == 0, kt == 1, [b_a1, b_xd])
                k.copy("act", x1t[0:32, :n], k.ps[bk][0:32, :n], [k.pb[bk]], [b_x1t])
                for ci in range(2):
                    b2 = k.bank()
                    k.mm(b2, k.ps[b2][:, :n], a2[:, ci * 128:(ci + 1) * 128], x1t[0:32, :n], True, True, [b_a2, b_x1t])
                    P.op("act", lambda e: e.activation(out=a_out[:, ci, :n], in_=k.ps[b2][:, :n], func=AF.Sigmoid,
                                                       bias=cols[:, ci, C_A0 + dr:C_A0 + dr + 1]),
                         reads=[k.pb[b2], b_cols], writes=[b_aout])

            def kmod_of(a_in, b_ain, ksrc, n, out, b_out_, tmpf, b_tmpf):
                for ci in range(2):
                    k.ts("dve", tmpf[:, ci, :n], a_in[:, ci, :n], -1.0, cols[:, ci, C_KA:C_KA + 1], ALU.add, ALU.mult, [b_ain, b_cols], [b_tmpf])
                    k.stt(out[:, ci, :n], tmpf[:, ci, :n], 1.0, ksrc[:, ci, :], ALU.add, ALU.mult, [b_tmpf, b_k], [b_out_])

            b_yt = [Buf(f"yacc{t}") for t in range(NTT)]
            for (c0_, n_) in GROUPS:
                k.memset("dve", yacc[:, :, c0_:c0_ + n_], 0.0, b_yt[c0_ // 128:(c0_ + n_) // 128])

            def scan_gen(dr, sd, banks):
                    w1, b_w1 = sw[f"w1_{dr}"]; w2, b_w2 = sw[f"w2_{dr}"]; w0r, b_w0r = sw[f"w0r_{dr}"]
                    tri, b_tri = sw[f"tri_{dr}"]; m1, b_m1 = sw[f"m1_{dr}"]; mnt, b_mnt = sw[f"mnt_{dr}"]; ind, b_ind = sw["ind"]
                    M32, b_M32 = k.sb(sd, [128, 2, 64], F32, "M32"); Mbf, b_Mbf = k.sb(sd, [128, 2, 128], BF16, "Mblk")
                    k.memset("dve", M32[:], 0.0, [b_M32]); k.memset("dve", Mbf[:], 0.0, [b_Mbf])
                    x1t, b_x1t = k.sb(sd, [32, 128], BF16, "x1t")
                    tnh, b_tnh = k.sb(sd, [32, 128], BF16, "tnh")
                    lwT, b_lwT = k.sb(sd, [128, 256], F32, "lwT")
                    lam, b_lam = k.sb(sd, [128, 2, 3, 128], F32, "lam")
                    gam, b_gam = k.sb(sd, [128, 2, 2], F32, "gam")
                    aF, b_aF = k.sb(sd, [128, 2, 128], F32, "aF")
                    tF, b_tF = k.sb(sd, [128, 2, 128], F32, "tF")
                    kmod, b_kmod = k.sb(sd, [128, 2, 128], F32, "kmod")
                    AR, b_AR = k.sb(sd, [128, 2, 2, 128], BF16, "AR")
                    BK, b_BK = k.sb(sd, [128, 2, 2, 128], BF16, "BK")
                    BKt2, b_BKt = k.sb(sd, [128, 2, 2, 2, 128], BF16, "BKt")
                    k.memset("pool", BKt2[:], 0.0, [b_BKt])
                    Vt, b_Vt = k.sb(sd, [128, 2, 128], BF16, "Vt")
                    SCb, b_SCb = k.sb(sd, [128, 4, 256], BF16, "SCb"); SCk, b_SCk = k.sb(sd, [128, 4, 256], BF16, "SCk")
                    Nb = [k.sb(sd, [128, 4, 128], BF16, f"Nb{i}") for i in range(2)]
                    Tb = [k.sb(sd, [128, 4, 128], BF16, f"Tb{i}") for i in range(2)]
                    R, b_R = k.sb(sd, [128, 4, 128], BF16, "Rinv")
                    Wsb, b_Wsb = k.sb(sd, [128, 256], BF16, "Wsb"); Usb, b_Usb = k.sb(sd, [128, 256], BF16, "Usb")
                    mt, b_mt = k.sb(sd, [128, 2, 64], F32, "mt")
                    v4h = lambda ap: ap.rearrange("p (h t) -> p h t", h=4)
                    order = list(range(NTT)) if dr == 0 else [1, 0] + list(range(NTT - 1, 1, -1))
                    yield
                    for tt in order:
                        tc_ = slice(tt * 128, (tt + 1) * 128)
                        bk = k.bank()
                        for kt in range(2):
                            k.mm(bk, k.ps[bk][0:32, 0:128], w1[:, kt, :], xd[:, kt, tc_], kt == 0, kt == 1, [b_w1, b_xd])
                        k.act(tnh[:], k.ps[bk][0:32, 0:128], AF.Tanh, [k.pb[bk]], [b_tnh])
                        a1, b_a1 = sw[f"a1_{dr}"]; a2, b_a2 = sw[f"a2_{dr}"]
                        bk = k.bank()
                        for kt in range(2):
                            k.mm(bk, k.ps[bk][0:32, 0:128], a1[:, kt, :], xd[:, kt, tc_], kt == 0, kt == 1, [b_a1, b_xd])
                        k.copy("dve", x1t[0:32, 0:128], k.ps[bk][0:32, 0:128], [k.pb[bk]], [b_x1t])
                        yield
                        bk = k.bank()
                        k.mm(bk, k.ps[bk][:, 0:256], tnh[:], w2[:], True, False, [b_tnh, b_w2])
                        k.mm(bk, k.ps[bk][:, 0:256], ones1[:], w0r[:], False, True, [b_ones1, b_w0r])
                        k.act(lwT[:], k.ps[bk][:, 0:256], AF.Sigmoid, [k.pb[bk]], [b_lwT])
                        k.ts("dve", lwT[:], lwT[:], -math.exp(-0.5), None, ALU.mult, None, [b_lwT], [b_lwT])
                        b2 = k.bank()
                        for ci in range(2):
                            k.mm(b2, k.ps[b2][:, ci * 128:(ci + 1) * 128], a2[:, ci * 128:(ci + 1) * 128], x1t[0:32, 0:128], True, True, [b_a2, b_x1t],
                                 inc=(ci == 1))
                        for ci in range(2):
                            P.op("act", lambda e: e.activation(out=aF[:, ci, :], in_=k.ps[b2][:, ci * 128:(ci + 1) * 128], func=AF.Sigmoid,
                                                               bias=cols[:, ci, C_A0 + dr:C_A0 + dr + 1]),
                                 reads=[k.pb[b2], b_cols], writes=[b_aF])
                        yield
                        kmod_of(aF, b_aF, kS[:, :, tc_], 128, kmod, b_kmod, tF, b_tF)
                        for ci in range(2):
                            k.tt("pool", tF[:, ci, :], kkS[:, ci, tc_], aF[:, ci, :], ALU.mult, [b_kk, b_aF], [b_tF])
                        lbk = []
                        for ci in range(2):
                            bk = k.bank(); lbk.append(bk)
                            k.mm(bk, k.ps[bk][:, 0:128], lwT[:, ci * 128:(ci + 1) * 128], tri[:, 0, :], True, True, [b_lwT, b_tri], inc=False)
                            k.mm(bk, k.ps[bk][:, 128:256], lwT[:, ci * 128:(ci + 1) * 128], tri[:, 1, :], True, True, [b_lwT, b_tri], inc=False)
                            k.mm(bk, k.ps[bk][:, 256:258], lwT[:, ci * 128:(ci + 1) * 128], ind[:], True, True, [b_lwT, b_ind])
                        yield
                        for ci in range(2):
                            bk = lbk[ci]
                            k.act(lam[:, ci, 0, :], k.ps[bk][:, 0:128], AF.Exp, [k.pb[bk]], [b_lam])
                            k.act(lam[:, ci, 1, :], k.ps[bk][:, 0:128], AF.Exp, [k.pb[bk]], [b_lam], scale=-1.0)
                            k.act(lam[:, ci, 2, :], k.ps[bk][:, 128:256], AF.Exp, [k.pb[bk]], [b_lam])
                            k.act(gam[:, ci, :], k.ps[bk][:, 256:258], AF.Exp, [k.pb[bk]], [b_gam])
                        yield
                        if RWSTOP <= 1:
                            continue
                        for ci in range(2):
                            k.tt("dve", AR[:, ci, 1, :], rS[:, ci, tc_], lam[:, ci, 0, :], ALU.mult, [b_r, b_lam], [b_AR])
                            k.stt(AR[:, ci, 0, :], kkS[:, ci, tc_], -1.0, lam[:, ci, 2, :], ALU.mult, ALU.mult, [b_kk, b_lam], [b_AR])
                            k.tt("dve", BK[:, ci, 1, :], kmod[:, ci, :], lam[:, ci, 1, :], ALU.mult, [b_kmod, b_lam], [b_BK])
                            k.tt("pool", BK[:, ci, 0, :], tF[:, ci, :], lam[:, ci, 1, :], ALU.mult, [b_tF, b_lam], [b_BK])
                        yield
                        if RWSTOP <= 2:
                            continue
                        bk = k.bank()
                        pv = k.ps[bk][:].bitcast(BF16)
                        for ci in range(2):
                            for x_ in range(2):
                                P.op("pe", lambda e: e.transpose(pv[:, (ci * 2 + x_) * 128:(ci * 2 + x_ + 1) * 128], BK[:, ci, x_, :], identb),
                                     reads=[b_BK, b_cstb], writes=[k.pb[bk]], inc=False)
                            P.op("pe", lambda e: e.transpose(pv[:, (4 + ci) * 128:(5 + ci) * 128], vS[:, ci, tc_], identb),
                                 reads=[b_v, b_cstb], writes=[k.pb[bk]], inc=(ci == 1))
                        k.copy("act", BKt2[0:64, 0].rearrange("p a b c -> p (a b c)"), pv[0:64, 0:512], [k.pb[bk]], [b_BKt])
                        k.copy("act", BKt2[64:128, 1].rearrange("p a b c -> p (a b c)"), pv[64:128, 0:512], [k.pb[bk]], [b_BKt])
                        k.copy("dve", Vt[:].rearrange("p a c -> p (a c)"), pv[:, 512:768], [k.pb[bk]], [b_Vt])
                        if RWSTOP <= 3:
                            continue
                        m1b = m1[:].unsqueeze(1).to_broadcast([128, 2, 256])
                        for hh in range(2):
                            pb = 64 * hh
                            bA = k.bank(); bB = k.bank()
                            for x_, bx in ((0, bA), (1, bB)):
                                for ci in range(2):
                                    arv = AR[pb:pb + 64, ci, :, :].rearrange("p a t -> p (a t)")
                                    k.mm(bx, k.ps[bx][:, ci * 256:(ci + 1) * 256], BK[pb:pb + 64, ci, x_, :], arv, True, True, [b_BK, b_AR], inc=(ci == 1))
                            k.tt("dve", SCb[:, hh * 2:hh * 2 + 2, :], k.ps[bA][:, 0:512].rearrange("p (h t) -> p h t", h=2), m1b, ALU.mult,
                                 [k.pb[bA], b_m1], [b_SCb])
                            k.tt("dve", SCk[:, hh * 2:hh * 2 + 2, :], k.ps[bB][:, 0:512].rearrange("p (h t) -> p h t", h=2), m1b, ALU.mult,
                                 [k.pb[bB], b_m1], [b_SCk])
                            yield
                        Tc, b_Tc = Tb[0]
                        for hh in range(2):
                            pb = 64 * hh
                            bC = k.bank()
                            for ci in range(2):
                                k.mm(bC, k.ps[bC][:, ci * 128:(ci + 1) * 128], AR[pb:pb + 64, ci, 0, :], BK[pb:pb + 64, ci, 0, :], True, True, [b_AR, b_BK],
                                     inc=(ci == 1))
                            k.tt("dve", Tc[:, hh * 2:hh * 2 + 2, :], k.ps[bC][:, 0:256].rearrange("p (h t) -> p h t", h=2),
                                 mnt[:].unsqueeze(1).to_broadcast([128, 2, 128]), ALU.mult, [k.pb[bC], b_mnt], [b_Tc])
                        k.tt("pool", R[:], SCb[:, :, 0:128], identb.unsqueeze(1).to_broadcast([128, 4, 128]), ALU.add, [b_SCb, b_cstb], [b_R])
                        yield
                        if RWSTOP <= 4:
                            continue
                        Nc, b_Nc = SCb[:, :, 0:128], b_SCb
                        for i in range(1, 6):
                            Nn, b_Nn = Nb[i % 2]; Tn, b_Tn = Tb[i % 2]
                            bt_ = k.bank()
                            for h in range(4):
                                k.mm(bt_, k.ps[bt_][:, h * 128:(h + 1) * 128], Nc[:, h, :], Tc[:, h, :], True, True, [b_Nc, b_Tc], inc=(h == 3))
                            k.copy("dve", Tn[:], v4h(k.ps[bt_][:, 0:512]), [k.pb[bt_]], [b_Tn])
                            if i < 5:
                                bn = k.bank()
                                for h in range(4):
                                    k.mm(bn, k.ps[bn][:, h * 128:(h + 1) * 128], Tc[:, h, :], Nc[:, h, :], True, True, [b_Tc, b_Nc], inc=(h == 3))
                                k.copy("act", Nn[:], v4h(k.ps[bn][:, 0:512]), [k.pb[bn]], [b_Nn])
                            yield
                            br_ = k.bank()
                            for h in range(4):
                                k.mm(br_, k.ps[br_][:, h * 128:(h + 1) * 128], Tn[:, h, :], R[:, h, :], True, True, [b_Tn, b_R], inc=(h == 3))
                            k.tt("dve", R[:], v4h(k.ps[br_][:, 0:512]), R[:], ALU.add, [k.pb[br_], b_R], [b_R])
                            yield
                            Nc, b_Nc = Nn[:], b_Nn
                            Tc, b_Tc = Tn, b_Tn
                        if RWSTOP <= 5:
                            continue
                        bY, bW, bU, bM = banks
                        for jj in ([0, 1] if dr == 0 else [1, 0]):
                            pj = 64 * jj
                            for ci in range(2):
                                k.mm(bW, k.ps[bW][:, ci * 128:(ci + 1) * 128], AR[:, ci, 0, :], Mbf[:, ci, :], True, False, [b_AR, b_Mbf], inc=False)
                                for hh in range(2):
                                    h = ci * 2 + hh
                                    pb = 64 * hh
                                    k.mm(bW, k.ps[bW][:, h * 64:(h + 1) * 64], SCk[:, hh * 2 + ci, 0:128], Vt[:, ci, pb:pb + 64], False, True, [b_SCk, b_Vt],
                                         inc=(h == 3))
                            k.copy("act", Wsb[:], k.ps[bW][:, 0:256], [k.pb[bW]], [b_Wsb])
                            yield
                            for h in range(4):
                                oc = slice(h * 64, (h + 1) * 64)
                                k.mm(bU, k.ps[bU][:, oc], R[:, (h % 2) * 2 + h // 2, :], Wsb[:, oc], True, True, [b_R, b_Wsb], inc=(h == 3))
                            k.copy("dve", Usb[:], k.ps[bU][:, 0:256], [k.pb[bU]], [b_Usb])
                            yield
                            for ci in range(2):
                                for hh in range(2):
                                    h = ci * 2 + hh
                                    pb = 64 * hh
                                    oc = slice(h * 64, (h + 1) * 64)
                                    mo = k.ps[bM][pb:pb + 64, ci * 64:(ci + 1) * 64]
                                    k.mm(bM, mo, BKt2[:, jj, ci, 0, pb:pb + 64], Usb[:, oc], True, False, [b_BKt, b_Usb], inc=False)
                                    k.mm(bM, mo, BKt2[:, jj, ci, 1, pb:pb + 64], Vt[:, ci, pb:pb + 64], False, True, [b_BKt, b_Vt], inc=(h == 3))
                            for ci in range(2):
                                ycol = slice(ci * 128 + pj, ci * 128 + pj + 64)
                                k.mm(bY, k.ps[bY][:, ycol], Mbf[:, ci, :], AR[:, ci, 1, pj:pj + 64], True, False, [b_Mbf, b_AR], inc=False)
                                for hh in range(2):
                                    h = ci * 2 + hh
                                    pb = 64 * hh
                                    oc = slice(h * 64, (h + 1) * 64)
                                    k.mm(bY, k.ps[bY][pb:pb + 64, ycol], Usb[:, oc], SCb[:, hh * 2 + ci, 128 + pj:128 + pj + 64], False, False, [b_Usb, b_SCb], inc=False)
                                    k.mm(bY, k.ps[bY][pb:pb + 64, ycol], Vt[:, ci, pb:pb + 64], SCk[:, hh * 2 + ci, 128 + pj:128 + pj + 64], False, True, [b_Vt, b_SCk],
                                         inc=(h == 3))
                            k.tt("dve", mt[:], k.ps[bM][:, 0:128].rearrange("p (c n) -> p c n", c=2), M32[:], ALU.add, [k.pb[bM], b_M32], [b_mt])
                            k.tt("dve", Mbf[0:64, :, 0:64], mt[0:64, :, :], gam[0:64, :, jj:jj + 1].to_broadcast([64, 2, 64]), ALU.mult,
                                 [b_mt, b_gam], [b_Mbf])
                            k.tt("dve", Mbf[64:128, :, 64:128], mt[64:128, :, :], gam[64:128, :, jj:jj + 1].to_broadcast([64, 2, 64]), ALU.mult,
                                 [b_mt, b_gam], [b_Mbf])
                            k.tt("dve", M32[:], mt[:], gam[:, :, jj:jj + 1].to_broadcast([128, 2, 64]), ALU.mult, [b_mt, b_gam], [b_M32])
                            yield
                        k.tt("dve", yacc[:, :, tc_], k.ps[bY][:, 0:256].rearrange("p (c t) -> p c t", c=2), yacc[:, :, tc_], ALU.add,
                             [k.pb[bY], b_yt[tt]], [b_yt[tt]])
                        yield

            with ExitStack() as sd:
                threads = [[scan_gen(dr, sd, [4 * dr + i for i in range(4)]), [4 * dr + i for i in range(4)], 0] for dr in range(2)]
                live = list(threads)
                k.rrset, k.rr = threads[0][1], threads[0][2]
                for _ in range(RWSKEW):
                    next(threads[0][0])
                threads[0][2] = k.rr
                if os.environ.get("RWSEQ"):
                    for th in threads:
                        k.rrset, k.rr = th[1], th[2]
                        for _ in th[0]:
                            pass
                    live = []
                while live:
                    for th in list(live):
                        k.rrset, k.rr = th[1], th[2]
                        try:
                            next(th[0])
                        except StopIteration:
                            live.remove(th)
                        th[2] = k.rr
                k.rrset = list(range(8)); k.rr = 0
                P.barrier()
            b_yacc = Buf("rw_yacc_all")
            dump("rw_yacc", yacc[:], b_yacc, [128, 2, NT], BF16)
            with ExitStack() as s5_:
                    g1, b_g1 = sw["g1"]; g2, b_g2 = sw["g2"]
                    lneps, b_lneps = k.sb(s5_, [128, 1], F32, "lneps")
                    k.memset("dve", lneps[:], 64e-5, [b_lneps])

                    def epi_bufs():
                        d = {}
                        for nm, shp, dt in (("x1t", [32, 512], BF16), ("aF", [128, 2, 512], F32), ("tF", [128, 2, 512], F32),
                                            ("kmod", [128, 2, 512], F32), ("bon", [128, 2, 512], F32), ("ybf", [128, 512], BF16),
                                            ("yc", [128, 512], F32), ("lnt", [128, 512], F32),
                                            ("gh", [64, 512], BF16), ("gg", [128, 2, 512], F32), ("rkb", [128, 512], BF16)):
                            d[nm] = k.sb(s5_, shp, dt, "e" + nm)
                        return d

                    def epi_gen(groups, d):
                        x1t, b_x1t = d["x1t"]; aF, b_aF = d["aF"]; tF, b_tF = d["tF"]; kmod, b_kmod = d["kmod"]
                        bon, b_bon = d["bon"]; ybf, b_ybf = d["ybf"]; yc, b_yc = d["yc"]
                        lnt, b_lnt = d["lnt"]; rs_, b_rs = lnt, b_lnt; gh, b_gh = d["gh"]; gg, b_gg = d["gg"]; rkb, b_rkb = d["rkb"]
                        for g in groups:
                            c0, n = GROUPS[g]
                            gc_ = slice(c0, c0 + n)
                            for dr in range(2):
                                a_of(dr, xd[:, :, gc_], n, aF, b_aF, x1t, b_x1t)
                                yield
                                kmod_of(aF, b_aF, kS[:, :, gc_], n, kmod, b_kmod, tF, b_tF)
                                for ci in range(2):
                                    k.stt(rkb[:, :n], kmod[:, ci, :n], cols[:, ci, C_RK:C_RK + 1], rS[:, ci, gc_], ALU.mult, ALU.mult,
                                          [b_kmod, b_cols, b_r], [b_rkb])
                                    bk = k.bank()
                                    k.mm(bk, k.ps[bk][:, :n], bo64, rkb[:, :n], True, True, [b_rkb, b_cstb])
                                    yield
                                    if dr == 0:
                                        k.tt("dve", bon[:, ci, :n], k.ps[bk][:, :n], vS[:, ci, gc_], ALU.mult, [k.pb[bk], b_v], [b_bon])
                                    else:
                                        k.tt("dve", tF[:, ci, :n], k.ps[bk][:, :n], vS[:, ci, gc_], ALU.mult, [k.pb[bk], b_v], [b_tF])
                                        k.tt("pool", bon[:, ci, :n], bon[:, ci, :n], tF[:, ci, :n], ALU.add, [b_bon, b_tF], [b_bon])
                            bk = k.bank()
                            for kt in range(2):
                                k.mm(bk, k.ps[bk][0:64, :n], g1[:, kt, :], xd[:, kt, gc_], kt == 0, kt == 1, [b_g1, b_xd])
                            k.act(gh[:, :n], k.ps[bk][0:64, :n], AF.Sigmoid, [k.pb[bk]], [b_gh])
                            yield
                            for ci in range(2):
                                b2 = k.bank()
                                k.mm(b2, k.ps[b2][:, :n], g2[:, ci * 128:(ci + 1) * 128], gh[:, :n], True, True, [b_g2, b_gh])
                                k.copy("act", gg[:, ci, :n], k.ps[b2][:, :n], [k.pb[b2]], [b_gg])
                            yield
                            for ci in range(2):
                                bk = k.bank()
                                k.mm(bk, k.ps[bk][:, :n], bo64, yacc[:, ci, gc_], True, True, [b_yacc, b_cstb])
                                k.stt(yc[:, :n], k.ps[bk][:, :n], -1.0 / 64, yacc[:, ci, gc_], ALU.mult, ALU.add, [k.pb[bk], b_yacc], [b_yc])
                                k.act(ybf[:, :n], yc[:, :n], AF.Square, [b_yc], [b_ybf])
                                yield
                                b2 = k.bank()
                                k.mm(b2, k.ps[b2][:, :n], bo64, ybf[:, :n], True, True, [b_ybf, b_cstb])
                                P.op("act", lambda e: e.activation(out=lnt[:, :n], in_=k.ps[b2][:, :n], func=AF.Ln, scale=1.0 / 64, bias=lneps[:]),
                                     reads=[k.pb[b2], b_lneps], writes=[b_lnt])
                                k.act(rs_[:, :n], lnt[:, :n], AF.Exp, [b_lnt], [b_rs], scale=-0.5)
                                yield
                                k.stt(yc[:, :n], yc[:, :n], cols[:, ci, C_LNG:C_LNG + 1], rs_[:, :n], ALU.mult, ALU.mult, [b_yc, b_cols, b_rs], [b_yc])
                                k.stt(yc[:, :n], yc[:, :n], cols[:, ci, C_LNB:C_LNB + 1], bon[:, ci, :n], ALU.add, ALU.add, [b_yc, b_cols, b_bon], [b_yc])
                                k.tt("dve", ydst[:, ci, gc_], yc[:, :n], gg[:, ci, :n], ALU.mult, [b_yc, b_gg, b_xd], [b_ydst])
                                yield

                    glist = [g for g in range(len(GROUPS)) if not (l == DEPTH - 1 and g == 0)]
                    eths = [[epi_gen(glist[0::2], epi_bufs()), [0, 1, 2, 3], 0], [epi_gen(glist[1::2], epi_bufs()), [4, 5, 6, 7], 0]]
                    live = list(eths)
                    while live:
                        for th in list(live):
                            k.rrset, k.rr = th[1], th[2]
                            try:
                                next(th[0])
                            except StopIteration:
                                live.remove(th)
                            th[2] = k.rr
                    k.rrset = list(range(8)); k.rr = 0
                    P.barrier()
        dump("yd", ydst[:], b_ydst, [128, 2, NT], BF16)

    def final_out():
        with ExitStack() as st:
            ot = [k.sb(st, [128, D], F32, "ostage") for _ in range(2)]
            for tt in range(NLAT // 128):
                o, ob = ot[tt % 2]
                c0 = NCTX + tt * 128
                for half in range(2):
                    bk = k.bank()
                    for j in range(4):
                        kt = half * 4 + j
                        P.op("pe", lambda e: e.transpose(k.ps[bk][:, j * 128:(j + 1) * 128], xT[:, kt, c0:c0 + 128], identf),
                             reads=[b_xT, b_cstf], writes=[k.pb[bk]], inc=(j == 3))
                    k.copy("dve" if half == 0 else "act", o[:, half * 512:(half + 1) * 512], k.ps[bk][:], [k.pb[bk]], [ob])
                P.dma("sp", out_d[tt * 128:(tt + 1) * 128, :], o[:], reads=[ob])
            P.finish([b for _, b in ot])
            P.barrier()

    for l in range(DEPTH):
        if ("L%d" % l) not in stages:
            continue
        with ExitStack() as st:
            rms_stats(st)
            P.barrier()
        if "rw" in stages:
            rwkv_mixer(l)
        if "da" in stages:
            da_mixer(l)
        if "mla" in stages:
            mla_mixer(l)
        if "s5" in stages:
            s5_mixer(l)
        if "merge" in stages:
            merge(l)
        if "moe" in stages:
            moe(l)
    final_out()
    for e in ("sp",):
        toks = [(kk, v) for kk, v in P.cnt.items() if v > 0 and isinstance(kk, tuple)]
        for tok in toks:
            P._wait(e, tok)
    top.close()
    return nc, k, dbg_out


_CONSTS = None


def prep_shared(inp):
    g = {}
    g.update(host_consts())
    f32 = np.float32
    w_in = np.asarray(inp["w_in"], f32)
    offs = np.cumsum([0, 256, 256, 256, 256, 192, 128, 16, 256, 256, 256, 256, 4096])
    seg = lambda i: w_in[:, :, offs[i]:offs[i + 1]]
    g["w_ada"] = np.ascontiguousarray(inp["w_ada"], f32)
    ba = np.asarray(inp["b_ada"], f32)
    g["bada"] = np.ascontiguousarray(ba.reshape(DEPTH, 48, 128).transpose(0, 2, 1))
    g["gmix"] = np.stack([fm_cols(inp["norm_mix_g"][l], 8) for l in range(DEPTH)])
    g["gffn"] = np.stack([fm_cols(inp["norm_ffn_g"][l], 8) for l in range(DEPTH)])
    def pad3(w):
        o = np.zeros((DEPTH, D, 384), f32)
        for j in range(8):
            o[:, :, (j // 3) * 128 + (j % 3) * 32:(j // 3) * 128 + (j % 3) * 32 + 32] = w[:, :, j * 32:(j + 1) * 32]
        return o
    g["w_da_qk"] = np.ascontiguousarray(np.concatenate([pad3(seg(0)), pad3(seg(1))], axis=2))
    g["w_da_v"] = np.ascontiguousarray(seg(2))
    dg = np.asarray(inp["da_qk_norm_g"], f32)
    g["da_g"] = np.ascontiguousarray(np.stack([np.tile(dg[:, 0, :], (1, 4)), np.tile(dg[:, 1, :], (1, 4))], axis=2))
    g["da_lam"] = np.ascontiguousarray(np.broadcast_to(np.asarray(inp["da_lambda"], f32).reshape(DEPTH, 1, 128), (DEPTH, 128, 128)))
    g["da_sub"] = np.ascontiguousarray(np.broadcast_to(np.asarray(inp["da_subln_g"], f32).reshape(DEPTH, 1, 64), (DEPTH, 128, 64)))
    g["w_mla_c"] = np.zeros((DEPTH, D, 384), f32)
    g["w_mla_c"][:, :, 0:192] = seg(4)
    g["w_mla_c"][:, :, 256:384] = seg(5)
    g["w_mla_kr"] = np.zeros((DEPTH, D, 128), f32)
    g["w_mla_kr"][:, :, 32:48] = seg(6)
    g["w_mla_kr"][:, :, 96:112] = seg(6)
    wuq = np.asarray(inp["mla_w_uq"], f32)
    wuqp = np.zeros((DEPTH, 256, 256), f32)
    for h in range(4):
        wuqp[:, 0:192, 64 * h:64 * h + 48] = wuq[:, :, 48 * h:48 * h + 48]
    g["w_uq"] = np.ascontiguousarray(wuqp.reshape(DEPTH, 2, 128, 256).transpose(0, 2, 1, 3))
    wukv = np.asarray(inp["mla_w_ukv"], f32)
    g["w_ukvk"] = np.zeros((DEPTH, 128, 256), f32)
    g["w_ukvv"] = np.zeros((DEPTH, 128, 256), f32)
    for h in range(4):
        g["w_ukvk"][:, :, 64 * h:64 * h + 32] = wukv[:, :, 96 * h:96 * h + 32]
        g["w_ukvv"][:, :, 64 * h:64 * h + 64] = wukv[:, :, 96 * h + 32:96 * h + 96]
    gcq = np.zeros((DEPTH, 256), f32); gcq[:, 0:192] = np.asarray(inp["mla_cq_norm_g"], f32)
    gkv = np.asarray(inp["mla_ckv_norm_g"], f32)
    g["mla_gc"] = np.ascontiguousarray(np.stack([gcq[:, 0:128], gcq[:, 128:256], gkv], axis=2))
    mg = np.asarray(inp["mla_qk_norm_g"], f32)
    mgp = np.zeros((DEPTH, 2, 128), f32)
    for h in range(2):
        mgp[:, :, 64 * h:64 * h + 48] = mg
    g["mla_g"] = np.ascontiguousarray(mgp.transpose(0, 2, 1))
    g["w_s5"] = np.ascontiguousarray(seg(3))
    Bre = np.asarray(inp["s5_b_re"], f32); Bim = np.asarray(inp["s5_b_im"], f32)
    Cre = np.asarray(inp["s5_c_re"], f32); Cim = np.asarray(inp["s5_c_im"], f32)
    s5B = np.zeros((DEPTH, 2, 2, 128, 1024), f32)
    s5C = np.zeros((DEPTH, 2, 2, 128, 8, 128), f32)
    for gi in range(16):
        kt, gl = gi // 8, gi % 8
        s5B[:, :, kt, gl * 16:(gl + 1) * 16, gl * 64:(gl + 1) * 64] = Bre[:, :, gi].transpose(0, 1, 3, 2)
        s5B[:, :, kt, gl * 16:(gl + 1) * 16, 512 + gl * 64:512 + (gl + 1) * 64] = Bim[:, :, gi].transpose(0, 1, 3, 2)
        i, po = gi // 2, (gi % 2) * 64
        s5C[:, :, 0, po:po + 64, i, gl * 16:(gl + 1) * 16] = Cre[:, :, gi].transpose(0, 1, 3, 2)
        s5C[:, :, 1, po:po + 64, i, gl * 16:(gl + 1) * 16] = Cim[:, :, gi].transpose(0, 1, 3, 2)
    g["s5B"] = s5B; g["s5C"] = s5C
    lre = np.asarray(inp["s5_lam_re"], f32).reshape(DEPTH, 2, 1024)
    lim = np.asarray(inp["s5_lam_im"], f32).reshape(DEPTH, 2, 1024)
    ldt = np.repeat(np.asarray(inp["s5_log_dt"], f32), 64, axis=2)
    tm = np.stack([lre, lim, ldt], axis=2)
    g["s5tm"] = np.ascontiguousarray(np.broadcast_to(tm[:, :, :, None, :], (DEPTH, 2, 3, 128, 1024)))
    g["s5fm"] = np.ascontiguousarray(tm.reshape(DEPTH, 2, 3, 8, 128).transpose(0, 1, 4, 2, 3))
    g["s5d"] = np.stack([fm_cols(np.asarray(inp["s5_d"], f32)[l].reshape(256), 2) for l in range(DEPTH)])
    g["s5bg"] = np.stack([fm_cols(np.asarray(inp["s5_b_glu"], f32)[l], 2) for l in range(DEPTH)])
    g["w_glu"] = np.ascontiguousarray(inp["s5_w_glu"], f32)
    pidx = np.arange(128, dtype=f32)
    posc = np.zeros((2, 128, 2), f32); posc[0, :, 0] = -pidx; posc[1, :, 0] = -(127 - pidx)
    posr = np.zeros((2, 128, 128), f32); posr[0] = pidx[None, :]; posr[1] = (127 - pidx)[None, :]
    tri = np.zeros((2, 128, 128), f32)
    tri[0] = (pidx[:, None] <= pidx[None, :]).astype(f32)
    tri[1] = (pidx[:, None] >= pidx[None, :]).astype(f32)
    g["posc"] = posc; g["posr"] = posr; g["tri"] = tri
    g["w_rw"] = np.ascontiguousarray(np.concatenate([seg(7), seg(8), seg(9), seg(10)], axis=2))
    mu = np.asarray(inp["rw_mu"], f32).reshape(DEPTH, 1, 1024)
    g["rw_mu_row"] = np.ascontiguousarray(np.broadcast_to(mu, (DEPTH, 128, 1024)))
    colsl = []
    for l in range(DEPTH):
        cs = [inp["rw_k_k"][l], inp["rw_k_a"][l], inp["rw_ln_g"][l], inp["rw_ln_b"][l], np.asarray(inp["rw_r_k"][l]).reshape(256),
              inp["rw_w0"][l, 0], inp["rw_w0"][l, 1], inp["rw_a0"][l, 0], inp["rw_a0"][l, 1]]
        colsl.append(np.stack([fm_cols(c, 2) for c in cs], axis=2))
    g["rw_cols"] = np.ascontiguousarray(np.stack(colsl))
    g["rw_w0row"] = np.ascontiguousarray(np.asarray(inp["rw_w0"], f32).reshape(DEPTH, 2, 1, 256))
    for nm in ("rw_w1", "rw_w2", "rw_a1", "rw_a2", "rw_g1", "rw_g2"):
        g[nm] = np.ascontiguousarray(inp[nm], f32)
    t = np.arange(128)
    same = (t[:, None] // 64) == (t[None, :] // 64)
    tri = np.zeros((2, 128, 2, 128), f32)
    tri[0, :, 0, :] = (same & (t[:, None] <= t[None, :])); tri[0, :, 1, :] = (same & (t[:, None] < t[None, :]))
    tri[1, :, 0, :] = (same & (t[:, None] >= t[None, :])); tri[1, :, 1, :] = (same & (t[:, None] > t[None, :]))
    g["rw_tri"] = tri
    ind = np.zeros((128, 2), f32); ind[:64, 0] = 1; ind[64:, 1] = 1
    g["rw_ind"] = ind
    m1 = np.zeros((2, 128, 256), f32)
    m1[0, :, :128] = (same & (t[:, None] < t[None, :])); m1[0, :, 128:] = (same & (t[:, None] <= t[None, :]))
    m1[1, :, :128] = (same & (t[:, None] > t[None, :])); m1[1, :, 128:] = (same & (t[:, None] >= t[None, :]))
    g["rw_m1"] = m1
    mnt = np.zeros((2, 128, 128), f32)
    mnt[0] = (same & (t[None, :] < t[:, None])); mnt[1] = (same & (t[None, :] > t[:, None]))
    g["rw_mnt"] = mnt
    g["w_gates"] = np.ascontiguousarray(seg(11).reshape(DEPTH, 8, 128, 4, 8, 128).transpose(0, 4, 2, 1, 3, 5)).reshape(DEPTH, 8, 128, 4096)
    g["w_branch"] = np.ascontiguousarray(np.asarray(inp["w_branch"], f32).reshape(DEPTH, 4, 2, 128, D).transpose(0, 3, 2, 1, 4)).reshape(DEPTH, 128, 8192)
    g["w_out"] = np.ascontiguousarray(inp["w_out"], f32)
    g["router_w"] = np.ascontiguousarray(inp["router_w"], f32)
    g["router_b"] = np.ascontiguousarray(np.broadcast_to(np.asarray(inp["router_bias"], f32).reshape(1, 16), (128, 16)))
    sel = np.zeros((16, 16, 128), f32)
    for e in range(16):
        sel[e, e, :] = 1.0
    g["sel"] = sel
    g["exp_w_gate"] = np.ascontiguousarray(inp["exp_w_gate"], f32)
    g["exp_w_up"] = np.ascontiguousarray(inp["exp_w_up"], f32)
    g["exp_w_down"] = np.ascontiguousarray(inp["exp_w_down"], f32)
    return g


def prep_core(inp, b):
    x = np.asarray(inp["x"], np.float32)[b]
    ctx = np.asarray(inp["ctx"], np.float32)[b]
    xin = np.ascontiguousarray(np.concatenate([ctx, x], axis=0))
    c2 = np.stack([np.asarray(inp["c"], np.float32)[b], np.asarray(inp["c_ctx"], np.float32)], axis=0)
    c2T = np.ascontiguousarray(c2.reshape(2, 8, 128).transpose(2, 1, 0))
    return {"xin": xin, "c2T": c2T}


RW_LEVEL = int(os.environ.get('RW_LEVEL', '9'))
RWSTOP = int(os.environ.get('RWSTOP', '9'))
S5SKEW = int(os.environ.get('S5SKEW', '1'))
RWSKEW = int(os.environ.get('RWSKEW', '0'))
RW_SUB = int(os.environ.get('RW_SUB', '9'))
STAGES_ALL = ("L0", "L1", "da", "mla", "s5", "rw", "merge", "moe")


def kernel(**inputs):
    nc, k, _ = build(STAGES_ALL)
    shared = prep_shared(inputs)
    in_maps = []
    for b in range(8):
        m = dict(shared)
        m.update(prep_core(inputs, b))
        in_maps.append({kk: m[kk] for kk in k.ins})
    res = run_bass_kernel_spmd(nc, in_maps, core_ids=list(range(8)))
    return np.stack([np.asarray(r["out"]) for r in res.results], axis=0).astype(np.float32)
```
